# Optimizing a Trainium2 kernel written in Bass

```python
import math
import jax, jax.numpy as jnp
from jax import lax
import numpy as np

D_MODEL = 1024
BATCH = 2
SEQ = 8192
DEPTH = 2

CHUNK = 64
Q_BLOCK = 128
D_MIX = D_MODEL
M_WIDTH = D_MIX // 2
M_HEADS = 4
M_HEAD_DIM = M_WIDTH // M_HEADS
CONV_WIDTH = 4
A_WIDTH = D_MIX - M_WIDTH
A_HEADS = 4
A_VDIM = A_WIDTH // A_HEADS
A_QK_DIM = A_VDIM // 2
ROPE_THETA = 10000.0
EPS = 1e-6
PROJ_SIZES = (M_WIDTH, M_WIDTH, M_WIDTH, M_HEADS, M_HEADS, M_WIDTH,
              A_WIDTH, A_WIDTH, A_WIDTH, A_WIDTH)
D_IN = 4 * M_WIDTH + 2 * M_HEADS + 4 * A_WIDTH

kernel_name = "hymba_mlstm_diffattn_trunk"


def rmsnorm(x, w):
    xf = x.astype(jnp.float32)
    y = xf * lax.rsqrt(jnp.mean(xf * xf, axis=-1, keepdims=True) + EPS)
    return (y * w.astype(jnp.float32)).astype(x.dtype)


def causal_conv(x, w, b):
    K, C = w.shape
    y = lax.conv_general_dilated(
        x, w.reshape(K, 1, C).astype(x.dtype), window_strides=(1,),
        padding=[(K - 1, 0)], dimension_numbers=('NWC', 'WIO', 'NWC'),
        feature_group_count=C)
    return y + b


def rope(x):
    S, dh = x.shape[1], x.shape[-1]
    inv = 1.0 / (ROPE_THETA ** (jnp.arange(0, dh, 2, dtype=jnp.float32) / dh))
    ang = jnp.arange(S, dtype=jnp.float32)[:, None] * inv[None, :]
    cos = jnp.concatenate([jnp.cos(ang), jnp.cos(ang)], -1)[None, :, None, None, :]
    sin = jnp.concatenate([jnp.sin(ang), jnp.sin(ang)], -1)[None, :, None, None, :]
    xf = x.astype(jnp.float32)
    x1, x2 = jnp.split(xf, 2, axis=-1)
    rot = jnp.concatenate([-x2, x1], -1)
    return (xf * cos + rot * sin).astype(x.dtype)


def mlstm_chunkwise(q, k, v, i_pre, f_pre):
    B, S, H, D = q.shape
    L = CHUNK
    N = S // L

    def to_chunks(t):
        return t.astype(jnp.float32).reshape(B, N, L, H, -1).transpose(1, 0, 3, 2, 4)

    qc = to_chunks(q)
    kc = to_chunks(k) * (D ** -0.5)
    vc = to_chunks(v)
    lf = jax.nn.log_sigmoid(f_pre.astype(jnp.float32)).reshape(B, N, L, H).transpose(1, 0, 3, 2)
    li = i_pre.astype(jnp.float32).reshape(B, N, L, H).transpose(1, 0, 3, 2)
    causal = jnp.tril(jnp.ones((L, L), dtype=bool))

    def step(carry, inp):
        C, n, m = carry
        q_, k_, v_, lf_, li_ = inp
        b = jnp.cumsum(lf_, axis=-1)
        dmat = jnp.where(causal, b[..., :, None] - b[..., None, :] + li_[..., None, :], -jnp.inf)
        inter = b + m[..., None]
        m_i = jnp.maximum(inter, jnp.max(dmat, axis=-1))
        w_intra = jnp.exp(dmat - m_i[..., None])
        w_inter = jnp.exp(inter - m_i)
        s = jnp.einsum('bhid,bhjd->bhij', q_, k_) * w_intra
        num = (w_inter[..., None] * jnp.einsum('bhid,bhde->bhie', q_, C)
               + jnp.einsum('bhij,bhje->bhie', s, v_))
        den = w_inter * jnp.einsum('bhid,bhd->bhi', q_, n) + jnp.sum(s, axis=-1)
        h = num / jnp.maximum(jnp.abs(den), jnp.exp(-m_i))[..., None]
        b_last = b[..., -1]
        g = b_last[..., None] - b + li_
        m_new = jnp.maximum(b_last + m, jnp.max(g, axis=-1))
        decay = jnp.exp(b_last + m - m_new)
        wk = jnp.exp(g - m_new[..., None])
        C_new = decay[..., None, None] * C + jnp.einsum('bhl,bhld,bhle->bhde', wk, k_, v_)
        n_new = decay[..., None] * n + jnp.einsum('bhl,bhld->bhd', wk, k_)
        return (C_new, n_new, m_new), h

    init = (jnp.zeros((B, H, D, D), jnp.float32), jnp.zeros((B, H, D), jnp.float32),
            jnp.zeros((B, H), jnp.float32))
    _, hs = lax.scan(step, init, (qc, kc, vc, lf, li))
    return hs.transpose(1, 0, 3, 2, 4).reshape(B, S, H, D).astype(q.dtype)


def diff_attention(q, k, v, lam):
    B, S, H, _, dk = q.shape
    dv = v.shape[-1]
    nb = S // Q_BLOCK
    qb = q.astype(jnp.float32).reshape(B, nb, Q_BLOCK, H, 2, dk).transpose(1, 0, 3, 4, 2, 5)
    kt = k.astype(jnp.float32).transpose(0, 2, 3, 1, 4)
    vt = v.astype(jnp.float32).transpose(0, 2, 1, 3)
    key_chunk = jnp.arange(S) // CHUNK
    scale = dk ** -0.5

    def block(args):
        qblk, start = args
        s = jnp.einsum('bhcqd,bhckd->bhcqk', qblk, kt) * scale
        q_chunk = (start + jnp.arange(Q_BLOCK)) // CHUNK
        mask = key_chunk[None, :] <= q_chunk[:, None]
        p = jax.nn.softmax(jnp.where(mask, s, -jnp.inf), axis=-1)
        a = p[:, :, 0] - lam * p[:, :, 1]
        return jnp.einsum('bhqk,bhke->bhqe', a, vt)

    out = lax.map(block, (qb, jnp.arange(nb) * Q_BLOCK))
    return out.transpose(1, 0, 3, 2, 4).reshape(B, S, H, dv).astype(v.dtype)


def hybrid_layer(x, norm_w, w_in, conv_w, conv_b, i_bias, f_bias, m_norm_w, m_skip,
                 lam_q1, lam_k1, lam_q2, lam_k2, a_norm_w, w_out, lambda_init):
    B, S, _ = x.shape
    h = rmsnorm(x, norm_w)
    proj = jnp.einsum('bsd,de->bse', h, w_in)
    idx = list(np.cumsum(PROJ_SIZES)[:-1])
    mq, mk, mv, mi, mf, mz, aq, ak, av, az = jnp.split(proj, idx, axis=-1)

    qk = jax.nn.silu(causal_conv(jnp.concatenate([mq, mk], -1), conv_w, conv_b))
    mq_c, mk_c = jnp.split(qk, 2, axis=-1)
    to_heads = lambda t: t.reshape(B, S, M_HEADS, M_HEAD_DIM)
    hm = mlstm_chunkwise(to_heads(mq_c), to_heads(mk_c), to_heads(mv), mi + i_bias, mf + f_bias)
    hm = rmsnorm(hm, m_norm_w.reshape(M_HEADS, M_HEAD_DIM)).reshape(B, S, M_WIDTH)
    ym = (hm + m_skip * mq_c) * jax.nn.silu(mz)

    qa = rope(aq.reshape(B, S, A_HEADS, 2, A_QK_DIM))
    ka = rope(ak.reshape(B, S, A_HEADS, 2, A_QK_DIM))
    lam = (jnp.exp(jnp.sum(lam_q1.astype(jnp.float32) * lam_k1.astype(jnp.float32)))
           - jnp.exp(jnp.sum(lam_q2.astype(jnp.float32) * lam_k2.astype(jnp.float32)))
           + lambda_init)
    ha = diff_attention(qa, ka, av.reshape(B, S, A_HEADS, A_VDIM), lam)
    ha = (rmsnorm(ha, a_norm_w) * (1.0 - lambda_init)).reshape(B, S, A_WIDTH)
    ya = ha * jax.nn.silu(az)

    y = jnp.einsum('bse,ed->bsd', jnp.concatenate([ym, ya], -1), w_out)
    return x + y


def setup_inputs(seed: int = 0) -> dict:
    key = jax.random.key(seed)
    ks = jax.random.split(key, 16)
    f32 = jnp.float32
    nrm = lambda k, shape, s: jax.random.normal(k, shape, f32) * s
    f_base = jnp.linspace(3.0, 6.0, M_HEADS, dtype=f32)[None, :]
    return {
        "x": nrm(ks[0], (BATCH, SEQ, D_MODEL), 1.0),
        "norm_w": 1.0 + nrm(ks[1], (DEPTH, D_MODEL), 0.02),
        "w_in": nrm(ks[2], (DEPTH, D_MODEL, D_IN), D_MODEL ** -0.5),
        "conv_w": nrm(ks[3], (DEPTH, CONV_WIDTH, 2 * M_WIDTH), CONV_WIDTH ** -0.5),
        "conv_b": nrm(ks[4], (DEPTH, 2 * M_WIDTH), 0.01),
        "i_bias": nrm(ks[5], (DEPTH, M_HEADS), 0.1),
        "f_bias": f_base + nrm(ks[6], (DEPTH, M_HEADS), 0.1),
        "m_norm_w": 1.0 + nrm(ks[7], (DEPTH, M_WIDTH), 0.02),
        "m_skip": 1.0 + nrm(ks[8], (DEPTH, M_WIDTH), 0.02),
        "lam_q1": nrm(ks[9], (DEPTH, A_QK_DIM), 0.1),
        "lam_k1": nrm(ks[10], (DEPTH, A_QK_DIM), 0.1),
        "lam_q2": nrm(ks[11], (DEPTH, A_QK_DIM), 0.1),
        "lam_k2": nrm(ks[12], (DEPTH, A_QK_DIM), 0.1),
        "a_norm_w": 1.0 + nrm(ks[13], (DEPTH, A_VDIM), 0.02),
        "w_out": nrm(ks[14], (DEPTH, D_MIX, D_MODEL), D_MIX ** -0.5),
        "final_norm_w": 1.0 + nrm(ks[15], (D_MODEL,), 0.02),
    }


def reference(x, norm_w, w_in, conv_w, conv_b, i_bias, f_bias, m_norm_w, m_skip,
              lam_q1, lam_k1, lam_q2, lam_k2, a_norm_w, w_out, final_norm_w):
    for l in range(DEPTH):
        lambda_init = 0.8 - 0.6 * math.exp(-0.3 * l)
        x = hybrid_layer(x, norm_w[l], w_in[l], conv_w[l], conv_b[l], i_bias[l], f_bias[l],
                         m_norm_w[l], m_skip[l], lam_q1[l], lam_k1[l], lam_q2[l], lam_k2[l],
                         a_norm_w[l], w_out[l], lambda_init)
    return rmsnorm(x, final_norm_w)
```

```python
import math
from contextlib import ExitStack

import numpy as np
import ml_dtypes

import concourse.bass as bass
import concourse.mybir as mybir
from concourse.bass_utils import run_bass_kernel_spmd

F32 = mybir.dt.float32
BF16 = mybir.dt.bfloat16
AF = mybir.ActivationFunctionType
ALU = mybir.AluOpType
AX = mybir.AxisListType

D_MODEL = 1024
BATCH = 2
SEQ = 8192
DEPTH = 2
NCORES = 8
TB = 512
NBLK = SEQ // TB
KT = D_MODEL // 128
EPS = 1e-6
NV = 32
LN_KSCALE = math.log(128.0 ** -0.5)
GROUPS = [[0, 1, 2, 3], [4, 5, 6, 7]]
import os
_STOP = int(os.environ.get("K_STOP", "9"))
_NB = int(os.environ.get("K_NBLK", str(NBLK)))


class Sched:
    CH = 30000
    ND = 48

    def __init__(self, nc, es):
        self.nc = nc
        self.engs = ["pe", "act", "dve", "pool", "sp"]
        self.recs = {e: [] for e in self.engs}
        self.cnt = {e: 0 for e in self.engs}
        self.seen = {e: {} for e in self.engs}
        self.lastw = {}
        self.readers = {}
        nsem = {"pe": 3, "act": 3, "dve": 4, "pool": 3, "sp": 1}
        self.esems = {e: [es.enter_context(nc.semaphore(f"s_{e}_{i}")) for i in range(nsem[e])]
                      for e in self.engs}
        self.dsems = [es.enter_context(nc.semaphore(f"d_{i}")) for i in range(self.ND)]
        self.dval = [0] * self.ND
        self.dnext = 0
        self.ccsem = es.enter_context(nc.semaphore("ccsem"))
        self.ccval = 0

    def _tok_wait(self, e, tok, waits):
        if tok[0] == "e":
            _, e2, n = tok
            if e2 == e and e == "pe":
                return
            if self.seen[e].get(e2, 0) >= n:
                return
            self.seen[e][e2] = n
            waits.append((self.esems[e2][(n - 1) // self.CH], (n - 1) % self.CH + 1))
        else:
            kind, i, v = tok
            key = (kind, i)
            if self.seen[e].get(key, 0) >= v:
                return
            self.seen[e][key] = v
            sem = self.dsems[i] if kind == "d" else self.ccsem
            waits.append((sem, v))

    def issue(self, e, fn, reads=(), writes=(), dma=False, cc=False):
        deps = []
        for k in reads:
            if k in self.lastw:
                deps.append(self.lastw[k])
        for k in writes:
            if k in self.lastw:
                deps.append(self.lastw[k])
            deps.extend(self.readers.get(k, {}).values())
        waits = []
        for tok in deps:
            self._tok_wait(e, tok, waits)
        if dma:
            i = self.dnext
            self.dnext = (self.dnext + 1) % self.ND
            prev = self.dval[i]
            if prev > 0:
                self._tok_wait(e, ("d", i, prev), waits)
            self.dval[i] += 16
            tok = ("d", i, self.dval[i])
            inc = (self.dsems[i], 16)
        elif cc:
            self.ccval += 1
            tok = ("c", 0, self.ccval)
            inc = (self.ccsem, 1)
        elif fn is None:
            tok = None
            inc = None
        else:
            self.cnt[e] += 1
            n = self.cnt[e]
            tok = ("e", e, n)
            inc = (self.esems[e][(n - 1) // self.CH], 1)
        self.recs[e].append((waits, fn, inc))
        if tok is not None:
            for k in reads:
                self.readers.setdefault(k, {})[(tok[0], tok[1])] = tok
            for k in writes:
                self.lastw[k] = tok
                self.readers[k] = {}
        return tok

    def emit(self, block):
        nc = self.nc

        def run(e, eng):
            for waits, fn, inc in self.recs[e]:
                for s, v in waits:
                    eng.wait_ge(s, v)
                if fn is not None:
                    ins = fn(eng)
                    ins.then_inc(inc[0], inc[1])

        @block.tensor
        def _(eng):
            run("pe", eng)

        @block.scalar
        def _(eng):
            run("act", eng)

        @block.vector
        def _(eng):
            run("dve", eng)

        @block.gpsimd
        def _(eng):
            run("pool", eng)

        @block.sync
        def _(eng):
            run("sp", eng)


def build_program(stage="all", debug=False):
    nc = bass.Bass("TRN2", target_bir_lowering=False)
    with ExitStack() as es:
        S = Sched(nc, es)

        def dram(name, shape, dt, kind):
            return nc.dram_tensor(name, shape, dt, kind=kind).ap()

        fused = stage == "all"
        layers = {"all": [0, 1], "l0": [0], "mid": [1], "fin": []}[stage]
        do_fin = stage in ("all", "fin")

        xT = dram("xT", [D_MODEL, SEQ], F32, "ExternalInput") if stage in ("all", "l0", "mid") else None
        cosT = dram("cosT", [128, SEQ], F32, "ExternalInput") if layers else None
        sinT = dram("sinT", [128, SEQ], F32, "ExternalInput") if layers else None
        consts = dram("consts", [128, 192], F32, "ExternalInput")
        wfm, wtm, wout, vecs, lamv = {}, {}, {}, {}, {}
        for l in layers:
            wfm[l] = dram(f"wfm{l}", [D_MODEL, 1026], F32, "ExternalInput")
            wtm[l] = dram(f"wtm{l}", [D_MODEL, 256], F32, "ExternalInput")
            lamv[l] = dram(f"lamv{l}", [128, 256], F32, "ExternalInput")
        for l in range(DEPTH):
            vecs[l] = dram(f"vecs{l}", [128, NV], F32, "ExternalInput")
        need_wout = {"all": [0, 1], "l0": [], "mid": [0], "fin": [1]}[stage]
        for l in need_wout:
            wout[l] = dram(f"wout{l}", [D_MODEL, D_MODEL], F32, "ExternalInput")

        ycl, ycf = {}, {}
        for l in layers:
            kind = "Internal" if fused else "ExternalOutput"
            ycl[l] = [dram(f"ycl{l}_{q}", [256, 2048], BF16, kind) for q in range(4)]
        for l in need_wout:
            kind = "Internal" if fused else "ExternalInput"
            ycf[l] = [dram(f"ycf{l}_{q}", [1024, 2048], BF16, kind) for q in range(4)]
        x1s = None
        if stage == "all":
            x1s = dram("x1s", [D_MODEL, SEQ], F32, "Internal")
        elif stage == "mid":
            x1s = dram("x1s", [D_MODEL, SEQ], F32, "ExternalOutput")
        elif stage == "fin":
            x1s = dram("x1s", [D_MODEL, SEQ], F32, "ExternalInput")
        outT = dram("outT", [D_MODEL, SEQ], F32, "ExternalOutput") if do_fin else None
        dbg = {}
        if debug:
            for nm, shp, dt in [("d_qm", [128, SEQ], BF16), ("d_km", [128, SEQ], BF16),
                                ("d_qa", [128, SEQ], BF16), ("d_ka", [128, SEQ], BF16),
                                ("d_rows", [1, 9 * SEQ], F32)]:
                dbg[nm] = dram(nm, shp, dt, "ExternalOutput")

        def sb(name, shape, dt):
            return es.enter_context(nc.sbuf_tensor(name, shape, dt))

        cst = sb("cst", [128, 192], F32)
        ident_f = cst[:, 0:128]
        identb = sb("identb", [128, 128], BF16)
        onesb = sb("onesb", [128, 128], BF16)
        onesf = sb("onesf", [1, 512], F32)
        vec = [sb(f"vec{l}", [128, NV], F32) for l in range(DEPTH)]
        lamt = sb("lamt", [128, 256], F32)
        lamw = sb("lamw", [128, 4], F32)
        neglam = sb("neglam", [128, 1], F32)
        negfb = sb("negfb", [1, 1], F32)

        wfm_b = sb("wfm_b", [128, KT, 1026], BF16)
        wtm_b = sb("wtm_b", [128, KT, 256], BF16)
        wout_b = sb("wout_b", [128, KT, D_MODEL], BF16)
        wstg = [sb(f"wstg{i}", [128, KT, 128], F32) for i in range(2)]

        xblk = sb("xblk", [128, KT, TB], F32)
        sqb = sb("sqb", [128, KT, TB], BF16)
        xnb = sb("xnb", [128, KT, TB], BF16)
        ycb = sqb
        rstd = sb("rstd", [128, TB], F32)
        cosb = sb("cosb", [128, TB], F32)
        sinb = sb("sinb", [128, TB], F32)
        preq = sb("preq", [128, TB + 4], F32)
        prek = sb("prek", [128, TB + 4], F32)
        cva = sb("tA", [128, TB], F32)
        cvb = sb("tB", [128, TB], F32)
        rpa, rpb = cva, cvb
        qa = sb("qa", [128, TB], BF16)
        qm = sb("qm", [128, TB], BF16)
        km = sb("km", [128, TB], BF16)
        qs = sb("qs", [128, TB], BF16)
        gm = sb("gm", [128, TB], F32)
        ga = sb("ga", [128, TB], F32)
        kaT = sb("kaT", [128, SEQ], BF16)
        va = sb("va", [128, SEQ // 128, 132], BF16)
        vmc = sb("vmc", [64, 8, 132], BF16)
        vmw = sb("vmw", [64, 8, 132], BF16)
        ktok = sb("ktok", [64, 8, 128], BF16)
        NR = 9
        rows = sb("rows", [1, NR, TB], F32)
        carry = sb("carry", [1, 4], F32)
        ape = sb("ape", [1, 8], F32)
        dec = sb("dec", [1, 8], F32)
        cols = sb("cols", [64, 24], F32)
        decb = sb("decb", [128, 8], F32)
        wT = sb("wT", [64, 8, 64], F32)
        stl = sb("stl", [64, 64], BF16)
        cf = sb("cf", [128, 132], F32)
        cb = sb("cb", [128, 132], BF16)
        nums = sb("nums", [64, 8, 132], F32)
        sq8 = sb("sq8", [64, 8, 128], F32)
        sm = sb("sm", [64, 8, 8], F32)
        hmb = sb("hmb", [64, 8, 128], BF16)
        gt1, gt2 = cva, cvb
        yo = sb("yo", [128, TB], BF16)
        pT = [sb(f"pT{i}", [128, 512], BF16) for i in range(2)]
        at1 = sb("at1", [128, 128], F32)
        at2 = sb("at2", [128, 128], F32)
        atj = sb("atj", [128, 128], F32)
        asm = sb("asm", [128, 8], F32)
        hab = sb("hab", [128, 128], BF16)
        yab = sb("yab", [128, TB], BF16)
        ob = xblk

        ps_all = es.enter_context(nc.psum_tensor("ps_all", [128, 8, 512], F32))
        ps = [ps_all[:, i, :] for i in range(8)]

        def I(e, fn, r=(), w=(), **kw):
            return S.issue(e, fn, r, w, **kw)

        def dma(q, out, in_, r, w):
            return I(q, lambda e: e.dma_start(out=out, in_=in_), r, w, dma=True)

        def act(out, in_, func, r, w, scale=1.0, bias=0.0, accum=None):
            if accum is None:
                return I("act", lambda e: e.activation(out, in_, func, bias=bias, scale=scale), r, w)
            return I("act", lambda e: e.activation(out, in_, func, bias=bias, scale=scale, accum_out=accum), r, w)

        def tt(eng, out, a, b, op, r, w):
            return I(eng, lambda e: e.tensor_tensor(out, a, b, op), r, w)

        def ts(eng, out, a, s1, op0, r, w, s2=None, op1=ALU.bypass):
            return I(eng, lambda e: e.tensor_scalar(out, a, s1, s2, op0, op1), r, w)

        def stt(out, a, sc, b, op0, op1, r, w):
            return I("dve", lambda e: e.scalar_tensor_tensor(out, a, sc, b, op0, op1), r, w)

        def cp(eng, out, in_, r, w):
            if eng == "act":
                return I(eng, lambda e: e.copy(out, in_), r, w)
            return I(eng, lambda e: e.tensor_copy(out, in_), r, w)

        def mm(out, lhsT, rhs, start, stop, r, w):
            return I("pe", lambda e: e.matmul(out, lhsT, rhs, start=start, stop=stop), r, w)

        def tr(out, in_, idn, r, w):
            return I("pe", lambda e: e.transpose(out, in_, idn), r, w)

        def psb(i):
            return ps[i].bitcast(BF16)

        dma("sp", cst[:, :], consts[:, :], (), ("cst",))
        for l in range(DEPTH):
            dma("sp", vec[l][:, :], vecs[l][:, :], (), (("vec", l),))
        cp("dve", identb[:, :], cst[:, 0:128], ("cst",), ("identb",))
        I("pool", lambda e: e.memset(onesb[:, :], 1.0), (), ("onesb",))
        I("pool", lambda e: e.memset(onesf[:, :], 1.0), (), ("onesf",))
        I("pool", lambda e: e.memset(va[:, :, 128:132], 1.0), (), ("va_ones",))
        I("pool", lambda e: e.memset(vmc[:, :, 128:132], 1.0), (), ("vmc_ones",))
        mask64 = cst[0:64, 128:192]

        def load_weights(l):
            nchunk = 11
            for ci in range(nchunk):
                st = wstg[ci % 2]
                sk = ("wstg", ci % 2)
                if ci < 8:
                    src = wfm[l][:, ci * 128:(ci + 1) * 128]
                    dst = wfm_b[:, :, ci * 128:(ci + 1) * 128]
                    ncol = 128
                elif ci == 8:
                    src = wfm[l][:, 1024:1026]
                    dst = wfm_b[:, :, 1024:1026]
                    ncol = 2
                else:
                    src = wtm[l][:, (ci - 9) * 128:(ci - 8) * 128]
                    dst = wtm_b[:, :, (ci - 9) * 128:(ci - 8) * 128]
                    ncol = 128
                dma("sp", st[:, :, 0:ncol], src.rearrange("(kt p) c -> p kt c", p=128), (), (sk,))
                eng = "pool" if ci % 2 == 0 else "dve"
                cp(eng, dst, st[:, :, 0:ncol], (sk,), ("win",))
                if ci in (4, 6):
                    for m in range(2):
                        sl = wfm_b[:, :, ci * 128 + m * 64: ci * 128 + m * 64 + 32]
                        ts("pool", sl, sl, -1.0, ALU.mult, ("win",), ("win",))
            dma("sp", lamt[:, :], lamv[l][:, :], (), ("lamt",))
            for i in range(2):
                tt("dve", lamt[:, i * 128:i * 128 + 64], lamt[:, i * 128:i * 128 + 64],
                   lamt[:, i * 128 + 64:i * 128 + 128], ALU.mult, ("lamt",), ("lamt",))
                I("dve", lambda e, i=i: e.reduce_sum(lamw[:, i:i + 1], lamt[:, i * 128:i * 128 + 64], AX.X),
                  ("lamt",), ("lamw",))
            act(lamw[:, 2:4], lamw[:, 0:2], AF.Exp, ("lamw",), ("lamw",))
            lam_init = 0.8 - 0.6 * math.exp(-0.3 * l)
            stt(neglam[:, :], lamw[:, 3:4], -lam_init, lamw[:, 2:3], ALU.add, ALU.subtract, ("lamw",), ("neglam",))
            ts("pool", negfb[:, :], vec[l][0:1, 22:23], -1.0, ALU.mult, (("vec", l),), ("negfb",))

        def load_wout(l):
            for ci in range(8):
                st = wstg[ci % 2]
                sk = ("wstg", ci % 2)
                dma("sp", st[:, :, :], wout[l][:, ci * 128:(ci + 1) * 128].rearrange("(kt p) c -> p kt c", p=128),
                    (), (sk,))
                eng = "pool" if ci % 2 == 0 else "dve"
                cp(eng, wout_b[:, :, ci * 128:(ci + 1) * 128], st[:, :, :], (sk,), ("wout",))

        def outproj_block(l_prev, tb, xsrc, xkey):
            t0 = tb * TB
            q4, tq = tb // 4, (tb % 4) * TB
            dma("sp", ycb[:, :, :], ycf[l_prev][q4][:, tq:tq + TB].rearrange("(kt p) t -> p kt t", p=128),
                (("ycf", l_prev, q4),), ("ycb",))
            dma("sp", xblk[:, :, :], xsrc[:, t0:t0 + TB].rearrange("(kt p) t -> p kt t", p=128),
                (xkey,), ("xblk",))
            for m in range(KT):
                bank = m % 2
                for et in range(KT):
                    mm(ps[bank][:, :], wout_b[:, et, m * 128:(m + 1) * 128], ycb[:, et, :], et == 0, et == KT - 1,
                       ("wout", "ycb"), (("ps", bank),))
                tt("dve", xblk[:, m, :], xblk[:, m, :], ps[bank][:, :], ALU.add, (("ps", bank), "xblk"), ("xblk",))

        def norm_block(nw_ap, to_out):
            for kt in range(KT):
                tt("pool", sqb[:, kt, :], xblk[:, kt, :], xblk[:, kt, :], ALU.mult, ("xblk",), ("sqb",))
            for kt in range(KT):
                mm(ps[2][:, :], onesb[:, :], sqb[:, kt, :], kt == 0, kt == KT - 1, ("onesb", "sqb"), (("ps", 2),))
            ts("dve", rstd[:, :], ps[2][:, :], 1.0 / D_MODEL, ALU.mult, (("ps", 2),), ("rstd",), s2=EPS, op1=ALU.add)
            act(rstd[:, :], rstd[:, :], AF.Ln, ("rstd",), ("rstd",))
            act(rstd[:, :], rstd[:, :], AF.Exp, ("rstd",), ("rstd",), scale=-0.5)
            for kt in range(KT):
                if to_out:
                    stt(ob[:, kt, :], xblk[:, kt, :], nw_ap[:, kt:kt + 1], rstd[:, :], ALU.mult, ALU.mult,
                        ("xblk", "rstd"), ("xblk",))
                else:
                    stt(xnb[:, kt, :], xblk[:, kt, :], nw_ap[:, kt:kt + 1], rstd[:, :], ALU.mult, ALU.mult,
                        ("xblk", "rstd"), ("xnb",))

        def inproj_block(l, tb):
            t0 = tb * TB
            V = vec[l]
            vk = ("vec", l)
            dma("sp", cosb[:, :], cosT[:, t0:t0 + TB], (), ("cosb",))
            dma("sp", sinb[:, :], sinT[:, t0:t0 + TB], (), ("sinb",))
            bank = [0]

            def group(c0, ncol):
                b = bank[0] % 2
                bank[0] += 1
                for kt in range(KT):
                    mm(ps[b][0:ncol, :], wfm_b[:, kt, c0:c0 + ncol], xnb[:, kt, :], kt == 0, kt == KT - 1,
                       ("win", "xnb"), (("ps", b),))
                return b

            for which, (pre, dst, cw0, cbc) in enumerate([(preq, qm, 8, 16), (prek, km, 12, 17)]):
                b = group(which * 128, 128)
                pk = ("pre", which)
                if tb == 0:
                    I("pool", lambda e, pre=pre: e.memset(pre[:, 0:4], 0.0), (), (pk,))
                else:
                    cp("pool", pre[:, 1:4], pre[:, TB + 1:TB + 4], (pk,), (pk,))
                cp("act", pre[:, 4:4 + TB], ps[b][:, :], (("ps", b), pk), (pk,))
                ts("dve", cva[:, :], pre[:, 4:4 + TB], V[:, cw0 + 3:cw0 + 4], ALU.mult, (pk, vk), ("tA",),
                   s2=V[:, cbc:cbc + 1], op1=ALU.add)
                stt(cvb[:, :], pre[:, 3:3 + TB], V[:, cw0 + 2:cw0 + 3], cva[:, :], ALU.mult, ALU.add,
                    (pk, vk, "tA"), ("tB",))
                stt(cva[:, :], pre[:, 2:2 + TB], V[:, cw0 + 1:cw0 + 2], cvb[:, :], ALU.mult, ALU.add,
                    (pk, vk, "tB"), ("tA",))
                stt(cvb[:, :], pre[:, 1:1 + TB], V[:, cw0:cw0 + 1], cva[:, :], ALU.mult, ALU.add,
                    (pk, vk, "tA"), ("tB",))
                act(dst[:, :], cvb[:, :], AF.Silu, ("tB",), ("qm" if which == 0 else "km",))
            b = group(2 * 128, 128)
            act(gm[:, :], ps[b][:, :], AF.Silu, (("ps", b),), ("gm",))
            for which in range(2):
                b1 = group((3 + 2 * which) * 128, 128)
                tt("dve", rpa[:, :], ps[b1][:, :], cosb[:, :], ALU.mult, (("ps", b1), "cosb"), ("tA",))
                b2 = group((4 + 2 * which) * 128, 128)
                tt("dve", rpb[:, :], ps[b2][:, :], sinb[:, :], ALU.mult, (("ps", b2), "sinb"), ("tB",))
                if which == 0:
                    tt("pool", qa[:, :], rpa[:, :], rpb[:, :], ALU.add, ("tA", "tB"), ("qa",))
                else:
                    tt("pool", kaT[:, t0:t0 + TB], rpa[:, :], rpb[:, :], ALU.add, ("tA", "tB"), (("ka", tb),))
            b = group(7 * 128, 128)
            act(ga[:, :], ps[b][:, :], AF.Silu, (("ps", b),), ("ga",))
            for g in range(2):
                b = group(1024 + g, 1)
                cp("act", rows[:, g, :], ps[b][0:1, :], (("ps", b),), (("row", g),))
            for c8 in range(8):
                b = bank[0] % 2
                bank[0] += 1
                for kt in range(KT):
                    mm(ps[b][0:64, 0:128], xnb[:, kt, c8 * 64:(c8 + 1) * 64], wtm_b[:, kt, 0:128], kt == 0,
                       kt == KT - 1, ("win", "xnb"), (("ps", b),))
                cp("act" if c8 % 2 else "dve", vmc[:, c8, 0:128], ps[b][0:64, 0:128], (("ps", b),), ("vmc",))
            for t4 in range(4):
                b = bank[0] % 2
                bank[0] += 1
                for kt in range(KT):
                    mm(ps[b][:, 0:128], xnb[:, kt, t4 * 128:(t4 + 1) * 128], wtm_b[:, kt, 128:256], kt == 0,
                       kt == KT - 1, ("win", "xnb"), (("ps", b),))
                tile = tb * 4 + t4
                cp("act" if t4 % 2 else "dve", va[:, tile, 0:128], ps[b][:, 0:128], (("ps", b), "va_ones"),
                   (("va", tile),))

        R_I, R_F, R_L1, R_BN, R_A_, R_AA, R_WI, R_WK, R_EM = range(9)
        R_T2, R_T1, R_ALS = R_I, R_F, R_L1

        def rw(i):
            return rows[:, i, :]

        def gates_block(l, tb):
            V = vec[l]
            vk = ("vec", l)
            rk = lambda i: ("row", i)
            if tb == 0:
                I("pool", lambda e: e.memset(carry[:, :], 0.0), (), ("carry",))
            act(rw(R_T1), rw(R_F), AF.Exp, (rk(R_F), "negfb"), (rk(R_T1),), scale=-1.0, bias=negfb[:, :])
            act(rw(R_L1), rw(R_T1), AF.Ln, (rk(R_T1),), (rk(R_L1),), bias=1.0)
            I("dve", lambda e: e.tensor_tensor_scan(rw(R_BN), onesf[:, :], rw(R_L1), carry[:, 0:1], ALU.mult, ALU.add),
              ("onesf", rk(R_L1), "carry"), (rk(R_BN),))
            stt(rw(R_A_), rw(R_I), V[0:1, 21:22], rw(R_BN), ALU.add, ALU.add, (rk(R_I), vk, rk(R_BN)), (rk(R_A_),))
            I("dve", lambda e: e.tensor_tensor_scan(rw(R_AA), onesf[:, :], rw(R_A_), carry[:, 1:2], ALU.mult, ALU.max),
              ("onesf", rk(R_A_), "carry"), (rk(R_AA),))
            A3 = rw(R_AA).rearrange("p (c i) -> p c i", i=64)
            aend = A3[:, :, 63]
            cp("pool", ape[:, 0:1], carry[:, 1:2], ("carry",), ("ape",))
            cp("pool", ape[:, 1:8], A3[:, 0:7, 63], (rk(R_AA),), ("ape",))
            tt("dve", rw(R_T1).rearrange("p (c i) -> p c i", i=64), ape[:, :].unsqueeze(2).broadcast_to([1, 8, 64]),
               A3, ALU.subtract, ("ape", rk(R_AA)), (rk(R_T1),))
            act(rw(R_WI), rw(R_T1), AF.Exp, (rk(R_T1),), (rk(R_WI),))
            tt("dve", dec[:, :], ape[:, :], aend, ALU.subtract, ("ape", rk(R_AA)), ("dec",))
            act(dec[:, :], dec[:, :], AF.Exp, ("dec",), ("dec",))
            tt("dve", rw(R_T2).rearrange("p (c i) -> p c i", i=64), rw(R_A_).rearrange("p (c i) -> p c i", i=64),
               aend.unsqueeze(2).broadcast_to([1, 8, 64]), ALU.subtract, (rk(R_A_), rk(R_AA)), (rk(R_T2),))
            act(rw(R_WK), rw(R_T2), AF.Exp, (rk(R_T2),), (rk(R_WK),), bias=LN_KSCALE)
            tt("dve", rw(R_T1), rw(R_BN), rw(R_AA), ALU.subtract, (rk(R_BN), rk(R_AA)), (rk(R_T1),))
            act(rw(R_EM), rw(R_T1), AF.Exp, (rk(R_T1),), (rk(R_EM),))
            ts("pool", rw(R_ALS), rw(R_A_), LN_KSCALE, ALU.add, (rk(R_A_),), (rk(R_ALS),))
            cp("pool", carry[:, 0:1], rw(R_BN)[:, TB - 1:TB], (rk(R_BN),), ("carry",))
            cp("pool", carry[:, 1:2], rw(R_AA)[:, TB - 1:TB], (rk(R_AA), "ape"), ("carry",))
            for qi, ri in enumerate([R_ALS, R_WK, R_EM]):
                for c8 in range(8):
                    mm(ps[3][0:64, 480 + qi * 8 + c8:480 + qi * 8 + c8 + 1], rw(ri)[:, c8 * 64:(c8 + 1) * 64],
                       onesf[:, 0:1], True, True, (rk(ri), "onesf"), (("ps", 3),))
            cp("dve", cols[:, :], ps[3][0:64, 480:504], (("ps", 3),), ("cols",))
            mm(ps[3][:, 504:512], onesf[:, 0:128], dec[:, :], True, True, ("onesf", "dec"), (("ps", 3),))
            cp("dve", decb[:, :], ps[3][:, 504:512], (("ps", 3),), ("decb",))
            mm(ps[2][0:64, :], onesf[:, 0:64], rw(R_AA), True, True, ("onesf", rk(R_AA)), (("ps", 2),))
            for c8 in range(8):
                act(wT[:, c8, :], ps[2][0:64, c8 * 64:(c8 + 1) * 64], AF.Exp, (("ps", 2), "cols"), ("wT",),
                    scale=-1.0, bias=cols[:, c8:c8 + 1])
            tt("pool", wT[:, :, :], wT[:, :, :], mask64.unsqueeze(1).broadcast_to([64, 8, 64]), ALU.mult,
               ("wT", "cst"), ("wT",))
            mm(ps[2][:, :], onesf[:, 0:128], rw(R_WI), True, True, ("onesf", rk(R_WI)), (("ps", 2),))
            tt("dve", qs[:, :], qm[:, :], ps[2][:, :], ALU.mult, ("qm", ("ps", 2)), ("qs",))
            tt("pool", vmw[:, :, 0:129], vmc[:, :, 0:129], cols[:, 8:16].unsqueeze(2).broadcast_to([64, 8, 129]),
               ALU.mult, ("vmc", "vmc_ones", "cols"), ("vmw",))

        def mlstm_block(l, tb, ycl_ap, okey):
            V = vec[l]
            vk = ("vec", l)
            if tb == 0:
                I("pool", lambda e: e.memset(cf[:, :], 0.0), (), ("cf",))
                I("pool", lambda e: e.memset(cb[:, :], 0.0), (), ("cb",))
            for c8 in range(8):
                tr(psb(2)[0:64, c8 * 128:(c8 + 1) * 128], km[:, c8 * 64:(c8 + 1) * 64], identb[:, :],
                   ("km", "identb"), (("ps", 2),))
            cp("act", ktok[:, :, :], psb(2)[0:64, :].rearrange("p (c d) -> p c d", d=128), (("ps", 2),), ("ktok",))
            for c8 in range(8):
                cs = c8 * 64
                mm(ps[3][0:64, 0:64], km[:, cs:cs + 64], qm[:, cs:cs + 64], True, True, ("km", "qm"),
                   (("ps", 3),))
                tt("dve", stl[:, :], ps[3][0:64, 0:64], wT[:, c8, :], ALU.mult, (("ps", 3), "wT"), ("stl",))
                mm(ps[3][0:64, 64:193], qs[:, cs:cs + 64], cb[:, 0:129], True, False, ("qs", "cb"), (("ps", 3),))
                mm(ps[3][0:64, 64:193], stl[:, :], vmc[:, c8, 0:129], False, True, ("stl", "vmc", "vmc_ones"),
                   (("ps", 3),))
                cp("act", nums[:, c8, 0:129], ps[3][0:64, 64:193], (("ps", 3),), ("nums",))
                kv = 200 + (c8 % 2) * 136
                mm(ps[3][:, kv:kv + 129], ktok[:, c8, :], vmw[:, c8, 0:129], True, True, ("ktok", "vmw"),
                   (("ps", 3),))
                stt(cf[:, 0:129], cf[:, 0:129], decb[:, c8:c8 + 1], ps[3][:, kv:kv + 129], ALU.mult, ALU.add,
                    ("cf", "decb", ("ps", 3)), ("cf",))
                cp("pool", cb[:, 0:129], cf[:, 0:129], ("cf",), ("cb",))
            den = nums[:, :, 128]
            tt("dve", sq8[:, :, :], nums[:, :, 0:128], nums[:, :, 0:128], ALU.mult, ("nums",), ("sq8",))
            I("dve", lambda e: e.reduce_sum(sm[:, :, 0], sq8[:, :, :], AX.X), ("sq8",), ("sm",))
            ts("dve", sm[:, :, 1], den, -1.0, ALU.mult, ("nums",), ("sm",))
            tt("dve", sm[:, :, 1], sm[:, :, 1], den, ALU.max, ("sm", "nums"), ("sm",))
            tt("dve", sm[:, :, 1], sm[:, :, 1], cols[:, 16:24], ALU.max, ("sm", "cols"), ("sm",))
            tt("dve", sm[:, :, 2], sm[:, :, 1], sm[:, :, 1], ALU.mult, ("sm",), ("sm",))
            ts("dve", sm[:, :, 0], sm[:, :, 0], 1.0 / 128.0, ALU.mult, ("sm",), ("sm",))
            stt(sm[:, :, 3], sm[:, :, 2], EPS, sm[:, :, 0], ALU.mult, ALU.add, ("sm",), ("sm",))
            act(sm[:, :, 4], sm[:, :, 3], AF.Ln, ("sm",), ("sm",))
            act(sm[:, :, 5], sm[:, :, 4], AF.Exp, ("sm",), ("sm",), scale=-0.5)
            tt("dve", hmb[:, :, :], nums[:, :, 0:128], sm[:, :, 5:6].broadcast_to([64, 8, 128]), ALU.mult,
               ("nums", "sm"), ("hmb",))
            for c8 in range(8):
                tr(psb(2)[:, c8 * 64:(c8 + 1) * 64], hmb[:, c8, :], identb[0:64, 0:64], ("hmb", "identb"),
                   (("ps", 2),))
            ts("dve", gt1[:, :], qm[:, :], V[:, 19:20], ALU.mult, ("qm", vk), ("tA",))
            stt(gt2[:, :], psb(2)[:, 0:TB], V[:, 18:19], gt1[:, :], ALU.mult, ALU.add, (("ps", 2), vk, "tA"),
                ("tB",))
            tt("dve", yo[:, :], gt2[:, :], gm[:, :], ALU.mult, ("tB", "gm"), ("yo",))
            return dma("sp", ycl_ap, yo[:, :], ("yo",), (okey,))

        def attn_block(l, tb, ycl_ap, okey):
            V = vec[l]
            vk = ("vec", l)
            lam_init = 0.8 - 0.6 * math.exp(-0.3 * l)
            pcount = [0]
            for g2 in range(2):
                qt0 = tb * 4 + g2 * 2
                qoff = g2 * 256
                touched = set()
                for j in range(qt0 + 2):
                    full = j <= qt0
                    lo = 0 if full else 128
                    half = pcount[0] % 2
                    pt = pT[pcount[0] % 2]
                    ptk = ("pT", pcount[0] % 2)
                    sk = ("ps", 4)
                    sk2 = ("ps", 5)
                    pcount[0] += 1
                    kb = j // 4
                    for m in range(2):
                        mm(ps[4 + m][:, half * 256 + lo:(half + 1) * 256],
                           kaT[m * 64:(m + 1) * 64, j * 128:(j + 1) * 128],
                           qa[m * 64:(m + 1) * 64, qoff + lo:qoff + 256], True, True, (("ka", kb), "qa"), (sk, sk2))
                    src_ = ps_all[:, 4:6, half * 256 + lo:(half + 1) * 256]
                    dst = pt[:, :].rearrange("p (m q) -> p m q", m=2)[:, :, lo:256]
                    act(dst, src_, AF.Exp, (sk, sk2), (ptk,), scale=0.125)
                    if j >= qt0:
                        d0 = (j - qt0) * 128
                        msl = pt[64:128, :].rearrange("p (m q) -> p m q", m=2)[:, :, d0:d0 + 64]
                        I("pool", lambda e, msl=msl: e.memset(msl, 0.0), (ptk,), (ptk,))
                    for qt in range(2):
                        if j > qt0 + qt:
                            continue
                        pb = 6 + qt
                        for m in range(2):
                            first = pb not in touched
                            touched.add(pb)
                            last = (j == qt0 + qt) and m == 1
                            mm(ps[pb][:, m * 129:(m + 1) * 129], pt[:, m * 256 + qt * 128:m * 256 + (qt + 1) * 128],
                               va[:, j, 0:129], first, last, (ptk, ("va", j), "va_ones"), (("ps", pb),))
                        if j == qt0 + qt:
                            P = ps[pb]
                            pk = ("ps", pb)
                            I("dve", lambda e, P=P: e.reciprocal(asm[:, 0:1], P[:, 128:129]), (pk,), ("asm0",))
                            I("dve", lambda e, P=P: e.reciprocal(asm[:, 1:2], P[:, 257:258]), (pk,), ("asm1",))
                            tt("dve", asm[:, 1:2], asm[:, 1:2], neglam[:, :], ALU.mult, ("asm1", "neglam"), ("asm1",))
                            ts("dve", at1[:, :], P[:, 0:128], asm[:, 0:1], ALU.mult, (pk, "asm0"), ("at1",))
                            stt(at2[:, :], P[:, 129:257], asm[:, 1:2], at1[:, :], ALU.mult, ALU.add,
                                (pk, "asm1", "at1"), ("at2",))
                            act(atj[:, :], at2[:, :], AF.Square, ("at2",), ("atj", "asm2"), accum=asm[:, 2:3])
                            ts("dve", asm[:, 3:4], asm[:, 2:3], 1.0 / 128.0, ALU.mult, ("asm2",), ("asm3",),
                               s2=EPS, op1=ALU.add)
                            act(asm[:, 4:5], asm[:, 3:4], AF.Ln, ("asm3",), ("asm4",))
                            act(asm[:, 5:6], asm[:, 4:5], AF.Exp, ("asm4",), ("asm5",), scale=-0.5)
                            ts("dve", hab[:, :], at2[:, :], asm[:, 5:6], ALU.mult, ("at2", "asm5"), ("hab",),
                               s2=1.0 - lam_init, op1=ALU.mult)
                            tr(psb(2)[:, 0:128], hab[:, :], identb[:, :], ("hab", "identb"), (("ps", 2),))
                            c0 = qoff + qt * 128
                            stt(yab[:, c0:c0 + 128], psb(2)[:, 0:128], V[:, 20:21], ga[:, c0:c0 + 128], ALU.mult,
                                ALU.mult, (("ps", 2), vk, "ga"), ("yab",))
            return dma("sp", ycl_ap, yab[:, :], ("yab",), (okey,))

        out_toks = []

        for l in layers:
            load_weights(l)
            if l > 0:
                load_wout(l - 1)
            for tb in range(_NB):
                t0 = tb * TB
                q4, tq = tb // 4, (tb % 4) * TB
                if l == 0:
                    dma("sp", xblk[:, :, :], xT[:, t0:t0 + TB].rearrange("(kt p) t -> p kt t", p=128), (), ("xblk",))
                else:
                    outproj_block(l - 1, tb, xT, ("xsrc0", tb))
                    tok = dma("sp", x1s[:, t0:t0 + TB].rearrange("(kt p) t -> p kt t", p=128), xblk[:, :, :],
                              ("xblk",), (("xsrc1", tb),))
                    if not do_fin:
                        out_toks.append(tok)
                if _STOP >= 1:
                    norm_block(vec[l][:, 0:8], False)
                if _STOP >= 2:
                    inproj_block(l, tb)
                if _STOP >= 3:
                    gates_block(l, tb)
                if _STOP >= 4:
                    tok = mlstm_block(l, tb, ycl[l][q4][0:128, tq:tq + TB], ("ycl", l, q4, tb % 4, 0))
                    if not fused:
                        out_toks.append(tok)
                if _STOP >= 5:
                    tok = attn_block(l, tb, ycl[l][q4][128:256, tq:tq + TB], ("ycl", l, q4, tb % 4, 1))
                    if not fused:
                        out_toks.append(tok)
                if debug and l == layers[0] and _STOP >= 3:
                    out_toks.append(dma("sp", dbg["d_qm"][:, t0:t0 + TB], qm[:, :], ("qm",), ()))
                    out_toks.append(dma("sp", dbg["d_km"][:, t0:t0 + TB], km[:, :], ("km",), ()))
                    out_toks.append(dma("sp", dbg["d_qa"][:, t0:t0 + TB], qa[:, :], ("qa",), ()))
                    out_toks.append(dma("sp", dbg["d_rows"].rearrange("p (r t) -> p r t", r=NR)[:, :, t0:t0 + TB],
                                        rows[:, :, :], tuple(("row", i) for i in range(NR)), ()))
                if fused and tb % 4 == 3:
                    rk = tuple(("ycl", l, q4, i, w) for i in range(4) for w in range(2))
                    I("pool", lambda e, l=l, q4=q4: e.collective_compute(
                        "AllGather", ALU.bypass, replica_groups=GROUPS,
                        ins=[ycl[l][q4][:, :]], outs=[ycf[l][q4][:, :]]), rk, (("ycf", l, q4),), cc=True)
            if debug and l == layers[0] and _STOP >= 3 and _NB == NBLK:
                out_toks.append(dma("sp", dbg["d_ka"][:, :], kaT[:, :], tuple(("ka", i) for i in range(NBLK)), ()))

        if do_fin:
            load_wout(DEPTH - 1)
            for tb in range(NBLK):
                t0 = tb * TB
                outproj_block(DEPTH - 1, tb, x1s, ("xsrc1", tb))
                norm_block(vec[DEPTH - 1][:, 23:31], True)
                out_toks.append(dma("sp", outT[:, t0:t0 + TB].rearrange("(kt p) t -> p kt t", p=128), xblk[:, :, :],
                                    ("xblk",), ()))

        waits = []
        for tok in out_toks:
            S._tok_wait("sp", tok, waits)
        S.recs["sp"].append((waits, None, None))

        with nc.Block() as block:
            S.emit(block)
    return nc


_OFF = {"mq": 0, "mk": 512, "mv": 1024, "mi": 1536, "mf": 1540, "mz": 1544,
        "aq": 2056, "ak": 2568, "av": 3080, "az": 3592}


def _rope_tables():
    inv = (1.0 / (np.float32(10000.0) ** (np.arange(0, 64, 2, dtype=np.float32) / np.float32(64.0)))).astype(np.float32)
    ang = np.arange(SEQ, dtype=np.float32)[:, None] * inv[None, :]
    cos = np.cos(ang).astype(np.float32)
    sin = np.sin(ang).astype(np.float32)
    cosT = np.ascontiguousarray(np.concatenate([cos, cos, cos, cos], 1).T)
    sinT = np.ascontiguousarray(np.concatenate([sin, sin, sin, sin], 1).T)
    return cosT, sinT


def _consts():
    c = np.zeros((128, 192), np.float32)
    c[:, 0:128] = np.eye(128, dtype=np.float32)
    c[0:64, 128:192] = np.triu(np.ones((64, 64), np.float32))
    return c


def _core_inputs(inp, c, stage_layers, need_wout):
    b, hd = c // 4, c % 4
    f32 = np.float32
    d = {}
    for l in stage_layers:
        w = np.asarray(inp["w_in"][l], f32)
        hs = slice(hd * 128, (hd + 1) * 128)

        def blk(name):
            return w[:, _OFF[name] + hd * 128:_OFF[name] + (hd + 1) * 128]

        def perm(m):
            return np.concatenate([m[:, 32:64], m[:, 0:32], m[:, 96:128], m[:, 64:96]], 1)

        aq, ak = blk("aq"), blk("ak")
        gi = w[:, _OFF["mi"] + hd:_OFF["mi"] + hd + 1]
        gf = w[:, _OFF["mf"] + hd:_OFF["mf"] + hd + 1]
        d[f"wfm{l}"] = np.ascontiguousarray(np.concatenate(
            [blk("mq"), blk("mk"), blk("mz"), aq, perm(aq), ak, perm(ak), blk("az"), gi, gf], 1))
        d[f"wtm{l}"] = np.ascontiguousarray(np.concatenate([blk("mv"), blk("av")], 1))
        lam = np.concatenate([np.asarray(inp[k][l], f32) for k in ("lam_q1", "lam_k1", "lam_q2", "lam_k2")])
        d[f"lamv{l}"] = np.ascontiguousarray(np.tile(lam[None, :], (128, 1)))
    for l in range(DEPTH):
        v = np.zeros((128, NV), f32)
        v[:, 0:8] = np.asarray(inp["norm_w"][l], f32).reshape(8, 128).T
        cw = np.asarray(inp["conv_w"][l], f32)
        cbv = np.asarray(inp["conv_b"][l], f32)
        v[:, 8:12] = cw[:, hd * 128:(hd + 1) * 128].T
        v[:, 12:16] = cw[:, 512 + hd * 128:512 + (hd + 1) * 128].T
        v[:, 16] = cbv[hd * 128:(hd + 1) * 128]
        v[:, 17] = cbv[512 + hd * 128:512 + (hd + 1) * 128]
        v[:, 18] = np.asarray(inp["m_norm_w"][l], f32)[hd * 128:(hd + 1) * 128]
        v[:, 19] = np.asarray(inp["m_skip"][l], f32)[hd * 128:(hd + 1) * 128]
        v[:, 20] = np.asarray(inp["a_norm_w"][l], f32)
        v[:, 21] = np.asarray(inp["i_bias"][l], f32)[hd]
        v[:, 22] = np.asarray(inp["f_bias"][l], f32)[hd]
        v[:, 23:31] = np.asarray(inp["final_norm_w"], f32).reshape(8, 128).T
        d[f"vecs{l}"] = v
    for l in need_wout:
        wo = np.asarray(inp["w_out"][l], f32)
        rows = []
        for r in range(4):
            rows.append(wo[r * 128:(r + 1) * 128])
            rows.append(wo[512 + r * 128:512 + (r + 1) * 128])
        d[f"wout{l}"] = np.ascontiguousarray(np.concatenate(rows, 0))
    return d


_PROG = {}


def _prog(stage, debug=False):
    key = (stage, debug)
    if key not in _PROG:
        _PROG[key] = build_program(stage, debug)
    return _PROG[key]


def kernel(**inp):
    x = np.asarray(inp["x"], np.float32)
    xTs = [np.ascontiguousarray(x[b].T) for b in range(BATCH)]
    cosT, sinT = _rope_tables()
    cst = _consts()
    nc = _prog("all")
    in_maps = []
    for c in range(NCORES):
        d = _core_inputs(inp, c, [0, 1], [0, 1])
        d.update({"xT": xTs[c // 4], "cosT": cosT, "sinT": sinT, "consts": cst})
        in_maps.append(d)
    res = run_bass_kernel_spmd(nc, in_maps, core_ids=list(range(NCORES)))
    out = np.empty((BATCH, SEQ, D_MODEL), np.float32)
    for b in range(BATCH):
        out[b] = res.results[4 * b]["outT"].T
    return out
```

```python
import math
from contextlib import ExitStack

import numpy as np
import ml_dtypes

import concourse.bass as bass
import concourse.mybir as mybir
from concourse.bass_utils import run_bass_kernel_spmd

F32 = mybir.dt.float32
BF16 = mybir.dt.bfloat16
AF = mybir.ActivationFunctionType
ALU = mybir.AluOpType
AX = mybir.AxisListType

D_MODEL = 1024
BATCH = 2
SEQ = 8192
DEPTH = 2
NCORES = 8
TB = 512
NBLK = SEQ // TB
KT = D_MODEL // 128
EPS = 1e-6
NV = 32
LN_KSCALE = math.log(128.0 ** -0.5)
GROUPS = [[0, 1, 2, 3], [4, 5, 6, 7]]
import os
_STOP = int(os.environ.get("K_STOP", "9"))
_NB = int(os.environ.get("K_NBLK", str(NBLK)))


class Sched:
    CH = 30000
    ND = 48

    def __init__(self, nc, es):
        self.nc = nc
        self.engs = ["pe", "act", "dve", "pool", "sp"]
        self.recs = {e: [] for e in self.engs}
        self.cnt = {e: 0 for e in self.engs}
        self.seen = {e: {} for e in self.engs}
        self.lastw = {}
        self.readers = {}
        nsem = {"pe": 3, "act": 3, "dve": 4, "pool": 3, "sp": 1}
        self.esems = {e: [es.enter_context(nc.semaphore(f"s_{e}_{i}")) for i in range(nsem[e])]
                      for e in self.engs}
        self.dsems = [es.enter_context(nc.semaphore(f"d_{i}")) for i in range(self.ND)]
        self.dval = [0] * self.ND
        self.dnext = 0
        self.ccsem = es.enter_context(nc.semaphore("ccsem"))
        self.ccval = 0

    def _tok_wait(self, e, tok, waits):
        if tok[0] == "e":
            _, e2, n = tok
            if e2 == e and e == "pe":
                return
            if self.seen[e].get(e2, 0) >= n:
                return
            self.seen[e][e2] = n
            waits.append((self.esems[e2][(n - 1) // self.CH], (n - 1) % self.CH + 1))
        else:
            kind, i, v = tok
            key = (kind, i)
            if self.seen[e].get(key, 0) >= v:
                return
            self.seen[e][key] = v
            sem = self.dsems[i] if kind == "d" else self.ccsem
            waits.append((sem, v))

    def issue(self, e, fn, reads=(), writes=(), dma=False, cc=False):
        deps = []
        for k in reads:
            if k in self.lastw:
                deps.append(self.lastw[k])
        for k in writes:
            if k in self.lastw:
                deps.append(self.lastw[k])
            deps.extend(self.readers.get(k, {}).values())
        waits = []
        for tok in deps:
            self._tok_wait(e, tok, waits)
        if dma:
            i = self.dnext
            self.dnext = (self.dnext + 1) % self.ND
            prev = self.dval[i]
            if prev > 0:
                self._tok_wait(e, ("d", i, prev), waits)
            self.dval[i] += 16
            tok = ("d", i, self.dval[i])
            inc = (self.dsems[i], 16)
        elif cc:
            self.ccval += 1
            tok = ("c", 0, self.ccval)
            inc = (self.ccsem, 1)
        elif fn is None:
            tok = None
            inc = None
        else:
            self.cnt[e] += 1
            n = self.cnt[e]
            tok = ("e", e, n)
            inc = (self.esems[e][(n - 1) // self.CH], 1)
        self.recs[e].append((waits, fn, inc))
        if tok is not None:
            for k in reads:
                self.readers.setdefault(k, {})[(tok[0], tok[1])] = tok
            for k in writes:
                self.lastw[k] = tok
                self.readers[k] = {}
        return tok

    def emit(self, block):
        nc = self.nc

        def run(e, eng):
            for waits, fn, inc in self.recs[e]:
                for s, v in waits:
                    eng.wait_ge(s, v)
                if fn is not None:
                    ins = fn(eng)
                    ins.then_inc(inc[0], inc[1])

        @block.tensor
        def _(eng):
            run("pe", eng)

        @block.scalar
        def _(eng):
            run("act", eng)

        @block.vector
        def _(eng):
            run("dve", eng)

        @block.gpsimd
        def _(eng):
            run("pool", eng)

        @block.sync
        def _(eng):
            run("sp", eng)


def build_program(stage="all", debug=False):
    nc = bass.Bass("TRN2", target_bir_lowering=False)
    with ExitStack() as es:
        S = Sched(nc, es)

        def dram(name, shape, dt, kind):
            return nc.dram_tensor(name, shape, dt, kind=kind).ap()

        fused = stage == "all"
        layers = {"all": [0, 1], "l0": [0], "mid": [1], "fin": []}[stage]
        do_fin = stage in ("all", "fin")

        xT = dram("xT", [D_MODEL, SEQ], F32, "ExternalInput") if stage in ("all", "l0", "mid") else None
        cosT = dram("cosT", [128, SEQ], F32, "ExternalInput") if layers else None
        sinT = dram("sinT", [128, SEQ], F32, "ExternalInput") if layers else None
        consts = dram("consts", [128, 192], F32, "ExternalInput")
        wfm, wtm, wout, vecs, lamv = {}, {}, {}, {}, {}
        for l in layers:
            wfm[l] = dram(f"wfm{l}", [D_MODEL, 1026], F32, "ExternalInput")
            wtm[l] = dram(f"wtm{l}", [D_MODEL, 256], F32, "ExternalInput")
            lamv[l] = dram(f"lamv{l}", [128, 256], F32, "ExternalInput")
        for l in range(DEPTH):
            vecs[l] = dram(f"vecs{l}", [128, NV], F32, "ExternalInput")
        need_wout = {"all": [0, 1], "l0": [], "mid": [0], "fin": [1]}[stage]
        for l in need_wout:
            wout[l] = dram(f"wout{l}", [D_MODEL, D_MODEL], F32, "ExternalInput")

        ycl, ycf = {}, {}
        for l in layers:
            kind = "Internal" if fused else "ExternalOutput"
            ycl[l] = [dram(f"ycl{l}_{q}", [256, 2048], BF16, kind) for q in range(4)]
        for l in need_wout:
            kind = "Internal" if fused else "ExternalInput"
            ycf[l] = [dram(f"ycf{l}_{q}", [1024, 2048], BF16, kind) for q in range(4)]
        x1s = None
        if stage == "all":
            x1s = dram("x1s", [D_MODEL, SEQ], F32, "Internal")
        elif stage == "mid":
            x1s = dram("x1s", [D_MODEL, SEQ], F32, "ExternalOutput")
        elif stage == "fin":
            x1s = dram("x1s", [D_MODEL, SEQ], F32, "ExternalInput")
        outT = dram("outT", [D_MODEL, SEQ], F32, "ExternalOutput") if do_fin else None
        dbg = {}
        if debug:
            for nm, shp, dt in [("d_qm", [128, SEQ], BF16), ("d_km", [128, SEQ], BF16),
                                ("d_qa", [128, SEQ], BF16), ("d_ka", [128, SEQ], BF16),
                                ("d_rows", [1, 9 * SEQ], F32)]:
                dbg[nm] = dram(nm, shp, dt, "ExternalOutput")

        def sb(name, shape, dt):
            return es.enter_context(nc.sbuf_tensor(name, shape, dt))

        cst = sb("cst", [128, 192], F32)
        ident_f = cst[:, 0:128]
        identb = sb("identb", [128, 128], BF16)
        onesb = sb("onesb", [128, 128], BF16)
        onesf = sb("onesf", [1, 512], F32)
        vec = [sb(f"vec{l}", [128, NV], F32) for l in range(DEPTH)]
        lamt = sb("lamt", [128, 256], F32)
        lamw = sb("lamw", [128, 4], F32)
        neglam = sb("neglam", [128, 1], F32)
        negfb = sb("negfb", [1, 1], F32)

        wfm_b = sb("wfm_b", [128, KT, 1026], BF16)
        wtm_b = sb("wtm_b", [128, KT, 256], BF16)
        wout_b = sb("wout_b", [128, KT, D_MODEL], BF16)
        wstg = [sb(f"wstg{i}", [128, KT, 128], F32) for i in range(2)]

        xblk = sb("xblk", [128, KT, TB], F32)
        sqb = sb("sqb", [128, KT, TB], BF16)
        xnb = sb("xnb", [128, KT, TB], BF16)
        ycb = sqb
        rstd = sb("rstd", [128, TB], F32)
        cosb = sb("cosb", [128, TB], F32)
        sinb = sb("sinb", [128, TB], F32)
        preq = sb("preq", [128, TB + 4], F32)
        prek = sb("prek", [128, TB + 4], F32)
        cva = sb("tA", [128, TB], F32)
        cvb = sb("tB", [128, TB], F32)
        rpa, rpb = cva, cvb
        qa2 = [sb(f"qa{i}", [128, TB], BF16) for i in range(2)]
        qm = sb("qm", [128, TB], BF16)
        km = sb("km", [128, TB], BF16)
        qs = sb("qs", [128, TB], BF16)
        gm = sb("gm", [128, TB], F32)
        ga2 = [sb(f"ga{i}", [128, TB], F32) for i in range(2)]
        kaT = sb("kaT", [128, SEQ], BF16)
        va = sb("va", [128, SEQ // 128, 132], BF16)
        vmc = sb("vmc", [64, 8, 132], BF16)
        vmw = sb("vmw", [64, 8, 132], BF16)
        ktok = sb("ktok", [64, 8, 128], BF16)
        NR = 9
        rows = sb("rows", [1, NR, TB], F32)
        carry = sb("carry", [1, 4], F32)
        ape = sb("ape", [1, 8], F32)
        dec = sb("dec", [1, 8], F32)
        cols = sb("cols", [64, 24], F32)
        decb = sb("decb", [128, 8], F32)
        wT = sb("wT", [64, 8, 64], F32)
        stl = sb("stl", [64, 64], BF16)
        cf = sb("cf", [128, 132], F32)
        cb = sb("cb", [128, 132], BF16)
        nums = sb("nums", [64, 8, 132], F32)
        sq8 = sb("sq8", [64, 8, 128], F32)
        sm = sb("sm", [64, 8, 8], F32)
        hmb = sb("hmb", [64, 8, 128], BF16)
        gt1, gt2 = cva, cvb
        yo = sb("yo", [128, TB], BF16)
        pT = [sb(f"pT{i}", [128, 512], BF16) for i in range(2)]
        at1 = sb("at1", [128, 128], F32)
        at2 = sb("at2", [128, 128], F32)
        atj = sb("atj", [128, 128], F32)
        asm = sb("asm", [128, 8], F32)
        hab = sb("hab", [128, 128], BF16)
        yab = sb("yab", [128, TB], BF16)
        ob = xblk

        ps_all = es.enter_context(nc.psum_tensor("ps_all", [128, 8, 512], F32))
        ps = [ps_all[:, i, :] for i in range(8)]

        def I(e, fn, r=(), w=(), **kw):
            return S.issue(e, fn, r, w, **kw)

        def dma(q, out, in_, r, w):
            return I(q, lambda e: e.dma_start(out=out, in_=in_), r, w, dma=True)

        def act(out, in_, func, r, w, scale=1.0, bias=0.0, accum=None):
            if accum is None:
                return I("act", lambda e: e.activation(out, in_, func, bias=bias, scale=scale), r, w)
            return I("act", lambda e: e.activation(out, in_, func, bias=bias, scale=scale, accum_out=accum), r, w)

        def tt(eng, out, a, b, op, r, w):
            return I(eng, lambda e: e.tensor_tensor(out, a, b, op), r, w)

        def ts(eng, out, a, s1, op0, r, w, s2=None, op1=ALU.bypass):
            return I(eng, lambda e: e.tensor_scalar(out, a, s1, s2, op0, op1), r, w)

        def stt(out, a, sc, b, op0, op1, r, w):
            return I("dve", lambda e: e.scalar_tensor_tensor(out, a, sc, b, op0, op1), r, w)

        def cp(eng, out, in_, r, w):
            if eng == "act":
                return I(eng, lambda e: e.copy(out, in_), r, w)
            return I(eng, lambda e: e.tensor_copy(out, in_), r, w)

        def mm(out, lhsT, rhs, start, stop, r, w):
            return I("pe", lambda e: e.matmul(out, lhsT, rhs, start=start, stop=stop), r, w)

        def tr(out, in_, idn, r, w):
            return I("pe", lambda e: e.transpose(out, in_, idn), r, w)

        def psb(i):
            return ps[i].bitcast(BF16)

        dma("sp", cst[:, :], consts[:, :], (), ("cst",))
        for l in range(DEPTH):
            dma("sp", vec[l][:, :], vecs[l][:, :], (), (("vec", l),))
        cp("dve", identb[:, :], cst[:, 0:128], ("cst",), ("identb",))
        I("pool", lambda e: e.memset(onesb[:, :], 1.0), (), ("onesb",))
        I("pool", lambda e: e.memset(onesf[:, :], 1.0), (), ("onesf",))
        I("pool", lambda e: e.memset(va[:, :, 128:132], 1.0), (), ("va_ones",))
        I("pool", lambda e: e.memset(vmc[:, :, 128:132], 1.0), (), ("vmc_ones",))
        mask64 = cst[0:64, 128:192]

        def load_weights(l):
            nchunk = 11
            for ci in range(nchunk):
                st = wstg[ci % 2]
                sk = ("wstg", ci % 2)
                if ci < 8:
                    src = wfm[l][:, ci * 128:(ci + 1) * 128]
                    dst = wfm_b[:, :, ci * 128:(ci + 1) * 128]
                    ncol = 128
                elif ci == 8:
                    src = wfm[l][:, 1024:1026]
                    dst = wfm_b[:, :, 1024:1026]
                    ncol = 2
                else:
                    src = wtm[l][:, (ci - 9) * 128:(ci - 8) * 128]
                    dst = wtm_b[:, :, (ci - 9) * 128:(ci - 8) * 128]
                    ncol = 128
                dma("sp", st[:, :, 0:ncol], src.rearrange("(kt p) c -> p kt c", p=128), (), (sk,))
                eng = "pool" if ci % 2 == 0 else "dve"
                cp(eng, dst, st[:, :, 0:ncol], (sk,), ("win",))
                if ci in (4, 6):
                    for m in range(2):
                        sl = wfm_b[:, :, ci * 128 + m * 64: ci * 128 + m * 64 + 32]
                        ts("pool", sl, sl, -1.0, ALU.mult, ("win",), ("win",))
            dma("sp", lamt[:, :], lamv[l][:, :], (), ("lamt",))
            for i in range(2):
                tt("dve", lamt[:, i * 128:i * 128 + 64], lamt[:, i * 128:i * 128 + 64],
                   lamt[:, i * 128 + 64:i * 128 + 128], ALU.mult, ("lamt",), ("lamt",))
                I("dve", lambda e, i=i: e.reduce_sum(lamw[:, i:i + 1], lamt[:, i * 128:i * 128 + 64], AX.X),
                  ("lamt",), ("lamw",))
            act(lamw[:, 2:4], lamw[:, 0:2], AF.Exp, ("lamw",), ("lamw",))
            lam_init = 0.8 - 0.6 * math.exp(-0.3 * l)
            stt(neglam[:, :], lamw[:, 3:4], -lam_init, lamw[:, 2:3], ALU.add, ALU.subtract, ("lamw",), ("neglam",))
            ts("pool", negfb[:, :], vec[l][0:1, 22:23], -1.0, ALU.mult, (("vec", l),), ("negfb",))

        def load_wout(l):
            for ci in range(8):
                st = wstg[ci % 2]
                sk = ("wstg", ci % 2)
                dma("sp", st[:, :, :], wout[l][:, ci * 128:(ci + 1) * 128].rearrange("(kt p) c -> p kt c", p=128),
                    (), (sk,))
                eng = "pool" if ci % 2 == 0 else "dve"
                cp(eng, wout_b[:, :, ci * 128:(ci + 1) * 128], st[:, :, :], (sk,), ("wout",))

        def outproj_block(l_prev, tb, xsrc, xkey):
            t0 = tb * TB
            q4, tq = tb // 4, (tb % 4) * TB
            dma("sp", ycb[:, :, :], ycf[l_prev][q4][:, tq:tq + TB].rearrange("(kt p) t -> p kt t", p=128),
                (("ycf", l_prev, q4),), ("ycb",))
            dma("sp", xblk[:, :, :], xsrc[:, t0:t0 + TB].rearrange("(kt p) t -> p kt t", p=128),
                (xkey,), ("xblk",))
            for m in range(KT):
                bank = m % 2
                for et in range(KT):
                    mm(ps[bank][:, :], wout_b[:, et, m * 128:(m + 1) * 128], ycb[:, et, :], et == 0, et == KT - 1,
                       ("wout", "ycb"), (("ps", bank),))
                tt("dve", xblk[:, m, :], xblk[:, m, :], ps[bank][:, :], ALU.add, (("ps", bank), "xblk"), ("xblk",))
                yield

        def norm_block(nw_ap, to_out):
            for kt in range(KT):
                tt("pool", sqb[:, kt, :], xblk[:, kt, :], xblk[:, kt, :], ALU.mult, ("xblk",), ("sqb",))
            for kt in range(KT):
                mm(ps[0][:, :], onesb[:, :], sqb[:, kt, :], kt == 0, kt == KT - 1, ("onesb", "sqb"), (("ps", 0),))
            yield
            ts("dve", rstd[:, :], ps[0][:, :], 1.0 / D_MODEL, ALU.mult, (("ps", 0),), ("rstd",), s2=EPS, op1=ALU.add)
            act(rstd[:, :], rstd[:, :], AF.Ln, ("rstd",), ("rstd",))
            act(rstd[:, :], rstd[:, :], AF.Exp, ("rstd",), ("rstd",), scale=-0.5)
            for kt in range(KT):
                if to_out:
                    stt(ob[:, kt, :], xblk[:, kt, :], nw_ap[:, kt:kt + 1], rstd[:, :], ALU.mult, ALU.mult,
                        ("xblk", "rstd"), ("xblk",))
                else:
                    stt(xnb[:, kt, :], xblk[:, kt, :], nw_ap[:, kt:kt + 1], rstd[:, :], ALU.mult, ALU.mult,
                        ("xblk", "rstd"), ("xnb",))
            yield

        def inproj_block(l, tb):
            t0 = tb * TB
            qa, ga = qa2[tb % 2], ga2[tb % 2]
            qak, gak = ("qa", tb % 2), ("ga", tb % 2)
            V = vec[l]
            vk = ("vec", l)
            dma("sp", cosb[:, :], cosT[:, t0:t0 + TB], (), ("cosb",))
            dma("sp", sinb[:, :], sinT[:, t0:t0 + TB], (), ("sinb",))
            bank = [0]

            def group(c0, ncol):
                b = bank[0] % 2
                bank[0] += 1
                for kt in range(KT):
                    mm(ps[b][0:ncol, :], wfm_b[:, kt, c0:c0 + ncol], xnb[:, kt, :], kt == 0, kt == KT - 1,
                       ("win", "xnb"), (("ps", b),))
                return b

            for which, (pre, dst, cw0, cbc) in enumerate([(preq, qm, 8, 16), (prek, km, 12, 17)]):
                b = group(which * 128, 128)
                pk = ("pre", which)
                if tb == 0:
                    I("pool", lambda e, pre=pre: e.memset(pre[:, 0:4], 0.0), (), (pk,))
                else:
                    cp("pool", pre[:, 1:4], pre[:, TB + 1:TB + 4], (pk,), (pk,))
                cp("act", pre[:, 4:4 + TB], ps[b][:, :], (("ps", b), pk), (pk,))
                ts("dve", cva[:, :], pre[:, 4:4 + TB], V[:, cw0 + 3:cw0 + 4], ALU.mult, (pk, vk), ("tA",),
                   s2=V[:, cbc:cbc + 1], op1=ALU.add)
                stt(cvb[:, :], pre[:, 3:3 + TB], V[:, cw0 + 2:cw0 + 3], cva[:, :], ALU.mult, ALU.add,
                    (pk, vk, "tA"), ("tB",))
                stt(cva[:, :], pre[:, 2:2 + TB], V[:, cw0 + 1:cw0 + 2], cvb[:, :], ALU.mult, ALU.add,
                    (pk, vk, "tB"), ("tA",))
                stt(cvb[:, :], pre[:, 1:1 + TB], V[:, cw0:cw0 + 1], cva[:, :], ALU.mult, ALU.add,
                    (pk, vk, "tA"), ("tB",))
                act(dst[:, :], cvb[:, :], AF.Silu, ("tB",), ("qm" if which == 0 else "km",))
                yield
            b = group(2 * 128, 128)
            act(gm[:, :], ps[b][:, :], AF.Silu, (("ps", b),), ("gm",))
            yield
            for which in range(2):
                b1 = group((3 + 2 * which) * 128, 128)
                tt("dve", rpa[:, :], ps[b1][:, :], cosb[:, :], ALU.mult, (("ps", b1), "cosb"), ("tA",))
                yield
                b2 = group((4 + 2 * which) * 128, 128)
                tt("dve", rpb[:, :], ps[b2][:, :], sinb[:, :], ALU.mult, (("ps", b2), "sinb"), ("tB",))
                if which == 0:
                    tt("pool", qa[:, :], rpa[:, :], rpb[:, :], ALU.add, ("tA", "tB"), (qak,))
                else:
                    tt("pool", kaT[:, t0:t0 + TB], rpa[:, :], rpb[:, :], ALU.add, ("tA", "tB"), (("ka", tb),))
                yield
            b = group(7 * 128, 128)
            act(ga[:, :], ps[b][:, :], AF.Silu, (("ps", b),), (gak,))
            yield
            for g in range(2):
                b = group(1024 + g, 1)
                cp("act", rows[:, g, :], ps[b][0:1, :], (("ps", b),), (("row", g),))
                yield
            for c8 in range(8):
                b = bank[0] % 2
                bank[0] += 1
                for kt in range(KT):
                    mm(ps[b][0:64, 0:128], xnb[:, kt, c8 * 64:(c8 + 1) * 64], wtm_b[:, kt, 0:128], kt == 0,
                       kt == KT - 1, ("win", "xnb"), (("ps", b),))
                cp("act" if c8 % 2 else "dve", vmc[:, c8, 0:128], ps[b][0:64, 0:128], (("ps", b),), ("vmc",))
                if c8 % 2:
                    yield
            for t4 in range(4):
                b = bank[0] % 2
                bank[0] += 1
                for kt in range(KT):
                    mm(ps[b][:, 0:128], xnb[:, kt, t4 * 128:(t4 + 1) * 128], wtm_b[:, kt, 128:256], kt == 0,
                       kt == KT - 1, ("win", "xnb"), (("ps", b),))
                tile = tb * 4 + t4
                cp("act" if t4 % 2 else "dve", va[:, tile, 0:128], ps[b][:, 0:128], (("ps", b), "va_ones"),
                   (("va", tile),))
                if t4 % 2:
                    yield

        R_I, R_F, R_L1, R_BN, R_A_, R_AA, R_WI, R_WK, R_EM = range(9)
        R_T2, R_T1, R_ALS = R_I, R_F, R_L1

        def rw(i):
            return rows[:, i, :]

        def gates_block(l, tb):
            V = vec[l]
            vk = ("vec", l)
            rk = lambda i: ("row", i)
            if tb == 0:
                I("pool", lambda e: e.memset(carry[:, :], 0.0), (), ("carry",))
            act(rw(R_T1), rw(R_F), AF.Exp, (rk(R_F), "negfb"), (rk(R_T1),), scale=-1.0, bias=negfb[:, :])
            act(rw(R_L1), rw(R_T1), AF.Ln, (rk(R_T1),), (rk(R_L1),), bias=1.0)
            I("dve", lambda e: e.tensor_tensor_scan(rw(R_BN), onesf[:, :], rw(R_L1), carry[:, 0:1], ALU.mult, ALU.add),
              ("onesf", rk(R_L1), "carry"), (rk(R_BN),))
            stt(rw(R_A_), rw(R_I), V[0:1, 21:22], rw(R_BN), ALU.add, ALU.add, (rk(R_I), vk, rk(R_BN)), (rk(R_A_),))
            I("dve", lambda e: e.tensor_tensor_scan(rw(R_AA), onesf[:, :], rw(R_A_), carry[:, 1:2], ALU.mult, ALU.max),
              ("onesf", rk(R_A_), "carry"), (rk(R_AA),))
            yield
            A3 = rw(R_AA).rearrange("p (c i) -> p c i", i=64)
            aend = A3[:, :, 63]
            cp("pool", ape[:, 0:1], carry[:, 1:2], ("carry",), ("ape",))
            cp("pool", ape[:, 1:8], A3[:, 0:7, 63], (rk(R_AA),), ("ape",))
            tt("dve", rw(R_T1).rearrange("p (c i) -> p c i", i=64), ape[:, :].unsqueeze(2).broadcast_to([1, 8, 64]),
               A3, ALU.subtract, ("ape", rk(R_AA)), (rk(R_T1),))
            act(rw(R_WI), rw(R_T1), AF.Exp, (rk(R_T1),), (rk(R_WI),))
            tt("dve", dec[:, :], ape[:, :], aend, ALU.subtract, ("ape", rk(R_AA)), ("dec",))
            act(dec[:, :], dec[:, :], AF.Exp, ("dec",), ("dec",))
            tt("dve", rw(R_T2).rearrange("p (c i) -> p c i", i=64), rw(R_A_).rearrange("p (c i) -> p c i", i=64),
               aend.unsqueeze(2).broadcast_to([1, 8, 64]), ALU.subtract, (rk(R_A_), rk(R_AA)), (rk(R_T2),))
            act(rw(R_WK), rw(R_T2), AF.Exp, (rk(R_T2),), (rk(R_WK),), bias=LN_KSCALE)
            tt("dve", rw(R_T1), rw(R_BN), rw(R_AA), ALU.subtract, (rk(R_BN), rk(R_AA)), (rk(R_T1),))
            act(rw(R_EM), rw(R_T1), AF.Exp, (rk(R_T1),), (rk(R_EM),))
            ts("pool", rw(R_ALS), rw(R_A_), LN_KSCALE, ALU.add, (rk(R_A_),), (rk(R_ALS),))
            cp("pool", carry[:, 0:1], rw(R_BN)[:, TB - 1:TB], (rk(R_BN),), ("carry",))
            cp("pool", carry[:, 1:2], rw(R_AA)[:, TB - 1:TB], (rk(R_AA), "ape"), ("carry",))
            yield
            for qi, ri in enumerate([R_ALS, R_WK, R_EM]):
                for c8 in range(8):
                    mm(ps[1][0:64, 480 + qi * 8 + c8:480 + qi * 8 + c8 + 1], rw(ri)[:, c8 * 64:(c8 + 1) * 64],
                       onesf[:, 0:1], True, True, (rk(ri), "onesf"), (("ps", 1),))
            cp("dve", cols[:, :], ps[1][0:64, 480:504], (("ps", 1),), ("cols",))
            mm(ps[1][:, 504:512], onesf[:, 0:128], dec[:, :], True, True, ("onesf", "dec"), (("ps", 1),))
            cp("dve", decb[:, :], ps[1][:, 504:512], (("ps", 1),), ("decb",))
            yield
            mm(ps[0][0:64, :], onesf[:, 0:64], rw(R_AA), True, True, ("onesf", rk(R_AA)), (("ps", 0),))
            for c8 in range(8):
                act(wT[:, c8, :], ps[0][0:64, c8 * 64:(c8 + 1) * 64], AF.Exp, (("ps", 0), "cols"), ("wT",),
                    scale=-1.0, bias=cols[:, c8:c8 + 1])
            tt("pool", wT[:, :, :], wT[:, :, :], mask64.unsqueeze(1).broadcast_to([64, 8, 64]), ALU.mult,
               ("wT", "cst"), ("wT",))
            yield
            mm(ps[0][:, :], onesf[:, 0:128], rw(R_WI), True, True, ("onesf", rk(R_WI)), (("ps", 0),))
            tt("dve", qs[:, :], qm[:, :], ps[0][:, :], ALU.mult, ("qm", ("ps", 0)), ("qs",))
            tt("pool", vmw[:, :, 0:129], vmc[:, :, 0:129], cols[:, 8:16].unsqueeze(2).broadcast_to([64, 8, 129]),
               ALU.mult, ("vmc", "vmc_ones", "cols"), ("vmw",))
            yield

        def mlstm_block(l, tb, ycl_ap, okey, res):
            V = vec[l]
            vk = ("vec", l)
            if tb == 0:
                I("pool", lambda e: e.memset(cf[:, :], 0.0), (), ("cf",))
                I("pool", lambda e: e.memset(cb[:, :], 0.0), (), ("cb",))
            for c8 in range(8):
                tr(psb(0)[0:64, c8 * 128:(c8 + 1) * 128], km[:, c8 * 64:(c8 + 1) * 64], identb[:, :],
                   ("km", "identb"), (("ps", 0),))
            cp("act", ktok[:, :, :], psb(0)[0:64, :].rearrange("p (c d) -> p c d", d=128), (("ps", 0),), ("ktok",))
            yield
            for c8 in range(8):
                cs = c8 * 64
                mm(ps[1][0:64, 0:64], km[:, cs:cs + 64], qm[:, cs:cs + 64], True, True, ("km", "qm"),
                   (("ps", 1),))
                tt("dve", stl[:, :], ps[1][0:64, 0:64], wT[:, c8, :], ALU.mult, (("ps", 1), "wT"), ("stl",))
                mm(ps[1][0:64, 64:193], qs[:, cs:cs + 64], cb[:, 0:129], True, False, ("qs", "cb"), (("ps", 1),))
                mm(ps[1][0:64, 64:193], stl[:, :], vmc[:, c8, 0:129], False, True, ("stl", "vmc", "vmc_ones"),
                   (("ps", 1),))
                cp("act", nums[:, c8, 0:129], ps[1][0:64, 64:193], (("ps", 1),), ("nums",))
                kv = 200 + (c8 % 2) * 136
                mm(ps[1][:, kv:kv + 129], ktok[:, c8, :], vmw[:, c8, 0:129], True, True, ("ktok", "vmw"),
                   (("ps", 1),))
                stt(cf[:, 0:129], cf[:, 0:129], decb[:, c8:c8 + 1], ps[1][:, kv:kv + 129], ALU.mult, ALU.add,
                    ("cf", "decb", ("ps", 1)), ("cf",))
                cp("pool", cb[:, 0:129], cf[:, 0:129], ("cf",), ("cb",))
                yield
            den = nums[:, :, 128]
            tt("dve", sq8[:, :, :], nums[:, :, 0:128], nums[:, :, 0:128], ALU.mult, ("nums",), ("sq8",))
            I("dve", lambda e: e.reduce_sum(sm[:, :, 0], sq8[:, :, :], AX.X), ("sq8",), ("sm",))
            ts("dve", sm[:, :, 1], den, -1.0, ALU.mult, ("nums",), ("sm",))
            tt("dve", sm[:, :, 1], sm[:, :, 1], den, ALU.max, ("sm", "nums"), ("sm",))
            tt("dve", sm[:, :, 1], sm[:, :, 1], cols[:, 16:24], ALU.max, ("sm", "cols"), ("sm",))
            tt("dve", sm[:, :, 2], sm[:, :, 1], sm[:, :, 1], ALU.mult, ("sm",), ("sm",))
            ts("dve", sm[:, :, 0], sm[:, :, 0], 1.0 / 128.0, ALU.mult, ("sm",), ("sm",))
            stt(sm[:, :, 3], sm[:, :, 2], EPS, sm[:, :, 0], ALU.mult, ALU.add, ("sm",), ("sm",))
            act(sm[:, :, 4], sm[:, :, 3], AF.Ln, ("sm",), ("sm",))
            act(sm[:, :, 5], sm[:, :, 4], AF.Exp, ("sm",), ("sm",), scale=-0.5)
            tt("dve", hmb[:, :, :], nums[:, :, 0:128], sm[:, :, 5:6].broadcast_to([64, 8, 128]), ALU.mult,
               ("nums", "sm"), ("hmb",))
            for c8 in range(8):
                tr(psb(0)[:, c8 * 64:(c8 + 1) * 64], hmb[:, c8, :], identb[0:64, 0:64], ("hmb", "identb"),
                   (("ps", 0),))
            yield
            ts("dve", gt1[:, :], qm[:, :], V[:, 19:20], ALU.mult, ("qm", vk), ("tA",))
            stt(gt2[:, :], psb(0)[:, 0:TB], V[:, 18:19], gt1[:, :], ALU.mult, ALU.add, (("ps", 0), vk, "tA"),
                ("tB",))
            tt("dve", yo[:, :], gt2[:, :], gm[:, :], ALU.mult, ("tB", "gm"), ("yo",))
            res.append(dma("sp", ycl_ap, yo[:, :], ("yo",), (okey,)))
            yield

        def attn_block(l, tb, ycl_ap, okey, res):
            V = vec[l]
            vk = ("vec", l)
            qa, ga = qa2[tb % 2], ga2[tb % 2]
            qak, gak = ("qa", tb % 2), ("ga", tb % 2)
            lam_init = 0.8 - 0.6 * math.exp(-0.3 * l)
            tiles = []
            for g2 in range(2):
                qt0 = tb * 4 + g2 * 2
                for j in range(qt0 + 2):
                    tiles.append((g2, qt0, j))

            def emit_S(ti):
                g2, qt0, j = tiles[ti]
                qoff = g2 * 256
                lo = 0 if j <= qt0 else 128
                sb0 = 2 + 2 * (ti % 2)
                kb = j // 4
                for m in range(2):
                    mm(ps[sb0 + m][:, lo:256], kaT[m * 64:(m + 1) * 64, j * 128:(j + 1) * 128],
                       qa[m * 64:(m + 1) * 64, qoff + lo:qoff + 256], True, True, (("ka", kb), qak),
                       (("ps", sb0), ("ps", sb0 + 1)))

            touched = set()
            emit_S(0)
            for ti, (g2, qt0, j) in enumerate(tiles):
                if ti + 1 < len(tiles):
                    emit_S(ti + 1)
                if j == 0:
                    touched = set()
                qoff = g2 * 256
                lo = 0 if j <= qt0 else 128
                sb0 = 2 + 2 * (ti % 2)
                pt = pT[ti % 2]
                ptk = ("pT", ti % 2)
                src_ = ps_all[:, sb0:sb0 + 2, lo:256]
                dst = pt[:, :].rearrange("p (m q) -> p m q", m=2)[:, :, lo:256]
                act(dst, src_, AF.Exp, (("ps", sb0), ("ps", sb0 + 1)), (ptk,), scale=0.125)
                if j >= qt0:
                    d0 = (j - qt0) * 128
                    msl = pt[64:128, :].rearrange("p (m q) -> p m q", m=2)[:, :, d0:d0 + 64]
                    I("pool", lambda e, msl=msl: e.memset(msl, 0.0), (ptk,), (ptk,))
                for qt in range(2):
                    if j > qt0 + qt:
                        continue
                    pb = 6 + qt
                    for m in range(2):
                        first = pb not in touched
                        touched.add(pb)
                        last = (j == qt0 + qt) and m == 1
                        mm(ps[pb][:, m * 129:(m + 1) * 129], pt[:, m * 256 + qt * 128:m * 256 + (qt + 1) * 128],
                           va[:, j, 0:129], first, last, (ptk, ("va", j), "va_ones"), (("ps", pb),))
                    if j == qt0 + qt:
                        P = ps[pb]
                        pk = ("ps", pb)
                        I("dve", lambda e, P=P: e.reciprocal(asm[:, 0:1], P[:, 128:129]), (pk,), ("asm0",))
                        I("dve", lambda e, P=P: e.reciprocal(asm[:, 1:2], P[:, 257:258]), (pk,), ("asm1",))
                        tt("dve", asm[:, 1:2], asm[:, 1:2], neglam[:, :], ALU.mult, ("asm1", "neglam"), ("asm1",))
                        ts("dve", at1[:, :], P[:, 0:128], asm[:, 0:1], ALU.mult, (pk, "asm0"), ("at1",))
                        stt(at2[:, :], P[:, 129:257], asm[:, 1:2], at1[:, :], ALU.mult, ALU.add,
                            (pk, "asm1", "at1"), ("at2",))
                        tt("pool", atj[:, :], at2[:, :], at2[:, :], ALU.mult, ("at2",), ("atj",))
                        I("dve", lambda e: e.reduce_sum(asm[:, 2:3], atj[:, :], AX.X), ("atj",), ("asm2",))
                        ts("dve", asm[:, 3:4], asm[:, 2:3], 1.0 / 128.0, ALU.mult, ("asm2",), ("asm3",),
                           s2=EPS, op1=ALU.add)
                        act(asm[:, 4:5], asm[:, 3:4], AF.Ln, ("asm3",), ("asm4",))
                        act(asm[:, 5:6], asm[:, 4:5], AF.Exp, ("asm4",), ("asm5",), scale=-0.5)
                        ts("dve", hab[:, :], at2[:, :], asm[:, 5:6], ALU.mult, ("at2", "asm5"), ("hab",),
                           s2=1.0 - lam_init, op1=ALU.mult)
                        tr(psb(0)[:, 0:128], hab[:, :], identb[:, :], ("hab", "identb"), (("ps", 0),))
                        c0 = qoff + qt * 128
                        stt(yab[:, c0:c0 + 128], psb(0)[:, 0:128], V[:, 20:21], ga[:, c0:c0 + 128], ALU.mult,
                            ALU.mult, (("ps", 0), vk, gak), ("yab",))
                yield
            res.append(dma("sp", ycl_ap, yab[:, :], ("yab",), (okey,)))

        out_toks = []

        def front(l, tb):
            t0 = tb * TB
            q4, tq = tb // 4, (tb % 4) * TB
            if l == 0:
                dma("sp", xblk[:, :, :], xT[:, t0:t0 + TB].rearrange("(kt p) t -> p kt t", p=128), (), ("xblk",))
            else:
                yield from outproj_block(l - 1, tb, xT, ("xsrc0", tb))
                tok = dma("sp", x1s[:, t0:t0 + TB].rearrange("(kt p) t -> p kt t", p=128), xblk[:, :, :],
                          ("xblk",), (("xsrc1", tb),))
                if not do_fin:
                    out_toks.append(tok)
            yield from norm_block(vec[l][:, 0:8], False)
            yield from inproj_block(l, tb)
            yield from gates_block(l, tb)
            res = []
            yield from mlstm_block(l, tb, ycl[l][q4][0:128, tq:tq + TB], ("ycl", l, q4, tb % 4, 0), res)
            if not fused:
                out_toks.extend(res)

        def drain(g):
            for _ in g:
                pass

        NF = 60.0

        for l in layers:
            load_weights(l)
            if l > 0:
                load_wout(l - 1)
            drain(front(l, 0))
            for tb in range(_NB):
                q4, tq = tb // 4, (tb % 4) * TB
                res = []
                ag = attn_block(l, tb, ycl[l][q4][128:256, tq:tq + TB], ("ycl", l, q4, tb % 4, 1), res)
                fg = front(l, tb + 1) if tb + 1 < _NB else iter(())
                ntile = 8 * tb + 6
                ratio = NF / ntile
                acc = 0.0
                fdone = False
                for _ in ag:
                    acc += ratio
                    while acc >= 1.0 and not fdone:
                        acc -= 1.0
                        try:
                            next(fg)
                        except StopIteration:
                            fdone = True
                drain(fg)
                if not fused:
                    out_toks.extend(res)
                if fused and tb % 4 == 3:
                    rk = tuple(("ycl", l, q4, i, w) for i in range(4) for w in range(2))
                    I("pool", lambda e, l=l, q4=q4: e.collective_compute(
                        "AllGather", ALU.bypass, replica_groups=GROUPS,
                        ins=[ycl[l][q4][:, :]], outs=[ycf[l][q4][:, :]]), rk, (("ycf", l, q4),), cc=True)

        if do_fin:
            load_wout(DEPTH - 1)
            for tb in range(_NB):
                t0 = tb * TB
                drain(outproj_block(DEPTH - 1, tb, x1s, ("xsrc1", tb)))
                drain(norm_block(vec[DEPTH - 1][:, 23:31], True))
                out_toks.append(dma("sp", outT[:, t0:t0 + TB].rearrange("(kt p) t -> p kt t", p=128), xblk[:, :, :],
                                    ("xblk",), ()))

        waits = []
        for tok in out_toks:
            S._tok_wait("sp", tok, waits)
        S.recs["sp"].append((waits, None, None))

        with nc.Block() as block:
            S.emit(block)
    return nc


_OFF = {"mq": 0, "mk": 512, "mv": 1024, "mi": 1536, "mf": 1540, "mz": 1544,
        "aq": 2056, "ak": 2568, "av": 3080, "az": 3592}


def _rope_tables():
    inv = (1.0 / (np.float32(10000.0) ** (np.arange(0, 64, 2, dtype=np.float32) / np.float32(64.0)))).astype(np.float32)
    ang = np.arange(SEQ, dtype=np.float32)[:, None] * inv[None, :]
    cos = np.cos(ang).astype(np.float32)
    sin = np.sin(ang).astype(np.float32)
    cosT = np.ascontiguousarray(np.concatenate([cos, cos, cos, cos], 1).T)
    sinT = np.ascontiguousarray(np.concatenate([sin, sin, sin, sin], 1).T)
    return cosT, sinT


def _consts():
    c = np.zeros((128, 192), np.float32)
    c[:, 0:128] = np.eye(128, dtype=np.float32)
    c[0:64, 128:192] = np.triu(np.ones((64, 64), np.float32))
    return c


def _core_inputs(inp, c, stage_layers, need_wout):
    b, hd = c // 4, c % 4
    f32 = np.float32
    d = {}
    for l in stage_layers:
        w = np.asarray(inp["w_in"][l], f32)
        hs = slice(hd * 128, (hd + 1) * 128)

        def blk(name):
            return w[:, _OFF[name] + hd * 128:_OFF[name] + (hd + 1) * 128]

        def perm(m):
            return np.concatenate([m[:, 32:64], m[:, 0:32], m[:, 96:128], m[:, 64:96]], 1)

        aq, ak = blk("aq"), blk("ak")
        gi = w[:, _OFF["mi"] + hd:_OFF["mi"] + hd + 1]
        gf = w[:, _OFF["mf"] + hd:_OFF["mf"] + hd + 1]
        d[f"wfm{l}"] = np.ascontiguousarray(np.concatenate(
            [blk("mq"), blk("mk"), blk("mz"), aq, perm(aq), ak, perm(ak), blk("az"), gi, gf], 1))
        d[f"wtm{l}"] = np.ascontiguousarray(np.concatenate([blk("mv"), blk("av")], 1))
        lam = np.concatenate([np.asarray(inp[k][l], f32) for k in ("lam_q1", "lam_k1", "lam_q2", "lam_k2")])
        d[f"lamv{l}"] = np.ascontiguousarray(np.tile(lam[None, :], (128, 1)))
    for l in range(DEPTH):
        v = np.zeros((128, NV), f32)
        v[:, 0:8] = np.asarray(inp["norm_w"][l], f32).reshape(8, 128).T
        cw = np.asarray(inp["conv_w"][l], f32)
        cbv = np.asarray(inp["conv_b"][l], f32)
        v[:, 8:12] = cw[:, hd * 128:(hd + 1) * 128].T
        v[:, 12:16] = cw[:, 512 + hd * 128:512 + (hd + 1) * 128].T
        v[:, 16] = cbv[hd * 128:(hd + 1) * 128]
        v[:, 17] = cbv[512 + hd * 128:512 + (hd + 1) * 128]
        v[:, 18] = np.asarray(inp["m_norm_w"][l], f32)[hd * 128:(hd + 1) * 128]
        v[:, 19] = np.asarray(inp["m_skip"][l], f32)[hd * 128:(hd + 1) * 128]
        v[:, 20] = np.asarray(inp["a_norm_w"][l], f32)
        v[:, 21] = np.asarray(inp["i_bias"][l], f32)[hd]
        v[:, 22] = np.asarray(inp["f_bias"][l], f32)[hd]
        v[:, 23:31] = np.asarray(inp["final_norm_w"], f32).reshape(8, 128).T
        d[f"vecs{l}"] = v
    for l in need_wout:
        wo = np.asarray(inp["w_out"][l], f32)
        rows = []
        for r in range(4):
            rows.append(wo[r * 128:(r + 1) * 128])
            rows.append(wo[512 + r * 128:512 + (r + 1) * 128])
        d[f"wout{l}"] = np.ascontiguousarray(np.concatenate(rows, 0))
    return d


_PROG = {}


def _prog(stage, debug=False):
    key = (stage, debug)
    if key not in _PROG:
        _PROG[key] = build_program(stage, debug)
    return _PROG[key]


def kernel(**inp):
    x = np.asarray(inp["x"], np.float32)
    xTs = [np.ascontiguousarray(x[b].T) for b in range(BATCH)]
    cosT, sinT = _rope_tables()
    cst = _consts()
    nc = _prog("all")
    in_maps = []
    for c in range(NCORES):
        d = _core_inputs(inp, c, [0, 1], [0, 1])
        d.update({"xT": xTs[c // 4], "cosT": cosT, "sinT": sinT, "consts": cst})
        in_maps.append(d)
    res = run_bass_kernel_spmd(nc, in_maps, core_ids=list(range(NCORES)))
    out = np.empty((BATCH, SEQ, D_MODEL), np.float32)
    for b in range(BATCH):
        out[b] = res.results[4 * b]["outT"].T
    return out
```

```python
import math
from contextlib import ExitStack

import numpy as np
import ml_dtypes

import concourse.bass as bass
import concourse.mybir as mybir
from concourse.bass_utils import run_bass_kernel_spmd

F32 = mybir.dt.float32
BF16 = mybir.dt.bfloat16
AF = mybir.ActivationFunctionType
ALU = mybir.AluOpType
AX = mybir.AxisListType

D_MODEL = 1024
BATCH = 2
SEQ = 8192
DEPTH = 2
NCORES = 8
TB = 512
NBLK = SEQ // TB
KT = D_MODEL // 128
EPS = 1e-6
NV = 32
LN_KSCALE = math.log(128.0 ** -0.5)
GROUPS = [[0, 1, 2, 3], [4, 5, 6, 7]]
import os
_STOP = int(os.environ.get("K_STOP", "9"))
_NB = int(os.environ.get("K_NBLK", str(NBLK)))


class Sched:
    CH = 30000
    ND = 48

    def __init__(self, nc, es):
        self.nc = nc
        self.engs = ["pe", "act", "dve", "pool", "sp"]
        self.recs = {e: [] for e in self.engs}
        self.cnt = {e: 0 for e in self.engs}
        self.seen = {e: {} for e in self.engs}
        self.lastw = {}
        self.readers = {}
        nsem = {"pe": 3, "act": 3, "dve": 4, "pool": 3, "sp": 1}
        self.esems = {e: [es.enter_context(nc.semaphore(f"s_{e}_{i}")) for i in range(nsem[e])]
                      for e in self.engs}
        self.dsems = [es.enter_context(nc.semaphore(f"d_{i}")) for i in range(self.ND)]
        self.dval = [0] * self.ND
        self.dnext = 0
        self.ccsem = es.enter_context(nc.semaphore("ccsem"))
        self.ccval = 0

    def _tok_wait(self, e, tok, waits):
        if tok[0] == "e":
            _, e2, n = tok
            if e2 == e and e == "pe":
                return
            if self.seen[e].get(e2, 0) >= n:
                return
            self.seen[e][e2] = n
            waits.append((self.esems[e2][(n - 1) // self.CH], (n - 1) % self.CH + 1))
        else:
            kind, i, v = tok
            key = (kind, i)
            if self.seen[e].get(key, 0) >= v:
                return
            self.seen[e][key] = v
            sem = self.dsems[i] if kind == "d" else self.ccsem
            waits.append((sem, v))

    def issue(self, e, fn, reads=(), writes=(), dma=False, cc=False):
        deps = []
        for k in reads:
            if k in self.lastw:
                deps.append(self.lastw[k])
        for k in writes:
            if k in self.lastw:
                deps.append(self.lastw[k])
            deps.extend(self.readers.get(k, {}).values())
        waits = []
        for tok in deps:
            self._tok_wait(e, tok, waits)
        if dma:
            i = self.dnext
            self.dnext = (self.dnext + 1) % self.ND
            prev = self.dval[i]
            if prev > 0:
                self._tok_wait(e, ("d", i, prev), waits)
            self.dval[i] += 16
            tok = ("d", i, self.dval[i])
            inc = (self.dsems[i], 16)
        elif cc:
            self.ccval += 1
            tok = ("c", 0, self.ccval)
            inc = (self.ccsem, 1)
        elif fn is None:
            tok = None
            inc = None
        else:
            self.cnt[e] += 1
            n = self.cnt[e]
            tok = ("e", e, n)
            inc = (self.esems[e][(n - 1) // self.CH], 1)
        self.recs[e].append((waits, fn, inc))
        if tok is not None:
            for k in reads:
                self.readers.setdefault(k, {})[(tok[0], tok[1])] = tok
            for k in writes:
                self.lastw[k] = tok
                self.readers[k] = {}
        return tok

    def emit(self, block):
        nc = self.nc

        def run(e, eng):
            for waits, fn, inc in self.recs[e]:
                for s, v in waits:
                    eng.wait_ge(s, v)
                if fn is not None:
                    ins = fn(eng)
                    ins.then_inc(inc[0], inc[1])

        @block.tensor
        def _(eng):
            run("pe", eng)

        @block.scalar
        def _(eng):
            run("act", eng)

        @block.vector
        def _(eng):
            run("dve", eng)

        @block.gpsimd
        def _(eng):
            run("pool", eng)

        @block.sync
        def _(eng):
            run("sp", eng)


def build_program(stage="all", debug=False):
    nc = bass.Bass("TRN2", target_bir_lowering=False)
    with ExitStack() as es:
        S = Sched(nc, es)

        def dram(name, shape, dt, kind):
            return nc.dram_tensor(name, shape, dt, kind=kind).ap()

        fused = stage == "all"
        layers = {"all": [0, 1], "l0": [0], "mid": [1], "fin": []}[stage]
        do_fin = stage in ("all", "fin")

        xT = dram("xT", [D_MODEL, SEQ], F32, "ExternalInput") if stage in ("all", "l0", "mid") else None
        cosT = dram("cosT", [128, SEQ], F32, "ExternalInput") if layers else None
        sinT = dram("sinT", [128, SEQ], F32, "ExternalInput") if layers else None
        consts = dram("consts", [128, 192], F32, "ExternalInput")
        wfm, wtm, wout, vecs, lamv = {}, {}, {}, {}, {}
        for l in layers:
            wfm[l] = dram(f"wfm{l}", [D_MODEL, 1026], F32, "ExternalInput")
            wtm[l] = dram(f"wtm{l}", [D_MODEL, 256], F32, "ExternalInput")
            lamv[l] = dram(f"lamv{l}", [128, 256], F32, "ExternalInput")
        for l in range(DEPTH):
            vecs[l] = dram(f"vecs{l}", [128, NV], F32, "ExternalInput")
        need_wout = {"all": [0, 1], "l0": [], "mid": [0], "fin": [1]}[stage]
        for l in need_wout:
            wout[l] = dram(f"wout{l}", [D_MODEL, D_MODEL], F32, "ExternalInput")

        ycl, ycf = {}, {}
        for l in layers:
            kind = "Internal" if fused else "ExternalOutput"
            ycl[l] = [dram(f"ycl{l}_{q}", [256, 2048], BF16, kind) for q in range(4)]
        for l in need_wout:
            kind = "Internal" if fused else "ExternalInput"
            ycf[l] = [dram(f"ycf{l}_{q}", [1024, 2048], BF16, kind) for q in range(4)]
        x1s = None
        if stage == "all":
            x1s = dram("x1s", [D_MODEL, SEQ], F32, "Internal")
        elif stage == "mid":
            x1s = dram("x1s", [D_MODEL, SEQ], F32, "ExternalOutput")
        elif stage == "fin":
            x1s = dram("x1s", [D_MODEL, SEQ], F32, "ExternalInput")
        outT = dram("outT", [D_MODEL, SEQ], F32, "ExternalOutput") if do_fin else None
        dbg = {}
        if debug:
            for nm, shp, dt in [("d_qm", [128, SEQ], BF16), ("d_km", [128, SEQ], BF16),
                                ("d_qa", [128, SEQ], BF16), ("d_ka", [128, SEQ], BF16),
                                ("d_rows", [1, 9 * SEQ], F32)]:
                dbg[nm] = dram(nm, shp, dt, "ExternalOutput")

        def sb(name, shape, dt):
            return es.enter_context(nc.sbuf_tensor(name, shape, dt))

        cst = sb("cst", [128, 192], F32)
        ident_f = cst[:, 0:128]
        identb = sb("identb", [128, 128], BF16)
        onesb = sb("onesb", [128, 128], BF16)
        onesf = sb("onesf", [1, 512], F32)
        vec = [sb(f"vec{l}", [128, NV], F32) for l in range(DEPTH)]
        lamt = sb("lamt", [128, 256], F32)
        lamw = sb("lamw", [128, 4], F32)
        neglam = sb("neglam", [128, 1], F32)
        negfb = sb("negfb", [1, 1], F32)

        wfm_b = sb("wfm_b", [128, KT, 1026], BF16)
        wtm_b = sb("wtm_b", [128, KT, 256], BF16)
        wout_b = sb("wout_b", [128, KT, D_MODEL], BF16)
        wstg = [sb(f"wstg{i}", [128, KT, 128], F32) for i in range(2)]

        xblk = sb("xblk", [128, KT, TB], F32)
        sqb = sb("sqb", [128, KT, TB], BF16)
        xnb = sb("xnb", [128, KT, TB], BF16)
        ycb = sqb
        rstd = sb("rstd", [128, TB], F32)
        cosb = sb("cosb", [128, TB], F32)
        sinb = sb("sinb", [128, TB], F32)
        preq = sb("preq", [128, TB + 4], F32)
        prek = sb("prek", [128, TB + 4], F32)
        cva = sb("tA", [128, TB], F32)
        cvb = sb("tB", [128, TB], F32)
        tC = sb("tC", [128, TB], F32)
        rpa, rpb = cva, cvb
        qa2 = [sb(f"qa{i}", [128, TB], BF16) for i in range(2)]
        qm = sb("qm", [128, TB], BF16)
        km = sb("km", [128, TB], BF16)
        qs = sb("qs", [128, TB], BF16)
        gm = sb("gm", [128, TB], F32)
        ga2 = [sb(f"ga{i}", [128, TB], F32) for i in range(2)]
        kaT = sb("kaT", [128, SEQ], BF16)
        va = sb("va", [128, SEQ // 128, 132], BF16)
        vmc = sb("vmc", [64, 8, 132], BF16)
        vmw = sb("vmw", [64, 8, 132], BF16)
        ktok = sb("ktok", [64, 8, 128], BF16)
        NR = 9
        rows = sb("rows", [1, NR, TB], F32)
        carry = sb("carry", [1, 4], F32)
        ape = sb("ape", [1, 8], F32)
        dec = sb("dec", [1, 8], F32)
        cols = sb("cols", [64, 24], F32)
        decb = sb("decb", [128, 8], F32)
        wT = sb("wT", [64, 8, 64], F32)
        stl = [sb(f"stl{i}", [64, 64], BF16) for i in range(2)]
        cf = sb("cf", [128, 132], F32)
        cb = sb("cb", [128, 132], BF16)
        nums = sb("nums", [64, 8, 132], F32)
        sq8 = sb("sq8", [64, 8, 128], F32)
        sm = sb("sm", [64, 8, 8], F32)
        hmb = sb("hmb", [64, 8, 128], BF16)
        gt1, gt2 = cva, cvb
        yo = sb("yo", [128, TB], BF16)
        pT = [sb(f"pT{i}", [128, 512], BF16) for i in range(2)]
        at1 = sb("at1", [128, 128], F32)
        at2 = sb("at2", [128, 128], F32)
        atj = sb("atj", [128, 128], F32)
        asm = sb("asm", [128, 8], F32)
        hab = sb("hab", [128, 128], BF16)
        yab = sb("yab", [128, TB], BF16)
        ob = xblk

        ps_all = es.enter_context(nc.psum_tensor("ps_all", [128, 8, 512], F32))
        ps = [ps_all[:, i, :] for i in range(8)]

        def I(e, fn, r=(), w=(), **kw):
            return S.issue(e, fn, r, w, **kw)

        def dma(q, out, in_, r, w):
            return I(q, lambda e: e.dma_start(out=out, in_=in_), r, w, dma=True)

        def act(out, in_, func, r, w, scale=1.0, bias=0.0, accum=None):
            if accum is None:
                return I("act", lambda e: e.activation(out, in_, func, bias=bias, scale=scale), r, w)
            return I("act", lambda e: e.activation(out, in_, func, bias=bias, scale=scale, accum_out=accum), r, w)

        def tt(eng, out, a, b, op, r, w):
            return I(eng, lambda e: e.tensor_tensor(out, a, b, op), r, w)

        def ts(eng, out, a, s1, op0, r, w, s2=None, op1=ALU.bypass):
            return I(eng, lambda e: e.tensor_scalar(out, a, s1, s2, op0, op1), r, w)

        def stt(out, a, sc, b, op0, op1, r, w):
            return I("dve", lambda e: e.scalar_tensor_tensor(out, a, sc, b, op0, op1), r, w)

        def cp(eng, out, in_, r, w):
            if eng == "act":
                return I(eng, lambda e: e.copy(out, in_), r, w)
            return I(eng, lambda e: e.tensor_copy(out, in_), r, w)

        def mm(out, lhsT, rhs, start, stop, r, w):
            return I("pe", lambda e: e.matmul(out, lhsT, rhs, start=start, stop=stop), r, w)

        def tr(out, in_, idn, r, w):
            return I("pe", lambda e: e.transpose(out, in_, idn), r, w)

        def psb(i):
            return ps[i].bitcast(BF16)

        dma("sp", cst[:, :], consts[:, :], (), ("cst",))
        for l in range(DEPTH):
            dma("sp", vec[l][:, :], vecs[l][:, :], (), (("vec", l),))
        cp("dve", identb[:, :], cst[:, 0:128], ("cst",), ("identb",))
        I("pool", lambda e: e.memset(onesb[:, :], 1.0), (), ("onesb",))
        I("pool", lambda e: e.memset(onesf[:, :], 1.0), (), ("onesf",))
        I("pool", lambda e: e.memset(va[:, :, 128:132], 1.0), (), ("va_ones",))
        I("pool", lambda e: e.memset(vmc[:, :, 128:132], 1.0), (), ("vmc_ones",))
        mask64 = cst[0:64, 128:192]

        def load_weights(l):
            nchunk = 11
            for ci in range(nchunk):
                st = wstg[ci % 2]
                sk = ("wstg", ci % 2)
                if ci < 8:
                    src = wfm[l][:, ci * 128:(ci + 1) * 128]
                    dst = wfm_b[:, :, ci * 128:(ci + 1) * 128]
                    ncol = 128
                elif ci == 8:
                    src = wfm[l][:, 1024:1026]
                    dst = wfm_b[:, :, 1024:1026]
                    ncol = 2
                else:
                    src = wtm[l][:, (ci - 9) * 128:(ci - 8) * 128]
                    dst = wtm_b[:, :, (ci - 9) * 128:(ci - 8) * 128]
                    ncol = 128
                dma("sp", st[:, :, 0:ncol], src.rearrange("(kt p) c -> p kt c", p=128), (), (sk,))
                eng = "pool" if ci % 2 == 0 else "dve"
                cp(eng, dst, st[:, :, 0:ncol], (sk,), ("win",))
                if ci in (4, 6):
                    for m in range(2):
                        sl = wfm_b[:, :, ci * 128 + m * 64: ci * 128 + m * 64 + 32]
                        ts("pool", sl, sl, -1.0, ALU.mult, ("win",), ("win",))
            dma("sp", lamt[:, :], lamv[l][:, :], (), ("lamt",))
            for i in range(2):
                tt("dve", lamt[:, i * 128:i * 128 + 64], lamt[:, i * 128:i * 128 + 64],
                   lamt[:, i * 128 + 64:i * 128 + 128], ALU.mult, ("lamt",), ("lamt",))
                I("dve", lambda e, i=i: e.reduce_sum(lamw[:, i:i + 1], lamt[:, i * 128:i * 128 + 64], AX.X),
                  ("lamt",), ("lamw",))
            act(lamw[:, 2:4], lamw[:, 0:2], AF.Exp, ("lamw",), ("lamw",))
            lam_init = 0.8 - 0.6 * math.exp(-0.3 * l)
            stt(neglam[:, :], lamw[:, 3:4], -lam_init, lamw[:, 2:3], ALU.add, ALU.subtract, ("lamw",), ("neglam",))
            ts("pool", negfb[:, :], vec[l][0:1, 22:23], -1.0, ALU.mult, (("vec", l),), ("negfb",))

        def load_wout(l):
            for ci in range(8):
                st = wstg[ci % 2]
                sk = ("wstg", ci % 2)
                dma("sp", st[:, :, :], wout[l][:, ci * 128:(ci + 1) * 128].rearrange("(kt p) c -> p kt c", p=128),
                    (), (sk,))
                eng = "pool" if ci % 2 == 0 else "dve"
                cp(eng, wout_b[:, :, ci * 128:(ci + 1) * 128], st[:, :, :], (sk,), ("wout",))

        def outproj_block(l_prev, tb, xsrc, xkey):
            t0 = tb * TB
            q4, tq = tb // 4, (tb % 4) * TB
            dma("sp", ycb[:, :, :], ycf[l_prev][q4][:, tq:tq + TB].rearrange("(kt p) t -> p kt t", p=128),
                (("ycf", l_prev, q4),), ("ycb",))
            dma("sp", xblk[:, :, :], xsrc[:, t0:t0 + TB].rearrange("(kt p) t -> p kt t", p=128),
                (xkey,), ("xblk",))
            for m in range(KT):
                bank = m % 2
                for et in range(KT):
                    mm(ps[bank][:, :], wout_b[:, et, m * 128:(m + 1) * 128], ycb[:, et, :], et == 0, et == KT - 1,
                       ("wout", "ycb"), (("ps", bank),))
                tt("dve", xblk[:, m, :], xblk[:, m, :], ps[bank][:, :], ALU.add, (("ps", bank), "xblk"), ("xblk",))
                yield

        def norm_block(nw_ap, to_out):
            for kt in range(KT):
                tt("pool", sqb[:, kt, :], xblk[:, kt, :], xblk[:, kt, :], ALU.mult, ("xblk",), ("sqb",))
            for kt in range(KT):
                mm(ps[0][:, :], onesb[:, :], sqb[:, kt, :], kt == 0, kt == KT - 1, ("onesb", "sqb"), (("ps", 0),))
            yield
            ts("dve", rstd[:, :], ps[0][:, :], 1.0 / D_MODEL, ALU.mult, (("ps", 0),), ("rstd",), s2=EPS, op1=ALU.add)
            act(rstd[:, :], rstd[:, :], AF.Ln, ("rstd",), ("rstd",))
            act(rstd[:, :], rstd[:, :], AF.Exp, ("rstd",), ("rstd",), scale=-0.5)
            for kt in range(KT):
                if to_out:
                    stt(ob[:, kt, :], xblk[:, kt, :], nw_ap[:, kt:kt + 1], rstd[:, :], ALU.mult, ALU.mult,
                        ("xblk", "rstd"), ("xblk",))
                else:
                    stt(xnb[:, kt, :], xblk[:, kt, :], nw_ap[:, kt:kt + 1], rstd[:, :], ALU.mult, ALU.mult,
                        ("xblk", "rstd"), ("xnb",))
            yield

        def inproj_block(l, tb):
            t0 = tb * TB
            qa, ga = qa2[tb % 2], ga2[tb % 2]
            qak, gak = ("qa", tb % 2), ("ga", tb % 2)
            V = vec[l]
            vk = ("vec", l)
            dma("sp", cosb[:, :], cosT[:, t0:t0 + TB], (), ("cosb",))
            dma("sp", sinb[:, :], sinT[:, t0:t0 + TB], (), ("sinb",))

            def fm(c0, ncol):
                def f(b):
                    for kt in range(KT):
                        mm(ps[b][0:ncol, :], wfm_b[:, kt, c0:c0 + ncol], xnb[:, kt, :], kt == 0, kt == KT - 1,
                           ("win", "xnb"), (("ps", b),))
                return f

            def conv_ev(which):
                pre, dst, cw0, cbc, fin, fk = [(preq, qm, 8, 16, tC, "tC"), (prek, km, 12, 17, cvb, "tB")][which]
                pk = ("pre", which)

                def e1(b):
                    if tb == 0:
                        I("pool", lambda e: e.memset(pre[:, 0:4], 0.0), (), (pk,))
                    else:
                        cp("pool", pre[:, 1:4], pre[:, TB + 1:TB + 4], (pk,), (pk,))
                    cp("dve", pre[:, 4:4 + TB], ps[b][:, :], (("ps", b), pk), (pk,))
                    ts("dve", cva[:, :], pre[:, 4:4 + TB], V[:, cw0 + 3:cw0 + 4], ALU.mult, (pk, vk), ("tA",),
                       s2=V[:, cbc:cbc + 1], op1=ALU.add)
                    stt(cvb[:, :], pre[:, 3:3 + TB], V[:, cw0 + 2:cw0 + 3], cva[:, :], ALU.mult, ALU.add,
                        (pk, vk, "tA"), ("tB",))
                    stt(cva[:, :], pre[:, 2:2 + TB], V[:, cw0 + 1:cw0 + 2], cvb[:, :], ALU.mult, ALU.add,
                        (pk, vk, "tB"), ("tA",))
                    stt(fin[:, :], pre[:, 1:1 + TB], V[:, cw0:cw0 + 1], cva[:, :], ALU.mult, ALU.add,
                        (pk, vk, "tA"), (fk,))

                def e2(b):
                    act(dst[:, :], fin[:, :], AF.Silu, (fk,), ("qm" if which == 0 else "km",))
                return [e1, e2]

            def silu_ev(dst, dk):
                return [lambda b: act(dst[:, :], ps[b][:, :], AF.Silu, (("ps", b),), (dk,))]

            def rope_a(b):
                tt("dve", rpa[:, :], ps[b][:, :], cosb[:, :], ALU.mult, (("ps", b), "cosb"), ("tA",))

            def rope_b(which):
                def f(b):
                    tt("dve", rpb[:, :], ps[b][:, :], sinb[:, :], ALU.mult, (("ps", b), "sinb"), ("tB",))
                    if which == 0:
                        tt("pool", qa[:, :], rpa[:, :], rpb[:, :], ALU.add, ("tA", "tB"), (qak,))
                    else:
                        tt("pool", kaT[:, t0:t0 + TB], rpa[:, :], rpb[:, :], ALU.add, ("tA", "tB"), (("ka", tb),))
                return f

            def row_ev(g):
                return [lambda b: cp("dve", rows[:, g, :], ps[b][0:1, :], (("ps", b),), (("row", g),))]

            def vm_mm(h):
                def f(b):
                    for c4 in range(4):
                        c8 = h * 4 + c4
                        for kt in range(KT):
                            mm(ps[b][0:64, c4 * 128:(c4 + 1) * 128], xnb[:, kt, c8 * 64:(c8 + 1) * 64],
                               wtm_b[:, kt, 0:128], kt == 0, kt == KT - 1, ("win", "xnb"), (("ps", b),))
                return f

            def vm_ev(h):
                return [lambda b: cp("dve", vmc[:, h * 4:(h + 1) * 4, 0:128],
                                     ps[b][0:64, :].rearrange("p (c d) -> p c d", d=128), (("ps", b),), ("vmc",))]

            def va_mm(b):
                for t4 in range(4):
                    for kt in range(KT):
                        mm(ps[b][:, t4 * 128:(t4 + 1) * 128], xnb[:, kt, t4 * 128:(t4 + 1) * 128],
                           wtm_b[:, kt, 128:256], kt == 0, kt == KT - 1, ("win", "xnb"), (("ps", b),))

            def va_ev(b):
                cp("dve", va[:, tb * 4:(tb + 1) * 4, 0:128], ps[b][:, :].rearrange("p (c d) -> p c d", d=128),
                   (("ps", b), "va_ones"), tuple(("va", tb * 4 + i) for i in range(4)))

            stages = [
                (fm(0, 128), conv_ev(0)),
                (fm(128, 128), conv_ev(1)),
                (fm(256, 128), silu_ev(gm, "gm")),
                (fm(3 * 128, 128), [rope_a]),
                (fm(4 * 128, 128), [rope_b(0)]),
                (fm(5 * 128, 128), [rope_a]),
                (fm(6 * 128, 128), [rope_b(1)]),
                (fm(7 * 128, 128), silu_ev(ga, gak)),
                (fm(1024, 1), row_ev(0)),
                (fm(1025, 1), row_ev(1)),
                (vm_mm(0), vm_ev(0)),
                (vm_mm(1), vm_ev(1)),
                (va_mm, [va_ev]),
            ]
            pending = []
            for k, (mmf, evs) in enumerate(stages):
                b = k % 2
                mmf(b)
                for due, fn, bb in [p for p in pending if p[0] <= k]:
                    fn(bb)
                pending = [p for p in pending if p[0] > k]
                for i, fn in enumerate(evs):
                    pending.append((k + 1 + i, fn, b))
                yield
            for due, fn, bb in sorted(pending, key=lambda p: p[0]):
                fn(bb)
            yield

        R_I, R_F, R_L1, R_BN, R_A_, R_AA, R_WI, R_WK, R_EM = range(9)
        R_T2, R_T1, R_ALS = R_I, R_F, R_L1

        def rw(i):
            return rows[:, i, :]

        def gates_block(l, tb):
            V = vec[l]
            vk = ("vec", l)
            rk = lambda i: ("row", i)
            if tb == 0:
                I("pool", lambda e: e.memset(carry[:, :], 0.0), (), ("carry",))
            act(rw(R_T1), rw(R_F), AF.Exp, (rk(R_F), "negfb"), (rk(R_T1),), scale=-1.0, bias=negfb[:, :])
            act(rw(R_L1), rw(R_T1), AF.Ln, (rk(R_T1),), (rk(R_L1),), bias=1.0)
            I("dve", lambda e: e.tensor_tensor_scan(rw(R_BN), onesf[:, :], rw(R_L1), carry[:, 0:1], ALU.mult, ALU.add),
              ("onesf", rk(R_L1), "carry"), (rk(R_BN),))
            stt(rw(R_A_), rw(R_I), V[0:1, 21:22], rw(R_BN), ALU.add, ALU.add, (rk(R_I), vk, rk(R_BN)), (rk(R_A_),))
            I("dve", lambda e: e.tensor_tensor_scan(rw(R_AA), onesf[:, :], rw(R_A_), carry[:, 1:2], ALU.mult, ALU.max),
              ("onesf", rk(R_A_), "carry"), (rk(R_AA),))
            yield
            A3 = rw(R_AA).rearrange("p (c i) -> p c i", i=64)
            aend = A3[:, :, 63]
            cp("pool", ape[:, 0:1], carry[:, 1:2], ("carry",), ("ape",))
            cp("pool", ape[:, 1:8], A3[:, 0:7, 63], (rk(R_AA),), ("ape",))
            tt("dve", rw(R_T1).rearrange("p (c i) -> p c i", i=64), ape[:, :].unsqueeze(2).broadcast_to([1, 8, 64]),
               A3, ALU.subtract, ("ape", rk(R_AA)), (rk(R_T1),))
            act(rw(R_WI), rw(R_T1), AF.Exp, (rk(R_T1),), (rk(R_WI),))
            tt("dve", dec[:, :], ape[:, :], aend, ALU.subtract, ("ape", rk(R_AA)), ("dec",))
            act(dec[:, :], dec[:, :], AF.Exp, ("dec",), ("dec",))
            tt("dve", rw(R_T2).rearrange("p (c i) -> p c i", i=64), rw(R_A_).rearrange("p (c i) -> p c i", i=64),
               aend.unsqueeze(2).broadcast_to([1, 8, 64]), ALU.subtract, (rk(R_A_), rk(R_AA)), (rk(R_T2),))
            act(rw(R_WK), rw(R_T2), AF.Exp, (rk(R_T2),), (rk(R_WK),), bias=LN_KSCALE)
            tt("dve", rw(R_T1), rw(R_BN), rw(R_AA), ALU.subtract, (rk(R_BN), rk(R_AA)), (rk(R_T1),))
            act(rw(R_EM), rw(R_T1), AF.Exp, (rk(R_T1),), (rk(R_EM),))
            ts("pool", rw(R_ALS), rw(R_A_), LN_KSCALE, ALU.add, (rk(R_A_),), (rk(R_ALS),))
            cp("pool", carry[:, 0:1], rw(R_BN)[:, TB - 1:TB], (rk(R_BN),), ("carry",))
            cp("pool", carry[:, 1:2], rw(R_AA)[:, TB - 1:TB], (rk(R_AA), "ape"), ("carry",))
            yield
            for qi, ri in enumerate([R_ALS, R_WK, R_EM]):
                for c8 in range(8):
                    mm(ps[1][0:64, 480 + qi * 8 + c8:480 + qi * 8 + c8 + 1], rw(ri)[:, c8 * 64:(c8 + 1) * 64],
                       onesf[:, 0:1], True, True, (rk(ri), "onesf"), (("ps", 1),))
            cp("dve", cols[:, :], ps[1][0:64, 480:504], (("ps", 1),), ("cols",))
            mm(ps[1][:, 504:512], onesf[:, 0:128], dec[:, :], True, True, ("onesf", "dec"), (("ps", 1),))
            cp("dve", decb[:, :], ps[1][:, 504:512], (("ps", 1),), ("decb",))
            yield
            mm(ps[0][0:64, :], onesf[:, 0:64], rw(R_AA), True, True, ("onesf", rk(R_AA)), (("ps", 0),))
            for c8 in range(8):
                act(wT[:, c8, :], ps[0][0:64, c8 * 64:(c8 + 1) * 64], AF.Exp, (("ps", 0), "cols"), ("wT",),
                    scale=-1.0, bias=cols[:, c8:c8 + 1])
            tt("pool", wT[:, :, :], wT[:, :, :], mask64.unsqueeze(1).broadcast_to([64, 8, 64]), ALU.mult,
               ("wT", "cst"), ("wT",))
            yield
            mm(ps[0][:, :], onesf[:, 0:128], rw(R_WI), True, True, ("onesf", rk(R_WI)), (("ps", 0),))
            tt("dve", qs[:, :], qm[:, :], ps[0][:, :], ALU.mult, ("qm", ("ps", 0)), ("qs",))
            tt("pool", vmw[:, :, 0:129], vmc[:, :, 0:129], cols[:, 8:16].unsqueeze(2).broadcast_to([64, 8, 129]),
               ALU.mult, ("vmc", "vmc_ones", "cols"), ("vmw",))
            yield

        def mlstm_block(l, tb, ycl_ap, okey, res):
            V = vec[l]
            vk = ("vec", l)
            if tb == 0:
                I("pool", lambda e: e.memset(cf[:, :], 0.0), (), ("cf",))
                I("pool", lambda e: e.memset(cb[:, :], 0.0), (), ("cb",))
            for c8 in range(8):
                tr(psb(0)[0:64, c8 * 128:(c8 + 1) * 128], km[:, c8 * 64:(c8 + 1) * 64], identb[:, :],
                   ("km", "identb"), (("ps", 0),))
            cp("dve", ktok[:, :, :], psb(0)[0:64, :].rearrange("p (c d) -> p c d", d=128), (("ps", 0),), ("ktok",))
            yield

            def st_kv(c8):
                cs = c8 * 64
                bk = c8 % 2
                mm(ps[bk][0:64, 0:64], km[:, cs:cs + 64], qm[:, cs:cs + 64], True, True, ("km", "qm"), (("ps", bk),))
                mm(ps[bk][:, 200:329], ktok[:, c8, :], vmw[:, c8, 0:129], True, True, ("ktok", "vmw"), (("ps", bk),))

            st_kv(0)
            for c8 in range(8):
                cs = c8 * 64
                bk = c8 % 2
                if c8 + 1 < 8:
                    st_kv(c8 + 1)
                tt("dve", stl[c8 % 2][:, :], ps[bk][0:64, 0:64], wT[:, c8, :], ALU.mult, (("ps", bk), "wT"),
                   (("stl", c8 % 2),))
                mm(ps[bk][0:64, 64:193], qs[:, cs:cs + 64], cb[:, 0:129], True, False, ("qs", "cb"), (("ps", bk),))
                mm(ps[bk][0:64, 64:193], stl[c8 % 2][:, :], vmc[:, c8, 0:129], False, True,
                   (("stl", c8 % 2), "vmc", "vmc_ones"), (("ps", bk),))
                stt(cf[:, 0:129], cf[:, 0:129], decb[:, c8:c8 + 1], ps[bk][:, 200:329], ALU.mult, ALU.add,
                    ("cf", "decb", ("ps", bk)), ("cf",))
                cp("pool", cb[:, 0:129], cf[:, 0:129], ("cf",), ("cb",))
                cp("dve", nums[:, c8, 0:129], ps[bk][0:64, 64:193], (("ps", bk),), ("nums",))
                yield
            den = nums[:, :, 128]
            tt("dve", sq8[:, :, :], nums[:, :, 0:128], nums[:, :, 0:128], ALU.mult, ("nums",), ("sq8",))
            I("dve", lambda e: e.reduce_sum(sm[:, :, 0], sq8[:, :, :], AX.X), ("sq8",), ("sm",))
            ts("dve", sm[:, :, 1], den, -1.0, ALU.mult, ("nums",), ("sm",))
            tt("dve", sm[:, :, 1], sm[:, :, 1], den, ALU.max, ("sm", "nums"), ("sm",))
            tt("dve", sm[:, :, 1], sm[:, :, 1], cols[:, 16:24], ALU.max, ("sm", "cols"), ("sm",))
            tt("dve", sm[:, :, 2], sm[:, :, 1], sm[:, :, 1], ALU.mult, ("sm",), ("sm",))
            ts("dve", sm[:, :, 0], sm[:, :, 0], 1.0 / 128.0, ALU.mult, ("sm",), ("sm",))
            stt(sm[:, :, 3], sm[:, :, 2], EPS, sm[:, :, 0], ALU.mult, ALU.add, ("sm",), ("sm",))
            act(sm[:, :, 4], sm[:, :, 3], AF.Ln, ("sm",), ("sm",))
            act(sm[:, :, 5], sm[:, :, 4], AF.Exp, ("sm",), ("sm",), scale=-0.5)
            tt("dve", hmb[:, :, :], nums[:, :, 0:128], sm[:, :, 5:6].broadcast_to([64, 8, 128]), ALU.mult,
               ("nums", "sm"), ("hmb",))
            for c8 in range(8):
                tr(psb(0)[:, c8 * 64:(c8 + 1) * 64], hmb[:, c8, :], identb[0:64, 0:64], ("hmb", "identb"),
                   (("ps", 0),))
            yield
            ts("dve", gt1[:, :], qm[:, :], V[:, 19:20], ALU.mult, ("qm", vk), ("tA",))
            stt(gt2[:, :], psb(0)[:, 0:TB], V[:, 18:19], gt1[:, :], ALU.mult, ALU.add, (("ps", 0), vk, "tA"),
                ("tB",))
            tt("dve", yo[:, :], gt2[:, :], gm[:, :], ALU.mult, ("tB", "gm"), ("yo",))
            res.append(dma("sp", ycl_ap, yo[:, :], ("yo",), (okey,)))
            yield

        def attn_block(l, tb, ycl_ap, okey, res):
            V = vec[l]
            vk = ("vec", l)
            qa, ga = qa2[tb % 2], ga2[tb % 2]
            qak, gak = ("qa", tb % 2), ("ga", tb % 2)
            lam_init = 0.8 - 0.6 * math.exp(-0.3 * l)
            tiles = []
            for g2 in range(2):
                qt0 = tb * 4 + g2 * 2
                for j in range(qt0 + 2):
                    tiles.append((g2, qt0, j))

            def emit_S(ti):
                g2, qt0, j = tiles[ti]
                qoff = g2 * 256
                lo = 0 if j <= qt0 else 128
                sb0 = 2 + 2 * (ti % 2)
                kb = j // 4
                for m in range(2):
                    mm(ps[sb0 + m][:, lo:256], kaT[m * 64:(m + 1) * 64, j * 128:(j + 1) * 128],
                       qa[m * 64:(m + 1) * 64, qoff + lo:qoff + 256], True, True, (("ka", kb), qak),
                       (("ps", sb0), ("ps", sb0 + 1)))

            touched = set()
            emit_S(0)
            for ti, (g2, qt0, j) in enumerate(tiles):
                if ti + 1 < len(tiles):
                    emit_S(ti + 1)
                if j == 0:
                    touched = set()
                qoff = g2 * 256
                lo = 0 if j <= qt0 else 128
                sb0 = 2 + 2 * (ti % 2)
                pt = pT[ti % 2]
                ptk = ("pT", ti % 2)
                src_ = ps_all[:, sb0:sb0 + 2, lo:256]
                dst = pt[:, :].rearrange("p (m q) -> p m q", m=2)[:, :, lo:256]
                act(dst, src_, AF.Exp, (("ps", sb0), ("ps", sb0 + 1)), (ptk,), scale=0.125)
                if j >= qt0:
                    d0 = (j - qt0) * 128
                    msl = pt[64:128, :].rearrange("p (m q) -> p m q", m=2)[:, :, d0:d0 + 64]
                    I("pool", lambda e, msl=msl: e.memset(msl, 0.0), (ptk,), (ptk,))
                for qt in range(2):
                    if j > qt0 + qt:
                        continue
                    pb = 6 + qt
                    for m in range(2):
                        first = pb not in touched
                        touched.add(pb)
                        last = (j == qt0 + qt) and m == 1
                        mm(ps[pb][:, m * 129:(m + 1) * 129], pt[:, m * 256 + qt * 128:m * 256 + (qt + 1) * 128],
                           va[:, j, 0:129], first, last, (ptk, ("va", j), "va_ones"), (("ps", pb),))
                    if j == qt0 + qt:
                        P = ps[pb]
                        pk = ("ps", pb)
                        I("dve", lambda e, P=P: e.reciprocal(asm[:, 0:1], P[:, 128:129]), (pk,), ("asm0",))
                        I("dve", lambda e, P=P: e.reciprocal(asm[:, 1:2], P[:, 257:258]), (pk,), ("asm1",))
                        tt("dve", asm[:, 1:2], asm[:, 1:2], neglam[:, :], ALU.mult, ("asm1", "neglam"), ("asm1",))
                        ts("dve", at1[:, :], P[:, 0:128], asm[:, 0:1], ALU.mult, (pk, "asm0"), ("at1",))
                        stt(at2[:, :], P[:, 129:257], asm[:, 1:2], at1[:, :], ALU.mult, ALU.add,
                            (pk, "asm1", "at1"), ("at2",))
                        tt("pool", atj[:, :], at2[:, :], at2[:, :], ALU.mult, ("at2",), ("atj",))
                        I("dve", lambda e: e.reduce_sum(asm[:, 2:3], atj[:, :], AX.X), ("atj",), ("asm2",))
                        ts("dve", asm[:, 3:4], asm[:, 2:3], 1.0 / 128.0, ALU.mult, ("asm2",), ("asm3",),
                           s2=EPS, op1=ALU.add)
                        act(asm[:, 4:5], asm[:, 3:4], AF.Ln, ("asm3",), ("asm4",))
                        act(asm[:, 5:6], asm[:, 4:5], AF.Exp, ("asm4",), ("asm5",), scale=-0.5)
                        ts("dve", hab[:, :], at2[:, :], asm[:, 5:6], ALU.mult, ("at2", "asm5"), ("hab",),
                           s2=1.0 - lam_init, op1=ALU.mult)
                        tr(psb(pb)[:, 768:896], hab[:, :], identb[:, :], ("hab", "identb"), (pk,))
                        c0 = qoff + qt * 128
                        stt(yab[:, c0:c0 + 128], psb(pb)[:, 768:896], V[:, 20:21], ga[:, c0:c0 + 128], ALU.mult,
                            ALU.mult, (pk, vk, gak), ("yab",))
                yield
            res.append(dma("sp", ycl_ap, yab[:, :], ("yab",), (okey,)))

        out_toks = []

        def front(l, tb):
            t0 = tb * TB
            q4, tq = tb // 4, (tb % 4) * TB
            if l == 0:
                dma("sp", xblk[:, :, :], xT[:, t0:t0 + TB].rearrange("(kt p) t -> p kt t", p=128), (), ("xblk",))
            else:
                yield from outproj_block(l - 1, tb, xT, ("xsrc0", tb))
                tok = dma("sp", x1s[:, t0:t0 + TB].rearrange("(kt p) t -> p kt t", p=128), xblk[:, :, :],
                          ("xblk",), (("xsrc1", tb),))
                if not do_fin:
                    out_toks.append(tok)
            yield from norm_block(vec[l][:, 0:8], False)
            yield from inproj_block(l, tb)
            yield from gates_block(l, tb)
            res = []
            yield from mlstm_block(l, tb, ycl[l][q4][0:128, tq:tq + TB], ("ycl", l, q4, tb % 4, 0), res)
            if not fused:
                out_toks.extend(res)

        def drain(g):
            for _ in g:
                pass

        NF = 60.0

        for l in layers:
            load_weights(l)
            if l > 0:
                load_wout(l - 1)
            drain(front(l, 0))
            for tb in range(_NB):
                q4, tq = tb // 4, (tb % 4) * TB
                res = []
                ag = attn_block(l, tb, ycl[l][q4][128:256, tq:tq + TB], ("ycl", l, q4, tb % 4, 1), res)
                fg = front(l, tb + 1) if tb + 1 < _NB else iter(())
                ntile = 8 * tb + 6
                ratio = NF / ntile
                acc = 0.0
                fdone = False
                for _ in ag:
                    acc += ratio
                    while acc >= 1.0 and not fdone:
                        acc -= 1.0
                        try:
                            next(fg)
                        except StopIteration:
                            fdone = True
                drain(fg)
                if not fused:
                    out_toks.extend(res)
                if fused and tb % 4 == 3:
                    rk = tuple(("ycl", l, q4, i, w) for i in range(4) for w in range(2))
                    I("pool", lambda e, l=l, q4=q4: e.collective_compute(
                        "AllGather", ALU.bypass, replica_groups=GROUPS,
                        ins=[ycl[l][q4][:, :]], outs=[ycf[l][q4][:, :]]), rk, (("ycf", l, q4),), cc=True)

        if do_fin:
            load_wout(DEPTH - 1)
            for tb in range(_NB):
                t0 = tb * TB
                drain(outproj_block(DEPTH - 1, tb, x1s, ("xsrc1", tb)))
                drain(norm_block(vec[DEPTH - 1][:, 23:31], True))
                out_toks.append(dma("sp", outT[:, t0:t0 + TB].rearrange("(kt p) t -> p kt t", p=128), xblk[:, :, :],
                                    ("xblk",), ()))

        waits = []
        for tok in out_toks:
            S._tok_wait("sp", tok, waits)
        S.recs["sp"].append((waits, None, None))

        with nc.Block() as block:
            S.emit(block)
    return nc


_OFF = {"mq": 0, "mk": 512, "mv": 1024, "mi": 1536, "mf": 1540, "mz": 1544,
        "aq": 2056, "ak": 2568, "av": 3080, "az": 3592}


def _rope_tables():
    inv = (1.0 / (np.float32(10000.0) ** (np.arange(0, 64, 2, dtype=np.float32) / np.float32(64.0)))).astype(np.float32)
    ang = np.arange(SEQ, dtype=np.float32)[:, None] * inv[None, :]
    cos = np.cos(ang).astype(np.float32)
    sin = np.sin(ang).astype(np.float32)
    cosT = np.ascontiguousarray(np.concatenate([cos, cos, cos, cos], 1).T)
    sinT = np.ascontiguousarray(np.concatenate([sin, sin, sin, sin], 1).T)
    return cosT, sinT


def _consts():
    c = np.zeros((128, 192), np.float32)
    c[:, 0:128] = np.eye(128, dtype=np.float32)
    c[0:64, 128:192] = np.triu(np.ones((64, 64), np.float32))
    return c


def _core_inputs(inp, c, stage_layers, need_wout):
    b, hd = c // 4, c % 4
    f32 = np.float32
    d = {}
    for l in stage_layers:
        w = np.asarray(inp["w_in"][l], f32)
        hs = slice(hd * 128, (hd + 1) * 128)

        def blk(name):
            return w[:, _OFF[name] + hd * 128:_OFF[name] + (hd + 1) * 128]

        def perm(m):
            return np.concatenate([m[:, 32:64], m[:, 0:32], m[:, 96:128], m[:, 64:96]], 1)

        aq, ak = blk("aq"), blk("ak")
        gi = w[:, _OFF["mi"] + hd:_OFF["mi"] + hd + 1]
        gf = w[:, _OFF["mf"] + hd:_OFF["mf"] + hd + 1]
        d[f"wfm{l}"] = np.ascontiguousarray(np.concatenate(
            [blk("mq"), blk("mk"), blk("mz"), aq, perm(aq), ak, perm(ak), blk("az"), gi, gf], 1))
        d[f"wtm{l}"] = np.ascontiguousarray(np.concatenate([blk("mv"), blk("av")], 1))
        lam = np.concatenate([np.asarray(inp[k][l], f32) for k in ("lam_q1", "lam_k1", "lam_q2", "lam_k2")])
        d[f"lamv{l}"] = np.ascontiguousarray(np.tile(lam[None, :], (128, 1)))
    for l in range(DEPTH):
        v = np.zeros((128, NV), f32)
        v[:, 0:8] = np.asarray(inp["norm_w"][l], f32).reshape(8, 128).T
        cw = np.asarray(inp["conv_w"][l], f32)
        cbv = np.asarray(inp["conv_b"][l], f32)
        v[:, 8:12] = cw[:, hd * 128:(hd + 1) * 128].T
        v[:, 12:16] = cw[:, 512 + hd * 128:512 + (hd + 1) * 128].T
        v[:, 16] = cbv[hd * 128:(hd + 1) * 128]
        v[:, 17] = cbv[512 + hd * 128:512 + (hd + 1) * 128]
        v[:, 18] = np.asarray(inp["m_norm_w"][l], f32)[hd * 128:(hd + 1) * 128]
        v[:, 19] = np.asarray(inp["m_skip"][l], f32)[hd * 128:(hd + 1) * 128]
        v[:, 20] = np.asarray(inp["a_norm_w"][l], f32)
        v[:, 21] = np.asarray(inp["i_bias"][l], f32)[hd]
        v[:, 22] = np.asarray(inp["f_bias"][l], f32)[hd]
        v[:, 23:31] = np.asarray(inp["final_norm_w"], f32).reshape(8, 128).T
        d[f"vecs{l}"] = v
    for l in need_wout:
        wo = np.asarray(inp["w_out"][l], f32)
        rows = []
        for r in range(4):
            rows.append(wo[r * 128:(r + 1) * 128])
            rows.append(wo[512 + r * 128:512 + (r + 1) * 128])
        d[f"wout{l}"] = np.ascontiguousarray(np.concatenate(rows, 0))
    return d


_PROG = {}


def _prog(stage, debug=False):
    key = (stage, debug)
    if key not in _PROG:
        _PROG[key] = build_program(stage, debug)
    return _PROG[key]


def kernel(**inp):
    x = np.asarray(inp["x"], np.float32)
    xTs = [np.ascontiguousarray(x[b].T) for b in range(BATCH)]
    cosT, sinT = _rope_tables()
    cst = _consts()
    nc = _prog("all")
    in_maps = []
    for c in range(NCORES):
        d = _core_inputs(inp, c, [0, 1], [0, 1])
        d.update({"xT": xTs[c // 4], "cosT": cosT, "sinT": sinT, "consts": cst})
        in_maps.append(d)
    res = run_bass_kernel_spmd(nc, in_maps, core_ids=list(range(NCORES)))
    out = np.empty((BATCH, SEQ, D_MODEL), np.float32)
    for b in range(BATCH):
        out[b] = res.results[4 * b]["outT"].T
    return out
```

```python
import math
from contextlib import ExitStack

import numpy as np
import ml_dtypes

import concourse.bass as bass
import concourse.mybir as mybir
from concourse.bass_utils import run_bass_kernel_spmd

F32 = mybir.dt.float32
BF16 = mybir.dt.bfloat16
AF = mybir.ActivationFunctionType
ALU = mybir.AluOpType
AX = mybir.AxisListType

D_MODEL = 1024
BATCH = 2
SEQ = 8192
DEPTH = 2
NCORES = 8
TB = 512
NBLK = SEQ // TB
KT = D_MODEL // 128
EPS = 1e-6
NV = 32
LN_KSCALE = math.log(128.0 ** -0.5)
GROUPS = [[0, 1, 2, 3], [4, 5, 6, 7]]
import os
_STOP = int(os.environ.get("K_STOP", "9"))
_NB = int(os.environ.get("K_NBLK", str(NBLK)))


class Sched:
    CH = 30000
    ND = 48

    def __init__(self, nc, es):
        self.nc = nc
        self.engs = ["pe", "act", "dve", "pool", "sp"]
        self.recs = {e: [] for e in self.engs}
        self.cnt = {e: 0 for e in self.engs}
        self.seen = {e: {} for e in self.engs}
        self.lastw = {}
        self.readers = {}
        nsem = {"pe": 3, "act": 3, "dve": 4, "pool": 3, "sp": 1}
        self.esems = {e: [es.enter_context(nc.semaphore(f"s_{e}_{i}")) for i in range(nsem[e])]
                      for e in self.engs}
        self.dsems = [es.enter_context(nc.semaphore(f"d_{i}")) for i in range(self.ND)]
        self.dval = [0] * self.ND
        self.dnext = 0
        self.ccsem = es.enter_context(nc.semaphore("ccsem"))
        self.ccval = 0

    def _tok_wait(self, e, tok, waits):
        if tok[0] == "e":
            _, e2, n = tok
            if e2 == e and e == "pe":
                return
            if self.seen[e].get(e2, 0) >= n:
                return
            self.seen[e][e2] = n
            waits.append((self.esems[e2][(n - 1) // self.CH], (n - 1) % self.CH + 1))
        else:
            kind, i, v = tok
            key = (kind, i)
            if self.seen[e].get(key, 0) >= v:
                return
            self.seen[e][key] = v
            sem = self.dsems[i] if kind == "d" else self.ccsem
            waits.append((sem, v))

    def issue(self, e, fn, reads=(), writes=(), dma=False, cc=False):
        deps = []
        for k in reads:
            if k in self.lastw:
                deps.append(self.lastw[k])
        for k in writes:
            if k in self.lastw:
                deps.append(self.lastw[k])
            deps.extend(self.readers.get(k, {}).values())
        waits = []
        for tok in deps:
            self._tok_wait(e, tok, waits)
        if dma:
            i = self.dnext
            self.dnext = (self.dnext + 1) % self.ND
            prev = self.dval[i]
            if prev > 0:
                self._tok_wait(e, ("d", i, prev), waits)
            self.dval[i] += 16
            tok = ("d", i, self.dval[i])
            inc = (self.dsems[i], 16)
        elif cc:
            self.ccval += 1
            tok = ("c", 0, self.ccval)
            inc = (self.ccsem, 1)
        elif fn is None:
            tok = None
            inc = None
        else:
            self.cnt[e] += 1
            n = self.cnt[e]
            tok = ("e", e, n)
            inc = (self.esems[e][(n - 1) // self.CH], 1)
        self.recs[e].append((waits, fn, inc))
        if tok is not None:
            for k in reads:
                self.readers.setdefault(k, {})[(tok[0], tok[1])] = tok
            for k in writes:
                self.lastw[k] = tok
                self.readers[k] = {}
        return tok

    def emit(self, block):
        nc = self.nc

        def run(e, eng):
            for waits, fn, inc in self.recs[e]:
                for s, v in waits:
                    eng.wait_ge(s, v)
                if fn is not None:
                    ins = fn(eng)
                    ins.then_inc(inc[0], inc[1])

        @block.tensor
        def _(eng):
            run("pe", eng)

        @block.scalar
        def _(eng):
            run("act", eng)

        @block.vector
        def _(eng):
            run("dve", eng)

        @block.gpsimd
        def _(eng):
            run("pool", eng)

        @block.sync
        def _(eng):
            run("sp", eng)


def build_program(stage="all", debug=False):
    nc = bass.Bass("TRN2", target_bir_lowering=False)
    with ExitStack() as es:
        S = Sched(nc, es)

        def dram(name, shape, dt, kind):
            return nc.dram_tensor(name, shape, dt, kind=kind).ap()

        fused = stage == "all"
        layers = {"all": [0, 1], "l0": [0], "mid": [1], "fin": []}[stage]
        do_fin = stage in ("all", "fin")

        xT = dram("xT", [D_MODEL, SEQ], F32, "ExternalInput") if stage in ("all", "l0", "mid") else None
        cosT = dram("cosT", [128, SEQ], F32, "ExternalInput") if layers else None
        sinT = dram("sinT", [128, SEQ], F32, "ExternalInput") if layers else None
        consts = dram("consts", [128, 192], F32, "ExternalInput")
        wfm, wtm, wout, vecs, lamv = {}, {}, {}, {}, {}
        for l in layers:
            wfm[l] = dram(f"wfm{l}", [D_MODEL, 1026], F32, "ExternalInput")
            wtm[l] = dram(f"wtm{l}", [D_MODEL, 256], F32, "ExternalInput")
            lamv[l] = dram(f"lamv{l}", [128, 256], F32, "ExternalInput")
        for l in range(DEPTH):
            vecs[l] = dram(f"vecs{l}", [128, NV], F32, "ExternalInput")
        need_wout = {"all": [0, 1], "l0": [], "mid": [0], "fin": [1]}[stage]
        for l in need_wout:
            wout[l] = dram(f"wout{l}", [D_MODEL, D_MODEL], F32, "ExternalInput")

        ycl, ycf = {}, {}
        for l in layers:
            kind = "Internal" if fused else "ExternalOutput"
            ycl[l] = [dram(f"ycl{l}_{q}", [256, 2048], BF16, kind) for q in range(4)]
        for l in need_wout:
            kind = "Internal" if fused else "ExternalInput"
            ycf[l] = [dram(f"ycf{l}_{q}", [1024, 2048], BF16, kind) for q in range(4)]
        x1s = None
        if stage == "all":
            x1s = dram("x1s", [D_MODEL, SEQ], F32, "Internal")
        elif stage == "mid":
            x1s = dram("x1s", [D_MODEL, SEQ], F32, "ExternalOutput")
        elif stage == "fin":
            x1s = dram("x1s", [D_MODEL, SEQ], F32, "ExternalInput")
        outT = dram("outT", [D_MODEL, SEQ], F32, "ExternalOutput") if do_fin else None
        dbg = {}
        if debug:
            for nm, shp, dt in [("d_qm", [128, SEQ], BF16), ("d_km", [128, SEQ], BF16),
                                ("d_qa", [128, SEQ], BF16), ("d_ka", [128, SEQ], BF16),
                                ("d_rows", [1, 9 * SEQ], F32)]:
                dbg[nm] = dram(nm, shp, dt, "ExternalOutput")

        def sb(name, shape, dt):
            return es.enter_context(nc.sbuf_tensor(name, shape, dt))

        cst = sb("cst", [128, 192], F32)
        ident_f = cst[:, 0:128]
        identb = sb("identb", [128, 128], BF16)
        onesb = sb("onesb", [128, 128], BF16)
        onesf = sb("onesf", [1, 512], F32)
        vec = [sb(f"vec{l}", [128, NV], F32) for l in range(DEPTH)]
        lamt = sb("lamt", [128, 256], F32)
        lamw = sb("lamw", [128, 4], F32)
        neglam = sb("neglam", [128, 1], F32)
        negfb = sb("negfb", [1, 1], F32)

        wfm_b = sb("wfm_b", [128, KT, 1026], BF16)
        wtm_b = sb("wtm_b", [128, KT, 256], BF16)
        wout_b = sb("wout_b", [128, KT, D_MODEL], BF16)
        wstg = [sb(f"wstg{i}", [128, KT, 128], F32) for i in range(2)]

        xblk = sb("xblk", [128, KT, TB], F32)
        sqb = sb("sqb", [128, KT, TB], BF16)
        xnb = sb("xnb", [128, KT, TB], BF16)
        ycb = sqb
        rstd = sb("rstd", [128, TB], F32)
        cosb = sb("cosb", [128, TB], F32)
        sinb = sb("sinb", [128, TB], F32)
        preq = sb("preq", [128, TB + 4], F32)
        prek = sb("prek", [128, TB + 4], F32)
        cva = sb("tA", [128, TB], F32)
        cvb = sb("tB", [128, TB], F32)
        tC = sb("tC", [128, TB], F32)
        rpa, rpb = cva, cvb
        qa2 = [sb(f"qa{i}", [128, TB], BF16) for i in range(2)]
        qm2 = [sb(f"qm{i}", [128, TB], BF16) for i in range(2)]
        km2 = [sb(f"km{i}", [128, TB], BF16) for i in range(2)]
        qs = sb("qs", [128, TB], BF16)
        gm2 = [sb(f"gm{i}", [128, TB], F32) for i in range(2)]
        gtA = sb("gtA", [128, TB], F32)
        gtB = sb("gtB", [128, TB], F32)
        ga2 = [sb(f"ga{i}", [128, TB], F32) for i in range(2)]
        kaT = sb("kaT", [128, SEQ], BF16)
        va = sb("va", [128, SEQ // 128, 132], BF16)
        vmc2 = [sb(f"vmc{i}", [64, 8, 132], BF16) for i in range(2)]
        vmw = sb("vmw", [64, 8, 132], BF16)
        ktok = sb("ktok", [64, 8, 128], BF16)
        NR = 9
        rows = sb("rows", [1, NR, TB], F32)
        gif2 = [sb(f"gif{i}", [1, 2, TB], F32) for i in range(2)]
        carry = sb("carry", [1, 4], F32)
        ape = sb("ape", [1, 8], F32)
        dec = sb("dec", [1, 8], F32)
        cols = sb("cols", [64, 24], F32)
        decb = sb("decb", [128, 8], F32)
        wT = sb("wT", [64, 8, 64], F32)
        stl = [sb(f"stl{i}", [64, 64], BF16) for i in range(2)]
        cf = sb("cf", [128, 132], F32)
        cb = sb("cb", [128, 132], BF16)
        nums = sb("nums", [64, 8, 132], F32)
        sq8 = sb("sq8", [64, 8, 128], F32)
        sm = sb("sm", [64, 8, 8], F32)
        hmb = sb("hmb", [64, 8, 128], BF16)
        gt1, gt2 = gtA, gtB
        yo = sb("yo", [128, TB], BF16)
        pT = [sb(f"pT{i}", [128, 512], BF16) for i in range(2)]
        at1 = sb("at1", [128, 128], F32)
        at2 = sb("at2", [128, 128], F32)
        atj = sb("atj", [128, 128], F32)
        asm = sb("asm", [128, 8], F32)
        hab = sb("hab", [128, 128], BF16)
        yab = sb("yab", [128, TB], BF16)
        ob = xblk

        ps_all = es.enter_context(nc.psum_tensor("ps_all", [128, 8, 512], F32))
        ps = [ps_all[:, i, :] for i in range(8)]

        def I(e, fn, r=(), w=(), **kw):
            return S.issue(e, fn, r, w, **kw)

        def dma(q, out, in_, r, w):
            return I(q, lambda e: e.dma_start(out=out, in_=in_), r, w, dma=True)

        def act(out, in_, func, r, w, scale=1.0, bias=0.0, accum=None):
            if accum is None:
                return I("act", lambda e: e.activation(out, in_, func, bias=bias, scale=scale), r, w)
            return I("act", lambda e: e.activation(out, in_, func, bias=bias, scale=scale, accum_out=accum), r, w)

        def tt(eng, out, a, b, op, r, w):
            return I(eng, lambda e: e.tensor_tensor(out, a, b, op), r, w)

        def ts(eng, out, a, s1, op0, r, w, s2=None, op1=ALU.bypass):
            return I(eng, lambda e: e.tensor_scalar(out, a, s1, s2, op0, op1), r, w)

        def stt(out, a, sc, b, op0, op1, r, w):
            return I("dve", lambda e: e.scalar_tensor_tensor(out, a, sc, b, op0, op1), r, w)

        def cp(eng, out, in_, r, w):
            if eng == "act":
                return I(eng, lambda e: e.copy(out, in_), r, w)
            return I(eng, lambda e: e.tensor_copy(out, in_), r, w)

        def mm(out, lhsT, rhs, start, stop, r, w):
            return I("pe", lambda e: e.matmul(out, lhsT, rhs, start=start, stop=stop), r, w)

        def tr(out, in_, idn, r, w):
            return I("pe", lambda e: e.transpose(out, in_, idn), r, w)

        def psb(i):
            return ps[i].bitcast(BF16)

        dma("sp", cst[:, :], consts[:, :], (), ("cst",))
        for l in range(DEPTH):
            dma("sp", vec[l][:, :], vecs[l][:, :], (), (("vec", l),))
        cp("dve", identb[:, :], cst[:, 0:128], ("cst",), ("identb",))
        I("pool", lambda e: e.memset(onesb[:, :], 1.0), (), ("onesb",))
        I("pool", lambda e: e.memset(onesf[:, :], 1.0), (), ("onesf",))
        I("pool", lambda e: e.memset(va[:, :, 128:132], 1.0), (), ("va_ones",))
        for i in range(2):
            I("pool", lambda e, i=i: e.memset(vmc2[i][:, :, 128:132], 1.0), (), ("vmc_ones",))
        mask64 = cst[0:64, 128:192]

        def load_weights(l):
            nchunk = 11
            for ci in range(nchunk):
                st = wstg[ci % 2]
                sk = ("wstg", ci % 2)
                if ci < 8:
                    src = wfm[l][:, ci * 128:(ci + 1) * 128]
                    dst = wfm_b[:, :, ci * 128:(ci + 1) * 128]
                    ncol = 128
                elif ci == 8:
                    src = wfm[l][:, 1024:1026]
                    dst = wfm_b[:, :, 1024:1026]
                    ncol = 2
                else:
                    src = wtm[l][:, (ci - 9) * 128:(ci - 8) * 128]
                    dst = wtm_b[:, :, (ci - 9) * 128:(ci - 8) * 128]
                    ncol = 128
                dma("sp", st[:, :, 0:ncol], src.rearrange("(kt p) c -> p kt c", p=128), (), (sk,))
                eng = "pool" if ci % 2 == 0 else "dve"
                cp(eng, dst, st[:, :, 0:ncol], (sk,), ("win",))
                if ci in (4, 6):
                    for m in range(2):
                        sl = wfm_b[:, :, ci * 128 + m * 64: ci * 128 + m * 64 + 32]
                        ts("pool", sl, sl, -1.0, ALU.mult, ("win",), ("win",))
            dma("sp", lamt[:, :], lamv[l][:, :], (), ("lamt",))
            for i in range(2):
                tt("dve", lamt[:, i * 128:i * 128 + 64], lamt[:, i * 128:i * 128 + 64],
                   lamt[:, i * 128 + 64:i * 128 + 128], ALU.mult, ("lamt",), ("lamt",))
                I("dve", lambda e, i=i: e.reduce_sum(lamw[:, i:i + 1], lamt[:, i * 128:i * 128 + 64], AX.X),
                  ("lamt",), ("lamw",))
            act(lamw[:, 2:4], lamw[:, 0:2], AF.Exp, ("lamw",), ("lamw",))
            lam_init = 0.8 - 0.6 * math.exp(-0.3 * l)
            stt(neglam[:, :], lamw[:, 3:4], -lam_init, lamw[:, 2:3], ALU.add, ALU.subtract, ("lamw",), ("neglam",))
            ts("pool", negfb[:, :], vec[l][0:1, 22:23], -1.0, ALU.mult, (("vec", l),), ("negfb",))

        def load_wout(l):
            for ci in range(8):
                st = wstg[ci % 2]
                sk = ("wstg", ci % 2)
                dma("sp", st[:, :, :], wout[l][:, ci * 128:(ci + 1) * 128].rearrange("(kt p) c -> p kt c", p=128),
                    (), (sk,))
                eng = "pool" if ci % 2 == 0 else "dve"
                cp(eng, wout_b[:, :, ci * 128:(ci + 1) * 128], st[:, :, :], (sk,), ("wout",))

        def outproj_block(l_prev, tb, xsrc, xkey, banks=(0,)):
            t0 = tb * TB
            q4, tq = tb // 4, (tb % 4) * TB
            dma("sp", ycb[:, :, :], ycf[l_prev][q4][:, tq:tq + TB].rearrange("(kt p) t -> p kt t", p=128),
                (("ycf", l_prev, q4),), ("sqb",))
            dma("sp", xblk[:, :, :], xsrc[:, t0:t0 + TB].rearrange("(kt p) t -> p kt t", p=128),
                (xkey,), ("xblk",))
            for m in range(KT):
                bank = banks[m % len(banks)]
                for et in range(KT):
                    mm(ps[bank][:, :], wout_b[:, et, m * 128:(m + 1) * 128], ycb[:, et, :], et == 0, et == KT - 1,
                       ("wout", "sqb"), (("ps", bank),))
                tt("dve", xblk[:, m, :], xblk[:, m, :], ps[bank][:, :], ALU.add, (("ps", bank), "xblk"), ("xblk",))
                yield

        def norm_block(nw_ap, to_out):
            for kt in range(KT):
                tt("pool", sqb[:, kt, :], xblk[:, kt, :], xblk[:, kt, :], ALU.mult, ("xblk",), ("sqb",))
            for kt in range(KT):
                mm(ps[0][:, :], onesb[:, :], sqb[:, kt, :], kt == 0, kt == KT - 1, ("onesb", "sqb"), (("ps", 0),))
            yield
            ts("dve", rstd[:, :], ps[0][:, :], 1.0 / D_MODEL, ALU.mult, (("ps", 0),), ("rstd",), s2=EPS, op1=ALU.add)
            act(rstd[:, :], rstd[:, :], AF.Ln, ("rstd",), ("rstd",))
            act(rstd[:, :], rstd[:, :], AF.Exp, ("rstd",), ("rstd",), scale=-0.5)
            for kt in range(KT):
                if to_out:
                    stt(ob[:, kt, :], xblk[:, kt, :], nw_ap[:, kt:kt + 1], rstd[:, :], ALU.mult, ALU.mult,
                        ("xblk", "rstd"), ("xblk",))
                else:
                    stt(xnb[:, kt, :], xblk[:, kt, :], nw_ap[:, kt:kt + 1], rstd[:, :], ALU.mult, ALU.mult,
                        ("xblk", "rstd"), ("xnb",))
            yield

        def inproj_block(l, tb):
            t0 = tb * TB
            pp = tb % 2
            qa, ga = qa2[pp], ga2[pp]
            qak, gak = ("qa", pp), ("ga", pp)
            qm, km, gm, vmc, gif = qm2[pp], km2[pp], gm2[pp], vmc2[pp], gif2[pp]
            V = vec[l]
            vk = ("vec", l)
            dma("sp", cosb[:, :], cosT[:, t0:t0 + TB], (), ("cosb",))
            dma("sp", sinb[:, :], sinT[:, t0:t0 + TB], (), ("sinb",))

            def fm(c0, ncol):
                def f(b):
                    for kt in range(KT):
                        mm(ps[b][0:ncol, :], wfm_b[:, kt, c0:c0 + ncol], xnb[:, kt, :], kt == 0, kt == KT - 1,
                           ("win", "xnb"), (("ps", b),))
                return f

            def conv_ev(which):
                pre, dst, cw0, cbc, fin, fk = [(preq, qm, 8, 16, tC, "tC"), (prek, km, 12, 17, cvb, "tB")][which]
                pk = ("pre", which)

                def e1(b):
                    if tb == 0:
                        I("pool", lambda e: e.memset(pre[:, 0:4], 0.0), (), (pk,))
                    else:
                        cp("pool", pre[:, 1:4], pre[:, TB + 1:TB + 4], (pk,), (pk,))
                    cp("dve", pre[:, 4:4 + TB], ps[b][:, :], (("ps", b), pk), (pk,))
                    ts("dve", cva[:, :], pre[:, 4:4 + TB], V[:, cw0 + 3:cw0 + 4], ALU.mult, (pk, vk), ("tA",),
                       s2=V[:, cbc:cbc + 1], op1=ALU.add)
                    stt(cvb[:, :], pre[:, 3:3 + TB], V[:, cw0 + 2:cw0 + 3], cva[:, :], ALU.mult, ALU.add,
                        (pk, vk, "tA"), ("tB",))
                    stt(cva[:, :], pre[:, 2:2 + TB], V[:, cw0 + 1:cw0 + 2], cvb[:, :], ALU.mult, ALU.add,
                        (pk, vk, "tB"), ("tA",))
                    stt(fin[:, :], pre[:, 1:1 + TB], V[:, cw0:cw0 + 1], cva[:, :], ALU.mult, ALU.add,
                        (pk, vk, "tA"), (fk,))

                def e2(b):
                    act(dst[:, :], fin[:, :], AF.Silu, (fk,), (("qm", pp) if which == 0 else ("km", pp),))
                return [e1, e2]

            def silu_ev(dst, dk):
                return [lambda b: act(dst[:, :], ps[b][:, :], AF.Silu, (("ps", b),), (dk,))]

            def rope_a(b):
                tt("dve", rpa[:, :], ps[b][:, :], cosb[:, :], ALU.mult, (("ps", b), "cosb"), ("tA",))

            def rope_b(which):
                def f(b):
                    tt("dve", rpb[:, :], ps[b][:, :], sinb[:, :], ALU.mult, (("ps", b), "sinb"), ("tB",))
                    if which == 0:
                        tt("pool", qa[:, :], rpa[:, :], rpb[:, :], ALU.add, ("tA", "tB"), (qak,))
                    else:
                        tt("pool", kaT[:, t0:t0 + TB], rpa[:, :], rpb[:, :], ALU.add, ("tA", "tB"), (("ka", tb),))
                return f

            def row_ev(g):
                return [lambda b: cp("dve", gif[:, g, :], ps[b][0:1, :], (("ps", b),), (("gif", pp),))]

            def vm_mm(h):
                def f(b):
                    for c4 in range(4):
                        c8 = h * 4 + c4
                        for kt in range(KT):
                            mm(ps[b][0:64, c4 * 128:(c4 + 1) * 128], xnb[:, kt, c8 * 64:(c8 + 1) * 64],
                               wtm_b[:, kt, 0:128], kt == 0, kt == KT - 1, ("win", "xnb"), (("ps", b),))
                return f

            def vm_ev(h):
                return [lambda b: cp("dve", vmc[:, h * 4:(h + 1) * 4, 0:128],
                                     ps[b][0:64, :].rearrange("p (c d) -> p c d", d=128), (("ps", b),), (("vmc", pp),))]

            def va_mm(b):
                for t4 in range(4):
                    for kt in range(KT):
                        mm(ps[b][:, t4 * 128:(t4 + 1) * 128], xnb[:, kt, t4 * 128:(t4 + 1) * 128],
                           wtm_b[:, kt, 128:256], kt == 0, kt == KT - 1, ("win", "xnb"), (("ps", b),))

            def va_ev(b):
                cp("dve", va[:, tb * 4:(tb + 1) * 4, 0:128], ps[b][:, :].rearrange("p (c d) -> p c d", d=128),
                   (("ps", b), "va_ones"), tuple(("va", tb * 4 + i) for i in range(4)))

            stages = [
                (fm(0, 128), conv_ev(0)),
                (fm(128, 128), conv_ev(1)),
                (fm(256, 128), silu_ev(gm, ("gm", pp))),
                (fm(3 * 128, 128), [rope_a]),
                (fm(4 * 128, 128), [rope_b(0)]),
                (fm(5 * 128, 128), [rope_a]),
                (fm(6 * 128, 128), [rope_b(1)]),
                (fm(7 * 128, 128), silu_ev(ga, gak)),
                (fm(1024, 1), row_ev(0)),
                (fm(1025, 1), row_ev(1)),
                (vm_mm(0), vm_ev(0)),
                (vm_mm(1), vm_ev(1)),
                (va_mm, [va_ev]),
            ]
            pending = []
            for k, (mmf, evs) in enumerate(stages):
                b = 0
                mmf(b)
                for i, fn in enumerate(evs):
                    pending.append((k + i, fn, b))
                for due, fn, bb in [p for p in pending if p[0] <= k]:
                    fn(bb)
                pending = [p for p in pending if p[0] > k]
                yield
            for due, fn, bb in sorted(pending, key=lambda p: p[0]):
                fn(bb)
            yield

        R_I, R_F, R_L1, R_BN, R_A_, R_AA, R_WI, R_WK, R_EM = range(9)
        R_T2, R_T1, R_ALS = R_I, R_F, R_L1

        def rw(i):
            return rows[:, i, :]

        def gates_block(l, tb):
            V = vec[l]
            vk = ("vec", l)
            rk = lambda i: ("row", i)
            pp = tb % 2
            qm, vmc, gif = qm2[pp], vmc2[pp], gif2[pp]
            gk = ("gif", pp)
            if tb == 0:
                I("pool", lambda e: e.memset(carry[:, :], 0.0), (), ("carry",))
            act(rw(R_T1), gif[:, 1, :], AF.Exp, (gk, "negfb"), (rk(R_T1),), scale=-1.0, bias=negfb[:, :])
            act(rw(R_L1), rw(R_T1), AF.Ln, (rk(R_T1),), (rk(R_L1),), bias=1.0)
            I("dve", lambda e: e.tensor_tensor_scan(rw(R_BN), onesf[:, :], rw(R_L1), carry[:, 0:1], ALU.mult, ALU.add),
              ("onesf", rk(R_L1), "carry"), (rk(R_BN),))
            stt(rw(R_A_), gif[:, 0, :], V[0:1, 21:22], rw(R_BN), ALU.add, ALU.add, (gk, vk, rk(R_BN)), (rk(R_A_),))
            I("dve", lambda e: e.tensor_tensor_scan(rw(R_AA), onesf[:, :], rw(R_A_), carry[:, 1:2], ALU.mult, ALU.max),
              ("onesf", rk(R_A_), "carry"), (rk(R_AA),))
            yield
            A3 = rw(R_AA).rearrange("p (c i) -> p c i", i=64)
            aend = A3[:, :, 63]
            cp("pool", ape[:, 0:1], carry[:, 1:2], ("carry",), ("ape",))
            cp("pool", ape[:, 1:8], A3[:, 0:7, 63], (rk(R_AA),), ("ape",))
            tt("dve", rw(R_T1).rearrange("p (c i) -> p c i", i=64), ape[:, :].unsqueeze(2).broadcast_to([1, 8, 64]),
               A3, ALU.subtract, ("ape", rk(R_AA)), (rk(R_T1),))
            act(rw(R_WI), rw(R_T1), AF.Exp, (rk(R_T1),), (rk(R_WI),))
            tt("dve", dec[:, :], ape[:, :], aend, ALU.subtract, ("ape", rk(R_AA)), ("dec",))
            act(dec[:, :], dec[:, :], AF.Exp, ("dec",), ("dec",))
            tt("dve", rw(R_T2).rearrange("p (c i) -> p c i", i=64), rw(R_A_).rearrange("p (c i) -> p c i", i=64),
               aend.unsqueeze(2).broadcast_to([1, 8, 64]), ALU.subtract, (rk(R_A_), rk(R_AA)), (rk(R_T2),))
            act(rw(R_WK), rw(R_T2), AF.Exp, (rk(R_T2),), (rk(R_WK),), bias=LN_KSCALE)
            tt("dve", rw(R_T1), rw(R_BN), rw(R_AA), ALU.subtract, (rk(R_BN), rk(R_AA)), (rk(R_T1),))
            act(rw(R_EM), rw(R_T1), AF.Exp, (rk(R_T1),), (rk(R_EM),))
            ts("pool", rw(R_ALS), rw(R_A_), LN_KSCALE, ALU.add, (rk(R_A_),), (rk(R_ALS),))
            cp("pool", carry[:, 0:1], rw(R_BN)[:, TB - 1:TB], (rk(R_BN),), ("carry",))
            cp("pool", carry[:, 1:2], rw(R_AA)[:, TB - 1:TB], (rk(R_AA), "ape"), ("carry",))
            yield
            for qi, ri in enumerate([R_ALS, R_WK, R_EM]):
                for c8 in range(8):
                    mm(ps[1][0:64, 480 + qi * 8 + c8:480 + qi * 8 + c8 + 1], rw(ri)[:, c8 * 64:(c8 + 1) * 64],
                       onesf[:, 0:1], True, True, (rk(ri), "onesf"), (("ps", 1),))
            cp("dve", cols[:, :], ps[1][0:64, 480:504], (("ps", 1),), ("cols",))
            mm(ps[1][:, 504:512], onesf[:, 0:128], dec[:, :], True, True, ("onesf", "dec"), (("ps", 1),))
            cp("dve", decb[:, :], ps[1][:, 504:512], (("ps", 1),), ("decb",))
            yield
            mm(ps[1][0:64, :], onesf[:, 0:64], rw(R_AA), True, True, ("onesf", rk(R_AA)), (("ps", 1),))
            for c8 in range(8):
                act(wT[:, c8, :], ps[1][0:64, c8 * 64:(c8 + 1) * 64], AF.Exp, (("ps", 1), "cols"), ("wT",),
                    scale=-1.0, bias=cols[:, c8:c8 + 1])
            tt("pool", wT[:, :, :], wT[:, :, :], mask64.unsqueeze(1).broadcast_to([64, 8, 64]), ALU.mult,
               ("wT", "cst"), ("wT",))
            yield
            mm(ps[1][:, :], onesf[:, 0:128], rw(R_WI), True, True, ("onesf", rk(R_WI)), (("ps", 1),))
            tt("dve", qs[:, :], qm[:, :], ps[1][:, :], ALU.mult, (("qm", pp), ("ps", 1)), ("qs",))
            tt("pool", vmw[:, :, 0:129], vmc[:, :, 0:129], cols[:, 8:16].unsqueeze(2).broadcast_to([64, 8, 129]),
               ALU.mult, (("vmc", pp), "vmc_ones", "cols"), ("vmw",))
            yield

        def mlstm_block(l, tb, ycl_ap, okey, res):
            V = vec[l]
            vk = ("vec", l)
            pp = tb % 2
            qm, km, gm, vmc = qm2[pp], km2[pp], gm2[pp], vmc2[pp]
            qmk, kmk, gmk, vmck = ("qm", pp), ("km", pp), ("gm", pp), ("vmc", pp)
            if tb == 0:
                I("pool", lambda e: e.memset(cf[:, :], 0.0), (), ("cf",))
                I("pool", lambda e: e.memset(cb[:, :], 0.0), (), ("cb",))
            for c8 in range(8):
                tr(psb(1)[0:64, c8 * 128:(c8 + 1) * 128], km[:, c8 * 64:(c8 + 1) * 64], identb[:, :],
                   (kmk, "identb"), (("ps", 1),))
            cp("dve", ktok[:, :, :], psb(1)[0:64, :].rearrange("p (c d) -> p c d", d=128), (("ps", 1),), ("ktok",))
            yield

            def st(c8):
                cs = c8 * 64
                so = (c8 % 2) * 64
                mm(ps[1][0:64, so:so + 64], km[:, cs:cs + 64], qm[:, cs:cs + 64], True, True, (kmk, qmk), (("ps", 1),))

            st(0)
            for c8 in range(8):
                cs = c8 * 64
                so = (c8 % 2) * 64
                if c8 + 1 < 8:
                    st(c8 + 1)
                tt("dve", stl[c8 % 2][:, :], ps[1][0:64, so:so + 64], wT[:, c8, :], ALU.mult, (("ps", 1), "wT"),
                   (("stl", c8 % 2),))
                mm(ps[1][:, 257:386], ktok[:, c8, :], vmw[:, c8, 0:129], True, True, ("ktok", "vmw"), (("ps", 1),))
                mm(ps[1][0:64, 128:257], qs[:, cs:cs + 64], cb[:, 0:129], True, False, ("qs", "cb"), (("ps", 1),))
                mm(ps[1][0:64, 128:257], stl[c8 % 2][:, :], vmc[:, c8, 0:129], False, True,
                   (("stl", c8 % 2), vmck, "vmc_ones"), (("ps", 1),))
                stt(cf[:, 0:129], cf[:, 0:129], decb[:, c8:c8 + 1], ps[1][:, 257:386], ALU.mult, ALU.add,
                    ("cf", "decb", ("ps", 1)), ("cf",))
                cp("pool", cb[:, 0:129], cf[:, 0:129], ("cf",), ("cb",))
                cp("dve", nums[:, c8, 0:129], ps[1][0:64, 128:257], (("ps", 1),), ("nums",))
                yield
            den = nums[:, :, 128]
            tt("dve", sq8[:, :, :], nums[:, :, 0:128], nums[:, :, 0:128], ALU.mult, ("nums",), ("sq8",))
            I("dve", lambda e: e.reduce_sum(sm[:, :, 0], sq8[:, :, :], AX.X), ("sq8",), ("sm",))
            ts("dve", sm[:, :, 1], den, -1.0, ALU.mult, ("nums",), ("sm",))
            tt("dve", sm[:, :, 1], sm[:, :, 1], den, ALU.max, ("sm", "nums"), ("sm",))
            tt("dve", sm[:, :, 1], sm[:, :, 1], cols[:, 16:24], ALU.max, ("sm", "cols"), ("sm",))
            tt("dve", sm[:, :, 2], sm[:, :, 1], sm[:, :, 1], ALU.mult, ("sm",), ("sm",))
            ts("dve", sm[:, :, 0], sm[:, :, 0], 1.0 / 128.0, ALU.mult, ("sm",), ("sm",))
            stt(sm[:, :, 3], sm[:, :, 2], EPS, sm[:, :, 0], ALU.mult, ALU.add, ("sm",), ("sm",))
            act(sm[:, :, 4], sm[:, :, 3], AF.Ln, ("sm",), ("sm",))
            act(sm[:, :, 5], sm[:, :, 4], AF.Exp, ("sm",), ("sm",), scale=-0.5)
            tt("dve", hmb[:, :, :], nums[:, :, 0:128], sm[:, :, 5:6].broadcast_to([64, 8, 128]), ALU.mult,
               ("nums", "sm"), ("hmb",))
            for c8 in range(8):
                tr(psb(1)[:, c8 * 64:(c8 + 1) * 64], hmb[:, c8, :], identb[0:64, 0:64], ("hmb", "identb"),
                   (("ps", 1),))
            yield
            ts("dve", gt1[:, :], qm[:, :], V[:, 19:20], ALU.mult, (qmk, vk), ("gtA",))
            stt(gt2[:, :], psb(1)[:, 0:TB], V[:, 18:19], gt1[:, :], ALU.mult, ALU.add, (("ps", 1), vk, "gtA"),
                ("gtB",))
            tt("dve", yo[:, :], gt2[:, :], gm[:, :], ALU.mult, ("gtB", gmk), ("yo",))
            res.append(dma("sp", ycl_ap, yo[:, :], ("yo",), (okey,)))
            yield

        def attn_block(l, tb, ycl_ap, okey, res):
            V = vec[l]
            vk = ("vec", l)
            qa, ga = qa2[tb % 2], ga2[tb % 2]
            qak, gak = ("qa", tb % 2), ("ga", tb % 2)
            lam_init = 0.8 - 0.6 * math.exp(-0.3 * l)
            tiles = []
            for g2 in range(2):
                qt0 = tb * 4 + g2 * 2
                for j in range(qt0 + 2):
                    tiles.append((g2, qt0, j))

            def emit_S(ti):
                g2, qt0, j = tiles[ti]
                qoff = g2 * 256
                lo = 0 if j <= qt0 else 128
                sb0 = 2 + 2 * (ti % 2)
                kb = j // 4
                for m in range(2):
                    mm(ps[sb0 + m][:, lo:256], kaT[m * 64:(m + 1) * 64, j * 128:(j + 1) * 128],
                       qa[m * 64:(m + 1) * 64, qoff + lo:qoff + 256], True, True, (("ka", kb), qak),
                       (("ps", sb0), ("ps", sb0 + 1)))

            touched = set()
            emit_S(0)
            if len(tiles) > 1:
                emit_S(1)
            for ti, (g2, qt0, j) in enumerate(tiles):
                if j == 0:
                    touched = set()
                qoff = g2 * 256
                lo = 0 if j <= qt0 else 128
                sb0 = 2 + 2 * (ti % 2)
                pt = pT[ti % 2]
                ptk = ("pT", ti % 2)
                src_ = ps_all[:, sb0:sb0 + 2, lo:256]
                dst = pt[:, :].rearrange("p (m q) -> p m q", m=2)[:, :, lo:256]
                act(dst, src_, AF.Exp, (("ps", sb0), ("ps", sb0 + 1)), (ptk,), scale=0.125)
                if j >= qt0:
                    d0 = (j - qt0) * 128
                    msl = pt[64:128, :].rearrange("p (m q) -> p m q", m=2)[:, :, d0:d0 + 64]
                    I("pool", lambda e, msl=msl: e.memset(msl, 0.0), (ptk,), (ptk,))
                if ti + 2 < len(tiles):
                    emit_S(ti + 2)
                for qt in range(2):
                    if j > qt0 + qt:
                        continue
                    pb = 6 + qt
                    for m in range(2):
                        first = pb not in touched
                        touched.add(pb)
                        last = (j == qt0 + qt) and m == 1
                        mm(ps[pb][:, m * 129:(m + 1) * 129], pt[:, m * 256 + qt * 128:m * 256 + (qt + 1) * 128],
                           va[:, j, 0:129], first, last, (ptk, ("va", j), "va_ones"), (("ps", pb),))
                    if j == qt0 + qt:
                        P = ps[pb]
                        pk = ("ps", pb)
                        I("dve", lambda e, P=P: e.reciprocal(asm[:, 0:1], P[:, 128:129]), (pk,), ("asm0",))
                        I("dve", lambda e, P=P: e.reciprocal(asm[:, 1:2], P[:, 257:258]), (pk,), ("asm1",))
                        tt("dve", asm[:, 1:2], asm[:, 1:2], neglam[:, :], ALU.mult, ("asm1", "neglam"), ("asm1",))
                        ts("dve", at1[:, :], P[:, 0:128], asm[:, 0:1], ALU.mult, (pk, "asm0"), ("at1",))
                        stt(at2[:, :], P[:, 129:257], asm[:, 1:2], at1[:, :], ALU.mult, ALU.add,
                            (pk, "asm1", "at1"), ("at2",))
                        tt("pool", atj[:, :], at2[:, :], at2[:, :], ALU.mult, ("at2",), ("atj",))
                        I("dve", lambda e: e.reduce_sum(asm[:, 2:3], atj[:, :], AX.X), ("atj",), ("asm2",))
                        ts("dve", asm[:, 3:4], asm[:, 2:3], 1.0 / 128.0, ALU.mult, ("asm2",), ("asm3",),
                           s2=EPS, op1=ALU.add)
                        act(asm[:, 4:5], asm[:, 3:4], AF.Ln, ("asm3",), ("asm4",))
                        act(asm[:, 5:6], asm[:, 4:5], AF.Exp, ("asm4",), ("asm5",), scale=-0.5)
                        ts("dve", hab[:, :], at2[:, :], asm[:, 5:6], ALU.mult, ("at2", "asm5"), ("hab",),
                           s2=1.0 - lam_init, op1=ALU.mult)
                        tr(psb(pb)[:, 768:896], hab[:, :], identb[:, :], ("hab", "identb"), (pk,))
                        c0 = qoff + qt * 128
                        stt(yab[:, c0:c0 + 128], psb(pb)[:, 768:896], V[:, 20:21], ga[:, c0:c0 + 128], ALU.mult,
                            ALU.mult, (pk, vk, gak), ("yab",))
                yield
            res.append(dma("sp", ycl_ap, yab[:, :], ("yab",), (okey,)))

        out_toks = []

        def frontA(l, tb):
            t0 = tb * TB
            if l == 0:
                dma("sp", xblk[:, :, :], xT[:, t0:t0 + TB].rearrange("(kt p) t -> p kt t", p=128), (), ("xblk",))
            else:
                yield from outproj_block(l - 1, tb, xT, ("xsrc0", tb))
                tok = dma("sp", x1s[:, t0:t0 + TB].rearrange("(kt p) t -> p kt t", p=128), xblk[:, :, :],
                          ("xblk",), (("xsrc1", tb),))
                if not do_fin:
                    out_toks.append(tok)
            yield from norm_block(vec[l][:, 0:8], False)
            if _STOP >= 2:
                yield from inproj_block(l, tb)

        def frontB(l, tb):
            q4, tq = tb // 4, (tb % 4) * TB
            if _STOP >= 3:
                yield from gates_block(l, tb)
            res = []
            if _STOP >= 4:
                yield from mlstm_block(l, tb, ycl[l][q4][0:128, tq:tq + TB], ("ycl", l, q4, tb % 4, 0), res)
            if not fused:
                out_toks.extend(res)

        def drain(g):
            for _ in g:
                pass

        def adv(g, n):
            for _ in range(n):
                try:
                    next(g)
                except StopIteration:
                    return False
            return True

        NBU, NAU = 20.0, 28.0

        for l in layers:
            load_weights(l)
            if l > 0:
                load_wout(l - 1)
            drain(frontA(l, 0))
            for tb in range(_NB):
                q4, tq = tb // 4, (tb % 4) * TB
                res = []
                ag = (attn_block(l, tb, ycl[l][q4][128:256, tq:tq + TB], ("ycl", l, q4, tb % 4, 1), res)
                      if _STOP >= 5 else iter(()))
                bg = frontB(l, tb)
                fg = frontA(l, tb + 1) if tb + 1 < _NB else iter(())
                ntile = 8 * tb + 6
                accb = acca = 0.0
                bl = fl = True
                for _ in ag:
                    accb += NBU / ntile
                    acca += NAU / ntile
                    nb_, na_ = int(accb), int(acca)
                    accb -= nb_
                    acca -= na_
                    while nb_ > 0 or na_ > 0:
                        if nb_ > 0:
                            bl = bl and adv(bg, 1)
                            nb_ -= 1
                        if na_ > 0:
                            fl = fl and adv(fg, 1)
                            na_ -= 1
                while bl or fl:
                    if bl:
                        bl = adv(bg, 1)
                    if fl:
                        fl = adv(fg, 1)
                if not fused:
                    out_toks.extend(res)
                if fused and tb % 4 == 3:
                    rk = tuple(("ycl", l, q4, i, w) for i in range(4) for w in range(2))
                    I("pool", lambda e, l=l, q4=q4: e.collective_compute(
                        "AllGather", ALU.bypass, replica_groups=GROUPS,
                        ins=[ycl[l][q4][:, :]], outs=[ycf[l][q4][:, :]]), rk, (("ycf", l, q4),), cc=True)

        if do_fin:
            load_wout(DEPTH - 1)
            for tb in range(_NB):
                t0 = tb * TB
                drain(outproj_block(DEPTH - 1, tb, x1s, ("xsrc1", tb), banks=(0, 1)))
                drain(norm_block(vec[DEPTH - 1][:, 23:31], True))
                out_toks.append(dma("sp", outT[:, t0:t0 + TB].rearrange("(kt p) t -> p kt t", p=128), xblk[:, :, :],
                                    ("xblk",), ()))

        waits = []
        for tok in out_toks:
            S._tok_wait("sp", tok, waits)
        S.recs["sp"].append((waits, None, None))

        with nc.Block() as block:
            S.emit(block)
    return nc


_OFF = {"mq": 0, "mk": 512, "mv": 1024, "mi": 1536, "mf": 1540, "mz": 1544,
        "aq": 2056, "ak": 2568, "av": 3080, "az": 3592}


def _rope_tables():
    inv = (1.0 / (np.float32(10000.0) ** (np.arange(0, 64, 2, dtype=np.float32) / np.float32(64.0)))).astype(np.float32)
    ang = np.arange(SEQ, dtype=np.float32)[:, None] * inv[None, :]
    cos = np.cos(ang).astype(np.float32)
    sin = np.sin(ang).astype(np.float32)
    cosT = np.ascontiguousarray(np.concatenate([cos, cos, cos, cos], 1).T)
    sinT = np.ascontiguousarray(np.concatenate([sin, sin, sin, sin], 1).T)
    return cosT, sinT


def _consts():
    c = np.zeros((128, 192), np.float32)
    c[:, 0:128] = np.eye(128, dtype=np.float32)
    c[0:64, 128:192] = np.triu(np.ones((64, 64), np.float32))
    return c


def _core_inputs(inp, c, stage_layers, need_wout):
    b, hd = c // 4, c % 4
    f32 = np.float32
    d = {}
    for l in stage_layers:
        w = np.asarray(inp["w_in"][l], f32)
        hs = slice(hd * 128, (hd + 1) * 128)

        def blk(name):
            return w[:, _OFF[name] + hd * 128:_OFF[name] + (hd + 1) * 128]

        def perm(m):
            return np.concatenate([m[:, 32:64], m[:, 0:32], m[:, 96:128], m[:, 64:96]], 1)

        aq, ak = blk("aq"), blk("ak")
        gi = w[:, _OFF["mi"] + hd:_OFF["mi"] + hd + 1]
        gf = w[:, _OFF["mf"] + hd:_OFF["mf"] + hd + 1]
        d[f"wfm{l}"] = np.ascontiguousarray(np.concatenate(
            [blk("mq"), blk("mk"), blk("mz"), aq, perm(aq), ak, perm(ak), blk("az"), gi, gf], 1))
        d[f"wtm{l}"] = np.ascontiguousarray(np.concatenate([blk("mv"), blk("av")], 1))
        lam = np.concatenate([np.asarray(inp[k][l], f32) for k in ("lam_q1", "lam_k1", "lam_q2", "lam_k2")])
        d[f"lamv{l}"] = np.ascontiguousarray(np.tile(lam[None, :], (128, 1)))
    for l in range(DEPTH):
        v = np.zeros((128, NV), f32)
        v[:, 0:8] = np.asarray(inp["norm_w"][l], f32).reshape(8, 128).T
        cw = np.asarray(inp["conv_w"][l], f32)
        cbv = np.asarray(inp["conv_b"][l], f32)
        v[:, 8:12] = cw[:, hd * 128:(hd + 1) * 128].T
        v[:, 12:16] = cw[:, 512 + hd * 128:512 + (hd + 1) * 128].T
        v[:, 16] = cbv[hd * 128:(hd + 1) * 128]
        v[:, 17] = cbv[512 + hd * 128:512 + (hd + 1) * 128]
        v[:, 18] = np.asarray(inp["m_norm_w"][l], f32)[hd * 128:(hd + 1) * 128]
        v[:, 19] = np.asarray(inp["m_skip"][l], f32)[hd * 128:(hd + 1) * 128]
        v[:, 20] = np.asarray(inp["a_norm_w"][l], f32)
        v[:, 21] = np.asarray(inp["i_bias"][l], f32)[hd]
        v[:, 22] = np.asarray(inp["f_bias"][l], f32)[hd]
        v[:, 23:31] = np.asarray(inp["final_norm_w"], f32).reshape(8, 128).T
        d[f"vecs{l}"] = v
    for l in need_wout:
        wo = np.asarray(inp["w_out"][l], f32)
        rows = []
        for r in range(4):
            rows.append(wo[r * 128:(r + 1) * 128])
            rows.append(wo[512 + r * 128:512 + (r + 1) * 128])
        d[f"wout{l}"] = np.ascontiguousarray(np.concatenate(rows, 0))
    return d


_PROG = {}


def _prog(stage, debug=False):
    key = (stage, debug)
    if key not in _PROG:
        _PROG[key] = build_program(stage, debug)
    return _PROG[key]


def kernel(**inp):
    x = np.asarray(inp["x"], np.float32)
    xTs = [np.ascontiguousarray(x[b].T) for b in range(BATCH)]
    cosT, sinT = _rope_tables()
    cst = _consts()
    nc = _prog("all")
    in_maps = []
    for c in range(NCORES):
        d = _core_inputs(inp, c, [0, 1], [0, 1])
        d.update({"xT": xTs[c // 4], "cosT": cosT, "sinT": sinT, "consts": cst})
        in_maps.append(d)
    res = run_bass_kernel_spmd(nc, in_maps, core_ids=list(range(NCORES)))
    out = np.empty((BATCH, SEQ, D_MODEL), np.float32)
    for b in range(BATCH):
        out[b] = res.results[4 * b]["outT"].T
    return out
```

```python
import math
from contextlib import ExitStack

import numpy as np
import ml_dtypes

import concourse.bass as bass
import concourse.mybir as mybir
from concourse.bass_utils import run_bass_kernel_spmd

F32 = mybir.dt.float32
BF16 = mybir.dt.bfloat16
AF = mybir.ActivationFunctionType
ALU = mybir.AluOpType
AX = mybir.AxisListType

D_MODEL = 1024
BATCH = 2
SEQ = 8192
DEPTH = 2
NCORES = 8
TB = 512
NBLK = SEQ // TB
KT = D_MODEL // 128
EPS = 1e-6
NV = 32
LN_KSCALE = math.log(128.0 ** -0.5)
GROUPS = [[0, 1, 2, 3], [4, 5, 6, 7]]
import os
_STOP = int(os.environ.get("K_STOP", "9"))
_NB = int(os.environ.get("K_NBLK", str(NBLK)))


class Sched:
    CH = 30000
    ND = 48

    def __init__(self, nc, es):
        self.nc = nc
        self.engs = ["pe", "act", "dve", "pool", "sp"]
        self.recs = {e: [] for e in self.engs}
        self.cnt = {e: 0 for e in self.engs}
        self.seen = {e: {} for e in self.engs}
        self.lastw = {}
        self.readers = {}
        nsem = {"pe": 3, "act": 3, "dve": 4, "pool": 3, "sp": 1}
        self.esems = {e: [es.enter_context(nc.semaphore(f"s_{e}_{i}")) for i in range(nsem[e])]
                      for e in self.engs}
        self.dsems = [es.enter_context(nc.semaphore(f"d_{i}")) for i in range(self.ND)]
        self.dval = [0] * self.ND
        self.dnext = 0
        self.ccsem = es.enter_context(nc.semaphore("ccsem"))
        self.ccval = 0

    def _tok_wait(self, e, tok, waits):
        if tok[0] == "e":
            _, e2, n = tok
            if e2 == e and e == "pe":
                return
            if self.seen[e].get(e2, 0) >= n:
                return
            self.seen[e][e2] = n
            waits.append((self.esems[e2][(n - 1) // self.CH], (n - 1) % self.CH + 1))
        else:
            kind, i, v = tok
            key = (kind, i)
            if self.seen[e].get(key, 0) >= v:
                return
            self.seen[e][key] = v
            sem = self.dsems[i] if kind == "d" else self.ccsem
            waits.append((sem, v))

    def issue(self, e, fn, reads=(), writes=(), dma=False, cc=False):
        deps = []
        for k in reads:
            if k in self.lastw:
                deps.append(self.lastw[k])
        for k in writes:
            if k in self.lastw:
                deps.append(self.lastw[k])
            deps.extend(self.readers.get(k, {}).values())
        waits = []
        for tok in deps:
            self._tok_wait(e, tok, waits)
        if dma:
            i = self.dnext
            self.dnext = (self.dnext + 1) % self.ND
            prev = self.dval[i]
            if prev > 0:
                self._tok_wait(e, ("d", i, prev), waits)
            self.dval[i] += 16
            tok = ("d", i, self.dval[i])
            inc = (self.dsems[i], 16)
        elif cc:
            self.ccval += 1
            tok = ("c", 0, self.ccval)
            inc = (self.ccsem, 1)
        elif fn is None:
            tok = None
            inc = None
        else:
            self.cnt[e] += 1
            n = self.cnt[e]
            tok = ("e", e, n)
            inc = (self.esems[e][(n - 1) // self.CH], 1)
        self.recs[e].append((waits, fn, inc))
        if tok is not None:
            for k in reads:
                self.readers.setdefault(k, {})[(tok[0], tok[1])] = tok
            for k in writes:
                self.lastw[k] = tok
                self.readers[k] = {}
        return tok

    def emit(self, block):
        nc = self.nc

        def run(e, eng):
            for waits, fn, inc in self.recs[e]:
                for s, v in waits:
                    eng.wait_ge(s, v)
                if fn is not None:
                    ins = fn(eng)
                    ins.then_inc(inc[0], inc[1])

        @block.tensor
        def _(eng):
            run("pe", eng)

        @block.scalar
        def _(eng):
            run("act", eng)

        @block.vector
        def _(eng):
            run("dve", eng)

        @block.gpsimd
        def _(eng):
            run("pool", eng)

        @block.sync
        def _(eng):
            run("sp", eng)


def build_program(stage="all", debug=False):
    nc = bass.Bass("TRN2", target_bir_lowering=False)
    with ExitStack() as es:
        S = Sched(nc, es)

        def dram(name, shape, dt, kind):
            return nc.dram_tensor(name, shape, dt, kind=kind).ap()

        fused = stage == "all"
        layers = {"all": [0, 1], "l0": [0], "mid": [1], "fin": []}[stage]
        do_fin = stage in ("all", "fin")

        xT = dram("xT", [D_MODEL, SEQ], F32, "ExternalInput") if stage in ("all", "l0", "mid") else None
        cosT = dram("cosT", [128, SEQ], F32, "ExternalInput") if layers else None
        sinT = dram("sinT", [128, SEQ], F32, "ExternalInput") if layers else None
        consts = dram("consts", [128, 192], F32, "ExternalInput")
        wfm, wtm, wout, vecs, lamv = {}, {}, {}, {}, {}
        for l in layers:
            wfm[l] = dram(f"wfm{l}", [D_MODEL, 1026], F32, "ExternalInput")
            wtm[l] = dram(f"wtm{l}", [D_MODEL, 256], F32, "ExternalInput")
            lamv[l] = dram(f"lamv{l}", [128, 256], F32, "ExternalInput")
        for l in range(DEPTH):
            vecs[l] = dram(f"vecs{l}", [128, NV], F32, "ExternalInput")
        need_wout = {"all": [0, 1], "l0": [], "mid": [0], "fin": [1]}[stage]
        for l in need_wout:
            wout[l] = dram(f"wout{l}", [D_MODEL, D_MODEL], F32, "ExternalInput")

        ycl, ycf = {}, {}
        for l in layers:
            kind = "Internal" if fused else "ExternalOutput"
            ycl[l] = [dram(f"ycl{l}_{q}", [256, 2048], BF16, kind) for q in range(4)]
        for l in need_wout:
            kind = "Internal" if fused else "ExternalInput"
            ycf[l] = [dram(f"ycf{l}_{q}", [1024, 2048], BF16, kind) for q in range(4)]
        x1s = None
        if stage == "all":
            x1s = dram("x1s", [D_MODEL, SEQ], F32, "Internal")
        elif stage == "mid":
            x1s = dram("x1s", [D_MODEL, SEQ], F32, "ExternalOutput")
        elif stage == "fin":
            x1s = dram("x1s", [D_MODEL, SEQ], F32, "ExternalInput")
        outT = dram("outT", [D_MODEL, SEQ], F32, "ExternalOutput") if do_fin else None
        dbg = {}
        if debug:
            for nm, shp, dt in [("d_qm", [128, SEQ], BF16), ("d_km", [128, SEQ], BF16),
                                ("d_qa", [128, SEQ], BF16), ("d_ka", [128, SEQ], BF16),
                                ("d_rows", [1, 9 * SEQ], F32)]:
                dbg[nm] = dram(nm, shp, dt, "ExternalOutput")

        def sb(name, shape, dt):
            return es.enter_context(nc.sbuf_tensor(name, shape, dt))

        cst = sb("cst", [128, 192], F32)
        ident_f = cst[:, 0:128]
        identb = sb("identb", [128, 128], BF16)
        onesb = sb("onesb", [128, 128], BF16)
        onesf = sb("onesf", [1, 512], F32)
        vec = [sb(f"vec{l}", [128, NV], F32) for l in range(DEPTH)]
        lamt = sb("lamt", [128, 256], F32)
        lamw = sb("lamw", [128, 4], F32)
        neglam = sb("neglam", [128, 1], F32)
        negfb = sb("negfb", [1, 1], F32)

        wfm_b = sb("wfm_b", [128, KT, 1026], BF16)
        wtm_b = sb("wtm_b", [128, KT, 256], BF16)
        wout_b = sb("wout_b", [128, KT, D_MODEL], BF16)
        wstg = [sb(f"wstg{i}", [128, KT, 128], F32) for i in range(2)]

        xblk = sb("xblk", [128, KT, TB], F32)
        sqb = sb("sqb", [128, KT, TB], BF16)
        xnb = sb("xnb", [128, KT, TB], BF16)
        ycb = sqb
        rstd = sb("rstd", [128, TB], F32)
        cosb = sb("cosb", [128, TB], F32)
        sinb = sb("sinb", [128, TB], F32)
        preq = sb("preq", [128, TB + 4], F32)
        prek = sb("prek", [128, TB + 4], F32)
        cva = sb("tA", [128, TB], F32)
        cvb = sb("tB", [128, TB], F32)
        tC = sb("tC", [128, TB], F32)
        rpa, rpb = cva, cvb
        qa2 = [sb(f"qa{i}", [128, TB], BF16) for i in range(2)]
        qm2 = [sb(f"qm{i}", [128, TB], BF16) for i in range(2)]
        km2 = [sb(f"km{i}", [128, TB], BF16) for i in range(2)]
        qs = sb("qs", [128, TB], BF16)
        gm2 = [sb(f"gm{i}", [128, TB], F32) for i in range(2)]
        gtA = sb("gtA", [128, TB], F32)
        gtB = sb("gtB", [128, TB], F32)
        ga2 = [sb(f"ga{i}", [128, TB], F32) for i in range(2)]
        kaT = sb("kaT", [128, SEQ], BF16)
        va = sb("va", [128, SEQ // 128, 132], BF16)
        vmc2 = [sb(f"vmc{i}", [64, 8, 132], BF16) for i in range(2)]
        vmw = sb("vmw", [64, 8, 132], BF16)
        ktok = sb("ktok", [64, 8, 128], BF16)
        NR = 9
        rows = sb("rows", [1, NR, TB], F32)
        gif2 = [sb(f"gif{i}", [1, 2, TB], F32) for i in range(2)]
        carry = sb("carry", [1, 4], F32)
        ape = sb("ape", [1, 8], F32)
        dec = sb("dec", [1, 8], F32)
        cols = sb("cols", [64, 24], F32)
        decb = sb("decb", [128, 8], F32)
        wT = sb("wT", [64, 8, 64], F32)
        stl = [sb(f"stl{i}", [64, 64], BF16) for i in range(2)]
        cf = sb("cf", [128, 132], F32)
        cb = sb("cb", [128, 132], BF16)
        nums = sb("nums", [64, 8, 132], F32)
        sq8 = sb("sq8", [64, 8, 128], F32)
        sm = sb("sm", [64, 8, 8], F32)
        hmb = sb("hmb", [64, 8, 128], BF16)
        gt1, gt2 = gtA, gtB
        yo = sb("yo", [128, TB], BF16)
        pT = [sb(f"pT{i}", [128, 512], BF16) for i in range(2)]
        at1 = sb("at1", [128, 128], F32)
        at2 = sb("at2", [128, 128], F32)
        atj = sb("atj", [128, 128], F32)
        asm = sb("asm", [128, 8], F32)
        hab = sb("hab", [128, 128], BF16)
        yab = sb("yab", [128, TB], BF16)
        ob = xblk

        ps_all = es.enter_context(nc.psum_tensor("ps_all", [128, 8, 512], F32))
        ps = [ps_all[:, i, :] for i in range(8)]

        def I(e, fn, r=(), w=(), **kw):
            return S.issue(e, fn, r, w, **kw)

        def dma(q, out, in_, r, w):
            return I(q, lambda e: e.dma_start(out=out, in_=in_), r, w, dma=True)

        def act(out, in_, func, r, w, scale=1.0, bias=0.0, accum=None):
            if accum is None:
                return I("act", lambda e: e.activation(out, in_, func, bias=bias, scale=scale), r, w)
            return I("act", lambda e: e.activation(out, in_, func, bias=bias, scale=scale, accum_out=accum), r, w)

        def tt(eng, out, a, b, op, r, w):
            return I(eng, lambda e: e.tensor_tensor(out, a, b, op), r, w)

        def ts(eng, out, a, s1, op0, r, w, s2=None, op1=ALU.bypass):
            return I(eng, lambda e: e.tensor_scalar(out, a, s1, s2, op0, op1), r, w)

        def stt(out, a, sc, b, op0, op1, r, w):
            return I("dve", lambda e: e.scalar_tensor_tensor(out, a, sc, b, op0, op1), r, w)

        def cp(eng, out, in_, r, w):
            if eng == "act":
                return I(eng, lambda e: e.copy(out, in_), r, w)
            return I(eng, lambda e: e.tensor_copy(out, in_), r, w)

        def mm(out, lhsT, rhs, start, stop, r, w):
            return I("pe", lambda e: e.matmul(out, lhsT, rhs, start=start, stop=stop), r, w)

        def tr(out, in_, idn, r, w):
            return I("pe", lambda e: e.transpose(out, in_, idn), r, w)

        def psb(i):
            return ps[i].bitcast(BF16)

        dma("sp", cst[:, :], consts[:, :], (), ("cst",))
        for l in range(DEPTH):
            dma("sp", vec[l][:, :], vecs[l][:, :], (), (("vec", l),))
        cp("dve", identb[:, :], cst[:, 0:128], ("cst",), ("identb",))
        I("pool", lambda e: e.memset(onesb[:, :], 1.0), (), ("onesb",))
        I("pool", lambda e: e.memset(onesf[:, :], 1.0), (), ("onesf",))
        I("pool", lambda e: e.memset(va[:, :, 128:132], 1.0), (), ("va_ones",))
        for i in range(2):
            I("pool", lambda e, i=i: e.memset(vmc2[i][:, :, 128:132], 1.0), (), ("vmc_ones",))
        mask64 = cst[0:64, 128:192]

        def load_weights(l):
            nchunk = 11
            for ci in range(nchunk):
                st = wstg[ci % 2]
                sk = ("wstg", ci % 2)
                if ci < 8:
                    src = wfm[l][:, ci * 128:(ci + 1) * 128]
                    dst = wfm_b[:, :, ci * 128:(ci + 1) * 128]
                    ncol = 128
                elif ci == 8:
                    src = wfm[l][:, 1024:1026]
                    dst = wfm_b[:, :, 1024:1026]
                    ncol = 2
                else:
                    src = wtm[l][:, (ci - 9) * 128:(ci - 8) * 128]
                    dst = wtm_b[:, :, (ci - 9) * 128:(ci - 8) * 128]
                    ncol = 128
                dma("sp", st[:, :, 0:ncol], src.rearrange("(kt p) c -> p kt c", p=128), (), (sk,))
                eng = "pool" if ci % 2 == 0 else "dve"
                cp(eng, dst, st[:, :, 0:ncol], (sk,), ("win",))
                if ci in (4, 6):
                    for m in range(2):
                        sl = wfm_b[:, :, ci * 128 + m * 64: ci * 128 + m * 64 + 32]
                        ts("pool", sl, sl, -1.0, ALU.mult, ("win",), ("win",))
            dma("sp", lamt[:, :], lamv[l][:, :], (), ("lamt",))
            for i in range(2):
                tt("dve", lamt[:, i * 128:i * 128 + 64], lamt[:, i * 128:i * 128 + 64],
                   lamt[:, i * 128 + 64:i * 128 + 128], ALU.mult, ("lamt",), ("lamt",))
                I("dve", lambda e, i=i: e.reduce_sum(lamw[:, i:i + 1], lamt[:, i * 128:i * 128 + 64], AX.X),
                  ("lamt",), ("lamw",))
            act(lamw[:, 2:4], lamw[:, 0:2], AF.Exp, ("lamw",), ("lamw",))
            lam_init = 0.8 - 0.6 * math.exp(-0.3 * l)
            stt(neglam[:, :], lamw[:, 3:4], -lam_init, lamw[:, 2:3], ALU.add, ALU.subtract, ("lamw",), ("neglam",))
            ts("pool", negfb[:, :], vec[l][0:1, 22:23], -1.0, ALU.mult, (("vec", l),), ("negfb",))

        def load_wout(l):
            for ci in range(8):
                st = wstg[ci % 2]
                sk = ("wstg", ci % 2)
                dma("sp", st[:, :, :], wout[l][:, ci * 128:(ci + 1) * 128].rearrange("(kt p) c -> p kt c", p=128),
                    (), (sk,))
                eng = "pool" if ci % 2 == 0 else "dve"
                cp(eng, wout_b[:, :, ci * 128:(ci + 1) * 128], st[:, :, :], (sk,), ("wout",))

        def outproj_block(l_prev, tb, xsrc, xkey, banks=(0,)):
            t0 = tb * TB
            q4, tq = tb // 4, (tb % 4) * TB
            dma("sp", ycb[:, :, :], ycf[l_prev][q4][:, tq:tq + TB].rearrange("(kt p) t -> p kt t", p=128),
                (("ycf", l_prev, q4),), ("sqb",))
            dma("sp", xblk[:, :, :], xsrc[:, t0:t0 + TB].rearrange("(kt p) t -> p kt t", p=128),
                (xkey,), ("xblk",))
            for m in range(KT):
                bank = banks[m % len(banks)]
                for et in range(KT):
                    mm(ps[bank][:, :], wout_b[:, et, m * 128:(m + 1) * 128], ycb[:, et, :], et == 0, et == KT - 1,
                       ("wout", "sqb"), (("ps", bank),))
                tt("dve", xblk[:, m, :], xblk[:, m, :], ps[bank][:, :], ALU.add, (("ps", bank), "xblk"), ("xblk",))
                yield

        def norm_block(nw_ap, to_out):
            for kt in range(KT):
                tt("pool", sqb[:, kt, :], xblk[:, kt, :], xblk[:, kt, :], ALU.mult, ("xblk",), ("sqb",))
            for kt in range(KT):
                mm(ps[0][:, :], onesb[:, :], sqb[:, kt, :], kt == 0, kt == KT - 1, ("onesb", "sqb"), (("ps", 0),))
            yield
            ts("dve", rstd[:, :], ps[0][:, :], 1.0 / D_MODEL, ALU.mult, (("ps", 0),), ("rstd",), s2=EPS, op1=ALU.add)
            act(rstd[:, :], rstd[:, :], AF.Ln, ("rstd",), ("rstd",))
            act(rstd[:, :], rstd[:, :], AF.Exp, ("rstd",), ("rstd",), scale=-0.5)
            for kt in range(KT):
                if to_out:
                    stt(ob[:, kt, :], xblk[:, kt, :], nw_ap[:, kt:kt + 1], rstd[:, :], ALU.mult, ALU.mult,
                        ("xblk", "rstd"), ("xblk",))
                else:
                    stt(xnb[:, kt, :], xblk[:, kt, :], nw_ap[:, kt:kt + 1], rstd[:, :], ALU.mult, ALU.mult,
                        ("xblk", "rstd"), ("xnb",))
            yield

        def inproj_block(l, tb):
            t0 = tb * TB
            pp = tb % 2
            qa, ga = qa2[pp], ga2[pp]
            qak, gak = ("qa", pp), ("ga", pp)
            qm, km, gm, vmc, gif = qm2[pp], km2[pp], gm2[pp], vmc2[pp], gif2[pp]
            V = vec[l]
            vk = ("vec", l)
            dma("sp", cosb[:, :], cosT[:, t0:t0 + TB], (), ("cosb",))
            dma("sp", sinb[:, :], sinT[:, t0:t0 + TB], (), ("sinb",))

            def fm(c0, ncol):
                def f(b):
                    for kt in range(KT):
                        mm(ps[b][0:ncol, :], wfm_b[:, kt, c0:c0 + ncol], xnb[:, kt, :], kt == 0, kt == KT - 1,
                           ("win", "xnb"), (("ps", b),))
                return f

            def conv_ev(which):
                pre, dst, cw0, cbc, fin, fk = [(preq, qm, 8, 16, tC, "tC"), (prek, km, 12, 17, cvb, "tB")][which]
                pk = ("pre", which)

                def e1(b):
                    if tb == 0:
                        I("pool", lambda e: e.memset(pre[:, 0:4], 0.0), (), (pk,))
                    else:
                        cp("pool", pre[:, 1:4], pre[:, TB + 1:TB + 4], (pk,), (pk,))
                    cp("dve", pre[:, 4:4 + TB], ps[b][:, :], (("ps", b), pk), (pk,))
                    ts("dve", cva[:, :], pre[:, 4:4 + TB], V[:, cw0 + 3:cw0 + 4], ALU.mult, (pk, vk), ("tA",),
                       s2=V[:, cbc:cbc + 1], op1=ALU.add)
                    stt(cvb[:, :], pre[:, 3:3 + TB], V[:, cw0 + 2:cw0 + 3], cva[:, :], ALU.mult, ALU.add,
                        (pk, vk, "tA"), ("tB",))
                    stt(cva[:, :], pre[:, 2:2 + TB], V[:, cw0 + 1:cw0 + 2], cvb[:, :], ALU.mult, ALU.add,
                        (pk, vk, "tB"), ("tA",))
                    stt(fin[:, :], pre[:, 1:1 + TB], V[:, cw0:cw0 + 1], cva[:, :], ALU.mult, ALU.add,
                        (pk, vk, "tA"), (fk,))

                def e2(b):
                    act(dst[:, :], fin[:, :], AF.Silu, (fk,), (("qm", pp) if which == 0 else ("km", pp),))
                return [e1, e2]

            def silu_ev(dst, dk):
                return [lambda b: act(dst[:, :], ps[b][:, :], AF.Silu, (("ps", b),), (dk,))]

            def rope_a(b):
                tt("dve", rpa[:, :], ps[b][:, :], cosb[:, :], ALU.mult, (("ps", b), "cosb"), ("tA",))

            def rope_b(which):
                def f(b):
                    tt("dve", rpb[:, :], ps[b][:, :], sinb[:, :], ALU.mult, (("ps", b), "sinb"), ("tB",))
                    if which == 0:
                        tt("pool", qa[:, :], rpa[:, :], rpb[:, :], ALU.add, ("tA", "tB"), (qak,))
                    else:
                        tt("pool", kaT[:, t0:t0 + TB], rpa[:, :], rpb[:, :], ALU.add, ("tA", "tB"), (("ka", tb),))
                return f

            def row_ev(g):
                return [lambda b: cp("dve", gif[:, g, :], ps[b][0:1, :], (("ps", b),), (("gif", pp),))]

            def vm_mm(h):
                def f(b):
                    for c4 in range(4):
                        c8 = h * 4 + c4
                        for kt in range(KT):
                            mm(ps[b][0:64, c4 * 128:(c4 + 1) * 128], xnb[:, kt, c8 * 64:(c8 + 1) * 64],
                               wtm_b[:, kt, 0:128], kt == 0, kt == KT - 1, ("win", "xnb"), (("ps", b),))
                return f

            def vm_ev(h):
                return [lambda b: cp("dve", vmc[:, h * 4:(h + 1) * 4, 0:128],
                                     ps[b][0:64, :].rearrange("p (c d) -> p c d", d=128), (("ps", b),), (("vmc", pp),))]

            def va_mm(b):
                for t4 in range(4):
                    for kt in range(KT):
                        mm(ps[b][:, t4 * 128:(t4 + 1) * 128], xnb[:, kt, t4 * 128:(t4 + 1) * 128],
                           wtm_b[:, kt, 128:256], kt == 0, kt == KT - 1, ("win", "xnb"), (("ps", b),))

            def va_ev(b):
                cp("dve", va[:, tb * 4:(tb + 1) * 4, 0:128], ps[b][:, :].rearrange("p (c d) -> p c d", d=128),
                   (("ps", b), "va_ones"), tuple(("va", tb * 4 + i) for i in range(4)))

            stages = [
                (fm(0, 128), conv_ev(0)),
                (fm(128, 128), conv_ev(1)),
                (fm(256, 128), silu_ev(gm, ("gm", pp))),
                (fm(3 * 128, 128), [rope_a]),
                (fm(4 * 128, 128), [rope_b(0)]),
                (fm(5 * 128, 128), [rope_a]),
                (fm(6 * 128, 128), [rope_b(1)]),
                (fm(7 * 128, 128), silu_ev(ga, gak)),
                (fm(1024, 1), row_ev(0)),
                (fm(1025, 1), row_ev(1)),
                (vm_mm(0), vm_ev(0)),
                (vm_mm(1), vm_ev(1)),
                (va_mm, [va_ev]),
            ]
            pending = []
            for k, (mmf, evs) in enumerate(stages):
                b = 0
                mmf(b)
                for i, fn in enumerate(evs):
                    pending.append((k + i, fn, b))
                for due, fn, bb in [p for p in pending if p[0] <= k]:
                    fn(bb)
                pending = [p for p in pending if p[0] > k]
                yield
            for due, fn, bb in sorted(pending, key=lambda p: p[0]):
                fn(bb)
            yield

        R_I, R_F, R_L1, R_BN, R_A_, R_AA, R_WI, R_WK, R_EM = range(9)
        R_T2, R_T1, R_ALS = R_I, R_F, R_L1

        def rw(i):
            return rows[:, i, :]

        def gates_block(l, tb):
            V = vec[l]
            vk = ("vec", l)
            rk = lambda i: ("row", i)
            pp = tb % 2
            qm, vmc, gif = qm2[pp], vmc2[pp], gif2[pp]
            gk = ("gif", pp)
            if tb == 0:
                I("pool", lambda e: e.memset(carry[:, :], 0.0), (), ("carry",))
            act(rw(R_T1), gif[:, 1, :], AF.Exp, (gk, "negfb"), (rk(R_T1),), scale=-1.0, bias=negfb[:, :])
            act(rw(R_L1), rw(R_T1), AF.Ln, (rk(R_T1),), (rk(R_L1),), bias=1.0)
            I("dve", lambda e: e.tensor_tensor_scan(rw(R_BN), onesf[:, :], rw(R_L1), carry[:, 0:1], ALU.mult, ALU.add),
              ("onesf", rk(R_L1), "carry"), (rk(R_BN),))
            stt(rw(R_A_), gif[:, 0, :], V[0:1, 21:22], rw(R_BN), ALU.add, ALU.add, (gk, vk, rk(R_BN)), (rk(R_A_),))
            I("dve", lambda e: e.tensor_tensor_scan(rw(R_AA), onesf[:, :], rw(R_A_), carry[:, 1:2], ALU.mult, ALU.max),
              ("onesf", rk(R_A_), "carry"), (rk(R_AA),))
            yield
            A3 = rw(R_AA).rearrange("p (c i) -> p c i", i=64)
            aend = A3[:, :, 63]
            cp("pool", ape[:, 0:1], carry[:, 1:2], ("carry",), ("ape",))
            cp("pool", ape[:, 1:8], A3[:, 0:7, 63], (rk(R_AA),), ("ape",))
            tt("dve", rw(R_T1).rearrange("p (c i) -> p c i", i=64), ape[:, :].unsqueeze(2).broadcast_to([1, 8, 64]),
               A3, ALU.subtract, ("ape", rk(R_AA)), (rk(R_T1),))
            act(rw(R_WI), rw(R_T1), AF.Exp, (rk(R_T1),), (rk(R_WI),))
            tt("dve", dec[:, :], ape[:, :], aend, ALU.subtract, ("ape", rk(R_AA)), ("dec",))
            act(dec[:, :], dec[:, :], AF.Exp, ("dec",), ("dec",))
            tt("dve", rw(R_T2).rearrange("p (c i) -> p c i", i=64), rw(R_A_).rearrange("p (c i) -> p c i", i=64),
               aend.unsqueeze(2).broadcast_to([1, 8, 64]), ALU.subtract, (rk(R_A_), rk(R_AA)), (rk(R_T2),))
            act(rw(R_WK), rw(R_T2), AF.Exp, (rk(R_T2),), (rk(R_WK),), bias=LN_KSCALE)
            tt("dve", rw(R_T1), rw(R_BN), rw(R_AA), ALU.subtract, (rk(R_BN), rk(R_AA)), (rk(R_T1),))
            act(rw(R_EM), rw(R_T1), AF.Exp, (rk(R_T1),), (rk(R_EM),))
            ts("pool", rw(R_ALS), rw(R_A_), LN_KSCALE, ALU.add, (rk(R_A_),), (rk(R_ALS),))
            cp("pool", carry[:, 0:1], rw(R_BN)[:, TB - 1:TB], (rk(R_BN),), ("carry",))
            cp("pool", carry[:, 1:2], rw(R_AA)[:, TB - 1:TB], (rk(R_AA), "ape"), ("carry",))
            yield
            for qi, ri in enumerate([R_ALS, R_WK, R_EM]):
                for c8 in range(8):
                    mm(ps[1][0:64, 480 + qi * 8 + c8:480 + qi * 8 + c8 + 1], rw(ri)[:, c8 * 64:(c8 + 1) * 64],
                       onesf[:, 0:1], True, True, (rk(ri), "onesf"), (("ps", 1),))
            cp("dve", cols[:, :], ps[1][0:64, 480:504], (("ps", 1),), ("cols",))
            mm(ps[1][:, 504:512], onesf[:, 0:128], dec[:, :], True, True, ("onesf", "dec"), (("ps", 1),))
            cp("dve", decb[:, :], ps[1][:, 504:512], (("ps", 1),), ("decb",))
            yield
            mm(ps[1][0:64, :], onesf[:, 0:64], rw(R_AA), True, True, ("onesf", rk(R_AA)), (("ps", 1),))
            for c8 in range(8):
                act(wT[:, c8, :], ps[1][0:64, c8 * 64:(c8 + 1) * 64], AF.Exp, (("ps", 1), "cols"), ("wT",),
                    scale=-1.0, bias=cols[:, c8:c8 + 1])
            tt("pool", wT[:, :, :], wT[:, :, :], mask64.unsqueeze(1).broadcast_to([64, 8, 64]), ALU.mult,
               ("wT", "cst"), ("wT",))
            yield
            mm(ps[1][:, :], onesf[:, 0:128], rw(R_WI), True, True, ("onesf", rk(R_WI)), (("ps", 1),))
            tt("dve", qs[:, :], qm[:, :], ps[1][:, :], ALU.mult, (("qm", pp), ("ps", 1)), ("qs",))
            tt("pool", vmw[:, :, 0:129], vmc[:, :, 0:129], cols[:, 8:16].unsqueeze(2).broadcast_to([64, 8, 129]),
               ALU.mult, (("vmc", pp), "vmc_ones", "cols"), ("vmw",))
            yield

        def mlstm_block(l, tb, ycl_ap, okey, res):
            V = vec[l]
            vk = ("vec", l)
            pp = tb % 2
            qm, km, gm, vmc = qm2[pp], km2[pp], gm2[pp], vmc2[pp]
            qmk, kmk, gmk, vmck = ("qm", pp), ("km", pp), ("gm", pp), ("vmc", pp)
            if tb == 0:
                I("pool", lambda e: e.memset(cf[:, :], 0.0), (), ("cf",))
                I("pool", lambda e: e.memset(cb[:, :], 0.0), (), ("cb",))
            for c8 in range(8):
                tr(psb(1)[0:64, c8 * 128:(c8 + 1) * 128], km[:, c8 * 64:(c8 + 1) * 64], identb[:, :],
                   (kmk, "identb"), (("ps", 1),))
            cp("dve", ktok[:, :, :], psb(1)[0:64, :].rearrange("p (c d) -> p c d", d=128), (("ps", 1),), ("ktok",))
            yield

            def st(c8):
                cs = c8 * 64
                so = (c8 % 2) * 64
                mm(ps[1][0:64, so:so + 64], km[:, cs:cs + 64], qm[:, cs:cs + 64], True, True, (kmk, qmk), (("ps", 1),))

            st(0)
            for c8 in range(8):
                cs = c8 * 64
                so = (c8 % 2) * 64
                if c8 + 1 < 8:
                    st(c8 + 1)
                tt("dve", stl[c8 % 2][:, :], ps[1][0:64, so:so + 64], wT[:, c8, :], ALU.mult, (("ps", 1), "wT"),
                   (("stl", c8 % 2),))
                mm(ps[1][:, 257:386], ktok[:, c8, :], vmw[:, c8, 0:129], True, True, ("ktok", "vmw"), (("ps", 1),))
                mm(ps[1][0:64, 128:257], qs[:, cs:cs + 64], cb[:, 0:129], True, False, ("qs", "cb"), (("ps", 1),))
                mm(ps[1][0:64, 128:257], stl[c8 % 2][:, :], vmc[:, c8, 0:129], False, True,
                   (("stl", c8 % 2), vmck, "vmc_ones"), (("ps", 1),))
                stt(cf[:, 0:129], cf[:, 0:129], decb[:, c8:c8 + 1], ps[1][:, 257:386], ALU.mult, ALU.add,
                    ("cf", "decb", ("ps", 1)), ("cf",))
                cp("pool", cb[:, 0:129], cf[:, 0:129], ("cf",), ("cb",))
                cp("dve", nums[:, c8, 0:129], ps[1][0:64, 128:257], (("ps", 1),), ("nums",))
                yield
            den = nums[:, :, 128]
            tt("dve", sq8[:, :, :], nums[:, :, 0:128], nums[:, :, 0:128], ALU.mult, ("nums",), ("sq8",))
            I("dve", lambda e: e.reduce_sum(sm[:, :, 0], sq8[:, :, :], AX.X), ("sq8",), ("sm",))
            ts("dve", sm[:, :, 1], den, -1.0, ALU.mult, ("nums",), ("sm",))
            tt("dve", sm[:, :, 1], sm[:, :, 1], den, ALU.max, ("sm", "nums"), ("sm",))
            tt("dve", sm[:, :, 1], sm[:, :, 1], cols[:, 16:24], ALU.max, ("sm", "cols"), ("sm",))
            tt("dve", sm[:, :, 2], sm[:, :, 1], sm[:, :, 1], ALU.mult, ("sm",), ("sm",))
            ts("dve", sm[:, :, 0], sm[:, :, 0], 1.0 / 128.0, ALU.mult, ("sm",), ("sm",))
            stt(sm[:, :, 3], sm[:, :, 2], EPS, sm[:, :, 0], ALU.mult, ALU.add, ("sm",), ("sm",))
            act(sm[:, :, 4], sm[:, :, 3], AF.Ln, ("sm",), ("sm",))
            act(sm[:, :, 5], sm[:, :, 4], AF.Exp, ("sm",), ("sm",), scale=-0.5)
            tt("dve", hmb[:, :, :], nums[:, :, 0:128], sm[:, :, 5:6].broadcast_to([64, 8, 128]), ALU.mult,
               ("nums", "sm"), ("hmb",))
            for c8 in range(8):
                tr(psb(1)[:, c8 * 64:(c8 + 1) * 64], hmb[:, c8, :], identb[0:64, 0:64], ("hmb", "identb"),
                   (("ps", 1),))
            yield
            ts("dve", gt1[:, :], qm[:, :], V[:, 19:20], ALU.mult, (qmk, vk), ("gtA",))
            stt(gt2[:, :], psb(1)[:, 0:TB], V[:, 18:19], gt1[:, :], ALU.mult, ALU.add, (("ps", 1), vk, "gtA"),
                ("gtB",))
            tt("dve", yo[:, :], gt2[:, :], gm[:, :], ALU.mult, ("gtB", gmk), ("yo",))
            res.append(dma("sp", ycl_ap, yo[:, :], ("yo",), (okey,)))
            yield

        def attn_block(l, tb, ycl_ap, okey, res):
            V = vec[l]
            vk = ("vec", l)
            qa, ga = qa2[tb % 2], ga2[tb % 2]
            qak, gak = ("qa", tb % 2), ("ga", tb % 2)
            lam_init = 0.8 - 0.6 * math.exp(-0.3 * l)
            tiles = []
            for g2 in range(2):
                qt0 = tb * 4 + g2 * 2
                for j in range(qt0 + 2):
                    tiles.append((g2, qt0, j))

            def emit_S(ti):
                g2, qt0, j = tiles[ti]
                qoff = g2 * 256
                lo = 0 if j <= qt0 else 128
                sb0 = 2 + 2 * (ti % 2)
                kb = j // 4
                for m in range(2):
                    mm(ps[sb0 + m][:, lo:256], kaT[m * 64:(m + 1) * 64, j * 128:(j + 1) * 128],
                       qa[m * 64:(m + 1) * 64, qoff + lo:qoff + 256], True, True, (("ka", kb), qak),
                       (("ps", sb0), ("ps", sb0 + 1)))

            touched = set()
            emit_S(0)
            if len(tiles) > 1:
                emit_S(1)
            for ti, (g2, qt0, j) in enumerate(tiles):
                if j == 0:
                    touched = set()
                qoff = g2 * 256
                lo = 0 if j <= qt0 else 128
                sb0 = 2 + 2 * (ti % 2)
                pt = pT[ti % 2]
                ptk = ("pT", ti % 2)
                src_ = ps_all[:, sb0:sb0 + 2, lo:256]
                dst = pt[:, :].rearrange("p (m q) -> p m q", m=2)[:, :, lo:256]
                act(dst, src_, AF.Exp, (("ps", sb0), ("ps", sb0 + 1)), (ptk,), scale=0.125)
                if j >= qt0:
                    d0 = (j - qt0) * 128
                    msl = pt[64:128, :].rearrange("p (m q) -> p m q", m=2)[:, :, d0:d0 + 64]
                    I("pool", lambda e, msl=msl: e.memset(msl, 0.0), (ptk,), (ptk,))
                if ti + 2 < len(tiles):
                    emit_S(ti + 2)
                for qt in range(2):
                    if j > qt0 + qt:
                        continue
                    pb = 6 + qt
                    for m in range(2):
                        first = pb not in touched
                        touched.add(pb)
                        last = (j == qt0 + qt) and m == 1
                        mm(ps[pb][:, m * 129:(m + 1) * 129], pt[:, m * 256 + qt * 128:m * 256 + (qt + 1) * 128],
                           va[:, j, 0:129], first, last, (ptk, ("va", j), "va_ones"), (("ps", pb),))
                    if j == qt0 + qt:
                        P = ps[pb]
                        pk = ("ps", pb)
                        I("dve", lambda e, P=P: e.reciprocal(asm[:, 0:1], P[:, 128:129]), (pk,), ("asm0",))
                        I("dve", lambda e, P=P: e.reciprocal(asm[:, 1:2], P[:, 257:258]), (pk,), ("asm1",))
                        tt("dve", asm[:, 1:2], asm[:, 1:2], neglam[:, :], ALU.mult, ("asm1", "neglam"), ("asm1",))
                        ts("dve", at1[:, :], P[:, 0:128], asm[:, 0:1], ALU.mult, (pk, "asm0"), ("at1",))
                        stt(at2[:, :], P[:, 129:257], asm[:, 1:2], at1[:, :], ALU.mult, ALU.add,
                            (pk, "asm1", "at1"), ("at2",))
                        tt("pool", atj[:, :], at2[:, :], at2[:, :], ALU.mult, ("at2",), ("atj",))
                        I("dve", lambda e: e.reduce_sum(asm[:, 2:3], atj[:, :], AX.X), ("atj",), ("asm2",))
                        ts("dve", asm[:, 3:4], asm[:, 2:3], 1.0 / 128.0, ALU.mult, ("asm2",), ("asm3",),
                           s2=EPS, op1=ALU.add)
                        act(asm[:, 4:5], asm[:, 3:4], AF.Ln, ("asm3",), ("asm4",))
                        act(asm[:, 5:6], asm[:, 4:5], AF.Exp, ("asm4",), ("asm5",), scale=-0.5)
                        ts("dve", hab[:, :], at2[:, :], asm[:, 5:6], ALU.mult, ("at2", "asm5"), ("hab",),
                           s2=1.0 - lam_init, op1=ALU.mult)
                        tr(psb(pb)[:, 768:896], hab[:, :], identb[:, :], ("hab", "identb"), (pk,))
                        c0 = qoff + qt * 128
                        stt(yab[:, c0:c0 + 128], psb(pb)[:, 768:896], V[:, 20:21], ga[:, c0:c0 + 128], ALU.mult,
                            ALU.mult, (pk, vk, gak), ("yab",))
                yield
            res.append(dma("sp", ycl_ap, yab[:, :], ("yab",), (okey,)))

        out_toks = []

        def frontA(l, tb):
            t0 = tb * TB
            if l == 0:
                dma("sp", xblk[:, :, :], xT[:, t0:t0 + TB].rearrange("(kt p) t -> p kt t", p=128), (), ("xblk",))
            else:
                yield from outproj_block(l - 1, tb, xT, ("xsrc0", tb))
                tok = dma("sp", x1s[:, t0:t0 + TB].rearrange("(kt p) t -> p kt t", p=128), xblk[:, :, :],
                          ("xblk",), (("xsrc1", tb),))
                if not do_fin:
                    out_toks.append(tok)
            yield from norm_block(vec[l][:, 0:8], False)
            if _STOP >= 2:
                yield from inproj_block(l, tb)

        def frontB(l, tb):
            q4, tq = tb // 4, (tb % 4) * TB
            if _STOP >= 3:
                yield from gates_block(l, tb)
            res = []
            if _STOP >= 4:
                yield from mlstm_block(l, tb, ycl[l][q4][0:128, tq:tq + TB], ("ycl", l, q4, tb % 4, 0), res)
            if not fused:
                out_toks.extend(res)

        def drain(g):
            for _ in g:
                pass

        def adv(g, n):
            for _ in range(n):
                try:
                    next(g)
                except StopIteration:
                    return False
            return True

        NBU, NAU = float(os.environ.get("K_NBU", "20")), float(os.environ.get("K_NAU", "28"))

        for l in layers:
            load_weights(l)
            if l > 0:
                load_wout(l - 1)
            drain(frontA(l, 0))
            for tb in range(_NB):
                q4, tq = tb // 4, (tb % 4) * TB
                res = []
                ag = (attn_block(l, tb, ycl[l][q4][128:256, tq:tq + TB], ("ycl", l, q4, tb % 4, 1), res)
                      if _STOP >= 5 else iter(()))
                bg = frontB(l, tb)
                fg = frontA(l, tb + 1) if tb + 1 < _NB else iter(())
                ntile = 8 * tb + 6
                accb = acca = 0.0
                bl = fl = True
                for _ in ag:
                    accb += NBU / ntile
                    acca += NAU / ntile
                    nb_, na_ = int(accb), int(acca)
                    accb -= nb_
                    acca -= na_
                    while nb_ > 0 or na_ > 0:
                        if nb_ > 0:
                            bl = bl and adv(bg, 1)
                            nb_ -= 1
                        if na_ > 0:
                            fl = fl and adv(fg, 1)
                            na_ -= 1
                while bl or fl:
                    if bl:
                        bl = adv(bg, 1)
                    if fl:
                        fl = adv(fg, 1)
                if not fused:
                    out_toks.extend(res)
                if fused and tb % 4 == 3:
                    rk = tuple(("ycl", l, q4, i, w) for i in range(4) for w in range(2))
                    I("pool", lambda e, l=l, q4=q4: e.collective_compute(
                        "AllGather", ALU.bypass, replica_groups=GROUPS,
                        ins=[ycl[l][q4][:, :]], outs=[ycf[l][q4][:, :]]), rk, (("ycf", l, q4),), cc=True)

        if do_fin:
            load_wout(DEPTH - 1)
            lf = DEPTH - 1
            nw_ap = vec[lf][:, 23:31]
            xb = [xblk[:, :, :], kaT[:, :].bitcast(F32).rearrange("p (kt t) -> p kt t", kt=KT)]
            yb = [sqb[:, :, :], va[:, :, :].rearrange("p a b -> p (a b)")[:, 0:KT * TB].rearrange("p (kt t) -> p kt t", kt=KT)]
            xk = [("xblk",), tuple(("ka", i) for i in range(NBLK))]
            yk = [("sqb",), tuple(("va", i) for i in range(SEQ // 128)) + ("va_ones",)]

            def fin_load(tb):
                p = tb % 2
                t0 = tb * TB
                q4, tq = tb // 4, (tb % 4) * TB
                dma("sp", yb[p], ycf[lf][q4][:, tq:tq + TB].rearrange("(kt p) t -> p kt t", p=128),
                    (("ycf", lf, q4),), yk[p])
                dma("sp", xb[p], x1s[:, t0:t0 + TB].rearrange("(kt p) t -> p kt t", p=128),
                    (("xsrc1", tb),), xk[p])

            fin_load(0)
            for tb in range(_NB):
                p = tb % 2
                t0 = tb * TB
                if tb + 1 < _NB:
                    fin_load(tb + 1)
                X, Y = xb[p], yb[p]
                for m in range(KT):
                    bank = m % 2
                    for et in range(KT):
                        mm(ps[bank][:, :], wout_b[:, et, m * 128:(m + 1) * 128], Y[:, et, :], et == 0, et == KT - 1,
                           ("wout",) + yk[p], (("ps", bank),))
                    tt("dve", X[:, m, :], X[:, m, :], ps[bank][:, :], ALU.add, (("ps", bank),) + xk[p], xk[p])
                    if m % 2 == 0:
                        tt("pool", xnb[:, m, :], X[:, m, :], X[:, m, :], ALU.mult, xk[p], ("xnb",))
                    else:
                        act(xnb[:, m, :], X[:, m, :], AF.Square, xk[p], ("xnb",))
                for kt in range(KT):
                    mm(ps[2][:, :], onesb[:, :], xnb[:, kt, :], kt == 0, kt == KT - 1, ("onesb", "xnb"), (("ps", 2),))
                ts("dve", rstd[:, :], ps[2][:, :], 1.0 / D_MODEL, ALU.mult, (("ps", 2),), ("rstd",), s2=EPS, op1=ALU.add)
                act(rstd[:, :], rstd[:, :], AF.Ln, ("rstd",), ("rstd",))
                act(rstd[:, :], rstd[:, :], AF.Exp, ("rstd",), ("rstd",), scale=-0.5)
                for kt in range(KT):
                    stt(X[:, kt, :], X[:, kt, :], nw_ap[:, kt:kt + 1], rstd[:, :], ALU.mult, ALU.mult,
                        xk[p] + ("rstd", ("vec", lf)), xk[p])
                out_toks.append(dma("sp", outT[:, t0:t0 + TB].rearrange("(kt p) t -> p kt t", p=128), X, xk[p], ()))

        waits = []
        for tok in out_toks:
            S._tok_wait("sp", tok, waits)
        S.recs["sp"].append((waits, None, None))

        with nc.Block() as block:
            S.emit(block)
    return nc


_OFF = {"mq": 0, "mk": 512, "mv": 1024, "mi": 1536, "mf": 1540, "mz": 1544,
        "aq": 2056, "ak": 2568, "av": 3080, "az": 3592}


def _rope_tables():
    inv = (1.0 / (np.float32(10000.0) ** (np.arange(0, 64, 2, dtype=np.float32) / np.float32(64.0)))).astype(np.float32)
    ang = np.arange(SEQ, dtype=np.float32)[:, None] * inv[None, :]
    cos = np.cos(ang).astype(np.float32)
    sin = np.sin(ang).astype(np.float32)
    cosT = np.ascontiguousarray(np.concatenate([cos, cos, cos, cos], 1).T)
    sinT = np.ascontiguousarray(np.concatenate([sin, sin, sin, sin], 1).T)
    return cosT, sinT


def _consts():
    c = np.zeros((128, 192), np.float32)
    c[:, 0:128] = np.eye(128, dtype=np.float32)
    c[0:64, 128:192] = np.triu(np.ones((64, 64), np.float32))
    return c


def _core_inputs(inp, c, stage_layers, need_wout):
    b, hd = c // 4, c % 4
    f32 = np.float32
    d = {}
    for l in stage_layers:
        w = np.asarray(inp["w_in"][l], f32)
        hs = slice(hd * 128, (hd + 1) * 128)

        def blk(name):
            return w[:, _OFF[name] + hd * 128:_OFF[name] + (hd + 1) * 128]

        def perm(m):
            return np.concatenate([m[:, 32:64], m[:, 0:32], m[:, 96:128], m[:, 64:96]], 1)

        aq, ak = blk("aq"), blk("ak")
        gi = w[:, _OFF["mi"] + hd:_OFF["mi"] + hd + 1]
        gf = w[:, _OFF["mf"] + hd:_OFF["mf"] + hd + 1]
        d[f"wfm{l}"] = np.ascontiguousarray(np.concatenate(
            [blk("mq"), blk("mk"), blk("mz"), aq, perm(aq), ak, perm(ak), blk("az"), gi, gf], 1))
        d[f"wtm{l}"] = np.ascontiguousarray(np.concatenate([blk("mv"), blk("av")], 1))
        lam = np.concatenate([np.asarray(inp[k][l], f32) for k in ("lam_q1", "lam_k1", "lam_q2", "lam_k2")])
        d[f"lamv{l}"] = np.ascontiguousarray(np.tile(lam[None, :], (128, 1)))
    for l in range(DEPTH):
        v = np.zeros((128, NV), f32)
        v[:, 0:8] = np.asarray(inp["norm_w"][l], f32).reshape(8, 128).T
        cw = np.asarray(inp["conv_w"][l], f32)
        cbv = np.asarray(inp["conv_b"][l], f32)
        v[:, 8:12] = cw[:, hd * 128:(hd + 1) * 128].T
        v[:, 12:16] = cw[:, 512 + hd * 128:512 + (hd + 1) * 128].T
        v[:, 16] = cbv[hd * 128:(hd + 1) * 128]
        v[:, 17] = cbv[512 + hd * 128:512 + (hd + 1) * 128]
        v[:, 18] = np.asarray(inp["m_norm_w"][l], f32)[hd * 128:(hd + 1) * 128]
        v[:, 19] = np.asarray(inp["m_skip"][l], f32)[hd * 128:(hd + 1) * 128]
        v[:, 20] = np.asarray(inp["a_norm_w"][l], f32)
        v[:, 21] = np.asarray(inp["i_bias"][l], f32)[hd]
        v[:, 22] = np.asarray(inp["f_bias"][l], f32)[hd]
        v[:, 23:31] = np.asarray(inp["final_norm_w"], f32).reshape(8, 128).T
        d[f"vecs{l}"] = v
    for l in need_wout:
        wo = np.asarray(inp["w_out"][l], f32)
        rows = []
        for r in range(4):
            rows.append(wo[r * 128:(r + 1) * 128])
            rows.append(wo[512 + r * 128:512 + (r + 1) * 128])
        d[f"wout{l}"] = np.ascontiguousarray(np.concatenate(rows, 0))
    return d


_PROG = {}


def _prog(stage, debug=False):
    key = (stage, debug)
    if key not in _PROG:
        _PROG[key] = build_program(stage, debug)
    return _PROG[key]


def kernel(**inp):
    x = np.asarray(inp["x"], np.float32)
    xTs = [np.ascontiguousarray(x[b].T) for b in range(BATCH)]
    cosT, sinT = _rope_tables()
    cst = _consts()
    nc = _prog("all")
    in_maps = []
    for c in range(NCORES):
        d = _core_inputs(inp, c, [0, 1], [0, 1])
        d.update({"xT": xTs[c // 4], "cosT": cosT, "sinT": sinT, "consts": cst})
        in_maps.append(d)
    res = run_bass_kernel_spmd(nc, in_maps, core_ids=list(range(NCORES)))
    out = np.empty((BATCH, SEQ, D_MODEL), np.float32)
    for b in range(BATCH):
        out[b] = res.results[4 * b]["outT"].T
    return out
```

```python
import math
from contextlib import ExitStack

import numpy as np
import ml_dtypes

import concourse.bass as bass
import concourse.mybir as mybir
from concourse.bass_utils import run_bass_kernel_spmd

F32 = mybir.dt.float32
BF16 = mybir.dt.bfloat16
AF = mybir.ActivationFunctionType
ALU = mybir.AluOpType
AX = mybir.AxisListType

D_MODEL = 1024
BATCH = 2
SEQ = 8192
DEPTH = 2
NCORES = 8
TB = 512
NBLK = SEQ // TB
KT = D_MODEL // 128
EPS = 1e-6
NV = 32
LN_KSCALE = math.log(128.0 ** -0.5)
GROUPS = [[0, 1, 2, 3], [4, 5, 6, 7]]
import os
_STOP = int(os.environ.get("K_STOP", "9"))
_NB = int(os.environ.get("K_NBLK", str(NBLK)))


class Sched:
    CH = 30000
    ND = 48

    def __init__(self, nc, es):
        self.nc = nc
        self.engs = ["pe", "act", "dve", "pool", "sp"]
        self.recs = {e: [] for e in self.engs}
        self.cnt = {e: 0 for e in self.engs}
        self.seen = {e: {} for e in self.engs}
        self.lastw = {}
        self.readers = {}
        nsem = {"pe": 3, "act": 3, "dve": 4, "pool": 3, "sp": 1}
        self.esems = {e: [es.enter_context(nc.semaphore(f"s_{e}_{i}")) for i in range(nsem[e])]
                      for e in self.engs}
        self.dsems = [es.enter_context(nc.semaphore(f"d_{i}")) for i in range(self.ND)]
        self.dval = [0] * self.ND
        self.dnext = 0
        self.ccsem = es.enter_context(nc.semaphore("ccsem"))
        self.ccval = 0

    def _tok_wait(self, e, tok, waits):
        if tok[0] == "e":
            _, e2, n = tok
            if e2 == e and e == "pe":
                return
            if self.seen[e].get(e2, 0) >= n:
                return
            self.seen[e][e2] = n
            waits.append((self.esems[e2][(n - 1) // self.CH], (n - 1) % self.CH + 1))
        else:
            kind, i, v = tok
            key = (kind, i)
            if self.seen[e].get(key, 0) >= v:
                return
            self.seen[e][key] = v
            sem = self.dsems[i] if kind == "d" else self.ccsem
            waits.append((sem, v))

    def issue(self, e, fn, reads=(), writes=(), dma=False, cc=False):
        deps = []
        for k in reads:
            if k in self.lastw:
                deps.append(self.lastw[k])
        for k in writes:
            if k in self.lastw:
                deps.append(self.lastw[k])
            deps.extend(self.readers.get(k, {}).values())
        waits = []
        for tok in deps:
            self._tok_wait(e, tok, waits)
        if dma:
            i = self.dnext
            self.dnext = (self.dnext + 1) % self.ND
            prev = self.dval[i]
            if prev > 0:
                self._tok_wait(e, ("d", i, prev), waits)
            self.dval[i] += 16
            tok = ("d", i, self.dval[i])
            inc = (self.dsems[i], 16)
        elif cc:
            self.ccval += 1
            tok = ("c", 0, self.ccval)
            inc = (self.ccsem, 1)
        elif fn is None:
            tok = None
            inc = None
        else:
            self.cnt[e] += 1
            n = self.cnt[e]
            tok = ("e", e, n)
            inc = (self.esems[e][(n - 1) // self.CH], 1)
        self.recs[e].append((waits, fn, inc))
        if tok is not None:
            for k in reads:
                self.readers.setdefault(k, {})[(tok[0], tok[1])] = tok
            for k in writes:
                self.lastw[k] = tok
                self.readers[k] = {}
        return tok

    def emit(self, block):
        nc = self.nc

        def run(e, eng):
            for waits, fn, inc in self.recs[e]:
                for s, v in waits:
                    eng.wait_ge(s, v)
                if fn is not None:
                    ins = fn(eng)
                    ins.then_inc(inc[0], inc[1])

        @block.tensor
        def _(eng):
            run("pe", eng)

        @block.scalar
        def _(eng):
            run("act", eng)

        @block.vector
        def _(eng):
            run("dve", eng)

        @block.gpsimd
        def _(eng):
            run("pool", eng)

        @block.sync
        def _(eng):
            run("sp", eng)


def build_program(stage="all", debug=False):
    nc = bass.Bass("TRN2", target_bir_lowering=False)
    with ExitStack() as es:
        S = Sched(nc, es)

        def dram(name, shape, dt, kind):
            return nc.dram_tensor(name, shape, dt, kind=kind).ap()

        fused = stage == "all"
        layers = {"all": [0, 1], "l0": [0], "mid": [1], "fin": []}[stage]
        do_fin = stage in ("all", "fin")

        xT = dram("xT", [D_MODEL, SEQ], F32, "ExternalInput") if stage in ("all", "l0", "mid") else None
        cosT = dram("cosT", [128, SEQ], F32, "ExternalInput") if layers else None
        sinT = dram("sinT", [128, SEQ], F32, "ExternalInput") if layers else None
        consts = dram("consts", [128, 192], F32, "ExternalInput")
        wfm, wtm, wout, vecs, lamv = {}, {}, {}, {}, {}
        for l in layers:
            wfm[l] = dram(f"wfm{l}", [D_MODEL, 1026], F32, "ExternalInput")
            wtm[l] = dram(f"wtm{l}", [D_MODEL, 256], F32, "ExternalInput")
            lamv[l] = dram(f"lamv{l}", [128, 256], F32, "ExternalInput")
        for l in range(DEPTH):
            vecs[l] = dram(f"vecs{l}", [128, NV], F32, "ExternalInput")
        need_wout = {"all": [0, 1], "l0": [], "mid": [0], "fin": [1]}[stage]
        for l in need_wout:
            wout[l] = dram(f"wout{l}", [D_MODEL, D_MODEL], F32, "ExternalInput")

        ycl, ycf = {}, {}
        for l in layers:
            kind = "Internal" if fused else "ExternalOutput"
            ycl[l] = [dram(f"ycl{l}_{q}", [256, 2048], BF16, kind) for q in range(4)]
        for l in need_wout:
            kind = "Internal" if fused else "ExternalInput"
            ycf[l] = [dram(f"ycf{l}_{q}", [1024, 2048], BF16, kind) for q in range(4)]
        x1s = None
        if stage == "all":
            x1s = dram("x1s", [D_MODEL, SEQ], F32, "Internal")
        elif stage == "mid":
            x1s = dram("x1s", [D_MODEL, SEQ], F32, "ExternalOutput")
        elif stage == "fin":
            x1s = dram("x1s", [D_MODEL, SEQ], F32, "ExternalInput")
        outT = dram("outT", [D_MODEL, SEQ], F32, "ExternalOutput") if do_fin else None
        dbg = {}
        if debug:
            for nm, shp, dt in [("d_qm", [128, SEQ], BF16), ("d_km", [128, SEQ], BF16),
                                ("d_qa", [128, SEQ], BF16), ("d_ka", [128, SEQ], BF16),
                                ("d_rows", [1, 9 * SEQ], F32)]:
                dbg[nm] = dram(nm, shp, dt, "ExternalOutput")

        def sb(name, shape, dt):
            return es.enter_context(nc.sbuf_tensor(name, shape, dt))

        cst = sb("cst", [128, 192], F32)
        ident_f = cst[:, 0:128]
        identb = sb("identb", [128, 128], BF16)
        onesb = sb("onesb", [128, 128], BF16)
        onesf = sb("onesf", [1, 512], F32)
        vec = [sb(f"vec{l}", [128, NV], F32) for l in range(DEPTH)]
        lamt = sb("lamt", [128, 256], F32)
        lamw = sb("lamw", [128, 4], F32)
        neglam = sb("neglam", [128, 1], F32)
        negfb = sb("negfb", [1, 1], F32)

        wfm_b = sb("wfm_b", [128, KT, 1026], BF16)
        wtm_b = sb("wtm_b", [128, KT, 256], BF16)
        wout_b = sb("wout_b", [128, KT, D_MODEL], BF16)
        wstg = [sb(f"wstg{i}", [128, KT, 128], F32) for i in range(2)]

        xblk = sb("xblk", [128, KT, TB], F32)
        sqb = sb("sqb", [128, KT, TB], BF16)
        xnb = sb("xnb", [128, KT, TB], BF16)
        ycb = sqb
        rstd = sb("rstd", [128, TB], F32)
        cosb = sb("cosb", [128, TB], F32)
        sinb = sb("sinb", [128, TB], F32)
        preq = sb("preq", [128, TB + 4], F32)
        prek = sb("prek", [128, TB + 4], F32)
        cva = sb("tA", [128, TB], F32)
        cvb = sb("tB", [128, TB], F32)
        tC = sb("tC", [128, TB], F32)
        rpa, rpb = cva, cvb
        qa2 = [sb(f"qa{i}", [128, TB], BF16) for i in range(2)]
        qm2 = [sb(f"qm{i}", [128, TB], BF16) for i in range(2)]
        km2 = [sb(f"km{i}", [128, TB], BF16) for i in range(2)]
        qs = sb("qs", [128, TB], BF16)
        gm2 = [sb(f"gm{i}", [128, TB], F32) for i in range(2)]
        gtA = sb("gtA", [128, TB], F32)
        gtB = sb("gtB", [128, TB], F32)
        ga2 = [sb(f"ga{i}", [128, TB], F32) for i in range(2)]
        kaT = sb("kaT", [128, SEQ], BF16)
        va = sb("va", [128, SEQ // 128, 132], BF16)
        vmc2 = [sb(f"vmc{i}", [64, 8, 132], BF16) for i in range(2)]
        vmw = sb("vmw", [64, 8, 132], BF16)
        ktok = sb("ktok", [64, 8, 128], BF16)
        NR = 9
        rows = sb("rows", [1, NR, TB], F32)
        gif2 = [sb(f"gif{i}", [1, 2, TB], F32) for i in range(2)]
        carry = sb("carry", [1, 4], F32)
        ape = sb("ape", [1, 8], F32)
        dec = sb("dec", [1, 8], F32)
        cols = sb("cols", [64, 24], F32)
        decb = sb("decb", [128, 8], F32)
        wT = sb("wT", [64, 8, 64], F32)
        stl = [sb(f"stl{i}", [64, 64], BF16) for i in range(2)]
        cf = sb("cf", [128, 132], F32)
        cb = sb("cb", [128, 132], BF16)
        nums = sb("nums", [64, 8, 132], F32)
        sq8 = sb("sq8", [64, 8, 128], F32)
        sm = sb("sm", [64, 8, 8], F32)
        hmb = sb("hmb", [64, 8, 128], BF16)
        gt1, gt2 = gtA, gtB
        yo = sb("yo", [128, TB], BF16)
        pT = [sb(f"pT{i}", [128, 512], BF16) for i in range(2)]
        at1 = sb("at1", [128, 128], F32)
        at2 = sb("at2", [128, 128], F32)
        atj = sb("atj", [128, 128], F32)
        asm = sb("asm", [128, 8], F32)
        hab = sb("hab", [128, 128], BF16)
        yab = sb("yab", [128, TB], BF16)
        ob = xblk

        ps_all = es.enter_context(nc.psum_tensor("ps_all", [128, 8, 512], F32))
        ps = [ps_all[:, i, :] for i in range(8)]

        def I(e, fn, r=(), w=(), **kw):
            return S.issue(e, fn, r, w, **kw)

        def dma(q, out, in_, r, w):
            return I(q, lambda e: e.dma_start(out=out, in_=in_), r, w, dma=True)

        def act(out, in_, func, r, w, scale=1.0, bias=0.0, accum=None):
            if accum is None:
                return I("act", lambda e: e.activation(out, in_, func, bias=bias, scale=scale), r, w)
            return I("act", lambda e: e.activation(out, in_, func, bias=bias, scale=scale, accum_out=accum), r, w)

        def tt(eng, out, a, b, op, r, w):
            return I(eng, lambda e: e.tensor_tensor(out, a, b, op), r, w)

        def ts(eng, out, a, s1, op0, r, w, s2=None, op1=ALU.bypass):
            return I(eng, lambda e: e.tensor_scalar(out, a, s1, s2, op0, op1), r, w)

        def stt(out, a, sc, b, op0, op1, r, w):
            return I("dve", lambda e: e.scalar_tensor_tensor(out, a, sc, b, op0, op1), r, w)

        def cp(eng, out, in_, r, w):
            if eng == "act":
                return I(eng, lambda e: e.copy(out, in_), r, w)
            return I(eng, lambda e: e.tensor_copy(out, in_), r, w)

        def mm(out, lhsT, rhs, start, stop, r, w):
            return I("pe", lambda e: e.matmul(out, lhsT, rhs, start=start, stop=stop), r, w)

        def tr(out, in_, idn, r, w):
            return I("pe", lambda e: e.transpose(out, in_, idn), r, w)

        def psb(i):
            return ps[i].bitcast(BF16)

        XB = tuple(("xblk", k) for k in range(KT))
        SQ = tuple(("sqb", k) for k in range(KT))
        XN = tuple(("xnb", k) for k in range(KT))

        dma("sp", cst[:, :], consts[:, :], (), ("cst",))
        for l in range(DEPTH):
            dma("sp", vec[l][:, :], vecs[l][:, :], (), (("vec", l),))
        cp("dve", identb[:, :], cst[:, 0:128], ("cst",), ("identb",))
        I("pool", lambda e: e.memset(onesb[:, :], 1.0), (), ("onesb",))
        I("pool", lambda e: e.memset(onesf[:, :], 1.0), (), ("onesf",))
        I("pool", lambda e: e.memset(va[:, :, 128:132], 1.0), (), ("va_ones",))
        for i in range(2):
            I("pool", lambda e, i=i: e.memset(vmc2[i][:, :, 128:132], 1.0), (), ("vmc_ones",))
        mask64 = cst[0:64, 128:192]

        def load_weights(l):
            nchunk = 11
            for ci in range(nchunk):
                st = wstg[ci % 2]
                sk = ("wstg", ci % 2)
                if ci < 8:
                    src = wfm[l][:, ci * 128:(ci + 1) * 128]
                    dst = wfm_b[:, :, ci * 128:(ci + 1) * 128]
                    ncol = 128
                elif ci == 8:
                    src = wfm[l][:, 1024:1026]
                    dst = wfm_b[:, :, 1024:1026]
                    ncol = 2
                else:
                    src = wtm[l][:, (ci - 9) * 128:(ci - 8) * 128]
                    dst = wtm_b[:, :, (ci - 9) * 128:(ci - 8) * 128]
                    ncol = 128
                dma("sp", st[:, :, 0:ncol], src.rearrange("(kt p) c -> p kt c", p=128), (), (sk,))
                eng = "pool" if ci % 2 == 0 else "dve"
                cp(eng, dst, st[:, :, 0:ncol], (sk,), ("win",))
                if ci in (4, 6):
                    for m in range(2):
                        sl = wfm_b[:, :, ci * 128 + m * 64: ci * 128 + m * 64 + 32]
                        ts("pool", sl, sl, -1.0, ALU.mult, ("win",), ("win",))
        def load_lam(l):
            dma("sp", lamt[:, :], lamv[l][:, :], (), ("lamt",))
            for i in range(2):
                tt("dve", lamt[:, i * 128:i * 128 + 64], lamt[:, i * 128:i * 128 + 64],
                   lamt[:, i * 128 + 64:i * 128 + 128], ALU.mult, ("lamt",), ("lamt",))
                I("dve", lambda e, i=i: e.reduce_sum(lamw[:, i:i + 1], lamt[:, i * 128:i * 128 + 64], AX.X),
                  ("lamt",), ("lamw",))
            act(lamw[:, 2:4], lamw[:, 0:2], AF.Exp, ("lamw",), ("lamw",))
            lam_init = 0.8 - 0.6 * math.exp(-0.3 * l)
            stt(neglam[:, :], lamw[:, 3:4], -lam_init, lamw[:, 2:3], ALU.add, ALU.subtract, ("lamw",), ("neglam",))
            ts("pool", negfb[:, :], vec[l][0:1, 22:23], -1.0, ALU.mult, (("vec", l),), ("negfb",))

        def load_wout(l):
            for ci in range(8):
                st = wstg[ci % 2]
                sk = ("wstg", ci % 2)
                dma("sp", st[:, :, :], wout[l][:, ci * 128:(ci + 1) * 128].rearrange("(kt p) c -> p kt c", p=128),
                    (), (sk,))
                eng = "pool" if ci % 2 == 0 else "dve"
                cp(eng, wout_b[:, :, ci * 128:(ci + 1) * 128], st[:, :, :], (sk,), ("wout",))

        def outproj_block(l_prev, tb, xsrc, xkey, banks=(0,)):
            t0 = tb * TB
            q4, tq = tb // 4, (tb % 4) * TB
            dma("sp", ycb[:, :, :], ycf[l_prev][q4][:, tq:tq + TB].rearrange("(kt p) t -> p kt t", p=128),
                (("ycf", l_prev, q4),), SQ)
            dma("sp", xblk[:, :, :], xsrc[:, t0:t0 + TB].rearrange("(kt p) t -> p kt t", p=128),
                (xkey,), XB)
            for m in range(KT):
                bank = banks[m % len(banks)]
                for et in range(KT):
                    mm(ps[bank][:, :], wout_b[:, et, m * 128:(m + 1) * 128], ycb[:, et, :], et == 0, et == KT - 1,
                       ("wout", ("sqb", et)), (("ps", bank),))
                tt("dve", xblk[:, m, :], xblk[:, m, :], ps[bank][:, :], ALU.add, (("ps", bank), ("xblk", m)),
                   (("xblk", m),))
                yield

        def norm_block(nw_ap, to_out):
            for kt in range(KT):
                tt("pool" if kt % 2 == 0 else "dve", sqb[:, kt, :], xblk[:, kt, :], xblk[:, kt, :], ALU.mult,
                   (("xblk", kt),), (("sqb", kt),))
            for kt in range(KT):
                mm(ps[0][:, :], onesb[:, :], sqb[:, kt, :], kt == 0, kt == KT - 1, ("onesb", ("sqb", kt)), (("ps", 0),))
            yield
            ts("dve", rstd[:, :], ps[0][:, :], 1.0 / D_MODEL, ALU.mult, (("ps", 0),), ("rstd",), s2=EPS, op1=ALU.add)
            act(rstd[:, :], rstd[:, :], AF.Ln, ("rstd",), ("rstd",))
            act(rstd[:, :], rstd[:, :], AF.Exp, ("rstd",), ("rstd",), scale=-0.5)
            for kt in range(KT):
                if to_out:
                    stt(ob[:, kt, :], xblk[:, kt, :], nw_ap[:, kt:kt + 1], rstd[:, :], ALU.mult, ALU.mult,
                        (("xblk", kt), "rstd"), (("xblk", kt),))
                else:
                    stt(xnb[:, kt, :], xblk[:, kt, :], nw_ap[:, kt:kt + 1], rstd[:, :], ALU.mult, ALU.mult,
                        (("xblk", kt), "rstd"), (("xnb", kt),))
            yield

        def inproj_block(l, tb):
            t0 = tb * TB
            pp = tb % 2
            qa, ga = qa2[pp], ga2[pp]
            qak, gak = ("qa", pp), ("ga", pp)
            qm, km, gm, vmc, gif = qm2[pp], km2[pp], gm2[pp], vmc2[pp], gif2[pp]
            V = vec[l]
            vk = ("vec", l)
            dma("sp", cosb[:, :], cosT[:, t0:t0 + TB], (), ("cosb",))
            dma("sp", sinb[:, :], sinT[:, t0:t0 + TB], (), ("sinb",))

            def fm(c0, ncol):
                def f(b):
                    for kt in range(KT):
                        mm(ps[b][0:ncol, :], wfm_b[:, kt, c0:c0 + ncol], xnb[:, kt, :], kt == 0, kt == KT - 1,
                           ("win", ("xnb", kt)), (("ps", b),))
                return f

            def conv_ev(which):
                pre, dst, cw0, cbc, fin, fk = [(preq, qm, 8, 16, tC, "tC"), (prek, km, 12, 17, cvb, "tB")][which]
                pk = ("pre", which)

                def e1(b):
                    if tb == 0:
                        I("pool", lambda e: e.memset(pre[:, 0:4], 0.0), (), (pk,))
                    else:
                        cp("pool", pre[:, 1:4], pre[:, TB + 1:TB + 4], (pk,), (pk,))
                    cp("dve", pre[:, 4:4 + TB], ps[b][:, :], (("ps", b), pk), (pk,))
                    ts("dve", cva[:, :], pre[:, 4:4 + TB], V[:, cw0 + 3:cw0 + 4], ALU.mult, (pk, vk), ("tA",),
                       s2=V[:, cbc:cbc + 1], op1=ALU.add)
                    stt(cvb[:, :], pre[:, 3:3 + TB], V[:, cw0 + 2:cw0 + 3], cva[:, :], ALU.mult, ALU.add,
                        (pk, vk, "tA"), ("tB",))
                    stt(cva[:, :], pre[:, 2:2 + TB], V[:, cw0 + 1:cw0 + 2], cvb[:, :], ALU.mult, ALU.add,
                        (pk, vk, "tB"), ("tA",))
                    stt(fin[:, :], pre[:, 1:1 + TB], V[:, cw0:cw0 + 1], cva[:, :], ALU.mult, ALU.add,
                        (pk, vk, "tA"), (fk,))

                def e2(b):
                    act(dst[:, :], fin[:, :], AF.Silu, (fk,), (("qm", pp) if which == 0 else ("km", pp),))
                return [e1, e2]

            def silu_ev(dst, dk):
                return [lambda b: act(dst[:, :], ps[b][:, :], AF.Silu, (("ps", b),), (dk,))]

            def rope_a(b):
                tt("dve", rpa[:, :], ps[b][:, :], cosb[:, :], ALU.mult, (("ps", b), "cosb"), ("tA",))

            def rope_b(which):
                def f(b):
                    tt("dve", rpb[:, :], ps[b][:, :], sinb[:, :], ALU.mult, (("ps", b), "sinb"), ("tB",))
                    if which == 0:
                        tt("pool", qa[:, :], rpa[:, :], rpb[:, :], ALU.add, ("tA", "tB"), (qak,))
                    else:
                        tt("pool", kaT[:, t0:t0 + TB], rpa[:, :], rpb[:, :], ALU.add, ("tA", "tB"), (("ka", tb),))
                return f

            def row_ev(g):
                return [lambda b: cp("dve", gif[:, g, :], ps[b][0:1, :], (("ps", b),), (("gif", pp),))]

            def vm_mm(h):
                def f(b):
                    for c4 in range(4):
                        c8 = h * 4 + c4
                        for kt in range(KT):
                            mm(ps[b][0:64, c4 * 128:(c4 + 1) * 128], xnb[:, kt, c8 * 64:(c8 + 1) * 64],
                               wtm_b[:, kt, 0:128], kt == 0, kt == KT - 1, ("win", ("xnb", kt)), (("ps", b),))
                return f

            def vm_ev(h):
                return [lambda b: cp("dve", vmc[:, h * 4:(h + 1) * 4, 0:128],
                                     ps[b][0:64, :].rearrange("p (c d) -> p c d", d=128), (("ps", b),), (("vmc", pp),))]

            def va_mm(b):
                for t4 in range(4):
                    for kt in range(KT):
                        mm(ps[b][:, t4 * 128:(t4 + 1) * 128], xnb[:, kt, t4 * 128:(t4 + 1) * 128],
                           wtm_b[:, kt, 128:256], kt == 0, kt == KT - 1, ("win", ("xnb", kt)), (("ps", b),))

            def va_ev(b):
                cp("dve", va[:, tb * 4:(tb + 1) * 4, 0:128], ps[b][:, :].rearrange("p (c d) -> p c d", d=128),
                   (("ps", b), "va_ones"), tuple(("va", tb * 4 + i) for i in range(4)))

            stages = [
                (fm(0, 128), conv_ev(0)),
                (fm(128, 128), conv_ev(1)),
                (fm(256, 128), silu_ev(gm, ("gm", pp))),
                (fm(3 * 128, 128), [rope_a]),
                (fm(4 * 128, 128), [rope_b(0)]),
                (fm(5 * 128, 128), [rope_a]),
                (fm(6 * 128, 128), [rope_b(1)]),
                (fm(7 * 128, 128), silu_ev(ga, gak)),
                (fm(1024, 1), row_ev(0)),
                (fm(1025, 1), row_ev(1)),
                (vm_mm(0), vm_ev(0)),
                (vm_mm(1), vm_ev(1)),
                (va_mm, [va_ev]),
            ]
            pending = []
            for k, (mmf, evs) in enumerate(stages):
                b = 0
                mmf(b)
                for i, fn in enumerate(evs):
                    pending.append((k + i, fn, b))
                for due, fn, bb in [p for p in pending if p[0] <= k]:
                    fn(bb)
                pending = [p for p in pending if p[0] > k]
                yield
            for due, fn, bb in sorted(pending, key=lambda p: p[0]):
                fn(bb)
            yield

        R_I, R_F, R_L1, R_BN, R_A_, R_AA, R_WI, R_WK, R_EM = range(9)
        R_T2, R_T1, R_ALS = R_I, R_F, R_L1

        def rw(i):
            return rows[:, i, :]

        def gates_block(l, tb):
            V = vec[l]
            vk = ("vec", l)
            rk = lambda i: ("row", i)
            pp = tb % 2
            qm, vmc, gif = qm2[pp], vmc2[pp], gif2[pp]
            gk = ("gif", pp)
            if tb == 0:
                I("pool", lambda e: e.memset(carry[:, :], 0.0), (), ("carry",))
            act(rw(R_T1), gif[:, 1, :], AF.Exp, (gk, "negfb"), (rk(R_T1),), scale=-1.0, bias=negfb[:, :])
            act(rw(R_L1), rw(R_T1), AF.Ln, (rk(R_T1),), (rk(R_L1),), bias=1.0)
            I("dve", lambda e: e.tensor_tensor_scan(rw(R_BN), onesf[:, :], rw(R_L1), carry[:, 0:1], ALU.mult, ALU.add),
              ("onesf", rk(R_L1), "carry"), (rk(R_BN),))
            stt(rw(R_A_), gif[:, 0, :], V[0:1, 21:22], rw(R_BN), ALU.add, ALU.add, (gk, vk, rk(R_BN)), (rk(R_A_),))
            I("dve", lambda e: e.tensor_tensor_scan(rw(R_AA), onesf[:, :], rw(R_A_), carry[:, 1:2], ALU.mult, ALU.max),
              ("onesf", rk(R_A_), "carry"), (rk(R_AA),))
            yield
            A3 = rw(R_AA).rearrange("p (c i) -> p c i", i=64)
            aend = A3[:, :, 63]
            cp("pool", ape[:, 0:1], carry[:, 1:2], ("carry",), ("ape",))
            cp("pool", ape[:, 1:8], A3[:, 0:7, 63], (rk(R_AA),), ("ape",))
            tt("dve", rw(R_T1).rearrange("p (c i) -> p c i", i=64), ape[:, :].unsqueeze(2).broadcast_to([1, 8, 64]),
               A3, ALU.subtract, ("ape", rk(R_AA)), (rk(R_T1),))
            act(rw(R_WI), rw(R_T1), AF.Exp, (rk(R_T1),), (rk(R_WI),))
            tt("dve", dec[:, :], ape[:, :], aend, ALU.subtract, ("ape", rk(R_AA)), ("dec",))
            act(dec[:, :], dec[:, :], AF.Exp, ("dec",), ("dec",))
            tt("dve", rw(R_T2).rearrange("p (c i) -> p c i", i=64), rw(R_A_).rearrange("p (c i) -> p c i", i=64),
               aend.unsqueeze(2).broadcast_to([1, 8, 64]), ALU.subtract, (rk(R_A_), rk(R_AA)), (rk(R_T2),))
            act(rw(R_WK), rw(R_T2), AF.Exp, (rk(R_T2),), (rk(R_WK),), bias=LN_KSCALE)
            tt("dve", rw(R_T1), rw(R_BN), rw(R_AA), ALU.subtract, (rk(R_BN), rk(R_AA)), (rk(R_T1),))
            act(rw(R_EM), rw(R_T1), AF.Exp, (rk(R_T1),), (rk(R_EM),))
            ts("pool", rw(R_ALS), rw(R_A_), LN_KSCALE, ALU.add, (rk(R_A_),), (rk(R_ALS),))
            cp("pool", carry[:, 0:1], rw(R_BN)[:, TB - 1:TB], (rk(R_BN),), ("carry",))
            cp("pool", carry[:, 1:2], rw(R_AA)[:, TB - 1:TB], (rk(R_AA), "ape"), ("carry",))
            yield
            for qi, ri in enumerate([R_ALS, R_WK, R_EM]):
                for c8 in range(8):
                    mm(ps[1][0:64, 480 + qi * 8 + c8:480 + qi * 8 + c8 + 1], rw(ri)[:, c8 * 64:(c8 + 1) * 64],
                       onesf[:, 0:1], True, True, (rk(ri), "onesf"), (("ps", 1),))
            cp("dve", cols[:, :], ps[1][0:64, 480:504], (("ps", 1),), ("cols",))
            mm(ps[1][:, 504:512], onesf[:, 0:128], dec[:, :], True, True, ("onesf", "dec"), (("ps", 1),))
            cp("dve", decb[:, :], ps[1][:, 504:512], (("ps", 1),), ("decb",))
            yield
            mm(ps[1][0:64, :], onesf[:, 0:64], rw(R_AA), True, True, ("onesf", rk(R_AA)), (("ps", 1),))
            for c8 in range(8):
                act(wT[:, c8, :], ps[1][0:64, c8 * 64:(c8 + 1) * 64], AF.Exp, (("ps", 1), "cols"), ("wT",),
                    scale=-1.0, bias=cols[:, c8:c8 + 1])
            tt("pool", wT[:, :, :], wT[:, :, :], mask64.unsqueeze(1).broadcast_to([64, 8, 64]), ALU.mult,
               ("wT", "cst"), ("wT",))
            yield
            mm(ps[1][:, :], onesf[:, 0:128], rw(R_WI), True, True, ("onesf", rk(R_WI)), (("ps", 1),))
            tt("dve", qs[:, :], qm[:, :], ps[1][:, :], ALU.mult, (("qm", pp), ("ps", 1)), ("qs",))
            tt("pool", vmw[:, :, 0:129], vmc[:, :, 0:129], cols[:, 8:16].unsqueeze(2).broadcast_to([64, 8, 129]),
               ALU.mult, (("vmc", pp), "vmc_ones", "cols"), ("vmw",))
            yield

        def mlstm_block(l, tb, ycl_ap, okey, res):
            V = vec[l]
            vk = ("vec", l)
            pp = tb % 2
            qm, km, gm, vmc = qm2[pp], km2[pp], gm2[pp], vmc2[pp]
            qmk, kmk, gmk, vmck = ("qm", pp), ("km", pp), ("gm", pp), ("vmc", pp)
            if tb == 0:
                I("pool", lambda e: e.memset(cf[:, :], 0.0), (), ("cf",))
                I("pool", lambda e: e.memset(cb[:, :], 0.0), (), ("cb",))
            for c8 in range(8):
                tr(psb(1)[0:64, c8 * 128:(c8 + 1) * 128], km[:, c8 * 64:(c8 + 1) * 64], identb[:, :],
                   (kmk, "identb"), (("ps", 1),))
            cp("dve", ktok[:, :, :], psb(1)[0:64, :].rearrange("p (c d) -> p c d", d=128), (("ps", 1),), ("ktok",))
            yield

            def st(c8):
                cs = c8 * 64
                so = (c8 % 2) * 64
                mm(ps[1][0:64, so:so + 64], km[:, cs:cs + 64], qm[:, cs:cs + 64], True, True, (kmk, qmk), (("ps", 1),))

            st(0)
            for c8 in range(8):
                cs = c8 * 64
                so = (c8 % 2) * 64
                if c8 + 1 < 8:
                    st(c8 + 1)
                tt("dve", stl[c8 % 2][:, :], ps[1][0:64, so:so + 64], wT[:, c8, :], ALU.mult, (("ps", 1), "wT"),
                   (("stl", c8 % 2),))
                mm(ps[1][:, 257:386], ktok[:, c8, :], vmw[:, c8, 0:129], True, True, ("ktok", "vmw"), (("ps", 1),))
                mm(ps[1][0:64, 128:257], qs[:, cs:cs + 64], cb[:, 0:129], True, False, ("qs", "cb"), (("ps", 1),))
                mm(ps[1][0:64, 128:257], stl[c8 % 2][:, :], vmc[:, c8, 0:129], False, True,
                   (("stl", c8 % 2), vmck, "vmc_ones"), (("ps", 1),))
                stt(cf[:, 0:129], cf[:, 0:129], decb[:, c8:c8 + 1], ps[1][:, 257:386], ALU.mult, ALU.add,
                    ("cf", "decb", ("ps", 1)), ("cf",))
                cp("pool", cb[:, 0:129], cf[:, 0:129], ("cf",), ("cb",))
                cp("dve", nums[:, c8, 0:129], ps[1][0:64, 128:257], (("ps", 1),), ("nums",))
                yield
            den = nums[:, :, 128]
            tt("dve", sq8[:, :, :], nums[:, :, 0:128], nums[:, :, 0:128], ALU.mult, ("nums",), ("sq8",))
            I("dve", lambda e: e.reduce_sum(sm[:, :, 0], sq8[:, :, :], AX.X), ("sq8",), ("sm",))
            ts("dve", sm[:, :, 1], den, -1.0, ALU.mult, ("nums",), ("sm",))
            tt("dve", sm[:, :, 1], sm[:, :, 1], den, ALU.max, ("sm", "nums"), ("sm",))
            tt("dve", sm[:, :, 1], sm[:, :, 1], cols[:, 16:24], ALU.max, ("sm", "cols"), ("sm",))
            tt("dve", sm[:, :, 2], sm[:, :, 1], sm[:, :, 1], ALU.mult, ("sm",), ("sm",))
            ts("dve", sm[:, :, 0], sm[:, :, 0], 1.0 / 128.0, ALU.mult, ("sm",), ("sm",))
            stt(sm[:, :, 3], sm[:, :, 2], EPS, sm[:, :, 0], ALU.mult, ALU.add, ("sm",), ("sm",))
            act(sm[:, :, 4], sm[:, :, 3], AF.Ln, ("sm",), ("sm",))
            act(sm[:, :, 5], sm[:, :, 4], AF.Exp, ("sm",), ("sm",), scale=-0.5)
            tt("dve", hmb[:, :, :], nums[:, :, 0:128], sm[:, :, 5:6].broadcast_to([64, 8, 128]), ALU.mult,
               ("nums", "sm"), ("hmb",))
            for c8 in range(8):
                tr(psb(1)[:, c8 * 64:(c8 + 1) * 64], hmb[:, c8, :], identb[0:64, 0:64], ("hmb", "identb"),
                   (("ps", 1),))
            yield
            ts("dve", gt1[:, :], qm[:, :], V[:, 19:20], ALU.mult, (qmk, vk), ("gtA",))
            stt(gt2[:, :], psb(1)[:, 0:TB], V[:, 18:19], gt1[:, :], ALU.mult, ALU.add, (("ps", 1), vk, "gtA"),
                ("gtB",))
            tt("dve", yo[:, :], gt2[:, :], gm[:, :], ALU.mult, ("gtB", gmk), ("yo",))
            res.append(dma("sp", ycl_ap, yo[:, :], ("yo",), (okey,)))
            yield

        def attn_block(l, tb, ycl_ap, okey, res):
            V = vec[l]
            vk = ("vec", l)
            qa, ga = qa2[tb % 2], ga2[tb % 2]
            qak, gak = ("qa", tb % 2), ("ga", tb % 2)
            lam_init = 0.8 - 0.6 * math.exp(-0.3 * l)
            tiles = []
            for g2 in range(2):
                qt0 = tb * 4 + g2 * 2
                for j in range(qt0 + 2):
                    tiles.append((g2, qt0, j))

            def emit_S(ti):
                g2, qt0, j = tiles[ti]
                qoff = g2 * 256
                lo = 0 if j <= qt0 else 128
                sb0 = 2 + 2 * (ti % 2)
                kb = j // 4
                for m in range(2):
                    mm(ps[sb0 + m][:, lo:256], kaT[m * 64:(m + 1) * 64, j * 128:(j + 1) * 128],
                       qa[m * 64:(m + 1) * 64, qoff + lo:qoff + 256], True, True, (("ka", kb), qak),
                       (("ps", sb0), ("ps", sb0 + 1)))

            touched = set()
            emit_S(0)
            if len(tiles) > 1:
                emit_S(1)
            for ti, (g2, qt0, j) in enumerate(tiles):
                if j == 0:
                    touched = set()
                qoff = g2 * 256
                lo = 0 if j <= qt0 else 128
                sb0 = 2 + 2 * (ti % 2)
                pt = pT[ti % 2]
                ptk = ("pT", ti % 2)
                src_ = ps_all[:, sb0:sb0 + 2, lo:256]
                dst = pt[:, :].rearrange("p (m q) -> p m q", m=2)[:, :, lo:256]
                act(dst, src_, AF.Exp, (("ps", sb0), ("ps", sb0 + 1)), (ptk,), scale=0.125)
                if j >= qt0:
                    d0 = (j - qt0) * 128
                    msl = pt[64:128, :].rearrange("p (m q) -> p m q", m=2)[:, :, d0:d0 + 64]
                    I("pool", lambda e, msl=msl: e.memset(msl, 0.0), (ptk,), (ptk,))
                if ti + 2 < len(tiles):
                    emit_S(ti + 2)
                for qt in range(2):
                    if j > qt0 + qt:
                        continue
                    pb = 6 + qt
                    for m in range(2):
                        first = pb not in touched
                        touched.add(pb)
                        last = (j == qt0 + qt) and m == 1
                        mm(ps[pb][:, m * 129:(m + 1) * 129], pt[:, m * 256 + qt * 128:m * 256 + (qt + 1) * 128],
                           va[:, j, 0:129], first, last, (ptk, ("va", j), "va_ones"), (("ps", pb),))
                    if j == qt0 + qt:
                        P = ps[pb]
                        pk = ("ps", pb)
                        I("dve", lambda e, P=P: e.reciprocal(asm[:, 0:1], P[:, 128:129]), (pk,), ("asm0",))
                        I("dve", lambda e, P=P: e.reciprocal(asm[:, 1:2], P[:, 257:258]), (pk,), ("asm1",))
                        tt("dve", asm[:, 1:2], asm[:, 1:2], neglam[:, :], ALU.mult, ("asm1", "neglam"), ("asm1",))
                        ts("dve", at1[:, :], P[:, 0:128], asm[:, 0:1], ALU.mult, (pk, "asm0"), ("at1",))
                        stt(at2[:, :], P[:, 129:257], asm[:, 1:2], at1[:, :], ALU.mult, ALU.add,
                            (pk, "asm1", "at1"), ("at2",))
                        tt("pool", atj[:, :], at2[:, :], at2[:, :], ALU.mult, ("at2",), ("atj",))
                        I("dve", lambda e: e.reduce_sum(asm[:, 2:3], atj[:, :], AX.X), ("atj",), ("asm2",))
                        ts("dve", asm[:, 3:4], asm[:, 2:3], 1.0 / 128.0, ALU.mult, ("asm2",), ("asm3",),
                           s2=EPS, op1=ALU.add)
                        act(asm[:, 4:5], asm[:, 3:4], AF.Ln, ("asm3",), ("asm4",))
                        act(asm[:, 5:6], asm[:, 4:5], AF.Exp, ("asm4",), ("asm5",), scale=-0.5)
                        ts("dve", hab[:, :], at2[:, :], asm[:, 5:6], ALU.mult, ("at2", "asm5"), ("hab",),
                           s2=1.0 - lam_init, op1=ALU.mult)
                        tr(psb(pb)[:, 768:896], hab[:, :], identb[:, :], ("hab", "identb"), (pk,))
                        c0 = qoff + qt * 128
                        stt(yab[:, c0:c0 + 128], psb(pb)[:, 768:896], V[:, 20:21], ga[:, c0:c0 + 128], ALU.mult,
                            ALU.mult, (pk, vk, gak), ("yab",))
                yield
            res.append(dma("sp", ycl_ap, yab[:, :], ("yab",), (okey,)))

        out_toks = []

        def frontA(l, tb):
            t0 = tb * TB
            if l == 0:
                dma("sp", xblk[:, :, :], xT[:, t0:t0 + TB].rearrange("(kt p) t -> p kt t", p=128), (), XB)
            else:
                yield from outproj_block(l - 1, tb, xT, ("xsrc0", tb))
                tok = dma("sp", x1s[:, t0:t0 + TB].rearrange("(kt p) t -> p kt t", p=128), xblk[:, :, :],
                          XB, (("xsrc1", tb),))
                if not do_fin:
                    out_toks.append(tok)
            yield from norm_block(vec[l][:, 0:8], False)
            if _STOP >= 2:
                yield from inproj_block(l, tb)

        def frontB(l, tb):
            q4, tq = tb // 4, (tb % 4) * TB
            if _STOP >= 3:
                yield from gates_block(l, tb)
            res = []
            if _STOP >= 4:
                yield from mlstm_block(l, tb, ycl[l][q4][0:128, tq:tq + TB], ("ycl", l, q4, tb % 4, 0), res)
            if not fused:
                out_toks.extend(res)

        def drain(g):
            for _ in g:
                pass

        def adv(g, n):
            for _ in range(n):
                try:
                    next(g)
                except StopIteration:
                    return False
            return True

        NBU, NAU = float(os.environ.get("K_NBU", "20")), float(os.environ.get("K_NAU", "28"))

        load_weights(layers[0]) if layers else None
        if layers and layers[0] > 0:
            load_wout(layers[0] - 1)
        elif len(layers) > 1:
            load_wout(layers[0])
        for li, l in enumerate(layers):
            load_lam(l)
            drain(frontA(l, 0))
            for tb in range(_NB):
                q4, tq = tb // 4, (tb % 4) * TB
                res = []
                ag = (attn_block(l, tb, ycl[l][q4][128:256, tq:tq + TB], ("ycl", l, q4, tb % 4, 1), res)
                      if _STOP >= 5 else iter(()))
                bg = frontB(l, tb)
                fg = frontA(l, tb + 1) if tb + 1 < _NB else iter(())
                if tb == _NB - 1:
                    if li + 1 < len(layers):
                        load_weights(layers[li + 1])
                        if li + 1 >= 2:
                            load_wout(layers[li + 1] - 1)
                    elif do_fin and l > layers[0]:
                        load_wout(DEPTH - 1)
                ntile = 8 * tb + 6
                accb = acca = 0.0
                bl = fl = True
                for _ in ag:
                    accb += NBU / ntile
                    acca += NAU / ntile
                    nb_, na_ = int(accb), int(acca)
                    accb -= nb_
                    acca -= na_
                    while nb_ > 0 or na_ > 0:
                        if nb_ > 0:
                            bl = bl and adv(bg, 1)
                            nb_ -= 1
                        if na_ > 0:
                            fl = fl and adv(fg, 1)
                            na_ -= 1
                while bl or fl:
                    if bl:
                        bl = adv(bg, 1)
                    if fl:
                        fl = adv(fg, 1)
                if not fused:
                    out_toks.extend(res)
                if fused and tb % 4 == 3:
                    rk = tuple(("ycl", l, q4, i, w) for i in range(4) for w in range(2))
                    I("pool", lambda e, l=l, q4=q4: e.collective_compute(
                        "AllGather", ALU.bypass, replica_groups=GROUPS,
                        ins=[ycl[l][q4][:, :]], outs=[ycf[l][q4][:, :]]), rk, (("ycf", l, q4),), cc=True)

        if do_fin:
            if not (layers and layers[-1] > layers[0]):
                load_wout(DEPTH - 1)
            lf = DEPTH - 1
            nw_ap = vec[lf][:, 23:31]
            xb = [xblk[:, :, :], kaT[:, :].bitcast(F32).rearrange("p (kt t) -> p kt t", kt=KT)]
            yb = [sqb[:, :, :], va[:, :, :].rearrange("p a b -> p (a b)")[:, 0:KT * TB].rearrange("p (kt t) -> p kt t", kt=KT)]
            xk = [XB, tuple(("ka", i) for i in range(NBLK))]
            yk = [SQ, tuple(("va", i) for i in range(SEQ // 128)) + ("va_ones",)]

            def fin_load(tb):
                p = tb % 2
                t0 = tb * TB
                q4, tq = tb // 4, (tb % 4) * TB
                dma("sp", yb[p], ycf[lf][q4][:, tq:tq + TB].rearrange("(kt p) t -> p kt t", p=128),
                    (("ycf", lf, q4),), yk[p])
                dma("sp", xb[p], x1s[:, t0:t0 + TB].rearrange("(kt p) t -> p kt t", p=128),
                    (("xsrc1", tb),), xk[p])

            fin_load(0)
            for tb in range(_NB):
                p = tb % 2
                t0 = tb * TB
                if tb + 1 < _NB:
                    fin_load(tb + 1)
                X, Y = xb[p], yb[p]
                for m in range(KT):
                    bank = m % 2
                    for et in range(KT):
                        mm(ps[bank][:, :], wout_b[:, et, m * 128:(m + 1) * 128], Y[:, et, :], et == 0, et == KT - 1,
                           ("wout",) + yk[p], (("ps", bank),))
                    tt("dve", X[:, m, :], X[:, m, :], ps[bank][:, :], ALU.add, (("ps", bank),) + xk[p], xk[p])
                    if m % 2 == 0:
                        tt("pool", xnb[:, m, :], X[:, m, :], X[:, m, :], ALU.mult, xk[p], (("xnb", m),))
                    else:
                        act(xnb[:, m, :], X[:, m, :], AF.Square, xk[p], (("xnb", m),))
                for kt in range(KT):
                    mm(ps[2][:, :], onesb[:, :], xnb[:, kt, :], kt == 0, kt == KT - 1, ("onesb", ("xnb", kt)), (("ps", 2),))
                ts("dve", rstd[:, :], ps[2][:, :], 1.0 / D_MODEL, ALU.mult, (("ps", 2),), ("rstd",), s2=EPS, op1=ALU.add)
                act(rstd[:, :], rstd[:, :], AF.Ln, ("rstd",), ("rstd",))
                act(rstd[:, :], rstd[:, :], AF.Exp, ("rstd",), ("rstd",), scale=-0.5)
                for kt in range(KT):
                    stt(X[:, kt, :], X[:, kt, :], nw_ap[:, kt:kt + 1], rstd[:, :], ALU.mult, ALU.mult,
                        xk[p] + ("rstd", ("vec", lf)), xk[p])
                out_toks.append(dma("sp", outT[:, t0:t0 + TB].rearrange("(kt p) t -> p kt t", p=128), X, xk[p], ()))

        waits = []
        for tok in out_toks:
            S._tok_wait("sp", tok, waits)
        S.recs["sp"].append((waits, None, None))

        with nc.Block() as block:
            S.emit(block)
    return nc


_OFF = {"mq": 0, "mk": 512, "mv": 1024, "mi": 1536, "mf": 1540, "mz": 1544,
        "aq": 2056, "ak": 2568, "av": 3080, "az": 3592}


def _rope_tables():
    inv = (1.0 / (np.float32(10000.0) ** (np.arange(0, 64, 2, dtype=np.float32) / np.float32(64.0)))).astype(np.float32)
    ang = np.arange(SEQ, dtype=np.float32)[:, None] * inv[None, :]
    cos = np.cos(ang).astype(np.float32)
    sin = np.sin(ang).astype(np.float32)
    cosT = np.ascontiguousarray(np.concatenate([cos, cos, cos, cos], 1).T)
    sinT = np.ascontiguousarray(np.concatenate([sin, sin, sin, sin], 1).T)
    return cosT, sinT


def _consts():
    c = np.zeros((128, 192), np.float32)
    c[:, 0:128] = np.eye(128, dtype=np.float32)
    c[0:64, 128:192] = np.triu(np.ones((64, 64), np.float32))
    return c


def _core_inputs(inp, c, stage_layers, need_wout):
    b, hd = c // 4, c % 4
    f32 = np.float32
    d = {}
    for l in stage_layers:
        w = np.asarray(inp["w_in"][l], f32)
        hs = slice(hd * 128, (hd + 1) * 128)

        def blk(name):
            return w[:, _OFF[name] + hd * 128:_OFF[name] + (hd + 1) * 128]

        def perm(m):
            return np.concatenate([m[:, 32:64], m[:, 0:32], m[:, 96:128], m[:, 64:96]], 1)

        aq, ak = blk("aq"), blk("ak")
        gi = w[:, _OFF["mi"] + hd:_OFF["mi"] + hd + 1]
        gf = w[:, _OFF["mf"] + hd:_OFF["mf"] + hd + 1]
        d[f"wfm{l}"] = np.ascontiguousarray(np.concatenate(
            [blk("mq"), blk("mk"), blk("mz"), aq, perm(aq), ak, perm(ak), blk("az"), gi, gf], 1))
        d[f"wtm{l}"] = np.ascontiguousarray(np.concatenate([blk("mv"), blk("av")], 1))
        lam = np.concatenate([np.asarray(inp[k][l], f32) for k in ("lam_q1", "lam_k1", "lam_q2", "lam_k2")])
        d[f"lamv{l}"] = np.ascontiguousarray(np.tile(lam[None, :], (128, 1)))
    for l in range(DEPTH):
        v = np.zeros((128, NV), f32)
        v[:, 0:8] = np.asarray(inp["norm_w"][l], f32).reshape(8, 128).T
        cw = np.asarray(inp["conv_w"][l], f32)
        cbv = np.asarray(inp["conv_b"][l], f32)
        v[:, 8:12] = cw[:, hd * 128:(hd + 1) * 128].T
        v[:, 12:16] = cw[:, 512 + hd * 128:512 + (hd + 1) * 128].T
        v[:, 16] = cbv[hd * 128:(hd + 1) * 128]
        v[:, 17] = cbv[512 + hd * 128:512 + (hd + 1) * 128]
        v[:, 18] = np.asarray(inp["m_norm_w"][l], f32)[hd * 128:(hd + 1) * 128]
        v[:, 19] = np.asarray(inp["m_skip"][l], f32)[hd * 128:(hd + 1) * 128]
        v[:, 20] = np.asarray(inp["a_norm_w"][l], f32)
        v[:, 21] = np.asarray(inp["i_bias"][l], f32)[hd]
        v[:, 22] = np.asarray(inp["f_bias"][l], f32)[hd]
        v[:, 23:31] = np.asarray(inp["final_norm_w"], f32).reshape(8, 128).T
        d[f"vecs{l}"] = v
    for l in need_wout:
        wo = np.asarray(inp["w_out"][l], f32)
        rows = []
        for r in range(4):
            rows.append(wo[r * 128:(r + 1) * 128])
            rows.append(wo[512 + r * 128:512 + (r + 1) * 128])
        d[f"wout{l}"] = np.ascontiguousarray(np.concatenate(rows, 0))
    return d


_PROG = {}


def _prog(stage, debug=False):
    key = (stage, debug)
    if key not in _PROG:
        _PROG[key] = build_program(stage, debug)
    return _PROG[key]


def kernel(**inp):
    x = np.asarray(inp["x"], np.float32)
    xTs = [np.ascontiguousarray(x[b].T) for b in range(BATCH)]
    cosT, sinT = _rope_tables()
    cst = _consts()
    nc = _prog("all")
    in_maps = []
    for c in range(NCORES):
        d = _core_inputs(inp, c, [0, 1], [0, 1])
        d.update({"xT": xTs[c // 4], "cosT": cosT, "sinT": sinT, "consts": cst})
        in_maps.append(d)
    res = run_bass_kernel_spmd(nc, in_maps, core_ids=list(range(NCORES)))
    out = np.empty((BATCH, SEQ, D_MODEL), np.float32)
    for b in range(BATCH):
        out[b] = res.results[4 * b]["outT"].T
    return out
```

```python
import math
from contextlib import ExitStack

import numpy as np
import ml_dtypes

import concourse.bass as bass
import concourse.mybir as mybir
from concourse.bass_utils import run_bass_kernel_spmd

F32 = mybir.dt.float32
BF16 = mybir.dt.bfloat16
AF = mybir.ActivationFunctionType
ALU = mybir.AluOpType
AX = mybir.AxisListType

D_MODEL = 1024
BATCH = 2
SEQ = 8192
DEPTH = 2
NCORES = 8
TB = 512
NBLK = SEQ // TB
KT = D_MODEL // 128
EPS = 1e-6
NV = 32
LN_KSCALE = math.log(128.0 ** -0.5)
GROUPS = [[0, 1, 2, 3], [4, 5, 6, 7]]
import os
_STOP = int(os.environ.get("K_STOP", "9"))
_NB = int(os.environ.get("K_NBLK", str(NBLK)))


class Sched:
    CH = 30000
    ND = 48

    def __init__(self, nc, es):
        self.nc = nc
        self.engs = ["pe", "act", "dve", "pool", "sp"]
        self.recs = {e: [] for e in self.engs}
        self.cnt = {e: 0 for e in self.engs}
        self.seen = {e: {} for e in self.engs}
        self.lastw = {}
        self.readers = {}
        nsem = {"pe": 3, "act": 3, "dve": 4, "pool": 3, "sp": 1}
        self.esems = {e: [es.enter_context(nc.semaphore(f"s_{e}_{i}")) for i in range(nsem[e])]
                      for e in self.engs}
        self.dsems = [es.enter_context(nc.semaphore(f"d_{i}")) for i in range(self.ND)]
        self.dval = [0] * self.ND
        self.dnext = 0
        self.ccsem = es.enter_context(nc.semaphore("ccsem"))
        self.ccval = 0

    def _tok_wait(self, e, tok, waits):
        if tok[0] == "e":
            _, e2, n = tok
            if e2 == e and e == "pe":
                return
            if self.seen[e].get(e2, 0) >= n:
                return
            self.seen[e][e2] = n
            waits.append((self.esems[e2][(n - 1) // self.CH], (n - 1) % self.CH + 1))
        else:
            kind, i, v = tok
            key = (kind, i)
            if self.seen[e].get(key, 0) >= v:
                return
            self.seen[e][key] = v
            sem = self.dsems[i] if kind == "d" else self.ccsem
            waits.append((sem, v))

    def issue(self, e, fn, reads=(), writes=(), dma=False, cc=False):
        deps = []
        for k in reads:
            if k in self.lastw:
                deps.append(self.lastw[k])
        for k in writes:
            if k in self.lastw:
                deps.append(self.lastw[k])
            deps.extend(self.readers.get(k, {}).values())
        waits = []
        for tok in deps:
            self._tok_wait(e, tok, waits)
        if dma:
            i = self.dnext
            self.dnext = (self.dnext + 1) % self.ND
            prev = self.dval[i]
            if prev > 0:
                self._tok_wait(e, ("d", i, prev), waits)
            self.dval[i] += 16
            tok = ("d", i, self.dval[i])
            inc = (self.dsems[i], 16)
        elif cc:
            self.ccval += 1
            tok = ("c", 0, self.ccval)
            inc = (self.ccsem, 1)
        elif fn is None:
            tok = None
            inc = None
        else:
            self.cnt[e] += 1
            n = self.cnt[e]
            tok = ("e", e, n)
            inc = (self.esems[e][(n - 1) // self.CH], 1)
        self.recs[e].append((waits, fn, inc))
        if tok is not None:
            for k in reads:
                self.readers.setdefault(k, {})[(tok[0], tok[1])] = tok
            for k in writes:
                self.lastw[k] = tok
                self.readers[k] = {}
        return tok

    def emit(self, block):
        nc = self.nc

        def run(e, eng):
            for waits, fn, inc in self.recs[e]:
                for s, v in waits:
                    eng.wait_ge(s, v)
                if fn is not None:
                    ins = fn(eng)
                    ins.then_inc(inc[0], inc[1])

        @block.tensor
        def _(eng):
            run("pe", eng)

        @block.scalar
        def _(eng):
            run("act", eng)

        @block.vector
        def _(eng):
            run("dve", eng)

        @block.gpsimd
        def _(eng):
            run("pool", eng)

        @block.sync
        def _(eng):
            run("sp", eng)


def build_program(stage="all", debug=False):
    nc = bass.Bass("TRN2", target_bir_lowering=False)
    with ExitStack() as es:
        S = Sched(nc, es)

        def dram(name, shape, dt, kind):
            return nc.dram_tensor(name, shape, dt, kind=kind).ap()

        fused = stage == "all"
        layers = {"all": [0, 1], "l0": [0], "mid": [1], "fin": []}[stage]
        do_fin = stage in ("all", "fin")

        xT = dram("xT", [D_MODEL, SEQ], F32, "ExternalInput") if stage in ("all", "l0", "mid") else None
        cosT = dram("cosT", [128, SEQ], F32, "ExternalInput") if layers else None
        sinT = dram("sinT", [128, SEQ], F32, "ExternalInput") if layers else None
        consts = dram("consts", [128, 192], F32, "ExternalInput")
        wfm, wtm, wout, vecs, lamv = {}, {}, {}, {}, {}
        for l in layers:
            wfm[l] = dram(f"wfm{l}", [D_MODEL, 1026], F32, "ExternalInput")
            wtm[l] = dram(f"wtm{l}", [D_MODEL, 256], F32, "ExternalInput")
            lamv[l] = dram(f"lamv{l}", [128, 256], F32, "ExternalInput")
        for l in range(DEPTH):
            vecs[l] = dram(f"vecs{l}", [128, NV], F32, "ExternalInput")
        need_wout = {"all": [0, 1], "l0": [], "mid": [0], "fin": [1]}[stage]
        for l in need_wout:
            wout[l] = dram(f"wout{l}", [D_MODEL, D_MODEL], F32, "ExternalInput")

        ycl, ycf = {}, {}
        for l in layers:
            kind = "Internal" if fused else "ExternalOutput"
            ycl[l] = [dram(f"ycl{l}_{q}", [256, 2048], BF16, kind) for q in range(4)]
        for l in need_wout:
            kind = "Internal" if fused else "ExternalInput"
            ycf[l] = [dram(f"ycf{l}_{q}", [1024, 2048], BF16, kind) for q in range(4)]
        x1s = None
        if stage == "all":
            x1s = dram("x1s", [D_MODEL, SEQ], F32, "Internal")
        elif stage == "mid":
            x1s = dram("x1s", [D_MODEL, SEQ], F32, "ExternalOutput")
        elif stage == "fin":
            x1s = dram("x1s", [D_MODEL, SEQ], F32, "ExternalInput")
        outT = dram("outT", [D_MODEL, SEQ], F32, "ExternalOutput") if do_fin else None
        dbg = {}
        if debug:
            for nm, shp, dt in [("d_qm", [128, SEQ], BF16), ("d_km", [128, SEQ], BF16),
                                ("d_qa", [128, SEQ], BF16), ("d_ka", [128, SEQ], BF16),
                                ("d_rows", [1, 9 * SEQ], F32)]:
                dbg[nm] = dram(nm, shp, dt, "ExternalOutput")

        def sb(name, shape, dt):
            return es.enter_context(nc.sbuf_tensor(name, shape, dt))

        cst = sb("cst", [128, 192], F32)
        ident_f = cst[:, 0:128]
        identb = sb("identb", [128, 128], BF16)
        onesb = sb("onesb", [128, 128], BF16)
        onesf = sb("onesf", [1, 512], F32)
        vec = [sb(f"vec{l}", [128, NV], F32) for l in range(DEPTH)]
        lamt = sb("lamt", [128, 256], F32)
        lamw = sb("lamw", [128, 4], F32)
        neglam = sb("neglam", [128, 1], F32)
        negfb = sb("negfb", [1, 1], F32)

        wfm_b = sb("wfm_b", [128, KT, 1026], BF16)
        wtm_b = sb("wtm_b", [128, KT, 256], BF16)
        wout_b = sb("wout_b", [128, KT, D_MODEL], BF16)
        wstg = [sb(f"wstg{i}", [128, KT, 128], F32) for i in range(2)]

        xblk = sb("xblk", [128, KT, TB], F32)
        sqb = sb("sqb", [128, KT, TB], BF16)
        xnb = sb("xnb", [128, KT, TB], BF16)
        ycb = sqb
        rstd = sb("rstd", [128, TB], F32)
        cosb = sb("cosb", [128, TB], F32)
        sinb = sb("sinb", [128, TB], F32)
        preq = sb("preq", [128, TB + 4], F32)
        prek = sb("prek", [128, TB + 4], F32)
        cva = sb("tA", [128, TB], F32)
        cvb = sb("tB", [128, TB], F32)
        tC = sb("tC", [128, TB], F32)
        rpa, rpb = cva, cvb
        qa2 = [sb(f"qa{i}", [128, TB], BF16) for i in range(2)]
        qm2 = [sb(f"qm{i}", [128, TB], BF16) for i in range(2)]
        km2 = [sb(f"km{i}", [128, TB], BF16) for i in range(2)]
        qs = sb("qs", [128, TB], BF16)
        gm2 = [sb(f"gm{i}", [128, TB], F32) for i in range(2)]
        gtA = sb("gtA", [128, TB], F32)
        gtB = sb("gtB", [128, TB], F32)
        ga2 = [sb(f"ga{i}", [128, TB], F32) for i in range(2)]
        kaT = sb("kaT", [128, SEQ], BF16)
        va = sb("va", [128, SEQ // 128, 132], BF16)
        vmc2 = [sb(f"vmc{i}", [64, 8, 132], BF16) for i in range(2)]
        vmw = sb("vmw", [64, 8, 132], BF16)
        ktok = sb("ktok", [64, 8, 128], BF16)
        NR = 9
        rows = sb("rows", [1, NR, TB], F32)
        gif2 = [sb(f"gif{i}", [1, 2, TB], F32) for i in range(2)]
        carry = sb("carry", [1, 4], F32)
        ape = sb("ape", [1, 8], F32)
        dec = sb("dec", [1, 8], F32)
        cols = sb("cols", [64, 24], F32)
        decb = sb("decb", [128, 8], F32)
        wT = sb("wT", [64, 8, 64], F32)
        stl = [sb(f"stl{i}", [64, 64], BF16) for i in range(2)]
        cf = sb("cf", [128, 132], F32)
        cb = sb("cb", [128, 132], BF16)
        nums = sb("nums", [64, 8, 132], F32)
        sq8 = sb("sq8", [64, 8, 128], F32)
        sm = sb("sm", [64, 8, 8], F32)
        hmb = sb("hmb", [64, 8, 128], BF16)
        gt1, gt2 = gtA, gtB
        yo = sb("yo", [128, TB], BF16)
        pT = [sb(f"pT{i}", [128, 512], BF16) for i in range(2)]
        at1 = sb("at1", [128, 128], F32)
        at2 = sb("at2", [128, 128], F32)
        atj = sb("atj", [128, 128], F32)
        asm = sb("asm", [128, 8], F32)
        hab = sb("hab", [128, 128], BF16)
        yab = sb("yab", [128, TB], BF16)
        ob = xblk

        ps_all = es.enter_context(nc.psum_tensor("ps_all", [128, 8, 512], F32))
        ps = [ps_all[:, i, :] for i in range(8)]

        def I(e, fn, r=(), w=(), **kw):
            return S.issue(e, fn, r, w, **kw)

        def dma(q, out, in_, r, w):
            return I(q, lambda e: e.dma_start(out=out, in_=in_), r, w, dma=True)

        def act(out, in_, func, r, w, scale=1.0, bias=0.0, accum=None):
            if accum is None:
                return I("act", lambda e: e.activation(out, in_, func, bias=bias, scale=scale), r, w)
            return I("act", lambda e: e.activation(out, in_, func, bias=bias, scale=scale, accum_out=accum), r, w)

        def tt(eng, out, a, b, op, r, w):
            return I(eng, lambda e: e.tensor_tensor(out, a, b, op), r, w)

        def ts(eng, out, a, s1, op0, r, w, s2=None, op1=ALU.bypass):
            return I(eng, lambda e: e.tensor_scalar(out, a, s1, s2, op0, op1), r, w)

        def stt(out, a, sc, b, op0, op1, r, w):
            return I("dve", lambda e: e.scalar_tensor_tensor(out, a, sc, b, op0, op1), r, w)

        def cp(eng, out, in_, r, w):
            if eng == "act":
                return I(eng, lambda e: e.copy(out, in_), r, w)
            return I(eng, lambda e: e.tensor_copy(out, in_), r, w)

        def mm(out, lhsT, rhs, start, stop, r, w):
            return I("pe", lambda e: e.matmul(out, lhsT, rhs, start=start, stop=stop), r, w)

        def tr(out, in_, idn, r, w):
            return I("pe", lambda e: e.transpose(out, in_, idn), r, w)

        def psb(i):
            return ps[i].bitcast(BF16)

        XB = tuple(("xblk", k) for k in range(KT))
        SQ = tuple(("sqb", k) for k in range(KT))
        XN = tuple(("xnb", k) for k in range(KT))

        dma("sp", cst[:, :], consts[:, :], (), ("cst",))
        for l in range(DEPTH):
            dma("sp", vec[l][:, :], vecs[l][:, :], (), (("vec", l),))
        cp("dve", identb[:, :], cst[:, 0:128], ("cst",), ("identb",))
        I("pool", lambda e: e.memset(onesb[:, :], 1.0), (), ("onesb",))
        I("pool", lambda e: e.memset(onesf[:, :], 1.0), (), ("onesf",))
        I("pool", lambda e: e.memset(va[:, :, 128:132], 1.0), (), ("va_ones",))
        for i in range(2):
            I("pool", lambda e, i=i: e.memset(vmc2[i][:, :, 128:132], 1.0), (), ("vmc_ones",))
        mask64 = cst[0:64, 128:192]

        def load_weights(l):
            nchunk = 11
            for ci in range(nchunk):
                st = wstg[ci % 2]
                sk = ("wstg", ci % 2)
                if ci < 8:
                    src = wfm[l][:, ci * 128:(ci + 1) * 128]
                    dst = wfm_b[:, :, ci * 128:(ci + 1) * 128]
                    ncol = 128
                elif ci == 8:
                    src = wfm[l][:, 1024:1026]
                    dst = wfm_b[:, :, 1024:1026]
                    ncol = 2
                else:
                    src = wtm[l][:, (ci - 9) * 128:(ci - 8) * 128]
                    dst = wtm_b[:, :, (ci - 9) * 128:(ci - 8) * 128]
                    ncol = 128
                dma("sp", st[:, :, 0:ncol], src.rearrange("(kt p) c -> p kt c", p=128), (), (sk,))
                eng = "pool" if ci % 2 == 0 else "dve"
                cp(eng, dst, st[:, :, 0:ncol], (sk,), ("win",))
                if ci in (4, 6):
                    for m in range(2):
                        sl = wfm_b[:, :, ci * 128 + m * 64: ci * 128 + m * 64 + 32]
                        ts("pool", sl, sl, -1.0, ALU.mult, ("win",), ("win",))
        def load_lam(l):
            dma("sp", lamt[:, :], lamv[l][:, :], (), ("lamt",))
            for i in range(2):
                tt("dve", lamt[:, i * 128:i * 128 + 64], lamt[:, i * 128:i * 128 + 64],
                   lamt[:, i * 128 + 64:i * 128 + 128], ALU.mult, ("lamt",), ("lamt",))
                I("dve", lambda e, i=i: e.reduce_sum(lamw[:, i:i + 1], lamt[:, i * 128:i * 128 + 64], AX.X),
                  ("lamt",), ("lamw",))
            act(lamw[:, 2:4], lamw[:, 0:2], AF.Exp, ("lamw",), ("lamw",))
            lam_init = 0.8 - 0.6 * math.exp(-0.3 * l)
            stt(neglam[:, :], lamw[:, 3:4], -lam_init, lamw[:, 2:3], ALU.add, ALU.subtract, ("lamw",), ("neglam",))
            ts("pool", negfb[:, :], vec[l][0:1, 22:23], -1.0, ALU.mult, (("vec", l),), ("negfb",))

        def load_wout(l):
            for ci in range(8):
                st = wstg[ci % 2]
                sk = ("wstg", ci % 2)
                dma("sp", st[:, :, :], wout[l][:, ci * 128:(ci + 1) * 128].rearrange("(kt p) c -> p kt c", p=128),
                    (), (sk,))
                eng = "pool" if ci % 2 == 0 else "dve"
                cp(eng, wout_b[:, :, ci * 128:(ci + 1) * 128], st[:, :, :], (sk,), ("wout",))

        def outproj_block(l_prev, tb, xsrc, xkey, banks=(0,)):
            t0 = tb * TB
            q4, tq = tb // 4, (tb % 4) * TB
            dma("sp", ycb[:, :, :], ycf[l_prev][q4][:, tq:tq + TB].rearrange("(kt p) t -> p kt t", p=128),
                (("ycf", l_prev, q4),), SQ)
            dma("sp", xblk[:, :, :], xsrc[:, t0:t0 + TB].rearrange("(kt p) t -> p kt t", p=128),
                (xkey,), XB)
            for m in range(KT):
                bank = banks[m % len(banks)]
                for et in range(KT):
                    mm(ps[bank][:, :], wout_b[:, et, m * 128:(m + 1) * 128], ycb[:, et, :], et == 0, et == KT - 1,
                       ("wout", ("sqb", et)), (("ps", bank),))
                tt("dve", xblk[:, m, :], xblk[:, m, :], ps[bank][:, :], ALU.add, (("ps", bank), ("xblk", m)),
                   (("xblk", m),))
                yield

        def norm_block(nw_ap, to_out):
            for kt in range(KT):
                tt("pool" if kt % 2 == 0 else "dve", sqb[:, kt, :], xblk[:, kt, :], xblk[:, kt, :], ALU.mult,
                   (("xblk", kt),), (("sqb", kt),))
            for kt in range(KT):
                mm(ps[0][:, :], onesb[:, :], sqb[:, kt, :], kt == 0, kt == KT - 1, ("onesb", ("sqb", kt)), (("ps", 0),))
            yield
            ts("dve", rstd[:, :], ps[0][:, :], 1.0 / D_MODEL, ALU.mult, (("ps", 0),), ("rstd",), s2=EPS, op1=ALU.add)
            act(rstd[:, :], rstd[:, :], AF.Ln, ("rstd",), ("rstd",))
            act(rstd[:, :], rstd[:, :], AF.Exp, ("rstd",), ("rstd",), scale=-0.5)
            for kt in range(KT):
                if to_out:
                    stt(ob[:, kt, :], xblk[:, kt, :], nw_ap[:, kt:kt + 1], rstd[:, :], ALU.mult, ALU.mult,
                        (("xblk", kt), "rstd"), (("xblk", kt),))
                else:
                    stt(xnb[:, kt, :], xblk[:, kt, :], nw_ap[:, kt:kt + 1], rstd[:, :], ALU.mult, ALU.mult,
                        (("xblk", kt), "rstd"), (("xnb", kt),))
            yield

        def inproj_block(l, tb):
            t0 = tb * TB
            pp = tb % 2
            qa, ga = qa2[pp], ga2[pp]
            qak, gak = ("qa", pp), ("ga", pp)
            qm, km, gm, vmc, gif = qm2[pp], km2[pp], gm2[pp], vmc2[pp], gif2[pp]
            V = vec[l]
            vk = ("vec", l)
            dma("sp", cosb[:, :], cosT[:, t0:t0 + TB], (), ("cosb",))
            dma("sp", sinb[:, :], sinT[:, t0:t0 + TB], (), ("sinb",))

            def fm(c0, ncol):
                def f(b):
                    for kt in range(KT):
                        mm(ps[b][0:ncol, :], wfm_b[:, kt, c0:c0 + ncol], xnb[:, kt, :], kt == 0, kt == KT - 1,
                           ("win", ("xnb", kt)), (("ps", b),))
                return f

            def conv_ev(which):
                pre, dst, cw0, cbc, fin, fk = [(preq, qm, 8, 16, tC, "tC"), (prek, km, 12, 17, cvb, "tB")][which]
                pk = ("pre", which)

                def e1(b):
                    if tb == 0:
                        I("pool", lambda e: e.memset(pre[:, 0:4], 0.0), (), (pk,))
                    else:
                        cp("pool", pre[:, 1:4], pre[:, TB + 1:TB + 4], (pk,), (pk,))
                    cp("dve", pre[:, 4:4 + TB], ps[b][:, :], (("ps", b), pk), (pk,))
                    ts("dve", cva[:, :], pre[:, 4:4 + TB], V[:, cw0 + 3:cw0 + 4], ALU.mult, (pk, vk), ("tA",),
                       s2=V[:, cbc:cbc + 1], op1=ALU.add)
                    stt(cvb[:, :], pre[:, 3:3 + TB], V[:, cw0 + 2:cw0 + 3], cva[:, :], ALU.mult, ALU.add,
                        (pk, vk, "tA"), ("tB",))
                    stt(cva[:, :], pre[:, 2:2 + TB], V[:, cw0 + 1:cw0 + 2], cvb[:, :], ALU.mult, ALU.add,
                        (pk, vk, "tB"), ("tA",))
                    stt(fin[:, :], pre[:, 1:1 + TB], V[:, cw0:cw0 + 1], cva[:, :], ALU.mult, ALU.add,
                        (pk, vk, "tA"), (fk,))

                def e2(b):
                    act(dst[:, :], fin[:, :], AF.Silu, (fk,), (("qm", pp) if which == 0 else ("km", pp),))
                return [e1, e2]

            def silu_ev(dst, dk):
                return [lambda b: act(dst[:, :], ps[b][:, :], AF.Silu, (("ps", b),), (dk,))]

            def rope_a(b):
                tt("dve", rpa[:, :], ps[b][:, :], cosb[:, :], ALU.mult, (("ps", b), "cosb"), ("tA",))

            def rope_b(which):
                def f(b):
                    tt("dve", rpb[:, :], ps[b][:, :], sinb[:, :], ALU.mult, (("ps", b), "sinb"), ("tB",))
                    if which == 0:
                        tt("pool", qa[:, :], rpa[:, :], rpb[:, :], ALU.add, ("tA", "tB"), (qak,))
                    else:
                        tt("pool", kaT[:, t0:t0 + TB], rpa[:, :], rpb[:, :], ALU.add, ("tA", "tB"), (("ka", tb),))
                return f

            def row_ev(g):
                return [lambda b: cp("dve", gif[:, g, :], ps[b][0:1, :], (("ps", b),), (("gif", pp),))]

            def vm_mm(h):
                def f(b):
                    for c4 in range(4):
                        c8 = h * 4 + c4
                        for kt in range(KT):
                            mm(ps[b][0:64, c4 * 128:(c4 + 1) * 128], xnb[:, kt, c8 * 64:(c8 + 1) * 64],
                               wtm_b[:, kt, 0:128], kt == 0, kt == KT - 1, ("win", ("xnb", kt)), (("ps", b),))
                return f

            def vm_ev(h):
                return [lambda b: cp("dve", vmc[:, h * 4:(h + 1) * 4, 0:128],
                                     ps[b][0:64, :].rearrange("p (c d) -> p c d", d=128), (("ps", b),), (("vmc", pp),))]

            def va_mm(b):
                for t4 in range(4):
                    for kt in range(KT):
                        mm(ps[b][:, t4 * 128:(t4 + 1) * 128], xnb[:, kt, t4 * 128:(t4 + 1) * 128],
                           wtm_b[:, kt, 128:256], kt == 0, kt == KT - 1, ("win", ("xnb", kt)), (("ps", b),))

            def va_ev(b):
                cp("dve", va[:, tb * 4:(tb + 1) * 4, 0:128], ps[b][:, :].rearrange("p (c d) -> p c d", d=128),
                   (("ps", b), "va_ones"), tuple(("va", tb * 4 + i) for i in range(4)))

            stages = [
                (fm(0, 128), conv_ev(0)),
                (fm(128, 128), conv_ev(1)),
                (fm(256, 128), silu_ev(gm, ("gm", pp))),
                (fm(3 * 128, 128), [rope_a]),
                (fm(4 * 128, 128), [rope_b(0)]),
                (fm(5 * 128, 128), [rope_a]),
                (fm(6 * 128, 128), [rope_b(1)]),
                (fm(7 * 128, 128), silu_ev(ga, gak)),
                (fm(1024, 1), row_ev(0)),
                (fm(1025, 1), row_ev(1)),
                (vm_mm(0), vm_ev(0)),
                (vm_mm(1), vm_ev(1)),
                (va_mm, [va_ev]),
            ]
            pending = []
            for k, (mmf, evs) in enumerate(stages):
                b = 0
                mmf(b)
                for i, fn in enumerate(evs):
                    pending.append((k + i, fn, b))
                for due, fn, bb in [p for p in pending if p[0] <= k]:
                    fn(bb)
                pending = [p for p in pending if p[0] > k]
                yield
            for due, fn, bb in sorted(pending, key=lambda p: p[0]):
                fn(bb)
            yield

        R_I, R_F, R_L1, R_BN, R_A_, R_AA, R_WI, R_WK, R_EM = range(9)
        R_T2, R_T1, R_ALS = R_I, R_F, R_L1

        def rw(i):
            return rows[:, i, :]

        def gates_block(l, tb):
            V = vec[l]
            vk = ("vec", l)
            rk = lambda i: ("row", i)
            pp = tb % 2
            qm, vmc, gif = qm2[pp], vmc2[pp], gif2[pp]
            gk = ("gif", pp)
            if tb == 0:
                I("pool", lambda e: e.memset(carry[:, :], 0.0), (), ("carry",))
            act(rw(R_T1), gif[:, 1, :], AF.Exp, (gk, "negfb"), (rk(R_T1),), scale=-1.0, bias=negfb[:, :])
            act(rw(R_L1), rw(R_T1), AF.Ln, (rk(R_T1),), (rk(R_L1),), bias=1.0)
            I("dve", lambda e: e.tensor_tensor_scan(rw(R_BN), onesf[:, :], rw(R_L1), carry[:, 0:1], ALU.mult, ALU.add),
              ("onesf", rk(R_L1), "carry"), (rk(R_BN),))
            stt(rw(R_A_), gif[:, 0, :], V[0:1, 21:22], rw(R_BN), ALU.add, ALU.add, (gk, vk, rk(R_BN)), (rk(R_A_),))
            I("dve", lambda e: e.tensor_tensor_scan(rw(R_AA), onesf[:, :], rw(R_A_), carry[:, 1:2], ALU.mult, ALU.max),
              ("onesf", rk(R_A_), "carry"), (rk(R_AA),))
            yield
            A3 = rw(R_AA).rearrange("p (c i) -> p c i", i=64)
            aend = A3[:, :, 63]
            cp("pool", ape[:, 0:1], carry[:, 1:2], ("carry",), ("ape",))
            cp("pool", ape[:, 1:8], A3[:, 0:7, 63], (rk(R_AA),), ("ape",))
            tt("dve", rw(R_T1).rearrange("p (c i) -> p c i", i=64), ape[:, :].unsqueeze(2).broadcast_to([1, 8, 64]),
               A3, ALU.subtract, ("ape", rk(R_AA)), (rk(R_T1),))
            act(rw(R_WI), rw(R_T1), AF.Exp, (rk(R_T1),), (rk(R_WI),))
            tt("dve", dec[:, :], ape[:, :], aend, ALU.subtract, ("ape", rk(R_AA)), ("dec",))
            act(dec[:, :], dec[:, :], AF.Exp, ("dec",), ("dec",))
            tt("dve", rw(R_T2).rearrange("p (c i) -> p c i", i=64), rw(R_A_).rearrange("p (c i) -> p c i", i=64),
               aend.unsqueeze(2).broadcast_to([1, 8, 64]), ALU.subtract, (rk(R_A_), rk(R_AA)), (rk(R_T2),))
            act(rw(R_WK), rw(R_T2), AF.Exp, (rk(R_T2),), (rk(R_WK),), bias=LN_KSCALE)
            tt("dve", rw(R_T1), rw(R_BN), rw(R_AA), ALU.subtract, (rk(R_BN), rk(R_AA)), (rk(R_T1),))
            act(rw(R_EM), rw(R_T1), AF.Exp, (rk(R_T1),), (rk(R_EM),))
            ts("pool", rw(R_ALS), rw(R_A_), LN_KSCALE, ALU.add, (rk(R_A_),), (rk(R_ALS),))
            cp("pool", carry[:, 0:1], rw(R_BN)[:, TB - 1:TB], (rk(R_BN),), ("carry",))
            cp("pool", carry[:, 1:2], rw(R_AA)[:, TB - 1:TB], (rk(R_AA), "ape"), ("carry",))
            yield
            for qi, ri in enumerate([R_ALS, R_WK, R_EM]):
                for c8 in range(8):
                    mm(ps[1][0:64, 480 + qi * 8 + c8:480 + qi * 8 + c8 + 1], rw(ri)[:, c8 * 64:(c8 + 1) * 64],
                       onesf[:, 0:1], True, True, (rk(ri), "onesf"), (("ps", 1),))
            cp("dve", cols[:, :], ps[1][0:64, 480:504], (("ps", 1),), ("cols",))
            mm(ps[1][:, 504:512], onesf[:, 0:128], dec[:, :], True, True, ("onesf", "dec"), (("ps", 1),))
            cp("dve", decb[:, :], ps[1][:, 504:512], (("ps", 1),), ("decb",))
            yield
            mm(ps[1][0:64, :], onesf[:, 0:64], rw(R_AA), True, True, ("onesf", rk(R_AA)), (("ps", 1),))
            for c8 in range(8):
                act(wT[:, c8, :], ps[1][0:64, c8 * 64:(c8 + 1) * 64], AF.Exp, (("ps", 1), "cols"), (("wT", c8),),
                    scale=-1.0, bias=cols[:, c8:c8 + 1])
            tt("pool", wT[:, :, :], wT[:, :, :], mask64.unsqueeze(1).broadcast_to([64, 8, 64]), ALU.mult,
               tuple(("wT", c) for c in range(8)) + ("cst",), tuple(("wT", c) for c in range(8)))
            yield
            mm(ps[1][:, :], onesf[:, 0:128], rw(R_WI), True, True, ("onesf", rk(R_WI)), (("ps", 1),))
            tt("dve", qs[:, :], qm[:, :], ps[1][:, :], ALU.mult, (("qm", pp), ("ps", 1)), ("qs",))
            tt("pool", vmw[:, :, 0:129], vmc[:, :, 0:129], cols[:, 8:16].unsqueeze(2).broadcast_to([64, 8, 129]),
               ALU.mult, (("vmc", pp), "vmc_ones", "cols"), ("vmw",))
            yield

        def mlstm_block(l, tb, ycl_ap, okey, res):
            V = vec[l]
            vk = ("vec", l)
            pp = tb % 2
            qm, km, gm, vmc = qm2[pp], km2[pp], gm2[pp], vmc2[pp]
            qmk, kmk, gmk, vmck = ("qm", pp), ("km", pp), ("gm", pp), ("vmc", pp)
            if tb == 0:
                I("pool", lambda e: e.memset(cf[:, :], 0.0), (), ("cf",))
                I("pool", lambda e: e.memset(cb[:, :], 0.0), (), ("cb",))
            for c8 in range(8):
                tr(psb(1)[0:64, c8 * 128:(c8 + 1) * 128], km[:, c8 * 64:(c8 + 1) * 64], identb[:, :],
                   (kmk, "identb"), (("ps", 1),))
            cp("dve", ktok[:, :, :], psb(1)[0:64, :].rearrange("p (c d) -> p c d", d=128), (("ps", 1),), ("ktok",))
            yield

            def st(c8):
                cs = c8 * 64
                so = (c8 % 2) * 64
                mm(ps[1][0:64, so:so + 64], km[:, cs:cs + 64], qm[:, cs:cs + 64], True, True, (kmk, qmk), (("ps", 1),))

            st(0)
            for c8 in range(8):
                cs = c8 * 64
                so = (c8 % 2) * 64
                if c8 + 1 < 8:
                    st(c8 + 1)
                tt("dve", stl[c8 % 2][:, :], ps[1][0:64, so:so + 64], wT[:, c8, :], ALU.mult, (("ps", 1), ("wT", c8)),
                   (("stl", c8 % 2),))
                mm(ps[1][:, 257:386], ktok[:, c8, :], vmw[:, c8, 0:129], True, True, ("ktok", "vmw"), (("ps", 1),))
                mm(ps[1][0:64, 128:257], qs[:, cs:cs + 64], cb[:, 0:129], True, False, ("qs", "cb"), (("ps", 1),))
                mm(ps[1][0:64, 128:257], stl[c8 % 2][:, :], vmc[:, c8, 0:129], False, True,
                   (("stl", c8 % 2), vmck, "vmc_ones"), (("ps", 1),))
                stt(cf[:, 0:129], cf[:, 0:129], decb[:, c8:c8 + 1], ps[1][:, 257:386], ALU.mult, ALU.add,
                    ("cf", "decb", ("ps", 1)), ("cf",))
                cp("pool", cb[:, 0:129], cf[:, 0:129], ("cf",), ("cb",))
                cp("dve", nums[:, c8, 0:129], ps[1][0:64, 128:257], (("ps", 1),), (("nums", c8),))
                yield
            NUMS = tuple(("nums", c) for c in range(8))
            den = nums[:, :, 128]
            tt("dve", sq8[:, :, :], nums[:, :, 0:128], nums[:, :, 0:128], ALU.mult, NUMS, ("sq8",))
            I("dve", lambda e: e.reduce_sum(sm[:, :, 0], sq8[:, :, :], AX.X), ("sq8",), ("sm",))
            ts("dve", sm[:, :, 1], den, -1.0, ALU.mult, NUMS, ("sm",))
            tt("dve", sm[:, :, 1], sm[:, :, 1], den, ALU.max, ("sm",) + NUMS, ("sm",))
            tt("dve", sm[:, :, 1], sm[:, :, 1], cols[:, 16:24], ALU.max, ("sm", "cols"), ("sm",))
            tt("dve", sm[:, :, 2], sm[:, :, 1], sm[:, :, 1], ALU.mult, ("sm",), ("sm",))
            ts("dve", sm[:, :, 0], sm[:, :, 0], 1.0 / 128.0, ALU.mult, ("sm",), ("sm",))
            stt(sm[:, :, 3], sm[:, :, 2], EPS, sm[:, :, 0], ALU.mult, ALU.add, ("sm",), ("sm",))
            act(sm[:, :, 4], sm[:, :, 3], AF.Ln, ("sm",), ("sm",))
            act(sm[:, :, 5], sm[:, :, 4], AF.Exp, ("sm",), ("sm",), scale=-0.5)
            tt("dve", hmb[:, :, :], nums[:, :, 0:128], sm[:, :, 5:6].broadcast_to([64, 8, 128]), ALU.mult,
               NUMS + ("sm",), ("hmb",))
            for c8 in range(8):
                tr(psb(1)[:, c8 * 64:(c8 + 1) * 64], hmb[:, c8, :], identb[0:64, 0:64], ("hmb", "identb"),
                   (("ps", 1),))
            yield
            ts("dve", gt1[:, :], qm[:, :], V[:, 19:20], ALU.mult, (qmk, vk), ("gtA",))
            stt(gt2[:, :], psb(1)[:, 0:TB], V[:, 18:19], gt1[:, :], ALU.mult, ALU.add, (("ps", 1), vk, "gtA"),
                ("gtB",))
            tt("dve", yo[:, :], gt2[:, :], gm[:, :], ALU.mult, ("gtB", gmk), ("yo",))
            res.append(dma("sp", ycl_ap, yo[:, :], ("yo",), (okey,)))
            yield

        def attn_block(l, tb, ycl_ap, okey, res):
            V = vec[l]
            vk = ("vec", l)
            qa, ga = qa2[tb % 2], ga2[tb % 2]
            qak, gak = ("qa", tb % 2), ("ga", tb % 2)
            lam_init = 0.8 - 0.6 * math.exp(-0.3 * l)
            tiles = []
            for g2 in range(2):
                qt0 = tb * 4 + g2 * 2
                for j in range(qt0 + 2):
                    tiles.append((g2, qt0, j))

            def emit_S(ti):
                g2, qt0, j = tiles[ti]
                qoff = g2 * 256
                lo = 0 if j <= qt0 else 128
                sb0 = 2 + 2 * (ti % 2)
                kb = j // 4
                for m in range(2):
                    mm(ps[sb0 + m][:, lo:256], kaT[m * 64:(m + 1) * 64, j * 128:(j + 1) * 128],
                       qa[m * 64:(m + 1) * 64, qoff + lo:qoff + 256], True, True, (("ka", kb), qak),
                       (("ps", sb0), ("ps", sb0 + 1)))

            touched = set()
            emit_S(0)
            if len(tiles) > 1:
                emit_S(1)
            for ti, (g2, qt0, j) in enumerate(tiles):
                if j == 0:
                    touched = set()
                qoff = g2 * 256
                lo = 0 if j <= qt0 else 128
                sb0 = 2 + 2 * (ti % 2)
                pt = pT[ti % 2]
                ptk = ("pT", ti % 2)
                src_ = ps_all[:, sb0:sb0 + 2, lo:256]
                dst = pt[:, :].rearrange("p (m q) -> p m q", m=2)[:, :, lo:256]
                act(dst, src_, AF.Exp, (("ps", sb0), ("ps", sb0 + 1)), (ptk,), scale=0.125)
                if j >= qt0:
                    d0 = (j - qt0) * 128
                    msl = pt[64:128, :].rearrange("p (m q) -> p m q", m=2)[:, :, d0:d0 + 64]
                    I("pool", lambda e, msl=msl: e.memset(msl, 0.0), (ptk,), (ptk,))
                if ti + 2 < len(tiles):
                    emit_S(ti + 2)
                for qt in range(2):
                    if j > qt0 + qt:
                        continue
                    pb = 6 + qt
                    for m in range(2):
                        first = pb not in touched
                        touched.add(pb)
                        last = (j == qt0 + qt) and m == 1
                        mm(ps[pb][:, m * 129:(m + 1) * 129], pt[:, m * 256 + qt * 128:m * 256 + (qt + 1) * 128],
                           va[:, j, 0:129], first, last, (ptk, ("va", j), "va_ones"), (("ps", pb),))
                    if j == qt0 + qt:
                        P = ps[pb]
                        pk = ("ps", pb)
                        I("dve", lambda e, P=P: e.reciprocal(asm[:, 0:1], P[:, 128:129]), (pk,), ("asm0",))
                        I("dve", lambda e, P=P: e.reciprocal(asm[:, 1:2], P[:, 257:258]), (pk,), ("asm1",))
                        tt("dve", asm[:, 1:2], asm[:, 1:2], neglam[:, :], ALU.mult, ("asm1", "neglam"), ("asm1",))
                        ts("dve", at1[:, :], P[:, 0:128], asm[:, 0:1], ALU.mult, (pk, "asm0"), ("at1",))
                        stt(at2[:, :], P[:, 129:257], asm[:, 1:2], at1[:, :], ALU.mult, ALU.add,
                            (pk, "asm1", "at1"), ("at2",))
                        tt("pool", atj[:, :], at2[:, :], at2[:, :], ALU.mult, ("at2",), ("atj",))
                        I("dve", lambda e: e.reduce_sum(asm[:, 2:3], atj[:, :], AX.X), ("atj",), ("asm2",))
                        ts("dve", asm[:, 3:4], asm[:, 2:3], 1.0 / 128.0, ALU.mult, ("asm2",), ("asm3",),
                           s2=EPS, op1=ALU.add)
                        act(asm[:, 4:5], asm[:, 3:4], AF.Ln, ("asm3",), ("asm4",))
                        act(asm[:, 5:6], asm[:, 4:5], AF.Exp, ("asm4",), ("asm5",), scale=-0.5)
                        ts("dve", hab[:, :], at2[:, :], asm[:, 5:6], ALU.mult, ("at2", "asm5"), ("hab",),
                           s2=1.0 - lam_init, op1=ALU.mult)
                        tr(psb(pb)[:, 768:896], hab[:, :], identb[:, :], ("hab", "identb"), (pk,))
                        c0 = qoff + qt * 128
                        stt(yab[:, c0:c0 + 128], psb(pb)[:, 768:896], V[:, 20:21], ga[:, c0:c0 + 128], ALU.mult,
                            ALU.mult, (pk, vk, gak), ("yab",))
                yield
            res.append(dma("sp", ycl_ap, yab[:, :], ("yab",), (okey,)))

        out_toks = []

        def frontA(l, tb):
            t0 = tb * TB
            if l == 0:
                dma("sp", xblk[:, :, :], xT[:, t0:t0 + TB].rearrange("(kt p) t -> p kt t", p=128), (), XB)
            else:
                yield from outproj_block(l - 1, tb, xT, ("xsrc0", tb))
                tok = dma("sp", x1s[:, t0:t0 + TB].rearrange("(kt p) t -> p kt t", p=128), xblk[:, :, :],
                          XB, (("xsrc1", tb),))
                if not do_fin:
                    out_toks.append(tok)
            yield from norm_block(vec[l][:, 0:8], False)
            if _STOP >= 2:
                yield from inproj_block(l, tb)

        def frontB(l, tb):
            q4, tq = tb // 4, (tb % 4) * TB
            if _STOP >= 3:
                yield from gates_block(l, tb)
            res = []
            if _STOP >= 4:
                yield from mlstm_block(l, tb, ycl[l][q4][0:128, tq:tq + TB], ("ycl", l, q4, tb % 4, 0), res)
            if not fused:
                out_toks.extend(res)

        def drain(g):
            for _ in g:
                pass

        def adv(g, n):
            for _ in range(n):
                try:
                    next(g)
                except StopIteration:
                    return False
            return True

        NBU, NAU = float(os.environ.get("K_NBU", "20")), float(os.environ.get("K_NAU", "16"))

        load_weights(layers[0]) if layers else None
        if layers and layers[0] > 0:
            load_wout(layers[0] - 1)
        elif len(layers) > 1:
            load_wout(layers[0])
        for li, l in enumerate(layers):
            load_lam(l)
            drain(frontA(l, 0))
            for tb in range(_NB):
                q4, tq = tb // 4, (tb % 4) * TB
                res = []
                ag = (attn_block(l, tb, ycl[l][q4][128:256, tq:tq + TB], ("ycl", l, q4, tb % 4, 1), res)
                      if _STOP >= 5 else iter(()))
                bg = frontB(l, tb)
                fg = frontA(l, tb + 1) if tb + 1 < _NB else iter(())
                if tb == _NB - 1:
                    if li + 1 < len(layers):
                        load_weights(layers[li + 1])
                        if li + 1 >= 2:
                            load_wout(layers[li + 1] - 1)
                    elif do_fin and l > layers[0]:
                        load_wout(DEPTH - 1)
                ntile = 8 * tb + 6
                accb = acca = 0.0
                bl = fl = True
                for _ in ag:
                    accb += NBU / ntile
                    acca += NAU / ntile
                    nb_, na_ = int(accb), int(acca)
                    accb -= nb_
                    acca -= na_
                    while nb_ > 0 or na_ > 0:
                        if nb_ > 0:
                            bl = bl and adv(bg, 1)
                            nb_ -= 1
                        if na_ > 0:
                            fl = fl and adv(fg, 1)
                            na_ -= 1
                while bl or fl:
                    if bl:
                        bl = adv(bg, 1)
                    if fl:
                        fl = adv(fg, 1)
                if not fused:
                    out_toks.extend(res)
                if fused and tb % 4 == 3:
                    rk = tuple(("ycl", l, q4, i, w) for i in range(4) for w in range(2))
                    I("pool", lambda e, l=l, q4=q4: e.collective_compute(
                        "AllGather", ALU.bypass, replica_groups=GROUPS,
                        ins=[ycl[l][q4][:, :]], outs=[ycf[l][q4][:, :]]), rk, (("ycf", l, q4),), cc=True)

        if do_fin:
            if not (layers and layers[-1] > layers[0]):
                load_wout(DEPTH - 1)
            lf = DEPTH - 1
            nw_ap = vec[lf][:, 23:31]
            xb = [xblk[:, :, :], kaT[:, :].bitcast(F32).rearrange("p (kt t) -> p kt t", kt=KT)]
            yb = [sqb[:, :, :], va[:, :, :].rearrange("p a b -> p (a b)")[:, 0:KT * TB].rearrange("p (kt t) -> p kt t", kt=KT)]
            xk = [XB, tuple(("ka", i) for i in range(NBLK))]
            yk = [SQ, tuple(("va", i) for i in range(SEQ // 128)) + ("va_ones",)]

            def fin_load(tb):
                p = tb % 2
                t0 = tb * TB
                q4, tq = tb // 4, (tb % 4) * TB
                dma("sp", yb[p], ycf[lf][q4][:, tq:tq + TB].rearrange("(kt p) t -> p kt t", p=128),
                    (("ycf", lf, q4),), yk[p])
                dma("sp", xb[p], x1s[:, t0:t0 + TB].rearrange("(kt p) t -> p kt t", p=128),
                    (("xsrc1", tb),), xk[p])

            fin_load(0)
            for tb in range(_NB):
                p = tb % 2
                t0 = tb * TB
                if tb + 1 < _NB:
                    fin_load(tb + 1)
                X, Y = xb[p], yb[p]
                for m in range(KT):
                    bank = m % 2
                    for et in range(KT):
                        mm(ps[bank][:, :], wout_b[:, et, m * 128:(m + 1) * 128], Y[:, et, :], et == 0, et == KT - 1,
                           ("wout",) + yk[p], (("ps", bank),))
                    tt("dve", X[:, m, :], X[:, m, :], ps[bank][:, :], ALU.add, (("ps", bank),) + xk[p], xk[p])
                    if m % 2 == 0:
                        tt("pool", xnb[:, m, :], X[:, m, :], X[:, m, :], ALU.mult, xk[p], (("xnb", m),))
                    else:
                        act(xnb[:, m, :], X[:, m, :], AF.Square, xk[p], (("xnb", m),))
                for kt in range(KT):
                    mm(ps[2][:, :], onesb[:, :], xnb[:, kt, :], kt == 0, kt == KT - 1, ("onesb", ("xnb", kt)), (("ps", 2),))
                ts("dve", rstd[:, :], ps[2][:, :], 1.0 / D_MODEL, ALU.mult, (("ps", 2),), ("rstd",), s2=EPS, op1=ALU.add)
                act(rstd[:, :], rstd[:, :], AF.Ln, ("rstd",), ("rstd",))
                act(rstd[:, :], rstd[:, :], AF.Exp, ("rstd",), ("rstd",), scale=-0.5)
                for kt in range(KT):
                    stt(X[:, kt, :], X[:, kt, :], nw_ap[:, kt:kt + 1], rstd[:, :], ALU.mult, ALU.mult,
                        xk[p] + ("rstd", ("vec", lf)), xk[p])
                out_toks.append(dma("sp", outT[:, t0:t0 + TB].rearrange("(kt p) t -> p kt t", p=128), X, xk[p], ()))

        waits = []
        for tok in out_toks:
            S._tok_wait("sp", tok, waits)
        S.recs["sp"].append((waits, None, None))

        with nc.Block() as block:
            S.emit(block)
    return nc


_OFF = {"mq": 0, "mk": 512, "mv": 1024, "mi": 1536, "mf": 1540, "mz": 1544,
        "aq": 2056, "ak": 2568, "av": 3080, "az": 3592}


def _rope_tables():
    inv = (1.0 / (np.float32(10000.0) ** (np.arange(0, 64, 2, dtype=np.float32) / np.float32(64.0)))).astype(np.float32)
    ang = np.arange(SEQ, dtype=np.float32)[:, None] * inv[None, :]
    cos = np.cos(ang).astype(np.float32)
    sin = np.sin(ang).astype(np.float32)
    cosT = np.ascontiguousarray(np.concatenate([cos, cos, cos, cos], 1).T)
    sinT = np.ascontiguousarray(np.concatenate([sin, sin, sin, sin], 1).T)
    return cosT, sinT


def _consts():
    c = np.zeros((128, 192), np.float32)
    c[:, 0:128] = np.eye(128, dtype=np.float32)
    c[0:64, 128:192] = np.triu(np.ones((64, 64), np.float32))
    return c


def _core_inputs(inp, c, stage_layers, need_wout):
    b, hd = c // 4, c % 4
    f32 = np.float32
    d = {}
    for l in stage_layers:
        w = np.asarray(inp["w_in"][l], f32)
        hs = slice(hd * 128, (hd + 1) * 128)

        def blk(name):
            return w[:, _OFF[name] + hd * 128:_OFF[name] + (hd + 1) * 128]

        def perm(m):
            return np.concatenate([m[:, 32:64], m[:, 0:32], m[:, 96:128], m[:, 64:96]], 1)

        aq, ak = blk("aq"), blk("ak")
        gi = w[:, _OFF["mi"] + hd:_OFF["mi"] + hd + 1]
        gf = w[:, _OFF["mf"] + hd:_OFF["mf"] + hd + 1]
        d[f"wfm{l}"] = np.ascontiguousarray(np.concatenate(
            [blk("mq"), blk("mk"), blk("mz"), aq, perm(aq), ak, perm(ak), blk("az"), gi, gf], 1))
        d[f"wtm{l}"] = np.ascontiguousarray(np.concatenate([blk("mv"), blk("av")], 1))
        lam = np.concatenate([np.asarray(inp[k][l], f32) for k in ("lam_q1", "lam_k1", "lam_q2", "lam_k2")])
        d[f"lamv{l}"] = np.ascontiguousarray(np.tile(lam[None, :], (128, 1)))
    for l in range(DEPTH):
        v = np.zeros((128, NV), f32)
        v[:, 0:8] = np.asarray(inp["norm_w"][l], f32).reshape(8, 128).T
        cw = np.asarray(inp["conv_w"][l], f32)
        cbv = np.asarray(inp["conv_b"][l], f32)
        v[:, 8:12] = cw[:, hd * 128:(hd + 1) * 128].T
        v[:, 12:16] = cw[:, 512 + hd * 128:512 + (hd + 1) * 128].T
        v[:, 16] = cbv[hd * 128:(hd + 1) * 128]
        v[:, 17] = cbv[512 + hd * 128:512 + (hd + 1) * 128]
        v[:, 18] = np.asarray(inp["m_norm_w"][l], f32)[hd * 128:(hd + 1) * 128]
        v[:, 19] = np.asarray(inp["m_skip"][l], f32)[hd * 128:(hd + 1) * 128]
        v[:, 20] = np.asarray(inp["a_norm_w"][l], f32)
        v[:, 21] = np.asarray(inp["i_bias"][l], f32)[hd]
        v[:, 22] = np.asarray(inp["f_bias"][l], f32)[hd]
        v[:, 23:31] = np.asarray(inp["final_norm_w"], f32).reshape(8, 128).T
        d[f"vecs{l}"] = v
    for l in need_wout:
        wo = np.asarray(inp["w_out"][l], f32)
        rows = []
        for r in range(4):
            rows.append(wo[r * 128:(r + 1) * 128])
            rows.append(wo[512 + r * 128:512 + (r + 1) * 128])
        d[f"wout{l}"] = np.ascontiguousarray(np.concatenate(rows, 0))
    return d


_PROG = {}


def _prog(stage, debug=False):
    key = (stage, debug)
    if key not in _PROG:
        _PROG[key] = build_program(stage, debug)
    return _PROG[key]


def kernel(**inp):
    x = np.asarray(inp["x"], np.float32)
    xTs = [np.ascontiguousarray(x[b].T) for b in range(BATCH)]
    cosT, sinT = _rope_tables()
    cst = _consts()
    nc = _prog("all")
    in_maps = []
    for c in range(NCORES):
        d = _core_inputs(inp, c, [0, 1], [0, 1])
        d.update({"xT": xTs[c // 4], "cosT": cosT, "sinT": sinT, "consts": cst})
        in_maps.append(d)
    res = run_bass_kernel_spmd(nc, in_maps, core_ids=list(range(NCORES)))
    out = np.empty((BATCH, SEQ, D_MODEL), np.float32)
    for b in range(BATCH):
        out[b] = res.results[4 * b]["outT"].T
    return out
```

```python
import math
from contextlib import ExitStack

import numpy as np
import ml_dtypes

import concourse.bass as bass
import concourse.mybir as mybir
from concourse.bass_utils import run_bass_kernel_spmd

F32 = mybir.dt.float32
BF16 = mybir.dt.bfloat16
AF = mybir.ActivationFunctionType
ALU = mybir.AluOpType
AX = mybir.AxisListType

D_MODEL = 1024
BATCH = 2
SEQ = 8192
DEPTH = 2
NCORES = 8
TB = 512
NBLK = SEQ // TB
KT = D_MODEL // 128
EPS = 1e-6
NV = 32
LN_KSCALE = math.log(128.0 ** -0.5)
GROUPS = [[0, 1, 2, 3], [4, 5, 6, 7]]
import os
_STOP = int(os.environ.get("K_STOP", "9"))
_NB = int(os.environ.get("K_NBLK", str(NBLK)))


class Sched:
    CH = 30000
    ND = 48

    def __init__(self, nc, es):
        self.nc = nc
        self.engs = ["pe", "act", "dve", "pool", "sp"]
        self.recs = {e: [] for e in self.engs}
        self.cnt = {e: 0 for e in self.engs}
        self.seen = {e: {} for e in self.engs}
        self.lastw = {}
        self.readers = {}
        nsem = {"pe": 3, "act": 3, "dve": 4, "pool": 3, "sp": 1}
        self.esems = {e: [es.enter_context(nc.semaphore(f"s_{e}_{i}")) for i in range(nsem[e])]
                      for e in self.engs}
        self.dsems = [es.enter_context(nc.semaphore(f"d_{i}")) for i in range(self.ND)]
        self.dval = [0] * self.ND
        self.dnext = 0
        self.ccsem = es.enter_context(nc.semaphore("ccsem"))
        self.ccval = 0

    def _tok_wait(self, e, tok, waits):
        if tok[0] == "e":
            _, e2, n = tok
            if e2 == e and e == "pe":
                return
            if self.seen[e].get(e2, 0) >= n:
                return
            self.seen[e][e2] = n
            waits.append((self.esems[e2][(n - 1) // self.CH], (n - 1) % self.CH + 1))
        else:
            kind, i, v = tok
            key = (kind, i)
            if self.seen[e].get(key, 0) >= v:
                return
            self.seen[e][key] = v
            sem = self.dsems[i] if kind == "d" else self.ccsem
            waits.append((sem, v))

    def issue(self, e, fn, reads=(), writes=(), dma=False, cc=False):
        deps = []
        for k in reads:
            if k in self.lastw:
                deps.append(self.lastw[k])
        for k in writes:
            if k in self.lastw:
                deps.append(self.lastw[k])
            deps.extend(self.readers.get(k, {}).values())
        waits = []
        for tok in deps:
            self._tok_wait(e, tok, waits)
        if dma:
            i = self.dnext
            self.dnext = (self.dnext + 1) % self.ND
            prev = self.dval[i]
            if prev > 0:
                self._tok_wait(e, ("d", i, prev), waits)
            self.dval[i] += 16
            tok = ("d", i, self.dval[i])
            inc = (self.dsems[i], 16)
        elif cc:
            self.ccval += 1
            tok = ("c", 0, self.ccval)
            inc = (self.ccsem, 1)
        elif fn is None:
            tok = None
            inc = None
        else:
            self.cnt[e] += 1
            n = self.cnt[e]
            tok = ("e", e, n)
            inc = (self.esems[e][(n - 1) // self.CH], 1)
        self.recs[e].append((waits, fn, inc))
        if tok is not None:
            for k in reads:
                self.readers.setdefault(k, {})[(tok[0], tok[1])] = tok
            for k in writes:
                self.lastw[k] = tok
                self.readers[k] = {}
        return tok

    def emit(self, block):
        nc = self.nc

        def run(e, eng):
            for waits, fn, inc in self.recs[e]:
                for s, v in waits:
                    eng.wait_ge(s, v)
                if fn is not None:
                    ins = fn(eng)
                    ins.then_inc(inc[0], inc[1])

        @block.tensor
        def _(eng):
            run("pe", eng)

        @block.scalar
        def _(eng):
            run("act", eng)

        @block.vector
        def _(eng):
            run("dve", eng)

        @block.gpsimd
        def _(eng):
            run("pool", eng)

        @block.sync
        def _(eng):
            run("sp", eng)


def build_program(stage="all", debug=False):
    nc = bass.Bass("TRN2", target_bir_lowering=False)
    with ExitStack() as es:
        S = Sched(nc, es)

        def dram(name, shape, dt, kind):
            return nc.dram_tensor(name, shape, dt, kind=kind).ap()

        fused = stage == "all"
        layers = {"all": [0, 1], "l0": [0], "mid": [1], "fin": []}[stage]
        do_fin = stage in ("all", "fin")

        xT = dram("xT", [D_MODEL, SEQ], F32, "ExternalInput") if stage in ("all", "l0", "mid") else None
        cosT = dram("cosT", [128, SEQ], F32, "ExternalInput") if layers else None
        sinT = dram("sinT", [128, SEQ], F32, "ExternalInput") if layers else None
        consts = dram("consts", [128, 192], F32, "ExternalInput")
        wfm, wtm, wout, vecs, lamv = {}, {}, {}, {}, {}
        for l in layers:
            wfm[l] = dram(f"wfm{l}", [D_MODEL, 1026], F32, "ExternalInput")
            wtm[l] = dram(f"wtm{l}", [D_MODEL, 256], F32, "ExternalInput")
            lamv[l] = dram(f"lamv{l}", [128, 256], F32, "ExternalInput")
        for l in range(DEPTH):
            vecs[l] = dram(f"vecs{l}", [128, NV], F32, "ExternalInput")
        need_wout = {"all": [0, 1], "l0": [], "mid": [0], "fin": [1]}[stage]
        for l in need_wout:
            wout[l] = dram(f"wout{l}", [D_MODEL, D_MODEL], F32, "ExternalInput")

        ycl, ycf = {}, {}
        for l in layers:
            kind = "Internal" if fused else "ExternalOutput"
            ycl[l] = [dram(f"ycl{l}_{q}", [256, 2048], BF16, kind) for q in range(4)]
        for l in need_wout:
            kind = "Internal" if fused else "ExternalInput"
            ycf[l] = [dram(f"ycf{l}_{q}", [1024, 2048], BF16, kind) for q in range(4)]
        x1s = None
        if stage == "all":
            x1s = dram("x1s", [D_MODEL, SEQ], F32, "Internal")
        elif stage == "mid":
            x1s = dram("x1s", [D_MODEL, SEQ], F32, "ExternalOutput")
        elif stage == "fin":
            x1s = dram("x1s", [D_MODEL, SEQ], F32, "ExternalInput")
        outT = dram("outT", [D_MODEL, SEQ], F32, "ExternalOutput") if do_fin else None
        dbg = {}
        if debug:
            for nm, shp, dt in [("d_qm", [128, SEQ], BF16), ("d_km", [128, SEQ], BF16),
                                ("d_qa", [128, SEQ], BF16), ("d_ka", [128, SEQ], BF16),
                                ("d_rows", [1, 9 * SEQ], F32)]:
                dbg[nm] = dram(nm, shp, dt, "ExternalOutput")

        def sb(name, shape, dt):
            return es.enter_context(nc.sbuf_tensor(name, shape, dt))

        cst = sb("cst", [128, 192], F32)
        ident_f = cst[:, 0:128]
        identb = sb("identb", [128, 128], BF16)
        onesb = sb("onesb", [128, 128], BF16)
        onesf = sb("onesf", [1, 512], F32)
        vec = [sb(f"vec{l}", [128, NV], F32) for l in range(DEPTH)]
        lamt = sb("lamt", [128, 256], F32)
        lamw = sb("lamw", [128, 4], F32)
        neglam = sb("neglam", [128, 1], F32)
        negfb = sb("negfb", [1, 1], F32)

        wfm_b = sb("wfm_b", [128, KT, 1026], BF16)
        wtm_b = sb("wtm_b", [128, KT, 256], BF16)
        wout_b = sb("wout_b", [128, KT, D_MODEL], BF16)
        wstg = [sb(f"wstg{i}", [128, KT, 128], F32) for i in range(2)]

        xblk = sb("xblk", [128, KT, TB], F32)
        sqb = sb("sqb", [128, KT, TB], BF16)
        xnb = sb("xnb", [128, KT, TB], BF16)
        ycb = sqb
        rstd = sb("rstd", [128, TB], F32)
        cosb = sb("cosb", [128, TB], F32)
        sinb = sb("sinb", [128, TB], F32)
        preq = sb("preq", [128, TB + 4], F32)
        prek = sb("prek", [128, TB + 4], F32)
        cva = sb("tA", [128, TB], F32)
        cvb = sb("tB", [128, TB], F32)
        tC = sb("tC", [128, TB], F32)
        rpa, rpb = cva, cvb
        qa2 = [sb(f"qa{i}", [128, TB], BF16) for i in range(2)]
        qm2 = [sb(f"qm{i}", [128, TB], BF16) for i in range(2)]
        km2 = [sb(f"km{i}", [128, TB], BF16) for i in range(2)]
        qs = sb("qs", [128, TB], BF16)
        gm2 = [sb(f"gm{i}", [128, TB], F32) for i in range(2)]
        gtA = sb("gtA", [128, TB], F32)
        gtB = sb("gtB", [128, TB], F32)
        ga2 = [sb(f"ga{i}", [128, TB], F32) for i in range(2)]
        kaT = sb("kaT", [128, SEQ], BF16)
        va = sb("va", [128, SEQ // 128, 132], BF16)
        vmc2 = [sb(f"vmc{i}", [64, 8, 132], BF16) for i in range(2)]
        vmw = sb("vmw", [64, 8, 132], BF16)
        ktok = sb("ktok", [64, 8, 128], BF16)
        NR = 9
        rows = sb("rows", [1, NR, TB], F32)
        gif2 = [sb(f"gif{i}", [1, 2, TB], F32) for i in range(2)]
        carry = sb("carry", [1, 4], F32)
        ape = sb("ape", [1, 8], F32)
        dec = sb("dec", [1, 8], F32)
        cols = sb("cols", [64, 24], F32)
        decb = sb("decb", [128, 8], F32)
        wT = sb("wT", [64, 8, 64], F32)
        stl = [sb(f"stl{i}", [64, 64], BF16) for i in range(2)]
        cf = sb("cf", [128, 132], F32)
        cb = sb("cb", [128, 132], BF16)
        nums = sb("nums", [64, 8, 132], F32)
        sq8 = sb("sq8", [64, 8, 128], F32)
        sm = sb("sm", [64, 8, 8], F32)
        hmb = sb("hmb", [64, 8, 128], BF16)
        gt1, gt2 = gtA, gtB
        yo = sb("yo", [128, TB], BF16)
        pT = [sb(f"pT{i}", [128, 512], BF16) for i in range(2)]
        at1 = sb("at1", [128, 128], F32)
        at2 = sb("at2", [128, 128], F32)
        atj = sb("atj", [128, 128], F32)
        asm = sb("asm", [128, 8], F32)
        hab = sb("hab", [128, 128], BF16)
        yab = sb("yab", [128, TB], BF16)
        ob = xblk

        ps_all = es.enter_context(nc.psum_tensor("ps_all", [128, 8, 512], F32))
        ps = [ps_all[:, i, :] for i in range(8)]

        def I(e, fn, r=(), w=(), **kw):
            return S.issue(e, fn, r, w, **kw)

        def dma(q, out, in_, r, w):
            return I(q, lambda e: e.dma_start(out=out, in_=in_), r, w, dma=True)

        def act(out, in_, func, r, w, scale=1.0, bias=0.0, accum=None):
            if accum is None:
                return I("act", lambda e: e.activation(out, in_, func, bias=bias, scale=scale), r, w)
            return I("act", lambda e: e.activation(out, in_, func, bias=bias, scale=scale, accum_out=accum), r, w)

        def tt(eng, out, a, b, op, r, w):
            return I(eng, lambda e: e.tensor_tensor(out, a, b, op), r, w)

        def ts(eng, out, a, s1, op0, r, w, s2=None, op1=ALU.bypass):
            return I(eng, lambda e: e.tensor_scalar(out, a, s1, s2, op0, op1), r, w)

        def stt(out, a, sc, b, op0, op1, r, w):
            return I("dve", lambda e: e.scalar_tensor_tensor(out, a, sc, b, op0, op1), r, w)

        def cp(eng, out, in_, r, w):
            if eng == "act":
                return I(eng, lambda e: e.copy(out, in_), r, w)
            return I(eng, lambda e: e.tensor_copy(out, in_), r, w)

        def mm(out, lhsT, rhs, start, stop, r, w):
            return I("pe", lambda e: e.matmul(out, lhsT, rhs, start=start, stop=stop), r, w)

        def tr(out, in_, idn, r, w):
            return I("pe", lambda e: e.transpose(out, in_, idn), r, w)

        def psb(i):
            return ps[i].bitcast(BF16)

        XB = tuple(("xblk", k) for k in range(KT))
        SQ = tuple(("sqb", k) for k in range(KT))
        XN = tuple(("xnb", k) for k in range(KT))

        dma("sp", cst[:, :], consts[:, :], (), ("cst",))
        for l in range(DEPTH):
            dma("sp", vec[l][:, :], vecs[l][:, :], (), (("vec", l),))
        cp("dve", identb[:, :], cst[:, 0:128], ("cst",), ("identb",))
        I("pool", lambda e: e.memset(onesb[:, :], 1.0), (), ("onesb",))
        I("pool", lambda e: e.memset(onesf[:, :], 1.0), (), ("onesf",))
        I("pool", lambda e: e.memset(va[:, :, 128:132], 1.0), (), ("va_ones",))
        for i in range(2):
            I("pool", lambda e, i=i: e.memset(vmc2[i][:, :, 128:132], 1.0), (), ("vmc_ones",))
        mask64 = cst[0:64, 128:192]

        def load_weights(l):
            nchunk = 11
            for ci in range(nchunk):
                st = wstg[ci % 2]
                sk = ("wstg", ci % 2)
                if ci < 8:
                    src = wfm[l][:, ci * 128:(ci + 1) * 128]
                    dst = wfm_b[:, :, ci * 128:(ci + 1) * 128]
                    ncol = 128
                elif ci == 8:
                    src = wfm[l][:, 1024:1026]
                    dst = wfm_b[:, :, 1024:1026]
                    ncol = 2
                else:
                    src = wtm[l][:, (ci - 9) * 128:(ci - 8) * 128]
                    dst = wtm_b[:, :, (ci - 9) * 128:(ci - 8) * 128]
                    ncol = 128
                dma("sp", st[:, :, 0:ncol], src.rearrange("(kt p) c -> p kt c", p=128), (), (sk,))
                eng = "pool" if ci % 2 == 0 else "dve"
                cp(eng, dst, st[:, :, 0:ncol], (sk,), ("win",))
                if ci in (4, 6):
                    for m in range(2):
                        sl = wfm_b[:, :, ci * 128 + m * 64: ci * 128 + m * 64 + 32]
                        ts("pool", sl, sl, -1.0, ALU.mult, ("win",), ("win",))
        def load_lam(l):
            dma("sp", lamt[:, :], lamv[l][:, :], (), ("lamt",))
            for i in range(2):
                tt("dve", lamt[:, i * 128:i * 128 + 64], lamt[:, i * 128:i * 128 + 64],
                   lamt[:, i * 128 + 64:i * 128 + 128], ALU.mult, ("lamt",), ("lamt",))
                I("dve", lambda e, i=i: e.reduce_sum(lamw[:, i:i + 1], lamt[:, i * 128:i * 128 + 64], AX.X),
                  ("lamt",), ("lamw",))
            act(lamw[:, 2:4], lamw[:, 0:2], AF.Exp, ("lamw",), ("lamw",))
            lam_init = 0.8 - 0.6 * math.exp(-0.3 * l)
            stt(neglam[:, :], lamw[:, 3:4], -lam_init, lamw[:, 2:3], ALU.add, ALU.subtract, ("lamw",), ("neglam",))
            ts("pool", negfb[:, :], vec[l][0:1, 22:23], -1.0, ALU.mult, (("vec", l),), ("negfb",))

        def load_wout(l):
            for ci in range(8):
                st = wstg[ci % 2]
                sk = ("wstg", ci % 2)
                dma("sp", st[:, :, :], wout[l][:, ci * 128:(ci + 1) * 128].rearrange("(kt p) c -> p kt c", p=128),
                    (), (sk,))
                eng = "pool" if ci % 2 == 0 else "dve"
                cp(eng, wout_b[:, :, ci * 128:(ci + 1) * 128], st[:, :, :], (sk,), ("wout",))

        def outproj_loads(l_prev, tb, xsrc, xkey):
            t0 = tb * TB
            q4, tq = tb // 4, (tb % 4) * TB
            dma("sp", ycb[:, :, :], ycf[l_prev][q4][:, tq:tq + TB].rearrange("(kt p) t -> p kt t", p=128),
                (("ycf", l_prev, q4),), SQ)
            dma("sp", xblk[:, :, :], xsrc[:, t0:t0 + TB].rearrange("(kt p) t -> p kt t", p=128),
                (xkey,), XB)

        def outproj_block(l_prev, tb, xsrc, xkey, banks=(0,), load=True):
            if load:
                outproj_loads(l_prev, tb, xsrc, xkey)
            for m in range(KT):
                bank = banks[m % len(banks)]
                for et in range(KT):
                    mm(ps[bank][:, :], wout_b[:, et, m * 128:(m + 1) * 128], ycb[:, et, :], et == 0, et == KT - 1,
                       ("wout", ("sqb", et)), (("ps", bank),))
                tt("dve", xblk[:, m, :], xblk[:, m, :], ps[bank][:, :], ALU.add, (("ps", bank), ("xblk", m)),
                   (("xblk", m),))
                yield

        def norm_block(nw_ap, to_out):
            for kt in range(KT):
                tt("pool" if kt % 2 == 0 else "dve", sqb[:, kt, :], xblk[:, kt, :], xblk[:, kt, :], ALU.mult,
                   (("xblk", kt),), (("sqb", kt),))
            for kt in range(KT):
                mm(ps[0][:, :], onesb[:, :], sqb[:, kt, :], kt == 0, kt == KT - 1, ("onesb", ("sqb", kt)), (("ps", 0),))
            yield
            ts("dve", rstd[:, :], ps[0][:, :], 1.0 / D_MODEL, ALU.mult, (("ps", 0),), ("rstd",), s2=EPS, op1=ALU.add)
            act(rstd[:, :], rstd[:, :], AF.Ln, ("rstd",), ("rstd",))
            act(rstd[:, :], rstd[:, :], AF.Exp, ("rstd",), ("rstd",), scale=-0.5)
            for kt in range(KT):
                if to_out:
                    stt(ob[:, kt, :], xblk[:, kt, :], nw_ap[:, kt:kt + 1], rstd[:, :], ALU.mult, ALU.mult,
                        (("xblk", kt), "rstd"), (("xblk", kt),))
                else:
                    stt(xnb[:, kt, :], xblk[:, kt, :], nw_ap[:, kt:kt + 1], rstd[:, :], ALU.mult, ALU.mult,
                        (("xblk", kt), "rstd"), (("xnb", kt),))
            yield

        def inproj_block(l, tb):
            t0 = tb * TB
            pp = tb % 2
            qa, ga = qa2[pp], ga2[pp]
            qak, gak = ("qa", pp), ("ga", pp)
            qm, km, gm, vmc, gif = qm2[pp], km2[pp], gm2[pp], vmc2[pp], gif2[pp]
            V = vec[l]
            vk = ("vec", l)
            dma("sp", cosb[:, :], cosT[:, t0:t0 + TB], (), ("cosb",))
            dma("sp", sinb[:, :], sinT[:, t0:t0 + TB], (), ("sinb",))

            def fm(c0, ncol):
                def f(b):
                    for kt in range(KT):
                        mm(ps[b][0:ncol, :], wfm_b[:, kt, c0:c0 + ncol], xnb[:, kt, :], kt == 0, kt == KT - 1,
                           ("win", ("xnb", kt)), (("ps", b),))
                return f

            def conv_ev(which):
                pre, dst, cw0, cbc, fin, fk = [(preq, qm, 8, 16, tC, "tC"), (prek, km, 12, 17, cvb, "tB")][which]
                pk = ("pre", which)

                def e1(b):
                    if tb == 0:
                        I("pool", lambda e: e.memset(pre[:, 0:4], 0.0), (), (pk,))
                    else:
                        cp("pool", pre[:, 1:4], pre[:, TB + 1:TB + 4], (pk,), (pk,))
                    cp("dve", pre[:, 4:4 + TB], ps[b][:, :], (("ps", b), pk), (pk,))
                    ts("dve", cva[:, :], pre[:, 4:4 + TB], V[:, cw0 + 3:cw0 + 4], ALU.mult, (pk, vk), ("tA",),
                       s2=V[:, cbc:cbc + 1], op1=ALU.add)
                    stt(cvb[:, :], pre[:, 3:3 + TB], V[:, cw0 + 2:cw0 + 3], cva[:, :], ALU.mult, ALU.add,
                        (pk, vk, "tA"), ("tB",))
                    stt(cva[:, :], pre[:, 2:2 + TB], V[:, cw0 + 1:cw0 + 2], cvb[:, :], ALU.mult, ALU.add,
                        (pk, vk, "tB"), ("tA",))
                    stt(fin[:, :], pre[:, 1:1 + TB], V[:, cw0:cw0 + 1], cva[:, :], ALU.mult, ALU.add,
                        (pk, vk, "tA"), (fk,))

                def e2(b):
                    act(dst[:, :], fin[:, :], AF.Silu, (fk,), (("qm", pp) if which == 0 else ("km", pp),))
                return [e1, e2]

            def silu_ev(dst, dk):
                return [lambda b: act(dst[:, :], ps[b][:, :], AF.Silu, (("ps", b),), (dk,))]

            def rope_a(b):
                tt("dve", rpa[:, :], ps[b][:, :], cosb[:, :], ALU.mult, (("ps", b), "cosb"), ("tA",))

            def rope_b(which):
                def f(b):
                    tt("dve", rpb[:, :], ps[b][:, :], sinb[:, :], ALU.mult, (("ps", b), "sinb"), ("tB",))
                    if which == 0:
                        tt("pool", qa[:, :], rpa[:, :], rpb[:, :], ALU.add, ("tA", "tB"), (qak,))
                    else:
                        tt("pool", kaT[:, t0:t0 + TB], rpa[:, :], rpb[:, :], ALU.add, ("tA", "tB"), (("ka", tb),))
                return f

            def row_ev(g):
                return [lambda b: cp("dve", gif[:, g, :], ps[b][0:1, :], (("ps", b),), (("gif", pp),))]

            def vm_mm(h):
                def f(b):
                    for c4 in range(4):
                        c8 = h * 4 + c4
                        for kt in range(KT):
                            mm(ps[b][0:64, c4 * 128:(c4 + 1) * 128], xnb[:, kt, c8 * 64:(c8 + 1) * 64],
                               wtm_b[:, kt, 0:128], kt == 0, kt == KT - 1, ("win", ("xnb", kt)), (("ps", b),))
                return f

            def vm_ev(h):
                return [lambda b: cp("dve", vmc[:, h * 4:(h + 1) * 4, 0:128],
                                     ps[b][0:64, :].rearrange("p (c d) -> p c d", d=128), (("ps", b),), (("vmc", pp),))]

            def va_mm(b):
                for t4 in range(4):
                    for kt in range(KT):
                        mm(ps[b][:, t4 * 128:(t4 + 1) * 128], xnb[:, kt, t4 * 128:(t4 + 1) * 128],
                           wtm_b[:, kt, 128:256], kt == 0, kt == KT - 1, ("win", ("xnb", kt)), (("ps", b),))

            def va_ev(b):
                cp("dve", va[:, tb * 4:(tb + 1) * 4, 0:128], ps[b][:, :].rearrange("p (c d) -> p c d", d=128),
                   (("ps", b), "va_ones"), tuple(("va", tb * 4 + i) for i in range(4)))

            stages = [
                (fm(0, 128), conv_ev(0)),
                (fm(128, 128), conv_ev(1)),
                (fm(256, 128), silu_ev(gm, ("gm", pp))),
                (fm(3 * 128, 128), [rope_a]),
                (fm(4 * 128, 128), [rope_b(0)]),
                (fm(5 * 128, 128), [rope_a]),
                (fm(6 * 128, 128), [rope_b(1)]),
                (fm(7 * 128, 128), silu_ev(ga, gak)),
                (fm(1024, 1), row_ev(0)),
                (fm(1025, 1), row_ev(1)),
                (vm_mm(0), vm_ev(0)),
                (vm_mm(1), vm_ev(1)),
                (va_mm, [va_ev]),
            ]
            pending = []
            for k, (mmf, evs) in enumerate(stages):
                b = 0
                mmf(b)
                for i, fn in enumerate(evs):
                    pending.append((k + i, fn, b))
                for due, fn, bb in [p for p in pending if p[0] <= k]:
                    fn(bb)
                pending = [p for p in pending if p[0] > k]
                yield
            for due, fn, bb in sorted(pending, key=lambda p: p[0]):
                fn(bb)
            yield

        R_I, R_F, R_L1, R_BN, R_A_, R_AA, R_WI, R_WK, R_EM = range(9)
        R_T2, R_T1, R_ALS = R_I, R_F, R_L1

        def rw(i):
            return rows[:, i, :]

        def gates_block(l, tb):
            V = vec[l]
            vk = ("vec", l)
            rk = lambda i: ("row", i)
            pp = tb % 2
            qm, vmc, gif = qm2[pp], vmc2[pp], gif2[pp]
            gk = ("gif", pp)
            if tb == 0:
                I("pool", lambda e: e.memset(carry[:, :], 0.0), (), ("carry",))
            act(rw(R_T1), gif[:, 1, :], AF.Exp, (gk, "negfb"), (rk(R_T1),), scale=-1.0, bias=negfb[:, :])
            act(rw(R_L1), rw(R_T1), AF.Ln, (rk(R_T1),), (rk(R_L1),), bias=1.0)
            I("dve", lambda e: e.tensor_tensor_scan(rw(R_BN), onesf[:, :], rw(R_L1), carry[:, 0:1], ALU.mult, ALU.add),
              ("onesf", rk(R_L1), "carry"), (rk(R_BN),))
            stt(rw(R_A_), gif[:, 0, :], V[0:1, 21:22], rw(R_BN), ALU.add, ALU.add, (gk, vk, rk(R_BN)), (rk(R_A_),))
            I("dve", lambda e: e.tensor_tensor_scan(rw(R_AA), onesf[:, :], rw(R_A_), carry[:, 1:2], ALU.mult, ALU.max),
              ("onesf", rk(R_A_), "carry"), (rk(R_AA),))
            yield
            A3 = rw(R_AA).rearrange("p (c i) -> p c i", i=64)
            aend = A3[:, :, 63]
            cp("pool", ape[:, 0:1], carry[:, 1:2], ("carry",), ("ape",))
            cp("pool", ape[:, 1:8], A3[:, 0:7, 63], (rk(R_AA),), ("ape",))
            tt("dve", rw(R_T1).rearrange("p (c i) -> p c i", i=64), ape[:, :].unsqueeze(2).broadcast_to([1, 8, 64]),
               A3, ALU.subtract, ("ape", rk(R_AA)), (rk(R_T1),))
            act(rw(R_WI), rw(R_T1), AF.Exp, (rk(R_T1),), (rk(R_WI),))
            tt("dve", dec[:, :], ape[:, :], aend, ALU.subtract, ("ape", rk(R_AA)), ("dec",))
            act(dec[:, :], dec[:, :], AF.Exp, ("dec",), ("dec",))
            tt("dve", rw(R_T2).rearrange("p (c i) -> p c i", i=64), rw(R_A_).rearrange("p (c i) -> p c i", i=64),
               aend.unsqueeze(2).broadcast_to([1, 8, 64]), ALU.subtract, (rk(R_A_), rk(R_AA)), (rk(R_T2),))
            act(rw(R_WK), rw(R_T2), AF.Exp, (rk(R_T2),), (rk(R_WK),), bias=LN_KSCALE)
            tt("dve", rw(R_T1), rw(R_BN), rw(R_AA), ALU.subtract, (rk(R_BN), rk(R_AA)), (rk(R_T1),))
            act(rw(R_EM), rw(R_T1), AF.Exp, (rk(R_T1),), (rk(R_EM),))
            ts("pool", rw(R_ALS), rw(R_A_), LN_KSCALE, ALU.add, (rk(R_A_),), (rk(R_ALS),))
            cp("pool", carry[:, 0:1], rw(R_BN)[:, TB - 1:TB], (rk(R_BN),), ("carry",))
            cp("pool", carry[:, 1:2], rw(R_AA)[:, TB - 1:TB], (rk(R_AA), "ape"), ("carry",))
            yield
            for qi, ri in enumerate([R_ALS, R_WK, R_EM]):
                for c8 in range(8):
                    mm(ps[1][0:64, 480 + qi * 8 + c8:480 + qi * 8 + c8 + 1], rw(ri)[:, c8 * 64:(c8 + 1) * 64],
                       onesf[:, 0:1], True, True, (rk(ri), "onesf"), (("ps", 1),))
            cp("dve", cols[:, :], ps[1][0:64, 480:504], (("ps", 1),), ("cols",))
            mm(ps[1][:, 504:512], onesf[:, 0:128], dec[:, :], True, True, ("onesf", "dec"), (("ps", 1),))
            cp("dve", decb[:, :], ps[1][:, 504:512], (("ps", 1),), ("decb",))
            yield
            mm(ps[1][0:64, :], onesf[:, 0:64], rw(R_AA), True, True, ("onesf", rk(R_AA)), (("ps", 1),))
            for c8 in range(8):
                act(wT[:, c8, :], ps[1][0:64, c8 * 64:(c8 + 1) * 64], AF.Exp, (("ps", 1), "cols"), (("wT", c8),),
                    scale=-1.0, bias=cols[:, c8:c8 + 1])
            tt("pool", wT[:, :, :], wT[:, :, :], mask64.unsqueeze(1).broadcast_to([64, 8, 64]), ALU.mult,
               tuple(("wT", c) for c in range(8)) + ("cst",), tuple(("wT", c) for c in range(8)))
            yield
            mm(ps[1][:, :], onesf[:, 0:128], rw(R_WI), True, True, ("onesf", rk(R_WI)), (("ps", 1),))
            tt("dve", qs[:, :], qm[:, :], ps[1][:, :], ALU.mult, (("qm", pp), ("ps", 1)), ("qs",))
            tt("pool", vmw[:, :, 0:129], vmc[:, :, 0:129], cols[:, 8:16].unsqueeze(2).broadcast_to([64, 8, 129]),
               ALU.mult, (("vmc", pp), "vmc_ones", "cols"), ("vmw",))
            yield

        def mlstm_block(l, tb, ycl_ap, okey, res):
            V = vec[l]
            vk = ("vec", l)
            pp = tb % 2
            qm, km, gm, vmc = qm2[pp], km2[pp], gm2[pp], vmc2[pp]
            qmk, kmk, gmk, vmck = ("qm", pp), ("km", pp), ("gm", pp), ("vmc", pp)
            if tb == 0:
                I("pool", lambda e: e.memset(cf[:, :], 0.0), (), ("cf",))
                I("pool", lambda e: e.memset(cb[:, :], 0.0), (), ("cb",))
            for c8 in range(8):
                tr(psb(1)[0:64, c8 * 128:(c8 + 1) * 128], km[:, c8 * 64:(c8 + 1) * 64], identb[:, :],
                   (kmk, "identb"), (("ps", 1),))
            cp("dve", ktok[:, :, :], psb(1)[0:64, :].rearrange("p (c d) -> p c d", d=128), (("ps", 1),), ("ktok",))
            yield

            def st(c8):
                cs = c8 * 64
                so = (c8 % 2) * 64
                mm(ps[1][0:64, so:so + 64], km[:, cs:cs + 64], qm[:, cs:cs + 64], True, True, (kmk, qmk), (("ps", 1),))

            st(0)
            for c8 in range(8):
                cs = c8 * 64
                so = (c8 % 2) * 64
                if c8 + 1 < 8:
                    st(c8 + 1)
                tt("dve", stl[c8 % 2][:, :], ps[1][0:64, so:so + 64], wT[:, c8, :], ALU.mult, (("ps", 1), ("wT", c8)),
                   (("stl", c8 % 2),))
                mm(ps[1][:, 257:386], ktok[:, c8, :], vmw[:, c8, 0:129], True, True, ("ktok", "vmw"), (("ps", 1),))
                mm(ps[1][0:64, 128:257], qs[:, cs:cs + 64], cb[:, 0:129], True, False, ("qs", "cb"), (("ps", 1),))
                mm(ps[1][0:64, 128:257], stl[c8 % 2][:, :], vmc[:, c8, 0:129], False, True,
                   (("stl", c8 % 2), vmck, "vmc_ones"), (("ps", 1),))
                stt(cf[:, 0:129], cf[:, 0:129], decb[:, c8:c8 + 1], ps[1][:, 257:386], ALU.mult, ALU.add,
                    ("cf", "decb", ("ps", 1)), ("cf",))
                cp("pool", cb[:, 0:129], cf[:, 0:129], ("cf",), ("cb",))
                cp("dve", nums[:, c8, 0:129], ps[1][0:64, 128:257], (("ps", 1),), (("nums", c8),))
                yield
            NUMS = tuple(("nums", c) for c in range(8))
            den = nums[:, :, 128]
            tt("dve", sq8[:, :, :], nums[:, :, 0:128], nums[:, :, 0:128], ALU.mult, NUMS, ("sq8",))
            I("dve", lambda e: e.reduce_sum(sm[:, :, 0], sq8[:, :, :], AX.X), ("sq8",), ("sm",))
            ts("dve", sm[:, :, 1], den, -1.0, ALU.mult, NUMS, ("sm",))
            tt("dve", sm[:, :, 1], sm[:, :, 1], den, ALU.max, ("sm",) + NUMS, ("sm",))
            tt("dve", sm[:, :, 1], sm[:, :, 1], cols[:, 16:24], ALU.max, ("sm", "cols"), ("sm",))
            tt("dve", sm[:, :, 2], sm[:, :, 1], sm[:, :, 1], ALU.mult, ("sm",), ("sm",))
            ts("dve", sm[:, :, 0], sm[:, :, 0], 1.0 / 128.0, ALU.mult, ("sm",), ("sm",))
            stt(sm[:, :, 3], sm[:, :, 2], EPS, sm[:, :, 0], ALU.mult, ALU.add, ("sm",), ("sm",))
            act(sm[:, :, 4], sm[:, :, 3], AF.Ln, ("sm",), ("sm",))
            act(sm[:, :, 5], sm[:, :, 4], AF.Exp, ("sm",), ("sm",), scale=-0.5)
            tt("dve", hmb[:, :, :], nums[:, :, 0:128], sm[:, :, 5:6].broadcast_to([64, 8, 128]), ALU.mult,
               NUMS + ("sm",), ("hmb",))
            for c8 in range(8):
                tr(psb(1)[:, c8 * 64:(c8 + 1) * 64], hmb[:, c8, :], identb[0:64, 0:64], ("hmb", "identb"),
                   (("ps", 1),))
            yield
            ts("dve", gt1[:, :], qm[:, :], V[:, 19:20], ALU.mult, (qmk, vk), ("gtA",))
            stt(gt2[:, :], psb(1)[:, 0:TB], V[:, 18:19], gt1[:, :], ALU.mult, ALU.add, (("ps", 1), vk, "gtA"),
                ("gtB",))
            tt("dve", yo[:, :], gt2[:, :], gm[:, :], ALU.mult, ("gtB", gmk), ("yo",))
            res.append(dma("sp", ycl_ap, yo[:, :], ("yo",), (okey,)))
            yield

        def attn_block(l, tb, ycl_ap, okey, res):
            V = vec[l]
            vk = ("vec", l)
            qa, ga = qa2[tb % 2], ga2[tb % 2]
            qak, gak = ("qa", tb % 2), ("ga", tb % 2)
            lam_init = 0.8 - 0.6 * math.exp(-0.3 * l)
            tiles = []
            for g2 in range(2):
                qt0 = tb * 4 + g2 * 2
                for j in range(qt0 + 2):
                    tiles.append((g2, qt0, j))

            def emit_S(ti):
                g2, qt0, j = tiles[ti]
                qoff = g2 * 256
                lo = 0 if j <= qt0 else 128
                sb0 = 2 + 2 * (ti % 2)
                kb = j // 4
                for m in range(2):
                    mm(ps[sb0 + m][:, lo:256], kaT[m * 64:(m + 1) * 64, j * 128:(j + 1) * 128],
                       qa[m * 64:(m + 1) * 64, qoff + lo:qoff + 256], True, True, (("ka", kb), qak),
                       (("ps", sb0), ("ps", sb0 + 1)))

            touched = set()
            emit_S(0)
            if len(tiles) > 1:
                emit_S(1)
            for ti, (g2, qt0, j) in enumerate(tiles):
                if j == 0:
                    touched = set()
                qoff = g2 * 256
                lo = 0 if j <= qt0 else 128
                sb0 = 2 + 2 * (ti % 2)
                pt = pT[ti % 2]
                ptk = ("pT", ti % 2)
                src_ = ps_all[:, sb0:sb0 + 2, lo:256]
                dst = pt[:, :].rearrange("p (m q) -> p m q", m=2)[:, :, lo:256]
                act(dst, src_, AF.Exp, (("ps", sb0), ("ps", sb0 + 1)), (ptk,), scale=0.125)
                if j >= qt0:
                    d0 = (j - qt0) * 128
                    msl = pt[64:128, :].rearrange("p (m q) -> p m q", m=2)[:, :, d0:d0 + 64]
                    I("pool", lambda e, msl=msl: e.memset(msl, 0.0), (ptk,), (ptk,))
                if ti + 2 < len(tiles):
                    emit_S(ti + 2)
                for qt in range(2):
                    if j > qt0 + qt:
                        continue
                    pb = 6 + qt
                    for m in range(2):
                        first = pb not in touched
                        touched.add(pb)
                        last = (j == qt0 + qt) and m == 1
                        mm(ps[pb][:, m * 129:(m + 1) * 129], pt[:, m * 256 + qt * 128:m * 256 + (qt + 1) * 128],
                           va[:, j, 0:129], first, last, (ptk, ("va", j), "va_ones"), (("ps", pb),))
                    if j == qt0 + qt:
                        P = ps[pb]
                        pk = ("ps", pb)
                        I("dve", lambda e, P=P: e.reciprocal(asm[:, 0:1], P[:, 128:129]), (pk,), ("asm0",))
                        I("dve", lambda e, P=P: e.reciprocal(asm[:, 1:2], P[:, 257:258]), (pk,), ("asm1",))
                        tt("dve", asm[:, 1:2], asm[:, 1:2], neglam[:, :], ALU.mult, ("asm1", "neglam"), ("asm1",))
                        ts("dve", at1[:, :], P[:, 0:128], asm[:, 0:1], ALU.mult, (pk, "asm0"), ("at1",))
                        stt(at2[:, :], P[:, 129:257], asm[:, 1:2], at1[:, :], ALU.mult, ALU.add,
                            (pk, "asm1", "at1"), ("at2",))
                        tt("pool", atj[:, :], at2[:, :], at2[:, :], ALU.mult, ("at2",), ("atj",))
                        I("dve", lambda e: e.reduce_sum(asm[:, 2:3], atj[:, :], AX.X), ("atj",), ("asm2",))
                        ts("dve", asm[:, 3:4], asm[:, 2:3], 1.0 / 128.0, ALU.mult, ("asm2",), ("asm3",),
                           s2=EPS, op1=ALU.add)
                        act(asm[:, 4:5], asm[:, 3:4], AF.Ln, ("asm3",), ("asm4",))
                        act(asm[:, 5:6], asm[:, 4:5], AF.Exp, ("asm4",), ("asm5",), scale=-0.5)
                        ts("dve", hab[:, :], at2[:, :], asm[:, 5:6], ALU.mult, ("at2", "asm5"), ("hab",),
                           s2=1.0 - lam_init, op1=ALU.mult)
                        tr(psb(pb)[:, 768:896], hab[:, :], identb[:, :], ("hab", "identb"), (pk,))
                        c0 = qoff + qt * 128
                        stt(yab[:, c0:c0 + 128], psb(pb)[:, 768:896], V[:, 20:21], ga[:, c0:c0 + 128], ALU.mult,
                            ALU.mult, (pk, vk, gak), ("yab",))
                yield
            res.append(dma("sp", ycl_ap, yab[:, :], ("yab",), (okey,)))

        out_toks = []

        def frontA(l, tb):
            t0 = tb * TB
            if l == 0:
                if tb == 0:
                    dma("sp", xblk[:, :, :], xT[:, t0:t0 + TB].rearrange("(kt p) t -> p kt t", p=128), (), XB)
            else:
                yield from outproj_block(l - 1, tb, xT, ("xsrc0", tb), load=(tb == 0))
                tok = dma("sp", x1s[:, t0:t0 + TB].rearrange("(kt p) t -> p kt t", p=128), xblk[:, :, :],
                          XB, (("xsrc1", tb),))
                if not do_fin:
                    out_toks.append(tok)
            yield from norm_block(vec[l][:, 0:8], False)
            if tb + 1 < _NB:
                t1 = (tb + 1) * TB
                if l == 0:
                    dma("sp", xblk[:, :, :], xT[:, t1:t1 + TB].rearrange("(kt p) t -> p kt t", p=128), (), XB)
                else:
                    outproj_loads(l - 1, tb + 1, xT, ("xsrc0", tb + 1))
            if _STOP >= 2:
                yield from inproj_block(l, tb)

        def frontB(l, tb):
            q4, tq = tb // 4, (tb % 4) * TB
            if _STOP >= 3:
                yield from gates_block(l, tb)
            res = []
            if _STOP >= 4:
                yield from mlstm_block(l, tb, ycl[l][q4][0:128, tq:tq + TB], ("ycl", l, q4, tb % 4, 0), res)
            if not fused:
                out_toks.extend(res)

        def drain(g):
            for _ in g:
                pass

        def adv(g, n):
            for _ in range(n):
                try:
                    next(g)
                except StopIteration:
                    return False
            return True

        NBU, NAU = float(os.environ.get("K_NBU", "20")), float(os.environ.get("K_NAU", "16"))

        load_weights(layers[0]) if layers else None
        if layers and layers[0] > 0:
            load_wout(layers[0] - 1)
        elif len(layers) > 1:
            load_wout(layers[0])
        for li, l in enumerate(layers):
            load_lam(l)
            drain(frontA(l, 0))
            for tb in range(_NB):
                q4, tq = tb // 4, (tb % 4) * TB
                res = []
                ag = (attn_block(l, tb, ycl[l][q4][128:256, tq:tq + TB], ("ycl", l, q4, tb % 4, 1), res)
                      if _STOP >= 5 else iter(()))
                bg = frontB(l, tb)
                fg = frontA(l, tb + 1) if tb + 1 < _NB else iter(())
                if tb == _NB - 1:
                    if li + 1 < len(layers):
                        load_weights(layers[li + 1])
                        if li + 1 >= 2:
                            load_wout(layers[li + 1] - 1)
                    elif do_fin and l > layers[0]:
                        load_wout(DEPTH - 1)
                ntile = 8 * tb + 6
                accb = acca = 0.0
                bl = fl = True
                for _ in ag:
                    accb += NBU / ntile
                    acca += NAU / ntile
                    nb_, na_ = int(accb), int(acca)
                    accb -= nb_
                    acca -= na_
                    while nb_ > 0 or na_ > 0:
                        if nb_ > 0:
                            bl = bl and adv(bg, 1)
                            nb_ -= 1
                        if na_ > 0:
                            fl = fl and adv(fg, 1)
                            na_ -= 1
                while bl or fl:
                    if bl:
                        bl = adv(bg, 1)
                    if fl:
                        fl = adv(fg, 1)
                if not fused:
                    out_toks.extend(res)
                if fused and tb % 4 == 3:
                    rk = tuple(("ycl", l, q4, i, w) for i in range(4) for w in range(2))
                    I("pool", lambda e, l=l, q4=q4: e.collective_compute(
                        "AllGather", ALU.bypass, replica_groups=GROUPS,
                        ins=[ycl[l][q4][:, :]], outs=[ycf[l][q4][:, :]]), rk, (("ycf", l, q4),), cc=True)

        if do_fin:
            if not (layers and layers[-1] > layers[0]):
                load_wout(DEPTH - 1)
            lf = DEPTH - 1
            nw_ap = vec[lf][:, 23:31]
            xb = [xblk[:, :, :], kaT[:, :].bitcast(F32).rearrange("p (kt t) -> p kt t", kt=KT)]
            yb = [sqb[:, :, :], va[:, :, :].rearrange("p a b -> p (a b)")[:, 0:KT * TB].rearrange("p (kt t) -> p kt t", kt=KT)]
            xk = [XB, tuple(("ka", i) for i in range(NBLK))]
            yk = [SQ, tuple(("va", i) for i in range(SEQ // 128)) + ("va_ones",)]

            def fin_load(tb):
                p = tb % 2
                t0 = tb * TB
                q4, tq = tb // 4, (tb % 4) * TB
                dma("sp", yb[p], ycf[lf][q4][:, tq:tq + TB].rearrange("(kt p) t -> p kt t", p=128),
                    (("ycf", lf, q4),), yk[p])
                dma("sp", xb[p], x1s[:, t0:t0 + TB].rearrange("(kt p) t -> p kt t", p=128),
                    (("xsrc1", tb),), xk[p])

            fin_load(0)
            for tb in range(_NB):
                p = tb % 2
                t0 = tb * TB
                if tb + 1 < _NB:
                    fin_load(tb + 1)
                X, Y = xb[p], yb[p]
                for m in range(KT):
                    bank = m % 2
                    for et in range(KT):
                        mm(ps[bank][:, :], wout_b[:, et, m * 128:(m + 1) * 128], Y[:, et, :], et == 0, et == KT - 1,
                           ("wout",) + yk[p], (("ps", bank),))
                    tt("dve", X[:, m, :], X[:, m, :], ps[bank][:, :], ALU.add, (("ps", bank),) + xk[p], xk[p])
                    if m % 2 == 0:
                        tt("pool", xnb[:, m, :], X[:, m, :], X[:, m, :], ALU.mult, xk[p], (("xnb", m),))
                    else:
                        act(xnb[:, m, :], X[:, m, :], AF.Square, xk[p], (("xnb", m),))
                for kt in range(KT):
                    mm(ps[2][:, :], onesb[:, :], xnb[:, kt, :], kt == 0, kt == KT - 1, ("onesb", ("xnb", kt)), (("ps", 2),))
                ts("dve", rstd[:, :], ps[2][:, :], 1.0 / D_MODEL, ALU.mult, (("ps", 2),), ("rstd",), s2=EPS, op1=ALU.add)
                act(rstd[:, :], rstd[:, :], AF.Ln, ("rstd",), ("rstd",))
                act(rstd[:, :], rstd[:, :], AF.Exp, ("rstd",), ("rstd",), scale=-0.5)
                for kt in range(KT):
                    stt(X[:, kt, :], X[:, kt, :], nw_ap[:, kt:kt + 1], rstd[:, :], ALU.mult, ALU.mult,
                        xk[p] + ("rstd", ("vec", lf)), xk[p])
                out_toks.append(dma("sp", outT[:, t0:t0 + TB].rearrange("(kt p) t -> p kt t", p=128), X, xk[p], ()))

        waits = []
        for tok in out_toks:
            S._tok_wait("sp", tok, waits)
        S.recs["sp"].append((waits, None, None))

        with nc.Block() as block:
            S.emit(block)
    return nc


_OFF = {"mq": 0, "mk": 512, "mv": 1024, "mi": 1536, "mf": 1540, "mz": 1544,
        "aq": 2056, "ak": 2568, "av": 3080, "az": 3592}


def _rope_tables():
    inv = (1.0 / (np.float32(10000.0) ** (np.arange(0, 64, 2, dtype=np.float32) / np.float32(64.0)))).astype(np.float32)
    ang = np.arange(SEQ, dtype=np.float32)[:, None] * inv[None, :]
    cos = np.cos(ang).astype(np.float32)
    sin = np.sin(ang).astype(np.float32)
    cosT = np.ascontiguousarray(np.concatenate([cos, cos, cos, cos], 1).T)
    sinT = np.ascontiguousarray(np.concatenate([sin, sin, sin, sin], 1).T)
    return cosT, sinT


def _consts():
    c = np.zeros((128, 192), np.float32)
    c[:, 0:128] = np.eye(128, dtype=np.float32)
    c[0:64, 128:192] = np.triu(np.ones((64, 64), np.float32))
    return c


def _core_inputs(inp, c, stage_layers, need_wout):
    b, hd = c // 4, c % 4
    f32 = np.float32
    d = {}
    for l in stage_layers:
        w = np.asarray(inp["w_in"][l], f32)
        hs = slice(hd * 128, (hd + 1) * 128)

        def blk(name):
            return w[:, _OFF[name] + hd * 128:_OFF[name] + (hd + 1) * 128]

        def perm(m):
            return np.concatenate([m[:, 32:64], m[:, 0:32], m[:, 96:128], m[:, 64:96]], 1)

        aq, ak = blk("aq"), blk("ak")
        gi = w[:, _OFF["mi"] + hd:_OFF["mi"] + hd + 1]
        gf = w[:, _OFF["mf"] + hd:_OFF["mf"] + hd + 1]
        d[f"wfm{l}"] = np.ascontiguousarray(np.concatenate(
            [blk("mq"), blk("mk"), blk("mz"), aq, perm(aq), ak, perm(ak), blk("az"), gi, gf], 1))
        d[f"wtm{l}"] = np.ascontiguousarray(np.concatenate([blk("mv"), blk("av")], 1))
        lam = np.concatenate([np.asarray(inp[k][l], f32) for k in ("lam_q1", "lam_k1", "lam_q2", "lam_k2")])
        d[f"lamv{l}"] = np.ascontiguousarray(np.tile(lam[None, :], (128, 1)))
    for l in range(DEPTH):
        v = np.zeros((128, NV), f32)
        v[:, 0:8] = np.asarray(inp["norm_w"][l], f32).reshape(8, 128).T
        cw = np.asarray(inp["conv_w"][l], f32)
        cbv = np.asarray(inp["conv_b"][l], f32)
        v[:, 8:12] = cw[:, hd * 128:(hd + 1) * 128].T
        v[:, 12:16] = cw[:, 512 + hd * 128:512 + (hd + 1) * 128].T
        v[:, 16] = cbv[hd * 128:(hd + 1) * 128]
        v[:, 17] = cbv[512 + hd * 128:512 + (hd + 1) * 128]
        v[:, 18] = np.asarray(inp["m_norm_w"][l], f32)[hd * 128:(hd + 1) * 128]
        v[:, 19] = np.asarray(inp["m_skip"][l], f32)[hd * 128:(hd + 1) * 128]
        v[:, 20] = np.asarray(inp["a_norm_w"][l], f32)
        v[:, 21] = np.asarray(inp["i_bias"][l], f32)[hd]
        v[:, 22] = np.asarray(inp["f_bias"][l], f32)[hd]
        v[:, 23:31] = np.asarray(inp["final_norm_w"], f32).reshape(8, 128).T
        d[f"vecs{l}"] = v
    for l in need_wout:
        wo = np.asarray(inp["w_out"][l], f32)
        rows = []
        for r in range(4):
            rows.append(wo[r * 128:(r + 1) * 128])
            rows.append(wo[512 + r * 128:512 + (r + 1) * 128])
        d[f"wout{l}"] = np.ascontiguousarray(np.concatenate(rows, 0))
    return d


_PROG = {}


def _prog(stage, debug=False):
    key = (stage, debug)
    if key not in _PROG:
        _PROG[key] = build_program(stage, debug)
    return _PROG[key]


def kernel(**inp):
    x = np.asarray(inp["x"], np.float32)
    xTs = [np.ascontiguousarray(x[b].T) for b in range(BATCH)]
    cosT, sinT = _rope_tables()
    cst = _consts()
    nc = _prog("all")
    in_maps = []
    for c in range(NCORES):
        d = _core_inputs(inp, c, [0, 1], [0, 1])
        d.update({"xT": xTs[c // 4], "cosT": cosT, "sinT": sinT, "consts": cst})
        in_maps.append(d)
    res = run_bass_kernel_spmd(nc, in_maps, core_ids=list(range(NCORES)))
    out = np.empty((BATCH, SEQ, D_MODEL), np.float32)
    for b in range(BATCH):
        out[b] = res.results[4 * b]["outT"].T
    return out
```

```python
import math
from contextlib import ExitStack

import numpy as np
import ml_dtypes

import concourse.bass as bass
import concourse.mybir as mybir
from concourse.bass_utils import run_bass_kernel_spmd

F32 = mybir.dt.float32
BF16 = mybir.dt.bfloat16
AF = mybir.ActivationFunctionType
ALU = mybir.AluOpType
AX = mybir.AxisListType

D_MODEL = 1024
BATCH = 2
SEQ = 8192
DEPTH = 2
NCORES = 8
TB = 512
NBLK = SEQ // TB
KT = D_MODEL // 128
EPS = 1e-6
NV = 32
LN_KSCALE = math.log(128.0 ** -0.5)
GROUPS = [[0, 1, 2, 3], [4, 5, 6, 7]]
import os
_STOP = int(os.environ.get("K_STOP", "9"))
_NB = int(os.environ.get("K_NBLK", str(NBLK)))


class Sched:
    CH = 30000
    ND = 48

    def __init__(self, nc, es):
        self.nc = nc
        self.engs = ["pe", "act", "dve", "pool", "sp"]
        self.recs = {e: [] for e in self.engs}
        self.cnt = {e: 0 for e in self.engs}
        self.seen = {e: {} for e in self.engs}
        self.lastw = {}
        self.readers = {}
        nsem = {"pe": 3, "act": 3, "dve": 4, "pool": 3, "sp": 1}
        self.esems = {e: [es.enter_context(nc.semaphore(f"s_{e}_{i}")) for i in range(nsem[e])]
                      for e in self.engs}
        self.dsems = [es.enter_context(nc.semaphore(f"d_{i}")) for i in range(self.ND)]
        self.dval = [0] * self.ND
        self.dnext = 0
        self.ccsem = es.enter_context(nc.semaphore("ccsem"))
        self.ccval = 0

    def _tok_wait(self, e, tok, waits):
        if tok[0] == "e":
            _, e2, n = tok
            if e2 == e and e == "pe":
                return
            if self.seen[e].get(e2, 0) >= n:
                return
            self.seen[e][e2] = n
            waits.append((self.esems[e2][(n - 1) // self.CH], (n - 1) % self.CH + 1))
        else:
            kind, i, v = tok
            key = (kind, i)
            if self.seen[e].get(key, 0) >= v:
                return
            self.seen[e][key] = v
            sem = self.dsems[i] if kind == "d" else self.ccsem
            waits.append((sem, v))

    def issue(self, e, fn, reads=(), writes=(), dma=False, cc=False):
        deps = []
        for k in reads:
            if k in self.lastw:
                deps.append(self.lastw[k])
        for k in writes:
            if k in self.lastw:
                deps.append(self.lastw[k])
            deps.extend(self.readers.get(k, {}).values())
        waits = []
        for tok in deps:
            self._tok_wait(e, tok, waits)
        if dma:
            i = self.dnext
            self.dnext = (self.dnext + 1) % self.ND
            prev = self.dval[i]
            if prev > 0:
                self._tok_wait(e, ("d", i, prev), waits)
            self.dval[i] += 16
            tok = ("d", i, self.dval[i])
            inc = (self.dsems[i], 16)
        elif cc:
            self.ccval += 1
            tok = ("c", 0, self.ccval)
            inc = (self.ccsem, 1)
        elif fn is None:
            tok = None
            inc = None
        else:
            self.cnt[e] += 1
            n = self.cnt[e]
            tok = ("e", e, n)
            inc = (self.esems[e][(n - 1) // self.CH], 1)
        self.recs[e].append((waits, fn, inc))
        if tok is not None:
            for k in reads:
                self.readers.setdefault(k, {})[(tok[0], tok[1])] = tok
            for k in writes:
                self.lastw[k] = tok
                self.readers[k] = {}
        return tok

    def emit(self, block):
        nc = self.nc

        def run(e, eng):
            for waits, fn, inc in self.recs[e]:
                for s, v in waits:
                    eng.wait_ge(s, v)
                if fn is not None:
                    ins = fn(eng)
                    ins.then_inc(inc[0], inc[1])

        @block.tensor
        def _(eng):
            run("pe", eng)

        @block.scalar
        def _(eng):
            run("act", eng)

        @block.vector
        def _(eng):
            run("dve", eng)

        @block.gpsimd
        def _(eng):
            run("pool", eng)

        @block.sync
        def _(eng):
            run("sp", eng)


def build_program(stage="all", debug=False):
    nc = bass.Bass("TRN2", target_bir_lowering=False)
    with ExitStack() as es:
        S = Sched(nc, es)

        def dram(name, shape, dt, kind):
            return nc.dram_tensor(name, shape, dt, kind=kind).ap()

        fused = stage == "all"
        layers = {"all": [0, 1], "l0": [0], "mid": [1], "fin": []}[stage]
        do_fin = stage in ("all", "fin")

        xT = dram("xT", [D_MODEL, SEQ], F32, "ExternalInput") if stage in ("all", "l0", "mid") else None
        cosT = dram("cosT", [128, SEQ], F32, "ExternalInput") if layers else None
        sinT = dram("sinT", [128, SEQ], F32, "ExternalInput") if layers else None
        consts = dram("consts", [128, 192], F32, "ExternalInput")
        wfm, wtm, wout, vecs, lamv = {}, {}, {}, {}, {}
        for l in layers:
            wfm[l] = dram(f"wfm{l}", [D_MODEL, 1026], F32, "ExternalInput")
            wtm[l] = dram(f"wtm{l}", [D_MODEL, 256], F32, "ExternalInput")
            lamv[l] = dram(f"lamv{l}", [128, 256], F32, "ExternalInput")
        for l in range(DEPTH):
            vecs[l] = dram(f"vecs{l}", [128, NV], F32, "ExternalInput")
        need_wout = {"all": [0, 1], "l0": [], "mid": [0], "fin": [1]}[stage]
        for l in need_wout:
            wout[l] = dram(f"wout{l}", [D_MODEL, D_MODEL], F32, "ExternalInput")

        ycl, ycf = {}, {}
        for l in layers:
            kind = "Internal" if fused else "ExternalOutput"
            ycl[l] = [dram(f"ycl{l}_{q}", [256, 2048], BF16, kind) for q in range(4)]
        for l in need_wout:
            kind = "Internal" if fused else "ExternalInput"
            ycf[l] = [dram(f"ycf{l}_{q}", [1024, 2048], BF16, kind) for q in range(4)]
        x1s = None
        if stage == "all":
            x1s = dram("x1s", [D_MODEL, SEQ], F32, "Internal")
        elif stage == "mid":
            x1s = dram("x1s", [D_MODEL, SEQ], F32, "ExternalOutput")
        elif stage == "fin":
            x1s = dram("x1s", [D_MODEL, SEQ], F32, "ExternalInput")
        outT = dram("outT", [D_MODEL, SEQ], F32, "ExternalOutput") if do_fin else None
        dbg = {}
        if debug:
            for nm, shp, dt in [("d_qm", [128, SEQ], BF16), ("d_km", [128, SEQ], BF16),
                                ("d_qa", [128, SEQ], BF16), ("d_ka", [128, SEQ], BF16),
                                ("d_rows", [1, 9 * SEQ], F32)]:
                dbg[nm] = dram(nm, shp, dt, "ExternalOutput")

        def sb(name, shape, dt):
            return es.enter_context(nc.sbuf_tensor(name, shape, dt))

        cst = sb("cst", [128, 192], F32)
        ident_f = cst[:, 0:128]
        identb = sb("identb", [128, 128], BF16)
        onesb = sb("onesb", [128, 128], BF16)
        onesf = sb("onesf", [1, 512], F32)
        vec = [sb(f"vec{l}", [128, NV], F32) for l in range(DEPTH)]
        lamt = sb("lamt", [128, 256], F32)
        lamw = sb("lamw", [128, 4], F32)
        neglam = sb("neglam", [128, 1], F32)
        negfb = sb("negfb", [1, 1], F32)

        wfm_b = sb("wfm_b", [128, KT, 1026], BF16)
        wtm_b = sb("wtm_b", [128, KT, 256], BF16)
        wout_b = sb("wout_b", [128, KT, D_MODEL], BF16)
        wstg = [sb(f"wstg{i}", [128, KT, 128], F32) for i in range(2)]

        xblk = sb("xblk", [128, KT, TB], F32)
        sqb = sb("sqb", [128, KT, TB], BF16)
        xnb = sb("xnb", [128, KT, TB], BF16)
        ycb = sqb
        rstd = sb("rstd", [128, TB], F32)
        cosb = sb("cosb", [128, TB], F32)
        sinb = sb("sinb", [128, TB], F32)
        preq = sb("preq", [128, TB + 4], F32)
        prek = sb("prek", [128, TB + 4], F32)
        cva = sb("tA", [128, TB], F32)
        cvb = sb("tB", [128, TB], F32)
        tC = sb("tC", [128, TB], F32)
        rpa, rpb = cva, cvb
        qa2 = [sb(f"qa{i}", [128, TB], BF16) for i in range(2)]
        qm2 = [sb(f"qm{i}", [128, TB], BF16) for i in range(2)]
        km2 = [sb(f"km{i}", [128, TB], BF16) for i in range(2)]
        qs = sb("qs", [128, TB], BF16)
        gm2 = [sb(f"gm{i}", [128, TB], F32) for i in range(2)]
        gtA = sb("gtA", [128, TB], F32)
        gtB = sb("gtB", [128, TB], F32)
        ga2 = [sb(f"ga{i}", [128, TB], F32) for i in range(2)]
        kaT = sb("kaT", [128, SEQ], BF16)
        va = sb("va", [128, SEQ // 128, 132], BF16)
        vmc2 = [sb(f"vmc{i}", [64, 8, 132], BF16) for i in range(2)]
        vmw = sb("vmw", [64, 8, 132], BF16)
        ktok = sb("ktok", [64, 8, 128], BF16)
        NR = 9
        rows = sb("rows", [1, NR, TB], F32)
        gif2 = [sb(f"gif{i}", [1, 2, TB], F32) for i in range(2)]
        carry = sb("carry", [1, 4], F32)
        ape = sb("ape", [1, 8], F32)
        dec = sb("dec", [1, 8], F32)
        cols = sb("cols", [64, 24], F32)
        decb = sb("decb", [128, 8], F32)
        wT = sb("wT", [64, 8, 64], F32)
        stl = [sb(f"stl{i}", [64, 64], BF16) for i in range(2)]
        cf = sb("cf", [128, 132], F32)
        cb = sb("cb", [128, 132], BF16)
        nums = sb("nums", [64, 8, 132], F32)
        sq8 = sb("sq8", [64, 8, 128], F32)
        sm = sb("sm", [64, 8, 8], F32)
        hmb = sb("hmb", [64, 8, 128], BF16)
        gt1, gt2 = gtA, gtB
        yo = sb("yo", [128, TB], BF16)
        pT = [sb(f"pT{i}", [128, 512], BF16) for i in range(2)]
        at1 = sb("at1", [128, 128], F32)
        at2 = sb("at2", [128, 128], F32)
        atj = sb("atj", [128, 128], F32)
        asm = sb("asm", [128, 8], F32)
        hab = sb("hab", [128, 128], BF16)
        yab = sb("yab", [128, TB], BF16)
        ob = xblk

        ps_all = es.enter_context(nc.psum_tensor("ps_all", [128, 8, 512], F32))
        ps = [ps_all[:, i, :] for i in range(8)]

        def I(e, fn, r=(), w=(), **kw):
            return S.issue(e, fn, r, w, **kw)

        def dma(q, out, in_, r, w):
            return I(q, lambda e: e.dma_start(out=out, in_=in_), r, w, dma=True)

        def act(out, in_, func, r, w, scale=1.0, bias=0.0, accum=None):
            if accum is None:
                return I("act", lambda e: e.activation(out, in_, func, bias=bias, scale=scale), r, w)
            return I("act", lambda e: e.activation(out, in_, func, bias=bias, scale=scale, accum_out=accum), r, w)

        def tt(eng, out, a, b, op, r, w):
            return I(eng, lambda e: e.tensor_tensor(out, a, b, op), r, w)

        def ts(eng, out, a, s1, op0, r, w, s2=None, op1=ALU.bypass):
            return I(eng, lambda e: e.tensor_scalar(out, a, s1, s2, op0, op1), r, w)

        def stt(out, a, sc, b, op0, op1, r, w):
            return I("dve", lambda e: e.scalar_tensor_tensor(out, a, sc, b, op0, op1), r, w)

        def cp(eng, out, in_, r, w):
            if eng == "act":
                return I(eng, lambda e: e.copy(out, in_), r, w)
            return I(eng, lambda e: e.tensor_copy(out, in_), r, w)

        def mm(out, lhsT, rhs, start, stop, r, w):
            return I("pe", lambda e: e.matmul(out, lhsT, rhs, start=start, stop=stop), r, w)

        def tr(out, in_, idn, r, w):
            return I("pe", lambda e: e.transpose(out, in_, idn), r, w)

        def psb(i):
            return ps[i].bitcast(BF16)

        XB = tuple(("xblk", k) for k in range(KT))
        SQ = tuple(("sqb", k) for k in range(KT))
        XN = tuple(("xnb", k) for k in range(KT))

        dma("sp", cst[:, :], consts[:, :], (), ("cst",))
        for l in range(DEPTH):
            dma("sp", vec[l][:, :], vecs[l][:, :], (), (("vec", l),))
        cp("dve", identb[:, :], cst[:, 0:128], ("cst",), ("identb",))
        I("pool", lambda e: e.memset(onesb[:, :], 1.0), (), ("onesb",))
        I("pool", lambda e: e.memset(onesf[:, :], 1.0), (), ("onesf",))
        I("pool", lambda e: e.memset(va[:, :, 128:132], 1.0), (), ("va_ones",))
        for i in range(2):
            I("pool", lambda e, i=i: e.memset(vmc2[i][:, :, 128:132], 1.0), (), ("vmc_ones",))
        mask64 = cst[0:64, 128:192]

        def load_weights(l):
            nchunk = 11
            for ci in range(nchunk):
                st = wstg[ci % 2]
                sk = ("wstg", ci % 2)
                if ci < 8:
                    src = wfm[l][:, ci * 128:(ci + 1) * 128]
                    dst = wfm_b[:, :, ci * 128:(ci + 1) * 128]
                    ncol = 128
                elif ci == 8:
                    src = wfm[l][:, 1024:1026]
                    dst = wfm_b[:, :, 1024:1026]
                    ncol = 2
                else:
                    src = wtm[l][:, (ci - 9) * 128:(ci - 8) * 128]
                    dst = wtm_b[:, :, (ci - 9) * 128:(ci - 8) * 128]
                    ncol = 128
                dma("sp", st[:, :, 0:ncol], src.rearrange("(kt p) c -> p kt c", p=128), (), (sk,))
                eng = "pool" if ci % 2 == 0 else "dve"
                cp(eng, dst, st[:, :, 0:ncol], (sk,), ("win",))
                if ci in (4, 6):
                    for m in range(2):
                        sl = wfm_b[:, :, ci * 128 + m * 64: ci * 128 + m * 64 + 32]
                        ts("pool", sl, sl, -1.0, ALU.mult, ("win",), ("win",))
        def load_lam(l):
            dma("sp", lamt[:, :], lamv[l][:, :], (), ("lamt",))
            for i in range(2):
                tt("dve", lamt[:, i * 128:i * 128 + 64], lamt[:, i * 128:i * 128 + 64],
                   lamt[:, i * 128 + 64:i * 128 + 128], ALU.mult, ("lamt",), ("lamt",))
                I("dve", lambda e, i=i: e.reduce_sum(lamw[:, i:i + 1], lamt[:, i * 128:i * 128 + 64], AX.X),
                  ("lamt",), ("lamw",))
            act(lamw[:, 2:4], lamw[:, 0:2], AF.Exp, ("lamw",), ("lamw",))
            lam_init = 0.8 - 0.6 * math.exp(-0.3 * l)
            stt(neglam[:, :], lamw[:, 3:4], -lam_init, lamw[:, 2:3], ALU.add, ALU.subtract, ("lamw",), ("neglam",))
            ts("pool", negfb[:, :], vec[l][0:1, 22:23], -1.0, ALU.mult, (("vec", l),), ("negfb",))

        def load_wout(l):
            for ci in range(8):
                st = wstg[ci % 2]
                sk = ("wstg", ci % 2)
                dma("sp", st[:, :, :], wout[l][:, ci * 128:(ci + 1) * 128].rearrange("(kt p) c -> p kt c", p=128),
                    (), (sk,))
                eng = "pool" if ci % 2 == 0 else "dve"
                cp(eng, wout_b[:, :, ci * 128:(ci + 1) * 128], st[:, :, :], (sk,), ("wout",))

        def outproj_loads(l_prev, tb, xsrc, xkey):
            t0 = tb * TB
            q4, tq = tb // 4, (tb % 4) * TB
            dma("sp", ycb[:, :, :], ycf[l_prev][q4][:, tq:tq + TB].rearrange("(kt p) t -> p kt t", p=128),
                (("ycf", l_prev, q4),), SQ)
            dma("sp", xblk[:, :, :], xsrc[:, t0:t0 + TB].rearrange("(kt p) t -> p kt t", p=128),
                (xkey,), XB)

        def outproj_block(l_prev, tb, xsrc, xkey, banks=(0,), load=True):
            if load:
                outproj_loads(l_prev, tb, xsrc, xkey)
            for m in range(KT):
                bank = banks[m % len(banks)]
                for et in range(KT):
                    mm(ps[bank][:, :], wout_b[:, et, m * 128:(m + 1) * 128], ycb[:, et, :], et == 0, et == KT - 1,
                       ("wout", ("sqb", et)), (("ps", bank),))
                tt("dve", xblk[:, m, :], xblk[:, m, :], ps[bank][:, :], ALU.add, (("ps", bank), ("xblk", m)),
                   (("xblk", m),))
                yield

        def norm_block(nw_ap, to_out):
            for kt in range(KT):
                tt("pool" if kt % 2 == 0 else "dve", sqb[:, kt, :], xblk[:, kt, :], xblk[:, kt, :], ALU.mult,
                   (("xblk", kt),), (("sqb", kt),))
            for kt in range(KT):
                mm(ps[0][:, :], onesb[:, :], sqb[:, kt, :], kt == 0, kt == KT - 1, ("onesb", ("sqb", kt)), (("ps", 0),))
            yield
            ts("dve", rstd[:, :], ps[0][:, :], 1.0 / D_MODEL, ALU.mult, (("ps", 0),), ("rstd",), s2=EPS, op1=ALU.add)
            act(rstd[:, :], rstd[:, :], AF.Ln, ("rstd",), ("rstd",))
            act(rstd[:, :], rstd[:, :], AF.Exp, ("rstd",), ("rstd",), scale=-0.5)
            for kt in range(KT):
                if to_out:
                    stt(ob[:, kt, :], xblk[:, kt, :], nw_ap[:, kt:kt + 1], rstd[:, :], ALU.mult, ALU.mult,
                        (("xblk", kt), "rstd"), (("xblk", kt),))
                else:
                    stt(xnb[:, kt, :], xblk[:, kt, :], nw_ap[:, kt:kt + 1], rstd[:, :], ALU.mult, ALU.mult,
                        (("xblk", kt), "rstd"), (("xnb", kt),))
            yield

        def inproj_block(l, tb):
            t0 = tb * TB
            pp = tb % 2
            qa, ga = qa2[pp], ga2[pp]
            qak, gak = ("qa", pp), ("ga", pp)
            qm, km, gm, vmc, gif = qm2[pp], km2[pp], gm2[pp], vmc2[pp], gif2[pp]
            V = vec[l]
            vk = ("vec", l)
            dma("sp", cosb[:, :], cosT[:, t0:t0 + TB], (), ("cosb",))
            dma("sp", sinb[:, :], sinT[:, t0:t0 + TB], (), ("sinb",))

            def fm(c0, ncol):
                def f(b):
                    for kt in range(KT):
                        mm(ps[b][0:ncol, :], wfm_b[:, kt, c0:c0 + ncol], xnb[:, kt, :], kt == 0, kt == KT - 1,
                           ("win", ("xnb", kt)), (("ps", b),))
                return f

            def conv_ev(which):
                pre, dst, cw0, cbc, fin, fk = [(preq, qm, 8, 16, tC, "tC"), (prek, km, 12, 17, cvb, "tB")][which]
                pk = ("pre", which)

                def e1(b):
                    if tb == 0:
                        I("pool", lambda e: e.memset(pre[:, 0:4], 0.0), (), (pk,))
                    else:
                        cp("pool", pre[:, 1:4], pre[:, TB + 1:TB + 4], (pk,), (pk,))
                    cp("dve", pre[:, 4:4 + TB], ps[b][:, :], (("ps", b), pk), (pk,))
                    ts("dve", cva[:, :], pre[:, 4:4 + TB], V[:, cw0 + 3:cw0 + 4], ALU.mult, (pk, vk), ("tA",),
                       s2=V[:, cbc:cbc + 1], op1=ALU.add)
                    stt(cvb[:, :], pre[:, 3:3 + TB], V[:, cw0 + 2:cw0 + 3], cva[:, :], ALU.mult, ALU.add,
                        (pk, vk, "tA"), ("tB",))
                    stt(cva[:, :], pre[:, 2:2 + TB], V[:, cw0 + 1:cw0 + 2], cvb[:, :], ALU.mult, ALU.add,
                        (pk, vk, "tB"), ("tA",))
                    stt(fin[:, :], pre[:, 1:1 + TB], V[:, cw0:cw0 + 1], cva[:, :], ALU.mult, ALU.add,
                        (pk, vk, "tA"), (fk,))

                def e2(b):
                    act(dst[:, :], fin[:, :], AF.Silu, (fk,), (("qm", pp) if which == 0 else ("km", pp),))
                return [e1, e2]

            def silu_ev(dst, dk):
                return [lambda b: act(dst[:, :], ps[b][:, :], AF.Silu, (("ps", b),), (dk,))]

            def rope_a(b):
                tt("dve", rpa[:, :], ps[b][:, :], cosb[:, :], ALU.mult, (("ps", b), "cosb"), ("tA",))

            def rope_b(which):
                def f(b):
                    tt("dve", rpb[:, :], ps[b][:, :], sinb[:, :], ALU.mult, (("ps", b), "sinb"), ("tB",))
                    if which == 0:
                        tt("pool", qa[:, :], rpa[:, :], rpb[:, :], ALU.add, ("tA", "tB"), (qak,))
                    else:
                        tt("pool", kaT[:, t0:t0 + TB], rpa[:, :], rpb[:, :], ALU.add, ("tA", "tB"), (("ka", tb),))
                return f

            def row_ev(g):
                return [lambda b: cp("dve", gif[:, g, :], ps[b][0:1, :], (("ps", b),), (("gif", pp),))]

            def vm_mm(h):
                def f(b):
                    for c4 in range(4):
                        c8 = h * 4 + c4
                        for kt in range(KT):
                            mm(ps[b][0:64, c4 * 128:(c4 + 1) * 128], xnb[:, kt, c8 * 64:(c8 + 1) * 64],
                               wtm_b[:, kt, 0:128], kt == 0, kt == KT - 1, ("win", ("xnb", kt)), (("ps", b),))
                return f

            def vm_ev(h):
                return [lambda b: cp("dve", vmc[:, h * 4:(h + 1) * 4, 0:128],
                                     ps[b][0:64, :].rearrange("p (c d) -> p c d", d=128), (("ps", b),), (("vmc", pp),))]

            def va_mm(b):
                for t4 in range(4):
                    for kt in range(KT):
                        mm(ps[b][:, t4 * 128:(t4 + 1) * 128], xnb[:, kt, t4 * 128:(t4 + 1) * 128],
                           wtm_b[:, kt, 128:256], kt == 0, kt == KT - 1, ("win", ("xnb", kt)), (("ps", b),))

            def va_ev(b):
                cp("dve", va[:, tb * 4:(tb + 1) * 4, 0:128], ps[b][:, :].rearrange("p (c d) -> p c d", d=128),
                   (("ps", b), "va_ones"), tuple(("va", tb * 4 + i) for i in range(4)))

            stages = [
                (fm(0, 128), conv_ev(0)),
                (fm(128, 128), conv_ev(1)),
                (fm(256, 128), silu_ev(gm, ("gm", pp))),
                (fm(3 * 128, 128), [rope_a]),
                (fm(4 * 128, 128), [rope_b(0)]),
                (fm(5 * 128, 128), [rope_a]),
                (fm(6 * 128, 128), [rope_b(1)]),
                (fm(7 * 128, 128), silu_ev(ga, gak)),
                (fm(1024, 1), row_ev(0)),
                (fm(1025, 1), row_ev(1)),
                (vm_mm(0), vm_ev(0)),
                (vm_mm(1), vm_ev(1)),
                (va_mm, [va_ev]),
            ]
            pending = []
            for k, (mmf, evs) in enumerate(stages):
                b = 0
                mmf(b)
                for i, fn in enumerate(evs):
                    pending.append((k + i, fn, b))
                for due, fn, bb in [p for p in pending if p[0] <= k]:
                    fn(bb)
                pending = [p for p in pending if p[0] > k]
                yield
            for due, fn, bb in sorted(pending, key=lambda p: p[0]):
                fn(bb)
            yield

        R_I, R_F, R_L1, R_BN, R_A_, R_AA, R_WI, R_WK, R_EM = range(9)
        R_T2, R_T1, R_ALS = R_I, R_F, R_L1

        def rw(i):
            return rows[:, i, :]

        def gates_block(l, tb):
            V = vec[l]
            vk = ("vec", l)
            rk = lambda i: ("row", i)
            pp = tb % 2
            qm, vmc, gif = qm2[pp], vmc2[pp], gif2[pp]
            gk = ("gif", pp)
            if tb == 0:
                I("pool", lambda e: e.memset(carry[:, :], 0.0), (), ("carry",))
            act(rw(R_T1), gif[:, 1, :], AF.Exp, (gk, "negfb"), (rk(R_T1),), scale=-1.0, bias=negfb[:, :])
            act(rw(R_L1), rw(R_T1), AF.Ln, (rk(R_T1),), (rk(R_L1),), bias=1.0)
            I("dve", lambda e: e.tensor_tensor_scan(rw(R_BN), onesf[:, :], rw(R_L1), carry[:, 0:1], ALU.mult, ALU.add),
              ("onesf", rk(R_L1), "carry"), (rk(R_BN),))
            stt(rw(R_A_), gif[:, 0, :], V[0:1, 21:22], rw(R_BN), ALU.add, ALU.add, (gk, vk, rk(R_BN)), (rk(R_A_),))
            I("dve", lambda e: e.tensor_tensor_scan(rw(R_AA), onesf[:, :], rw(R_A_), carry[:, 1:2], ALU.mult, ALU.max),
              ("onesf", rk(R_A_), "carry"), (rk(R_AA),))
            yield
            A3 = rw(R_AA).rearrange("p (c i) -> p c i", i=64)
            aend = A3[:, :, 63]
            cp("pool", ape[:, 0:1], carry[:, 1:2], ("carry",), ("ape",))
            cp("pool", ape[:, 1:8], A3[:, 0:7, 63], (rk(R_AA),), ("ape",))
            tt("dve", rw(R_T1).rearrange("p (c i) -> p c i", i=64), ape[:, :].unsqueeze(2).broadcast_to([1, 8, 64]),
               A3, ALU.subtract, ("ape", rk(R_AA)), (rk(R_T1),))
            act(rw(R_WI), rw(R_T1), AF.Exp, (rk(R_T1),), (rk(R_WI),))
            tt("dve", dec[:, :], ape[:, :], aend, ALU.subtract, ("ape", rk(R_AA)), ("dec",))
            act(dec[:, :], dec[:, :], AF.Exp, ("dec",), ("dec",))
            tt("dve", rw(R_T2).rearrange("p (c i) -> p c i", i=64), rw(R_A_).rearrange("p (c i) -> p c i", i=64),
               aend.unsqueeze(2).broadcast_to([1, 8, 64]), ALU.subtract, (rk(R_A_), rk(R_AA)), (rk(R_T2),))
            act(rw(R_WK), rw(R_T2), AF.Exp, (rk(R_T2),), (rk(R_WK),), bias=LN_KSCALE)
            tt("dve", rw(R_T1), rw(R_BN), rw(R_AA), ALU.subtract, (rk(R_BN), rk(R_AA)), (rk(R_T1),))
            act(rw(R_EM), rw(R_T1), AF.Exp, (rk(R_T1),), (rk(R_EM),))
            ts("dve", rw(R_ALS), rw(R_A_), LN_KSCALE, ALU.add, (rk(R_A_),), (rk(R_ALS),))
            cp("pool", carry[:, 0:1], rw(R_BN)[:, TB - 1:TB], (rk(R_BN),), ("carry",))
            cp("pool", carry[:, 1:2], rw(R_AA)[:, TB - 1:TB], (rk(R_AA), "ape"), ("carry",))
            yield
            for qi, ri in enumerate([R_ALS, R_WK, R_EM]):
                for c8 in range(8):
                    mm(ps[1][0:64, 480 + qi * 8 + c8:480 + qi * 8 + c8 + 1], rw(ri)[:, c8 * 64:(c8 + 1) * 64],
                       onesf[:, 0:1], True, True, (rk(ri), "onesf"), (("ps", 1),))
            cp("dve", cols[:, :], ps[1][0:64, 480:504], (("ps", 1),), ("cols",))
            mm(ps[1][:, 504:512], onesf[:, 0:128], dec[:, :], True, True, ("onesf", "dec"), (("ps", 1),))
            cp("dve", decb[:, :], ps[1][:, 504:512], (("ps", 1),), ("decb",))
            yield
            mm(ps[1][0:64, :], onesf[:, 0:64], rw(R_AA), True, True, ("onesf", rk(R_AA)), (("ps", 1),))
            for c8 in range(8):
                act(wT[:, c8, :], ps[1][0:64, c8 * 64:(c8 + 1) * 64], AF.Exp, (("ps", 1), "cols"), (("wT", c8),),
                    scale=-1.0, bias=cols[:, c8:c8 + 1])
            tt("pool", wT[:, :, :], wT[:, :, :], mask64.unsqueeze(1).broadcast_to([64, 8, 64]), ALU.mult,
               tuple(("wT", c) for c in range(8)) + ("cst",), tuple(("wT", c) for c in range(8)))
            yield
            mm(ps[1][:, :], onesf[:, 0:128], rw(R_WI), True, True, ("onesf", rk(R_WI)), (("ps", 1),))
            tt("dve", qs[:, :], qm[:, :], ps[1][:, :], ALU.mult, (("qm", pp), ("ps", 1)), ("qs",))
            tt("pool", vmw[:, :, 0:129], vmc[:, :, 0:129], cols[:, 8:16].unsqueeze(2).broadcast_to([64, 8, 129]),
               ALU.mult, (("vmc", pp), "vmc_ones", "cols"), ("vmw",))
            yield

        def mlstm_block(l, tb, ycl_ap, okey, res):
            V = vec[l]
            vk = ("vec", l)
            pp = tb % 2
            qm, km, gm, vmc = qm2[pp], km2[pp], gm2[pp], vmc2[pp]
            qmk, kmk, gmk, vmck = ("qm", pp), ("km", pp), ("gm", pp), ("vmc", pp)
            if tb == 0:
                I("pool", lambda e: e.memset(cf[:, :], 0.0), (), ("cf",))
                I("pool", lambda e: e.memset(cb[:, :], 0.0), (), ("cb",))
            for c8 in range(8):
                tr(psb(1)[0:64, c8 * 128:(c8 + 1) * 128], km[:, c8 * 64:(c8 + 1) * 64], identb[:, :],
                   (kmk, "identb"), (("ps", 1),))
            cp("dve", ktok[:, :, :], psb(1)[0:64, :].rearrange("p (c d) -> p c d", d=128), (("ps", 1),), ("ktok",))
            yield

            def st(c8):
                cs = c8 * 64
                so = (c8 % 2) * 64
                mm(ps[1][0:64, so:so + 64], km[:, cs:cs + 64], qm[:, cs:cs + 64], True, True, (kmk, qmk), (("ps", 1),))

            st(0)
            for c8 in range(8):
                cs = c8 * 64
                so = (c8 % 2) * 64
                if c8 + 1 < 8:
                    st(c8 + 1)
                tt("dve", stl[c8 % 2][:, :], ps[1][0:64, so:so + 64], wT[:, c8, :], ALU.mult, (("ps", 1), ("wT", c8)),
                   (("stl", c8 % 2),))
                mm(ps[1][:, 257:386], ktok[:, c8, :], vmw[:, c8, 0:129], True, True, ("ktok", "vmw"), (("ps", 1),))
                mm(ps[1][0:64, 128:257], qs[:, cs:cs + 64], cb[:, 0:129], True, False, ("qs", "cb"), (("ps", 1),))
                mm(ps[1][0:64, 128:257], stl[c8 % 2][:, :], vmc[:, c8, 0:129], False, True,
                   (("stl", c8 % 2), vmck, "vmc_ones"), (("ps", 1),))
                stt(cf[:, 0:129], cf[:, 0:129], decb[:, c8:c8 + 1], ps[1][:, 257:386], ALU.mult, ALU.add,
                    ("cf", "decb", ("ps", 1)), ("cf",))
                cp("dve", cb[:, 0:129], cf[:, 0:129], ("cf",), ("cb",))
                cp("dve", nums[:, c8, 0:129], ps[1][0:64, 128:257], (("ps", 1),), (("nums", c8),))
                yield
            NUMS = tuple(("nums", c) for c in range(8))
            den = nums[:, :, 128]
            tt("dve", sq8[:, :, :], nums[:, :, 0:128], nums[:, :, 0:128], ALU.mult, NUMS, ("sq8",))
            I("dve", lambda e: e.reduce_sum(sm[:, :, 0], sq8[:, :, :], AX.X), ("sq8",), ("sm",))
            ts("dve", sm[:, :, 1], den, -1.0, ALU.mult, NUMS, ("sm",))
            tt("dve", sm[:, :, 1], sm[:, :, 1], den, ALU.max, ("sm",) + NUMS, ("sm",))
            tt("dve", sm[:, :, 1], sm[:, :, 1], cols[:, 16:24], ALU.max, ("sm", "cols"), ("sm",))
            tt("dve", sm[:, :, 2], sm[:, :, 1], sm[:, :, 1], ALU.mult, ("sm",), ("sm",))
            ts("dve", sm[:, :, 0], sm[:, :, 0], 1.0 / 128.0, ALU.mult, ("sm",), ("sm",))
            stt(sm[:, :, 3], sm[:, :, 2], EPS, sm[:, :, 0], ALU.mult, ALU.add, ("sm",), ("sm",))
            act(sm[:, :, 4], sm[:, :, 3], AF.Ln, ("sm",), ("sm",))
            act(sm[:, :, 5], sm[:, :, 4], AF.Exp, ("sm",), ("sm",), scale=-0.5)
            tt("dve", hmb[:, :, :], nums[:, :, 0:128], sm[:, :, 5:6].broadcast_to([64, 8, 128]), ALU.mult,
               NUMS + ("sm",), ("hmb",))
            for c8 in range(8):
                tr(psb(1)[:, c8 * 64:(c8 + 1) * 64], hmb[:, c8, :], identb[0:64, 0:64], ("hmb", "identb"),
                   (("ps", 1),))
            yield
            ts("dve", gt1[:, :], qm[:, :], V[:, 19:20], ALU.mult, (qmk, vk), ("gtA",))
            stt(gt2[:, :], psb(1)[:, 0:TB], V[:, 18:19], gt1[:, :], ALU.mult, ALU.add, (("ps", 1), vk, "gtA"),
                ("gtB",))
            tt("dve", yo[:, :], gt2[:, :], gm[:, :], ALU.mult, ("gtB", gmk), ("yo",))
            res.append(dma("sp", ycl_ap, yo[:, :], ("yo",), (okey,)))
            yield

        def attn_block(l, tb, ycl_ap, okey, res):
            V = vec[l]
            vk = ("vec", l)
            qa, ga = qa2[tb % 2], ga2[tb % 2]
            qak, gak = ("qa", tb % 2), ("ga", tb % 2)
            lam_init = 0.8 - 0.6 * math.exp(-0.3 * l)
            tiles = []
            for g2 in range(2):
                qt0 = tb * 4 + g2 * 2
                for j in range(qt0 + 2):
                    tiles.append((g2, qt0, j))

            def emit_S(ti):
                g2, qt0, j = tiles[ti]
                qoff = g2 * 256
                lo = 0 if j <= qt0 else 128
                sb0 = 2 + 2 * (ti % 2)
                kb = j // 4
                for m in range(2):
                    mm(ps[sb0 + m][:, lo:256], kaT[m * 64:(m + 1) * 64, j * 128:(j + 1) * 128],
                       qa[m * 64:(m + 1) * 64, qoff + lo:qoff + 256], True, True, (("ka", kb), qak),
                       (("ps", sb0), ("ps", sb0 + 1)))

            touched = set()
            emit_S(0)
            if len(tiles) > 1:
                emit_S(1)
            for ti, (g2, qt0, j) in enumerate(tiles):
                if j == 0:
                    touched = set()
                qoff = g2 * 256
                lo = 0 if j <= qt0 else 128
                sb0 = 2 + 2 * (ti % 2)
                pt = pT[ti % 2]
                ptk = ("pT", ti % 2)
                src_ = ps_all[:, sb0:sb0 + 2, lo:256]
                dst = pt[:, :].rearrange("p (m q) -> p m q", m=2)[:, :, lo:256]
                act(dst, src_, AF.Exp, (("ps", sb0), ("ps", sb0 + 1)), (ptk,), scale=0.125)
                if j >= qt0:
                    d0 = (j - qt0) * 128
                    msl = pt[64:128, :].rearrange("p (m q) -> p m q", m=2)[:, :, d0:d0 + 64]
                    I("pool", lambda e, msl=msl: e.memset(msl, 0.0), (ptk,), (ptk,))
                if ti + 2 < len(tiles):
                    emit_S(ti + 2)
                for qt in range(2):
                    if j > qt0 + qt:
                        continue
                    pb = 6 + qt
                    for m in range(2):
                        first = pb not in touched
                        touched.add(pb)
                        last = (j == qt0 + qt) and m == 1
                        mm(ps[pb][:, m * 129:(m + 1) * 129], pt[:, m * 256 + qt * 128:m * 256 + (qt + 1) * 128],
                           va[:, j, 0:129], first, last, (ptk, ("va", j), "va_ones"), (("ps", pb),))
                    if j == qt0 + qt:
                        P = ps[pb]
                        pk = ("ps", pb)
                        I("dve", lambda e, P=P: e.reciprocal(asm[:, 0:1], P[:, 128:129]), (pk,), ("asm0",))
                        I("dve", lambda e, P=P: e.reciprocal(asm[:, 1:2], P[:, 257:258]), (pk,), ("asm1",))
                        tt("dve", asm[:, 1:2], asm[:, 1:2], neglam[:, :], ALU.mult, ("asm1", "neglam"), ("asm1",))
                        ts("dve", at1[:, :], P[:, 0:128], asm[:, 0:1], ALU.mult, (pk, "asm0"), ("at1",))
                        stt(at2[:, :], P[:, 129:257], asm[:, 1:2], at1[:, :], ALU.mult, ALU.add,
                            (pk, "asm1", "at1"), ("at2",))
                        tt("pool", atj[:, :], at2[:, :], at2[:, :], ALU.mult, ("at2",), ("atj",))
                        I("dve", lambda e: e.reduce_sum(asm[:, 2:3], atj[:, :], AX.X), ("atj",), ("asm2",))
                        ts("dve", asm[:, 3:4], asm[:, 2:3], 1.0 / 128.0, ALU.mult, ("asm2",), ("asm3",),
                           s2=EPS, op1=ALU.add)
                        act(asm[:, 4:5], asm[:, 3:4], AF.Ln, ("asm3",), ("asm4",))
                        act(asm[:, 5:6], asm[:, 4:5], AF.Exp, ("asm4",), ("asm5",), scale=-0.5)
                        ts("dve", hab[:, :], at2[:, :], asm[:, 5:6], ALU.mult, ("at2", "asm5"), ("hab",),
                           s2=1.0 - lam_init, op1=ALU.mult)
                        tr(psb(pb)[:, 768:896], hab[:, :], identb[:, :], ("hab", "identb"), (pk,))
                        c0 = qoff + qt * 128
                        stt(yab[:, c0:c0 + 128], psb(pb)[:, 768:896], V[:, 20:21], ga[:, c0:c0 + 128], ALU.mult,
                            ALU.mult, (pk, vk, gak), ("yab",))
                yield
            res.append(dma("sp", ycl_ap, yab[:, :], ("yab",), (okey,)))

        out_toks = []

        def frontA(l, tb):
            t0 = tb * TB
            if l == 0:
                if tb == 0:
                    dma("sp", xblk[:, :, :], xT[:, t0:t0 + TB].rearrange("(kt p) t -> p kt t", p=128), (), XB)
            else:
                yield from outproj_block(l - 1, tb, xT, ("xsrc0", tb), load=(tb == 0))
                tok = dma("sp", x1s[:, t0:t0 + TB].rearrange("(kt p) t -> p kt t", p=128), xblk[:, :, :],
                          XB, (("xsrc1", tb),))
                if not do_fin:
                    out_toks.append(tok)
            yield from norm_block(vec[l][:, 0:8], False)
            if tb + 1 < _NB:
                t1 = (tb + 1) * TB
                if l == 0:
                    dma("sp", xblk[:, :, :], xT[:, t1:t1 + TB].rearrange("(kt p) t -> p kt t", p=128), (), XB)
                else:
                    outproj_loads(l - 1, tb + 1, xT, ("xsrc0", tb + 1))
            if _STOP >= 2:
                yield from inproj_block(l, tb)

        def frontB(l, tb):
            q4, tq = tb // 4, (tb % 4) * TB
            if _STOP >= 3:
                yield from gates_block(l, tb)
            res = []
            if _STOP >= 4:
                yield from mlstm_block(l, tb, ycl[l][q4][0:128, tq:tq + TB], ("ycl", l, q4, tb % 4, 0), res)
            if not fused:
                out_toks.extend(res)

        def drain(g):
            for _ in g:
                pass

        def adv(g, n):
            for _ in range(n):
                try:
                    next(g)
                except StopIteration:
                    return False
            return True

        NBU, NAU = float(os.environ.get("K_NBU", "20")), float(os.environ.get("K_NAU", "16"))

        load_weights(layers[0]) if layers else None
        if layers and layers[0] > 0:
            load_wout(layers[0] - 1)
        elif len(layers) > 1:
            load_wout(layers[0])
        for li, l in enumerate(layers):
            load_lam(l)
            drain(frontA(l, 0))
            for tb in range(_NB):
                q4, tq = tb // 4, (tb % 4) * TB
                res = []
                ag = (attn_block(l, tb, ycl[l][q4][128:256, tq:tq + TB], ("ycl", l, q4, tb % 4, 1), res)
                      if _STOP >= 5 else iter(()))
                bg = frontB(l, tb)
                fg = frontA(l, tb + 1) if tb + 1 < _NB else iter(())
                if tb == _NB - 1:
                    if li + 1 < len(layers):
                        load_weights(layers[li + 1])
                        if li + 1 >= 2:
                            load_wout(layers[li + 1] - 1)
                    elif do_fin and l > layers[0]:
                        load_wout(DEPTH - 1)
                ntile = 8 * tb + 6
                accb = acca = 0.0
                bl = fl = True
                for _ in ag:
                    accb += NBU / ntile
                    acca += NAU / ntile
                    nb_, na_ = int(accb), int(acca)
                    accb -= nb_
                    acca -= na_
                    while nb_ > 0 or na_ > 0:
                        if nb_ > 0:
                            bl = bl and adv(bg, 1)
                            nb_ -= 1
                        if na_ > 0:
                            fl = fl and adv(fg, 1)
                            na_ -= 1
                while bl or fl:
                    if bl:
                        bl = adv(bg, 1)
                    if fl:
                        fl = adv(fg, 1)
                if not fused:
                    out_toks.extend(res)
                if fused and tb % 4 == 3:
                    rk = tuple(("ycl", l, q4, i, w) for i in range(4) for w in range(2))
                    I("pool", lambda e, l=l, q4=q4: e.collective_compute(
                        "AllGather", ALU.bypass, replica_groups=GROUPS,
                        ins=[ycl[l][q4][:, :]], outs=[ycf[l][q4][:, :]]), rk, (("ycf", l, q4),), cc=True)

        if do_fin:
            if not (layers and layers[-1] > layers[0]):
                load_wout(DEPTH - 1)
            lf = DEPTH - 1
            nw_ap = vec[lf][:, 23:31]
            xb = [xblk[:, :, :], kaT[:, :].bitcast(F32).rearrange("p (kt t) -> p kt t", kt=KT)]
            yb = [sqb[:, :, :], va[:, :, :].rearrange("p a b -> p (a b)")[:, 0:KT * TB].rearrange("p (kt t) -> p kt t", kt=KT)]
            xk = [XB, tuple(("ka", i) for i in range(NBLK))]
            yk = [SQ, tuple(("va", i) for i in range(SEQ // 128)) + ("va_ones",)]

            def fin_load(tb):
                p = tb % 2
                t0 = tb * TB
                q4, tq = tb // 4, (tb % 4) * TB
                dma("sp", yb[p], ycf[lf][q4][:, tq:tq + TB].rearrange("(kt p) t -> p kt t", p=128),
                    (("ycf", lf, q4),), yk[p])
                dma("sp", xb[p], x1s[:, t0:t0 + TB].rearrange("(kt p) t -> p kt t", p=128),
                    (("xsrc1", tb),), xk[p])

            fin_load(0)
            for tb in range(_NB):
                p = tb % 2
                t0 = tb * TB
                if tb + 1 < _NB:
                    fin_load(tb + 1)
                X, Y = xb[p], yb[p]
                for m in range(KT):
                    bank = m % 2
                    for et in range(KT):
                        mm(ps[bank][:, :], wout_b[:, et, m * 128:(m + 1) * 128], Y[:, et, :], et == 0, et == KT - 1,
                           ("wout",) + yk[p], (("ps", bank),))
                    tt("dve", X[:, m, :], X[:, m, :], ps[bank][:, :], ALU.add, (("ps", bank),) + xk[p], xk[p])
                    if m % 2 == 0:
                        tt("pool", xnb[:, m, :], X[:, m, :], X[:, m, :], ALU.mult, xk[p], (("xnb", m),))
                    else:
                        act(xnb[:, m, :], X[:, m, :], AF.Square, xk[p], (("xnb", m),))
                for kt in range(KT):
                    mm(ps[2][:, :], onesb[:, :], xnb[:, kt, :], kt == 0, kt == KT - 1, ("onesb", ("xnb", kt)), (("ps", 2),))
                ts("dve", rstd[:, :], ps[2][:, :], 1.0 / D_MODEL, ALU.mult, (("ps", 2),), ("rstd",), s2=EPS, op1=ALU.add)
                act(rstd[:, :], rstd[:, :], AF.Ln, ("rstd",), ("rstd",))
                act(rstd[:, :], rstd[:, :], AF.Exp, ("rstd",), ("rstd",), scale=-0.5)
                for kt in range(KT):
                    stt(X[:, kt, :], X[:, kt, :], nw_ap[:, kt:kt + 1], rstd[:, :], ALU.mult, ALU.mult,
                        xk[p] + ("rstd", ("vec", lf)), xk[p])
                out_toks.append(dma("sp", outT[:, t0:t0 + TB].rearrange("(kt p) t -> p kt t", p=128), X, xk[p], ()))

        waits = []
        for tok in out_toks:
            S._tok_wait("sp", tok, waits)
        S.recs["sp"].append((waits, None, None))

        with nc.Block() as block:
            S.emit(block)
    return nc


_OFF = {"mq": 0, "mk": 512, "mv": 1024, "mi": 1536, "mf": 1540, "mz": 1544,
        "aq": 2056, "ak": 2568, "av": 3080, "az": 3592}


def _rope_tables():
    inv = (1.0 / (np.float32(10000.0) ** (np.arange(0, 64, 2, dtype=np.float32) / np.float32(64.0)))).astype(np.float32)
    ang = np.arange(SEQ, dtype=np.float32)[:, None] * inv[None, :]
    cos = np.cos(ang).astype(np.float32)
    sin = np.sin(ang).astype(np.float32)
    cosT = np.ascontiguousarray(np.concatenate([cos, cos, cos, cos], 1).T)
    sinT = np.ascontiguousarray(np.concatenate([sin, sin, sin, sin], 1).T)
    return cosT, sinT


def _consts():
    c = np.zeros((128, 192), np.float32)
    c[:, 0:128] = np.eye(128, dtype=np.float32)
    c[0:64, 128:192] = np.triu(np.ones((64, 64), np.float32))
    return c


def _core_inputs(inp, c, stage_layers, need_wout):
    b, hd = c // 4, c % 4
    f32 = np.float32
    d = {}
    for l in stage_layers:
        w = np.asarray(inp["w_in"][l], f32)
        hs = slice(hd * 128, (hd + 1) * 128)

        def blk(name):
            return w[:, _OFF[name] + hd * 128:_OFF[name] + (hd + 1) * 128]

        def perm(m):
            return np.concatenate([m[:, 32:64], m[:, 0:32], m[:, 96:128], m[:, 64:96]], 1)

        aq, ak = blk("aq"), blk("ak")
        gi = w[:, _OFF["mi"] + hd:_OFF["mi"] + hd + 1]
        gf = w[:, _OFF["mf"] + hd:_OFF["mf"] + hd + 1]
        d[f"wfm{l}"] = np.ascontiguousarray(np.concatenate(
            [blk("mq"), blk("mk"), blk("mz"), aq, perm(aq), ak, perm(ak), blk("az"), gi, gf], 1))
        d[f"wtm{l}"] = np.ascontiguousarray(np.concatenate([blk("mv"), blk("av")], 1))
        lam = np.concatenate([np.asarray(inp[k][l], f32) for k in ("lam_q1", "lam_k1", "lam_q2", "lam_k2")])
        d[f"lamv{l}"] = np.ascontiguousarray(np.tile(lam[None, :], (128, 1)))
    for l in range(DEPTH):
        v = np.zeros((128, NV), f32)
        v[:, 0:8] = np.asarray(inp["norm_w"][l], f32).reshape(8, 128).T
        cw = np.asarray(inp["conv_w"][l], f32)
        cbv = np.asarray(inp["conv_b"][l], f32)
        v[:, 8:12] = cw[:, hd * 128:(hd + 1) * 128].T
        v[:, 12:16] = cw[:, 512 + hd * 128:512 + (hd + 1) * 128].T
        v[:, 16] = cbv[hd * 128:(hd + 1) * 128]
        v[:, 17] = cbv[512 + hd * 128:512 + (hd + 1) * 128]
        v[:, 18] = np.asarray(inp["m_norm_w"][l], f32)[hd * 128:(hd + 1) * 128]
        v[:, 19] = np.asarray(inp["m_skip"][l], f32)[hd * 128:(hd + 1) * 128]
        v[:, 20] = np.asarray(inp["a_norm_w"][l], f32)
        v[:, 21] = np.asarray(inp["i_bias"][l], f32)[hd]
        v[:, 22] = np.asarray(inp["f_bias"][l], f32)[hd]
        v[:, 23:31] = np.asarray(inp["final_norm_w"], f32).reshape(8, 128).T
        d[f"vecs{l}"] = v
    for l in need_wout:
        wo = np.asarray(inp["w_out"][l], f32)
        rows = []
        for r in range(4):
            rows.append(wo[r * 128:(r + 1) * 128])
            rows.append(wo[512 + r * 128:512 + (r + 1) * 128])
        d[f"wout{l}"] = np.ascontiguousarray(np.concatenate(rows, 0))
    return d


_PROG = {}


def _prog(stage, debug=False):
    key = (stage, debug)
    if key not in _PROG:
        _PROG[key] = build_program(stage, debug)
    return _PROG[key]


def kernel(**inp):
    x = np.asarray(inp["x"], np.float32)
    xTs = [np.ascontiguousarray(x[b].T) for b in range(BATCH)]
    cosT, sinT = _rope_tables()
    cst = _consts()
    nc = _prog("all")
    in_maps = []
    for c in range(NCORES):
        d = _core_inputs(inp, c, [0, 1], [0, 1])
        d.update({"xT": xTs[c // 4], "cosT": cosT, "sinT": sinT, "consts": cst})
        in_maps.append(d)
    res = run_bass_kernel_spmd(nc, in_maps, core_ids=list(range(NCORES)))
    out = np.empty((BATCH, SEQ, D_MODEL), np.float32)
    for b in range(BATCH):
        out[b] = res.results[4 * b]["outT"].T
    return out
```

```python
import math
from contextlib import ExitStack

import numpy as np
import ml_dtypes

import concourse.bass as bass
import concourse.mybir as mybir
from concourse.bass_utils import run_bass_kernel_spmd

F32 = mybir.dt.float32
BF16 = mybir.dt.bfloat16
AF = mybir.ActivationFunctionType
ALU = mybir.AluOpType
AX = mybir.AxisListType

D_MODEL = 1024
BATCH = 2
SEQ = 8192
DEPTH = 2
NCORES = 8
TB = 512
NBLK = SEQ // TB
KT = D_MODEL // 128
EPS = 1e-6
NV = 32
LN_KSCALE = math.log(128.0 ** -0.5)
GROUPS = [[0, 1, 2, 3], [4, 5, 6, 7]]
import os
_STOP = int(os.environ.get("K_STOP", "9"))
_NB = int(os.environ.get("K_NBLK", str(NBLK)))


class Sched:
    CH = 30000
    ND = 48

    def __init__(self, nc, es):
        self.nc = nc
        self.engs = ["pe", "act", "dve", "pool", "sp"]
        self.recs = {e: [] for e in self.engs}
        self.cnt = {e: 0 for e in self.engs}
        self.seen = {e: {} for e in self.engs}
        self.lastw = {}
        self.readers = {}
        nsem = {"pe": 3, "act": 3, "dve": 4, "pool": 3, "sp": 1}
        self.esems = {e: [es.enter_context(nc.semaphore(f"s_{e}_{i}")) for i in range(nsem[e])]
                      for e in self.engs}
        self.dsems = [es.enter_context(nc.semaphore(f"d_{i}")) for i in range(self.ND)]
        self.dval = [0] * self.ND
        self.dnext = 0
        self.ccsem = es.enter_context(nc.semaphore("ccsem"))
        self.ccval = 0

    def _tok_wait(self, e, tok, waits):
        if tok[0] == "e":
            _, e2, n = tok
            if e2 == e and e == "pe":
                return
            if self.seen[e].get(e2, 0) >= n:
                return
            self.seen[e][e2] = n
            waits.append((self.esems[e2][(n - 1) // self.CH], (n - 1) % self.CH + 1))
        else:
            kind, i, v = tok
            key = (kind, i)
            if self.seen[e].get(key, 0) >= v:
                return
            self.seen[e][key] = v
            sem = self.dsems[i] if kind == "d" else self.ccsem
            waits.append((sem, v))

    def issue(self, e, fn, reads=(), writes=(), dma=False, cc=False):
        deps = []
        for k in reads:
            if k in self.lastw:
                deps.append(self.lastw[k])
        for k in writes:
            if k in self.lastw:
                deps.append(self.lastw[k])
            deps.extend(self.readers.get(k, {}).values())
        waits = []
        for tok in deps:
            self._tok_wait(e, tok, waits)
        if dma:
            i = self.dnext
            self.dnext = (self.dnext + 1) % self.ND
            prev = self.dval[i]
            if prev > 0:
                self._tok_wait(e, ("d", i, prev), waits)
            self.dval[i] += 16
            tok = ("d", i, self.dval[i])
            inc = (self.dsems[i], 16)
        elif cc:
            self.ccval += 1
            tok = ("c", 0, self.ccval)
            inc = (self.ccsem, 1)
        elif fn is None:
            tok = None
            inc = None
        else:
            self.cnt[e] += 1
            n = self.cnt[e]
            tok = ("e", e, n)
            inc = (self.esems[e][(n - 1) // self.CH], 1)
        self.recs[e].append((waits, fn, inc))
        if tok is not None:
            for k in reads:
                self.readers.setdefault(k, {})[(tok[0], tok[1])] = tok
            for k in writes:
                self.lastw[k] = tok
                self.readers[k] = {}
        return tok

    def emit(self, block):
        nc = self.nc

        def run(e, eng):
            for waits, fn, inc in self.recs[e]:
                for s, v in waits:
                    eng.wait_ge(s, v)
                if fn is not None:
                    ins = fn(eng)
                    ins.then_inc(inc[0], inc[1])

        @block.tensor
        def _(eng):
            run("pe", eng)

        @block.scalar
        def _(eng):
            run("act", eng)

        @block.vector
        def _(eng):
            run("dve", eng)

        @block.gpsimd
        def _(eng):
            run("pool", eng)

        @block.sync
        def _(eng):
            run("sp", eng)


def build_program(stage="all", debug=False):
    nc = bass.Bass("TRN2", target_bir_lowering=False)
    with ExitStack() as es:
        S = Sched(nc, es)

        def dram(name, shape, dt, kind):
            return nc.dram_tensor(name, shape, dt, kind=kind).ap()

        fused = stage == "all"
        layers = {"all": [0, 1], "l0": [0], "mid": [1], "fin": []}[stage]
        do_fin = stage in ("all", "fin")

        xT = dram("xT", [D_MODEL, SEQ], F32, "ExternalInput") if stage in ("all", "l0", "mid") else None
        cosT = dram("cosT", [128, SEQ], F32, "ExternalInput") if layers else None
        sinT = dram("sinT", [128, SEQ], F32, "ExternalInput") if layers else None
        consts = dram("consts", [128, 192], F32, "ExternalInput")
        wfm, wtm, wout, vecs, lamv = {}, {}, {}, {}, {}
        for l in layers:
            wfm[l] = dram(f"wfm{l}", [D_MODEL, 1026], F32, "ExternalInput")
            wtm[l] = dram(f"wtm{l}", [D_MODEL, 256], F32, "ExternalInput")
            lamv[l] = dram(f"lamv{l}", [128, 256], F32, "ExternalInput")
        for l in range(DEPTH):
            vecs[l] = dram(f"vecs{l}", [128, NV], F32, "ExternalInput")
        need_wout = {"all": [0, 1], "l0": [], "mid": [0], "fin": [1]}[stage]
        for l in need_wout:
            wout[l] = dram(f"wout{l}", [D_MODEL, D_MODEL], F32, "ExternalInput")

        ycl, ycf = {}, {}
        for l in layers:
            kind = "Internal" if fused else "ExternalOutput"
            ycl[l] = [dram(f"ycl{l}_{q}", [256, 2048], BF16, kind) for q in range(4)]
        for l in need_wout:
            kind = "Internal" if fused else "ExternalInput"
            ycf[l] = [dram(f"ycf{l}_{q}", [1024, 2048], BF16, kind) for q in range(4)]
        x1s = None
        if stage == "all":
            x1s = dram("x1s", [D_MODEL, SEQ], F32, "Internal")
        elif stage == "mid":
            x1s = dram("x1s", [D_MODEL, SEQ], F32, "ExternalOutput")
        elif stage == "fin":
            x1s = dram("x1s", [D_MODEL, SEQ], F32, "ExternalInput")
        outT = dram("outT", [D_MODEL, SEQ], F32, "ExternalOutput") if do_fin else None
        dbg = {}
        if debug:
            for nm, shp, dt in [("d_qm", [128, SEQ], BF16), ("d_km", [128, SEQ], BF16),
                                ("d_qa", [128, SEQ], BF16), ("d_ka", [128, SEQ], BF16),
                                ("d_rows", [1, 9 * SEQ], F32)]:
                dbg[nm] = dram(nm, shp, dt, "ExternalOutput")

        def sb(name, shape, dt):
            return es.enter_context(nc.sbuf_tensor(name, shape, dt))

        cst = sb("cst", [128, 192], F32)
        ident_f = cst[:, 0:128]
        identb = sb("identb", [128, 128], BF16)
        onesb = sb("onesb", [128, 128], BF16)
        onesf = sb("onesf", [1, 512], F32)
        vec = [sb(f"vec{l}", [128, NV], F32) for l in range(DEPTH)]
        lamt = sb("lamt", [128, 256], F32)
        lamw = sb("lamw", [128, 4], F32)
        neglam = sb("neglam", [128, 1], F32)
        negfb = sb("negfb", [1, 1], F32)

        wfm_b = sb("wfm_b", [128, KT, 1026], BF16)
        wtm_b = sb("wtm_b", [128, KT, 256], BF16)
        wout_b = sb("wout_b", [128, KT, D_MODEL], BF16)
        wstg = [sb(f"wstg{i}", [128, KT, 128], F32) for i in range(2)]

        xblk = sb("xblk", [128, KT, TB], F32)
        sqb = sb("sqb", [128, KT, TB], BF16)
        xnb = sb("xnb", [128, KT, TB], BF16)
        ycb = sqb
        rstd = sb("rstd", [128, TB], F32)
        cosb = sb("cosb", [128, TB], F32)
        sinb = sb("sinb", [128, TB], F32)
        preq = sb("preq", [128, TB + 4], F32)
        prek = sb("prek", [128, TB + 4], F32)
        cva = sb("tA", [128, TB], F32)
        cvb = sb("tB", [128, TB], F32)
        tC = sb("tC", [128, TB], F32)
        tD = sb("tD", [128, TB], F32)
        rpa, rpb = cva, cvb
        qa2 = [sb(f"qa{i}", [128, TB], BF16) for i in range(2)]
        qm2 = [sb(f"qm{i}", [128, TB], BF16) for i in range(2)]
        km2 = [sb(f"km{i}", [128, TB], BF16) for i in range(2)]
        qs = sb("qs", [128, TB], BF16)
        gm2 = [sb(f"gm{i}", [128, TB], F32) for i in range(2)]
        gtA = sb("gtA", [128, TB], F32)
        gtB = sb("gtB", [128, TB], F32)
        ga2 = [sb(f"ga{i}", [128, TB], F32) for i in range(2)]
        kaT = sb("kaT", [128, SEQ], BF16)
        va = sb("va", [128, SEQ // 128, 132], BF16)
        vmc2 = [sb(f"vmc{i}", [64, 8, 132], BF16) for i in range(2)]
        vmw = sb("vmw", [64, 8, 132], BF16)
        ktok = sb("ktok", [64, 8, 128], BF16)
        NR = 9
        rows = sb("rows", [1, NR, TB], F32)
        gif2 = [sb(f"gif{i}", [1, 2, TB], F32) for i in range(2)]
        carry = sb("carry", [1, 4], F32)
        ape = sb("ape", [1, 8], F32)
        dec = sb("dec", [1, 8], F32)
        cols = sb("cols", [64, 24], F32)
        decb = sb("decb", [128, 8], F32)
        wT = sb("wT", [64, 8, 64], F32)
        stl = [sb(f"stl{i}", [64, 64], BF16) for i in range(2)]
        cf = sb("cf", [128, 132], F32)
        cb = sb("cb", [128, 132], BF16)
        nums = sb("nums", [64, 8, 132], F32)
        sq8 = sb("sq8", [64, 8, 128], F32)
        sm = sb("sm", [64, 8, 8], F32)
        hmb = sb("hmb", [64, 8, 128], BF16)
        gt1, gt2 = gtA, gtB
        yo = sb("yo", [128, TB], BF16)
        pT = [sb(f"pT{i}", [128, 512], BF16) for i in range(2)]
        at1 = sb("at1", [128, 128], F32)
        at2 = sb("at2", [128, 128], F32)
        atj = sb("atj", [128, 128], F32)
        asm = sb("asm", [128, 8], F32)
        hab = sb("hab", [128, 128], BF16)
        yab = sb("yab", [128, TB], BF16)
        ob = xblk

        ps_all = es.enter_context(nc.psum_tensor("ps_all", [128, 8, 512], F32))
        ps = [ps_all[:, i, :] for i in range(8)]

        def I(e, fn, r=(), w=(), **kw):
            return S.issue(e, fn, r, w, **kw)

        def dma(q, out, in_, r, w):
            return I(q, lambda e: e.dma_start(out=out, in_=in_), r, w, dma=True)

        def act(out, in_, func, r, w, scale=1.0, bias=0.0, accum=None):
            if accum is None:
                return I("act", lambda e: e.activation(out, in_, func, bias=bias, scale=scale), r, w)
            return I("act", lambda e: e.activation(out, in_, func, bias=bias, scale=scale, accum_out=accum), r, w)

        def tt(eng, out, a, b, op, r, w):
            return I(eng, lambda e: e.tensor_tensor(out, a, b, op), r, w)

        def ts(eng, out, a, s1, op0, r, w, s2=None, op1=ALU.bypass):
            return I(eng, lambda e: e.tensor_scalar(out, a, s1, s2, op0, op1), r, w)

        def stt(out, a, sc, b, op0, op1, r, w):
            return I("dve", lambda e: e.scalar_tensor_tensor(out, a, sc, b, op0, op1), r, w)

        def cp(eng, out, in_, r, w):
            if eng == "act":
                return I(eng, lambda e: e.copy(out, in_), r, w)
            return I(eng, lambda e: e.tensor_copy(out, in_), r, w)

        def mm(out, lhsT, rhs, start, stop, r, w):
            return I("pe", lambda e: e.matmul(out, lhsT, rhs, start=start, stop=stop), r, w)

        def tr(out, in_, idn, r, w):
            return I("pe", lambda e: e.transpose(out, in_, idn), r, w)

        def psb(i):
            return ps[i].bitcast(BF16)

        XB = tuple(("xblk", k) for k in range(KT))
        SQ = tuple(("sqb", k) for k in range(KT))
        XN = tuple(("xnb", k) for k in range(KT))

        dma("sp", cst[:, :], consts[:, :], (), ("cst",))
        for l in range(DEPTH):
            dma("sp", vec[l][:, :], vecs[l][:, :], (), (("vec", l),))
        cp("dve", identb[:, :], cst[:, 0:128], ("cst",), ("identb",))
        I("pool", lambda e: e.memset(onesb[:, :], 1.0), (), ("onesb",))
        I("pool", lambda e: e.memset(onesf[:, :], 1.0), (), ("onesf",))
        I("pool", lambda e: e.memset(va[:, :, 128:132], 1.0), (), ("va_ones",))
        for i in range(2):
            I("pool", lambda e, i=i: e.memset(vmc2[i][:, :, 128:132], 1.0), (), ("vmc_ones",))
        mask64 = cst[0:64, 128:192]

        def load_weights(l):
            nchunk = 11
            for ci in range(nchunk):
                st = wstg[ci % 2]
                sk = ("wstg", ci % 2)
                if ci < 8:
                    src = wfm[l][:, ci * 128:(ci + 1) * 128]
                    dst = wfm_b[:, :, ci * 128:(ci + 1) * 128]
                    ncol = 128
                elif ci == 8:
                    src = wfm[l][:, 1024:1026]
                    dst = wfm_b[:, :, 1024:1026]
                    ncol = 2
                else:
                    src = wtm[l][:, (ci - 9) * 128:(ci - 8) * 128]
                    dst = wtm_b[:, :, (ci - 9) * 128:(ci - 8) * 128]
                    ncol = 128
                dma("sp", st[:, :, 0:ncol], src.rearrange("(kt p) c -> p kt c", p=128), (), (sk,))
                eng = "pool" if ci % 2 == 0 else "dve"
                cp(eng, dst, st[:, :, 0:ncol], (sk,), ("win",))
                if ci in (4, 6):
                    for m in range(2):
                        sl = wfm_b[:, :, ci * 128 + m * 64: ci * 128 + m * 64 + 32]
                        ts("pool", sl, sl, -1.0, ALU.mult, ("win",), ("win",))
        def load_lam(l):
            dma("sp", lamt[:, :], lamv[l][:, :], (), ("lamt",))
            for i in range(2):
                tt("dve", lamt[:, i * 128:i * 128 + 64], lamt[:, i * 128:i * 128 + 64],
                   lamt[:, i * 128 + 64:i * 128 + 128], ALU.mult, ("lamt",), ("lamt",))
                I("dve", lambda e, i=i: e.reduce_sum(lamw[:, i:i + 1], lamt[:, i * 128:i * 128 + 64], AX.X),
                  ("lamt",), ("lamw",))
            act(lamw[:, 2:4], lamw[:, 0:2], AF.Exp, ("lamw",), ("lamw",))
            lam_init = 0.8 - 0.6 * math.exp(-0.3 * l)
            stt(neglam[:, :], lamw[:, 3:4], -lam_init, lamw[:, 2:3], ALU.add, ALU.subtract, ("lamw",), ("neglam",))
            ts("pool", negfb[:, :], vec[l][0:1, 22:23], -1.0, ALU.mult, (("vec", l),), ("negfb",))

        def load_wout(l):
            for ci in range(8):
                st = wstg[ci % 2]
                sk = ("wstg", ci % 2)
                dma("sp", st[:, :, :], wout[l][:, ci * 128:(ci + 1) * 128].rearrange("(kt p) c -> p kt c", p=128),
                    (), (sk,))
                eng = "pool" if ci % 2 == 0 else "dve"
                cp(eng, wout_b[:, :, ci * 128:(ci + 1) * 128], st[:, :, :], (sk,), ("wout",))

        def outproj_loads(l_prev, tb, xsrc, xkey):
            t0 = tb * TB
            q4, tq = tb // 4, (tb % 4) * TB
            dma("sp", ycb[:, :, :], ycf[l_prev][q4][:, tq:tq + TB].rearrange("(kt p) t -> p kt t", p=128),
                (("ycf", l_prev, q4),), SQ)
            dma("sp", xblk[:, :, :], xsrc[:, t0:t0 + TB].rearrange("(kt p) t -> p kt t", p=128),
                (xkey,), XB)

        def outproj_block(l_prev, tb, xsrc, xkey, banks=(0,), load=True):
            if load:
                outproj_loads(l_prev, tb, xsrc, xkey)
            for m in range(KT):
                bank = banks[m % len(banks)]
                for et in range(KT):
                    mm(ps[bank][:, :], wout_b[:, et, m * 128:(m + 1) * 128], ycb[:, et, :], et == 0, et == KT - 1,
                       ("wout", ("sqb", et)), (("ps", bank),))
                tt("dve", xblk[:, m, :], xblk[:, m, :], ps[bank][:, :], ALU.add, (("ps", bank), ("xblk", m)),
                   (("xblk", m),))
                yield

        def norm_block(nw_ap, to_out):
            for kt in range(KT):
                tt("pool" if kt % 2 == 0 else "dve", sqb[:, kt, :], xblk[:, kt, :], xblk[:, kt, :], ALU.mult,
                   (("xblk", kt),), (("sqb", kt),))
            for kt in range(KT):
                mm(ps[0][:, :], onesb[:, :], sqb[:, kt, :], kt == 0, kt == KT - 1, ("onesb", ("sqb", kt)), (("ps", 0),))
            yield
            ts("dve", rstd[:, :], ps[0][:, :], 1.0 / D_MODEL, ALU.mult, (("ps", 0),), ("rstd",), s2=EPS, op1=ALU.add)
            act(rstd[:, :], rstd[:, :], AF.Ln, ("rstd",), ("rstd",))
            act(rstd[:, :], rstd[:, :], AF.Exp, ("rstd",), ("rstd",), scale=-0.5)
            for kt in range(KT):
                if to_out:
                    stt(ob[:, kt, :], xblk[:, kt, :], nw_ap[:, kt:kt + 1], rstd[:, :], ALU.mult, ALU.mult,
                        (("xblk", kt), "rstd"), (("xblk", kt),))
                else:
                    stt(xnb[:, kt, :], xblk[:, kt, :], nw_ap[:, kt:kt + 1], rstd[:, :], ALU.mult, ALU.mult,
                        (("xblk", kt), "rstd"), (("xnb", kt),))
            yield

        def inproj_block(l, tb):
            t0 = tb * TB
            pp = tb % 2
            qa, ga = qa2[pp], ga2[pp]
            qak, gak = ("qa", pp), ("ga", pp)
            qm, km, gm, vmc, gif = qm2[pp], km2[pp], gm2[pp], vmc2[pp], gif2[pp]
            V = vec[l]
            vk = ("vec", l)
            dma("sp", cosb[:, :], cosT[:, t0:t0 + TB], (), ("cosb",))
            dma("sp", sinb[:, :], sinT[:, t0:t0 + TB], (), ("sinb",))

            def fm(c0, ncol):
                def f(b):
                    for kt in range(KT):
                        mm(ps[b][0:ncol, :], wfm_b[:, kt, c0:c0 + ncol], xnb[:, kt, :], kt == 0, kt == KT - 1,
                           ("win", ("xnb", kt)), (("ps", b),))
                return f

            def conv_ev(which):
                pre, dst, cw0, cbc, fin, fk = [(preq, qm, 8, 16, tC, "tC"), (prek, km, 12, 17, tD, "tD")][which]
                pk = ("pre", which)

                def e1(b):
                    if tb == 0:
                        I("pool", lambda e: e.memset(pre[:, 0:4], 0.0), (), (pk,))
                    else:
                        cp("pool", pre[:, 1:4], pre[:, TB + 1:TB + 4], (pk,), (pk,))
                    cp("dve", pre[:, 4:4 + TB], ps[b][:, :], (("ps", b), pk), (pk,))
                    ts("dve", cva[:, :], pre[:, 4:4 + TB], V[:, cw0 + 3:cw0 + 4], ALU.mult, (pk, vk), ("tA",),
                       s2=V[:, cbc:cbc + 1], op1=ALU.add)
                    stt(cvb[:, :], pre[:, 3:3 + TB], V[:, cw0 + 2:cw0 + 3], cva[:, :], ALU.mult, ALU.add,
                        (pk, vk, "tA"), ("tB",))
                    stt(cva[:, :], pre[:, 2:2 + TB], V[:, cw0 + 1:cw0 + 2], cvb[:, :], ALU.mult, ALU.add,
                        (pk, vk, "tB"), ("tA",))
                    stt(fin[:, :], pre[:, 1:1 + TB], V[:, cw0:cw0 + 1], cva[:, :], ALU.mult, ALU.add,
                        (pk, vk, "tA"), (fk,))

                def e2(b):
                    act(dst[:, :], fin[:, :], AF.Silu, (fk,), (("qm", pp) if which == 0 else ("km", pp),))
                return [e1, (SILU_LATE, e2)]

            SILU_LATE = 99

            def silu_ev(dst, dk):
                return [lambda b: cp("dve", dst[:, :], ps[b][:, :], (("ps", b),), (dk,)),
                        (SILU_LATE, lambda b: act(dst[:, :], dst[:, :], AF.Silu, (dk,), (dk,)))]

            def rope_a(b):
                tt("dve", rpa[:, :], ps[b][:, :], cosb[:, :], ALU.mult, (("ps", b), "cosb"), ("tA",))

            def rope_b(which):
                def f(b):
                    tt("dve", rpb[:, :], ps[b][:, :], sinb[:, :], ALU.mult, (("ps", b), "sinb"), ("tB",))
                    if which == 0:
                        tt("pool", qa[:, :], rpa[:, :], rpb[:, :], ALU.add, ("tA", "tB"), (qak,))
                    else:
                        tt("pool", kaT[:, t0:t0 + TB], rpa[:, :], rpb[:, :], ALU.add, ("tA", "tB"), (("ka", tb),))
                return f

            def row_ev(g):
                return [lambda b: cp("dve", gif[:, g, :], ps[b][0:1, :], (("ps", b),), (("gif", pp),))]

            def vm_mm(h):
                def f(b):
                    for c4 in range(4):
                        c8 = h * 4 + c4
                        for kt in range(KT):
                            mm(ps[b][0:64, c4 * 128:(c4 + 1) * 128], xnb[:, kt, c8 * 64:(c8 + 1) * 64],
                               wtm_b[:, kt, 0:128], kt == 0, kt == KT - 1, ("win", ("xnb", kt)), (("ps", b),))
                return f

            def vm_ev(h):
                return [lambda b: cp("dve", vmc[:, h * 4:(h + 1) * 4, 0:128],
                                     ps[b][0:64, :].rearrange("p (c d) -> p c d", d=128), (("ps", b),), (("vmc", pp),))]

            def va_mm(b):
                for t4 in range(4):
                    for kt in range(KT):
                        mm(ps[b][:, t4 * 128:(t4 + 1) * 128], xnb[:, kt, t4 * 128:(t4 + 1) * 128],
                           wtm_b[:, kt, 128:256], kt == 0, kt == KT - 1, ("win", ("xnb", kt)), (("ps", b),))

            def va_ev(b):
                cp("dve", va[:, tb * 4:(tb + 1) * 4, 0:128], ps[b][:, :].rearrange("p (c d) -> p c d", d=128),
                   (("ps", b), "va_ones"), tuple(("va", tb * 4 + i) for i in range(4)))

            stages = [
                (fm(0, 128), conv_ev(0)),
                (fm(128, 128), conv_ev(1)),
                (fm(256, 128), silu_ev(gm, ("gm", pp))),
                (fm(3 * 128, 128), [rope_a]),
                (fm(4 * 128, 128), [rope_b(0)]),
                (fm(5 * 128, 128), [rope_a]),
                (fm(6 * 128, 128), [rope_b(1)]),
                (fm(7 * 128, 128), silu_ev(ga, gak)),
                (fm(1024, 1), row_ev(0)),
                (fm(1025, 1), row_ev(1)),
                (vm_mm(0), vm_ev(0)),
                (vm_mm(1), vm_ev(1)),
                (va_mm, [va_ev]),
            ]
            pending = []
            for k, (mmf, evs) in enumerate(stages):
                b = 0
                mmf(b)
                for i, fn in enumerate(evs):
                    if isinstance(fn, tuple):
                        pending.append((fn[0], fn[1], b))
                    else:
                        pending.append((k + i, fn, b))
                for due, fn, bb in [p for p in pending if p[0] <= k]:
                    fn(bb)
                pending = [p for p in pending if p[0] > k]
                yield
            for due, fn, bb in sorted(pending, key=lambda p: p[0]):
                fn(bb)
            yield

        R_I, R_F, R_L1, R_BN, R_A_, R_AA, R_WI, R_WK, R_EM = range(9)
        R_T2, R_T1, R_ALS = R_I, R_F, R_L1

        def rw(i):
            return rows[:, i, :]

        def gates_block(l, tb):
            V = vec[l]
            vk = ("vec", l)
            rk = lambda i: ("row", i)
            pp = tb % 2
            qm, vmc, gif = qm2[pp], vmc2[pp], gif2[pp]
            gk = ("gif", pp)
            if tb == 0:
                I("pool", lambda e: e.memset(carry[:, :], 0.0), (), ("carry",))
            act(rw(R_T1), gif[:, 1, :], AF.Exp, (gk, "negfb"), (rk(R_T1),), scale=-1.0, bias=negfb[:, :])
            act(rw(R_L1), rw(R_T1), AF.Ln, (rk(R_T1),), (rk(R_L1),), bias=1.0)
            I("dve", lambda e: e.tensor_tensor_scan(rw(R_BN), onesf[:, :], rw(R_L1), carry[:, 0:1], ALU.mult, ALU.add),
              ("onesf", rk(R_L1), "carry"), (rk(R_BN),))
            stt(rw(R_A_), gif[:, 0, :], V[0:1, 21:22], rw(R_BN), ALU.add, ALU.add, (gk, vk, rk(R_BN)), (rk(R_A_),))
            I("dve", lambda e: e.tensor_tensor_scan(rw(R_AA), onesf[:, :], rw(R_A_), carry[:, 1:2], ALU.mult, ALU.max),
              ("onesf", rk(R_A_), "carry"), (rk(R_AA),))
            yield
            A3 = rw(R_AA).rearrange("p (c i) -> p c i", i=64)
            aend = A3[:, :, 63]
            cp("pool", ape[:, 0:1], carry[:, 1:2], ("carry",), ("ape",))
            cp("pool", ape[:, 1:8], A3[:, 0:7, 63], (rk(R_AA),), ("ape",))
            tt("dve", rw(R_T1).rearrange("p (c i) -> p c i", i=64), ape[:, :].unsqueeze(2).broadcast_to([1, 8, 64]),
               A3, ALU.subtract, ("ape", rk(R_AA)), (rk(R_T1),))
            act(rw(R_WI), rw(R_T1), AF.Exp, (rk(R_T1),), (rk(R_WI),))
            tt("dve", dec[:, :], ape[:, :], aend, ALU.subtract, ("ape", rk(R_AA)), ("dec",))
            act(dec[:, :], dec[:, :], AF.Exp, ("dec",), ("dec",))
            tt("dve", rw(R_T2).rearrange("p (c i) -> p c i", i=64), rw(R_A_).rearrange("p (c i) -> p c i", i=64),
               aend.unsqueeze(2).broadcast_to([1, 8, 64]), ALU.subtract, (rk(R_A_), rk(R_AA)), (rk(R_T2),))
            act(rw(R_WK), rw(R_T2), AF.Exp, (rk(R_T2),), (rk(R_WK),), bias=LN_KSCALE)
            tt("dve", rw(R_T1), rw(R_BN), rw(R_AA), ALU.subtract, (rk(R_BN), rk(R_AA)), (rk(R_T1),))
            act(rw(R_EM), rw(R_T1), AF.Exp, (rk(R_T1),), (rk(R_EM),))
            ts("dve", rw(R_ALS), rw(R_A_), LN_KSCALE, ALU.add, (rk(R_A_),), (rk(R_ALS),))
            cp("pool", carry[:, 0:1], rw(R_BN)[:, TB - 1:TB], (rk(R_BN),), ("carry",))
            cp("pool", carry[:, 1:2], rw(R_AA)[:, TB - 1:TB], (rk(R_AA), "ape"), ("carry",))
            yield
            for qi, ri in enumerate([R_ALS, R_WK, R_EM]):
                for c8 in range(8):
                    mm(ps[1][0:64, 480 + qi * 8 + c8:480 + qi * 8 + c8 + 1], rw(ri)[:, c8 * 64:(c8 + 1) * 64],
                       onesf[:, 0:1], True, True, (rk(ri), "onesf"), (("ps", 1),))
            cp("dve", cols[:, :], ps[1][0:64, 480:504], (("ps", 1),), ("cols",))
            mm(ps[1][:, 504:512], onesf[:, 0:128], dec[:, :], True, True, ("onesf", "dec"), (("ps", 1),))
            cp("dve", decb[:, :], ps[1][:, 504:512], (("ps", 1),), ("decb",))
            yield
            mm(ps[1][0:64, :], onesf[:, 0:64], rw(R_AA), True, True, ("onesf", rk(R_AA)), (("ps", 1),))
            for c8 in range(8):
                act(wT[:, c8, :], ps[1][0:64, c8 * 64:(c8 + 1) * 64], AF.Exp, (("ps", 1), "cols"), (("wT", c8),),
                    scale=-1.0, bias=cols[:, c8:c8 + 1])
            tt("pool", wT[:, :, :], wT[:, :, :], mask64.unsqueeze(1).broadcast_to([64, 8, 64]), ALU.mult,
               tuple(("wT", c) for c in range(8)) + ("cst",), tuple(("wT", c) for c in range(8)))
            yield
            mm(ps[1][:, :], onesf[:, 0:128], rw(R_WI), True, True, ("onesf", rk(R_WI)), (("ps", 1),))
            tt("dve", qs[:, :], qm[:, :], ps[1][:, :], ALU.mult, (("qm", pp), ("ps", 1)), ("qs",))
            tt("pool", vmw[:, :, 0:129], vmc[:, :, 0:129], cols[:, 8:16].unsqueeze(2).broadcast_to([64, 8, 129]),
               ALU.mult, (("vmc", pp), "vmc_ones", "cols"), ("vmw",))
            yield

        def mlstm_block(l, tb, ycl_ap, okey, res):
            V = vec[l]
            vk = ("vec", l)
            pp = tb % 2
            qm, km, gm, vmc = qm2[pp], km2[pp], gm2[pp], vmc2[pp]
            qmk, kmk, gmk, vmck = ("qm", pp), ("km", pp), ("gm", pp), ("vmc", pp)
            if tb == 0:
                I("pool", lambda e: e.memset(cf[:, :], 0.0), (), ("cf",))
                I("pool", lambda e: e.memset(cb[:, :], 0.0), (), ("cb",))
            for c8 in range(8):
                tr(psb(1)[0:64, c8 * 128:(c8 + 1) * 128], km[:, c8 * 64:(c8 + 1) * 64], identb[:, :],
                   (kmk, "identb"), (("ps", 1),))
            cp("dve", ktok[:, :, :], psb(1)[0:64, :].rearrange("p (c d) -> p c d", d=128), (("ps", 1),), ("ktok",))
            yield

            def st(c8):
                cs = c8 * 64
                so = (c8 % 2) * 64
                mm(ps[1][0:64, so:so + 64], km[:, cs:cs + 64], qm[:, cs:cs + 64], True, True, (kmk, qmk), (("ps", 1),))

            st(0)
            for c8 in range(8):
                cs = c8 * 64
                so = (c8 % 2) * 64
                if c8 + 1 < 8:
                    st(c8 + 1)
                tt("dve", stl[c8 % 2][:, :], ps[1][0:64, so:so + 64], wT[:, c8, :], ALU.mult, (("ps", 1), ("wT", c8)),
                   (("stl", c8 % 2),))
                mm(ps[1][:, 257:386], ktok[:, c8, :], vmw[:, c8, 0:129], True, True, ("ktok", "vmw"), (("ps", 1),))
                mm(ps[1][0:64, 128:257], qs[:, cs:cs + 64], cb[:, 0:129], True, False, ("qs", "cb"), (("ps", 1),))
                mm(ps[1][0:64, 128:257], stl[c8 % 2][:, :], vmc[:, c8, 0:129], False, True,
                   (("stl", c8 % 2), vmck, "vmc_ones"), (("ps", 1),))
                stt(cf[:, 0:129], cf[:, 0:129], decb[:, c8:c8 + 1], ps[1][:, 257:386], ALU.mult, ALU.add,
                    ("cf", "decb", ("ps", 1)), ("cf",))
                cp("dve", cb[:, 0:129], cf[:, 0:129], ("cf",), ("cb",))
                cp("dve", nums[:, c8, 0:129], ps[1][0:64, 128:257], (("ps", 1),), (("nums", c8),))
                yield
            NUMS = tuple(("nums", c) for c in range(8))
            den = nums[:, :, 128]
            tt("dve", sq8[:, :, :], nums[:, :, 0:128], nums[:, :, 0:128], ALU.mult, NUMS, ("sq8",))
            I("dve", lambda e: e.reduce_sum(sm[:, :, 0], sq8[:, :, :], AX.X), ("sq8",), ("sm",))
            ts("dve", sm[:, :, 1], den, -1.0, ALU.mult, NUMS, ("sm",))
            tt("dve", sm[:, :, 1], sm[:, :, 1], den, ALU.max, ("sm",) + NUMS, ("sm",))
            tt("dve", sm[:, :, 1], sm[:, :, 1], cols[:, 16:24], ALU.max, ("sm", "cols"), ("sm",))
            tt("dve", sm[:, :, 2], sm[:, :, 1], sm[:, :, 1], ALU.mult, ("sm",), ("sm",))
            ts("dve", sm[:, :, 0], sm[:, :, 0], 1.0 / 128.0, ALU.mult, ("sm",), ("sm",))
            stt(sm[:, :, 3], sm[:, :, 2], EPS, sm[:, :, 0], ALU.mult, ALU.add, ("sm",), ("sm",))
            act(sm[:, :, 4], sm[:, :, 3], AF.Ln, ("sm",), ("sm",))
            act(sm[:, :, 5], sm[:, :, 4], AF.Exp, ("sm",), ("sm",), scale=-0.5)
            tt("dve", hmb[:, :, :], nums[:, :, 0:128], sm[:, :, 5:6].broadcast_to([64, 8, 128]), ALU.mult,
               NUMS + ("sm",), ("hmb",))
            for c8 in range(8):
                tr(psb(1)[:, c8 * 64:(c8 + 1) * 64], hmb[:, c8, :], identb[0:64, 0:64], ("hmb", "identb"),
                   (("ps", 1),))
            yield
            ts("dve", gt1[:, :], qm[:, :], V[:, 19:20], ALU.mult, (qmk, vk), ("gtA",))
            stt(gt2[:, :], psb(1)[:, 0:TB], V[:, 18:19], gt1[:, :], ALU.mult, ALU.add, (("ps", 1), vk, "gtA"),
                ("gtB",))
            tt("dve", yo[:, :], gt2[:, :], gm[:, :], ALU.mult, ("gtB", gmk), ("yo",))
            res.append(dma("sp", ycl_ap, yo[:, :], ("yo",), (okey,)))
            yield

        def attn_block(l, tb, ycl_ap, okey, res):
            V = vec[l]
            vk = ("vec", l)
            qa, ga = qa2[tb % 2], ga2[tb % 2]
            qak, gak = ("qa", tb % 2), ("ga", tb % 2)
            lam_init = 0.8 - 0.6 * math.exp(-0.3 * l)
            tiles = []
            for g2 in range(2):
                qt0 = tb * 4 + g2 * 2
                for j in range(qt0 + 2):
                    tiles.append((g2, qt0, j))

            def emit_S(ti):
                g2, qt0, j = tiles[ti]
                qoff = g2 * 256
                lo = 0 if j <= qt0 else 128
                sb0 = 2 + 2 * (ti % 2)
                kb = j // 4
                for m in range(2):
                    mm(ps[sb0 + m][:, lo:256], kaT[m * 64:(m + 1) * 64, j * 128:(j + 1) * 128],
                       qa[m * 64:(m + 1) * 64, qoff + lo:qoff + 256], True, True, (("ka", kb), qak),
                       (("ps", sb0), ("ps", sb0 + 1)))

            touched = set()
            emit_S(0)
            if len(tiles) > 1:
                emit_S(1)
            for ti, (g2, qt0, j) in enumerate(tiles):
                if j == 0:
                    touched = set()
                qoff = g2 * 256
                lo = 0 if j <= qt0 else 128
                sb0 = 2 + 2 * (ti % 2)
                pt = pT[ti % 2]
                ptk = ("pT", ti % 2)
                src_ = ps_all[:, sb0:sb0 + 2, lo:256]
                dst = pt[:, :].rearrange("p (m q) -> p m q", m=2)[:, :, lo:256]
                act(dst, src_, AF.Exp, (("ps", sb0), ("ps", sb0 + 1)), (ptk,), scale=0.125)
                if j >= qt0:
                    d0 = (j - qt0) * 128
                    msl = pt[64:128, :].rearrange("p (m q) -> p m q", m=2)[:, :, d0:d0 + 64]
                    I("pool", lambda e, msl=msl: e.memset(msl, 0.0), (ptk,), (ptk,))
                if ti + 2 < len(tiles):
                    emit_S(ti + 2)
                for qt in range(2):
                    if j > qt0 + qt:
                        continue
                    pb = 6 + qt
                    for m in range(2):
                        first = pb not in touched
                        touched.add(pb)
                        last = (j == qt0 + qt) and m == 1
                        mm(ps[pb][:, m * 129:(m + 1) * 129], pt[:, m * 256 + qt * 128:m * 256 + (qt + 1) * 128],
                           va[:, j, 0:129], first, last, (ptk, ("va", j), "va_ones"), (("ps", pb),))
                    if j == qt0 + qt:
                        P = ps[pb]
                        pk = ("ps", pb)
                        I("dve", lambda e, P=P: e.reciprocal(asm[:, 0:1], P[:, 128:129]), (pk,), ("asm0",))
                        I("dve", lambda e, P=P: e.reciprocal(asm[:, 1:2], P[:, 257:258]), (pk,), ("asm1",))
                        tt("dve", asm[:, 1:2], asm[:, 1:2], neglam[:, :], ALU.mult, ("asm1", "neglam"), ("asm1",))
                        ts("dve", at1[:, :], P[:, 0:128], asm[:, 0:1], ALU.mult, (pk, "asm0"), ("at1",))
                        stt(at2[:, :], P[:, 129:257], asm[:, 1:2], at1[:, :], ALU.mult, ALU.add,
                            (pk, "asm1", "at1"), ("at2",))
                        tt("pool", atj[:, :], at2[:, :], at2[:, :], ALU.mult, ("at2",), ("atj",))
                        I("dve", lambda e: e.reduce_sum(asm[:, 2:3], atj[:, :], AX.X), ("atj",), ("asm2",))
                        ts("dve", asm[:, 3:4], asm[:, 2:3], 1.0 / 128.0, ALU.mult, ("asm2",), ("asm3",),
                           s2=EPS, op1=ALU.add)
                        act(asm[:, 4:5], asm[:, 3:4], AF.Ln, ("asm3",), ("asm4",))
                        act(asm[:, 5:6], asm[:, 4:5], AF.Exp, ("asm4",), ("asm5",), scale=-0.5)
                        ts("dve", hab[:, :], at2[:, :], asm[:, 5:6], ALU.mult, ("at2", "asm5"), ("hab",),
                           s2=1.0 - lam_init, op1=ALU.mult)
                        tr(psb(pb)[:, 768:896], hab[:, :], identb[:, :], ("hab", "identb"), (pk,))
                        c0 = qoff + qt * 128
                        stt(yab[:, c0:c0 + 128], psb(pb)[:, 768:896], V[:, 20:21], ga[:, c0:c0 + 128], ALU.mult,
                            ALU.mult, (pk, vk, gak), ("yab",))
                yield
            res.append(dma("sp", ycl_ap, yab[:, :], ("yab",), (okey,)))

        out_toks = []

        def frontA(l, tb):
            t0 = tb * TB
            if l == 0:
                if tb == 0:
                    dma("sp", xblk[:, :, :], xT[:, t0:t0 + TB].rearrange("(kt p) t -> p kt t", p=128), (), XB)
            else:
                yield from outproj_block(l - 1, tb, xT, ("xsrc0", tb), load=(tb == 0))
                tok = dma("sp", x1s[:, t0:t0 + TB].rearrange("(kt p) t -> p kt t", p=128), xblk[:, :, :],
                          XB, (("xsrc1", tb),))
                if not do_fin:
                    out_toks.append(tok)
            yield from norm_block(vec[l][:, 0:8], False)
            if tb + 1 < _NB:
                t1 = (tb + 1) * TB
                if l == 0:
                    dma("sp", xblk[:, :, :], xT[:, t1:t1 + TB].rearrange("(kt p) t -> p kt t", p=128), (), XB)
                else:
                    outproj_loads(l - 1, tb + 1, xT, ("xsrc0", tb + 1))
            if _STOP >= 2:
                yield from inproj_block(l, tb)

        def frontB(l, tb):
            q4, tq = tb // 4, (tb % 4) * TB
            if _STOP >= 3:
                yield from gates_block(l, tb)
            res = []
            if _STOP >= 4:
                yield from mlstm_block(l, tb, ycl[l][q4][0:128, tq:tq + TB], ("ycl", l, q4, tb % 4, 0), res)
            if not fused:
                out_toks.extend(res)

        def drain(g):
            for _ in g:
                pass

        def adv(g, n):
            for _ in range(n):
                try:
                    next(g)
                except StopIteration:
                    return False
            return True

        NBU, NAU = float(os.environ.get("K_NBU", "20")), float(os.environ.get("K_NAU", "16"))

        load_weights(layers[0]) if layers else None
        if layers and layers[0] > 0:
            load_wout(layers[0] - 1)
        elif len(layers) > 1:
            load_wout(layers[0])
        for li, l in enumerate(layers):
            load_lam(l)
            drain(frontA(l, 0))
            for tb in range(_NB):
                q4, tq = tb // 4, (tb % 4) * TB
                res = []
                ag = (attn_block(l, tb, ycl[l][q4][128:256, tq:tq + TB], ("ycl", l, q4, tb % 4, 1), res)
                      if _STOP >= 5 else iter(()))
                bg = frontB(l, tb)
                fg = frontA(l, tb + 1) if tb + 1 < _NB else iter(())
                if tb == _NB - 1:
                    if li + 1 < len(layers):
                        load_weights(layers[li + 1])
                        if li + 1 >= 2:
                            load_wout(layers[li + 1] - 1)
                    elif do_fin and l > layers[0]:
                        load_wout(DEPTH - 1)
                ntile = 8 * tb + 6
                accb = acca = 0.0
                bl = fl = True
                for _ in ag:
                    accb += NBU / ntile
                    acca += NAU / ntile
                    nb_, na_ = int(accb), int(acca)
                    accb -= nb_
                    acca -= na_
                    while nb_ > 0 or na_ > 0:
                        if nb_ > 0:
                            bl = bl and adv(bg, 1)
                            nb_ -= 1
                        if na_ > 0:
                            fl = fl and adv(fg, 1)
                            na_ -= 1
                while bl or fl:
                    if bl:
                        bl = adv(bg, 1)
                    if fl:
                        fl = adv(fg, 1)
                if not fused:
                    out_toks.extend(res)
                if fused and tb % 4 == 3:
                    rk = tuple(("ycl", l, q4, i, w) for i in range(4) for w in range(2))
                    I("pool", lambda e, l=l, q4=q4: e.collective_compute(
                        "AllGather", ALU.bypass, replica_groups=GROUPS,
                        ins=[ycl[l][q4][:, :]], outs=[ycf[l][q4][:, :]]), rk, (("ycf", l, q4),), cc=True)

        if do_fin:
            if not (layers and layers[-1] > layers[0]):
                load_wout(DEPTH - 1)
            lf = DEPTH - 1
            nw_ap = vec[lf][:, 23:31]
            xb = [xblk[:, :, :], kaT[:, :].bitcast(F32).rearrange("p (kt t) -> p kt t", kt=KT)]
            yb = [sqb[:, :, :], va[:, :, :].rearrange("p a b -> p (a b)")[:, 0:KT * TB].rearrange("p (kt t) -> p kt t", kt=KT)]
            xk = [XB, tuple(("ka", i) for i in range(NBLK))]
            yk = [SQ, tuple(("va", i) for i in range(SEQ // 128)) + ("va_ones",)]

            def fin_load(tb):
                p = tb % 2
                t0 = tb * TB
                q4, tq = tb // 4, (tb % 4) * TB
                dma("sp", yb[p], ycf[lf][q4][:, tq:tq + TB].rearrange("(kt p) t -> p kt t", p=128),
                    (("ycf", lf, q4),), yk[p])
                dma("sp", xb[p], x1s[:, t0:t0 + TB].rearrange("(kt p) t -> p kt t", p=128),
                    (("xsrc1", tb),), xk[p])

            fin_load(0)
            for tb in range(_NB):
                p = tb % 2
                t0 = tb * TB
                if tb + 1 < _NB:
                    fin_load(tb + 1)
                X, Y = xb[p], yb[p]
                for m in range(KT):
                    bank = m % 2
                    for et in range(KT):
                        mm(ps[bank][:, :], wout_b[:, et, m * 128:(m + 1) * 128], Y[:, et, :], et == 0, et == KT - 1,
                           ("wout",) + yk[p], (("ps", bank),))
                    tt("dve", X[:, m, :], X[:, m, :], ps[bank][:, :], ALU.add, (("ps", bank),) + xk[p], xk[p])
                    if m % 2 == 0:
                        tt("pool", xnb[:, m, :], X[:, m, :], X[:, m, :], ALU.mult, xk[p], (("xnb", m),))
                    else:
                        tt("dve", xnb[:, m, :], X[:, m, :], X[:, m, :], ALU.mult, xk[p], (("xnb", m),))
                for kt in range(KT):
                    mm(ps[2][:, :], onesb[:, :], xnb[:, kt, :], kt == 0, kt == KT - 1, ("onesb", ("xnb", kt)), (("ps", 2),))
                ts("dve", rstd[:, :], ps[2][:, :], 1.0 / D_MODEL, ALU.mult, (("ps", 2),), ("rstd",), s2=EPS, op1=ALU.add)
                act(rstd[:, :], rstd[:, :], AF.Ln, ("rstd",), ("rstd",))
                act(rstd[:, :], rstd[:, :], AF.Exp, ("rstd",), ("rstd",), scale=-0.5)
                for kt in range(KT):
                    stt(X[:, kt, :], X[:, kt, :], nw_ap[:, kt:kt + 1], rstd[:, :], ALU.mult, ALU.mult,
                        xk[p] + ("rstd", ("vec", lf)), xk[p])
                out_toks.append(dma("sp", outT[:, t0:t0 + TB].rearrange("(kt p) t -> p kt t", p=128), X, xk[p], ()))

        waits = []
        for tok in out_toks:
            S._tok_wait("sp", tok, waits)
        S.recs["sp"].append((waits, None, None))

        with nc.Block() as block:
            S.emit(block)
    return nc


_OFF = {"mq": 0, "mk": 512, "mv": 1024, "mi": 1536, "mf": 1540, "mz": 1544,
        "aq": 2056, "ak": 2568, "av": 3080, "az": 3592}


def _rope_tables():
    inv = (1.0 / (np.float32(10000.0) ** (np.arange(0, 64, 2, dtype=np.float32) / np.float32(64.0)))).astype(np.float32)
    ang = np.arange(SEQ, dtype=np.float32)[:, None] * inv[None, :]
    cos = np.cos(ang).astype(np.float32)
    sin = np.sin(ang).astype(np.float32)
    cosT = np.ascontiguousarray(np.concatenate([cos, cos, cos, cos], 1).T)
    sinT = np.ascontiguousarray(np.concatenate([sin, sin, sin, sin], 1).T)
    return cosT, sinT


def _consts():
    c = np.zeros((128, 192), np.float32)
    c[:, 0:128] = np.eye(128, dtype=np.float32)
    c[0:64, 128:192] = np.triu(np.ones((64, 64), np.float32))
    return c


def _core_inputs(inp, c, stage_layers, need_wout):
    b, hd = c // 4, c % 4
    f32 = np.float32
    d = {}
    for l in stage_layers:
        w = np.asarray(inp["w_in"][l], f32)
        hs = slice(hd * 128, (hd + 1) * 128)

        def blk(name):
            return w[:, _OFF[name] + hd * 128:_OFF[name] + (hd + 1) * 128]

        def perm(m):
            return np.concatenate([m[:, 32:64], m[:, 0:32], m[:, 96:128], m[:, 64:96]], 1)

        aq, ak = blk("aq"), blk("ak")
        gi = w[:, _OFF["mi"] + hd:_OFF["mi"] + hd + 1]
        gf = w[:, _OFF["mf"] + hd:_OFF["mf"] + hd + 1]
        d[f"wfm{l}"] = np.ascontiguousarray(np.concatenate(
            [blk("mq"), blk("mk"), blk("mz"), aq, perm(aq), ak, perm(ak), blk("az"), gi, gf], 1))
        d[f"wtm{l}"] = np.ascontiguousarray(np.concatenate([blk("mv"), blk("av")], 1))
        lam = np.concatenate([np.asarray(inp[k][l], f32) for k in ("lam_q1", "lam_k1", "lam_q2", "lam_k2")])
        d[f"lamv{l}"] = np.ascontiguousarray(np.tile(lam[None, :], (128, 1)))
    for l in range(DEPTH):
        v = np.zeros((128, NV), f32)
        v[:, 0:8] = np.asarray(inp["norm_w"][l], f32).reshape(8, 128).T
        cw = np.asarray(inp["conv_w"][l], f32)
        cbv = np.asarray(inp["conv_b"][l], f32)
        v[:, 8:12] = cw[:, hd * 128:(hd + 1) * 128].T
        v[:, 12:16] = cw[:, 512 + hd * 128:512 + (hd + 1) * 128].T
        v[:, 16] = cbv[hd * 128:(hd + 1) * 128]
        v[:, 17] = cbv[512 + hd * 128:512 + (hd + 1) * 128]
        v[:, 18] = np.asarray(inp["m_norm_w"][l], f32)[hd * 128:(hd + 1) * 128]
        v[:, 19] = np.asarray(inp["m_skip"][l], f32)[hd * 128:(hd + 1) * 128]
        v[:, 20] = np.asarray(inp["a_norm_w"][l], f32)
        v[:, 21] = np.asarray(inp["i_bias"][l], f32)[hd]
        v[:, 22] = np.asarray(inp["f_bias"][l], f32)[hd]
        v[:, 23:31] = np.asarray(inp["final_norm_w"], f32).reshape(8, 128).T
        d[f"vecs{l}"] = v
    for l in need_wout:
        wo = np.asarray(inp["w_out"][l], f32)
        rows = []
        for r in range(4):
            rows.append(wo[r * 128:(r + 1) * 128])
            rows.append(wo[512 + r * 128:512 + (r + 1) * 128])
        d[f"wout{l}"] = np.ascontiguousarray(np.concatenate(rows, 0))
    return d


_PROG = {}


def _prog(stage, debug=False):
    key = (stage, debug)
    if key not in _PROG:
        _PROG[key] = build_program(stage, debug)
    return _PROG[key]


def kernel(**inp):
    x = np.asarray(inp["x"], np.float32)
    xTs = [np.ascontiguousarray(x[b].T) for b in range(BATCH)]
    cosT, sinT = _rope_tables()
    cst = _consts()
    nc = _prog("all")
    in_maps = []
    for c in range(NCORES):
        d = _core_inputs(inp, c, [0, 1], [0, 1])
        d.update({"xT": xTs[c // 4], "cosT": cosT, "sinT": sinT, "consts": cst})
        in_maps.append(d)
    res = run_bass_kernel_spmd(nc, in_maps, core_ids=list(range(NCORES)))
    out = np.empty((BATCH, SEQ, D_MODEL), np.float32)
    for b in range(BATCH):
        out[b] = res.results[4 * b]["outT"].T
    return out
```

```python
import math
from contextlib import ExitStack

import numpy as np
import ml_dtypes

import concourse.bass as bass
import concourse.mybir as mybir
from concourse.bass_utils import run_bass_kernel_spmd

F32 = mybir.dt.float32
BF16 = mybir.dt.bfloat16
AF = mybir.ActivationFunctionType
ALU = mybir.AluOpType
AX = mybir.AxisListType

D_MODEL = 1024
BATCH = 2
SEQ = 8192
DEPTH = 2
NCORES = 8
TB = 512
NBLK = SEQ // TB
KT = D_MODEL // 128
EPS = 1e-6
NV = 32
LN_KSCALE = math.log(128.0 ** -0.5)
GROUPS = [[0, 1, 2, 3], [4, 5, 6, 7]]
import os
_STOP = int(os.environ.get("K_STOP", "9"))
_NB = int(os.environ.get("K_NBLK", str(NBLK)))


class Sched:
    CH = 30000
    ND = 48

    def __init__(self, nc, es):
        self.nc = nc
        self.engs = ["pe", "act", "dve", "pool", "sp"]
        self.recs = {e: [] for e in self.engs}
        self.cnt = {e: 0 for e in self.engs}
        self.seen = {e: {} for e in self.engs}
        self.lastw = {}
        self.readers = {}
        nsem = {"pe": 3, "act": 3, "dve": 4, "pool": 3, "sp": 1}
        self.esems = {e: [es.enter_context(nc.semaphore(f"s_{e}_{i}")) for i in range(nsem[e])]
                      for e in self.engs}
        self.dsems = [es.enter_context(nc.semaphore(f"d_{i}")) for i in range(self.ND)]
        self.dval = [0] * self.ND
        self.dnext = 0
        self.ccsem = es.enter_context(nc.semaphore("ccsem"))
        self.ccval = 0

    def _tok_wait(self, e, tok, waits):
        if tok[0] == "e":
            _, e2, n = tok
            if e2 == e and e == "pe":
                return
            if self.seen[e].get(e2, 0) >= n:
                return
            self.seen[e][e2] = n
            waits.append((self.esems[e2][(n - 1) // self.CH], (n - 1) % self.CH + 1))
        else:
            kind, i, v = tok
            key = (kind, i)
            if self.seen[e].get(key, 0) >= v:
                return
            self.seen[e][key] = v
            sem = self.dsems[i] if kind == "d" else self.ccsem
            waits.append((sem, v))

    def issue(self, e, fn, reads=(), writes=(), dma=False, cc=False):
        deps = []
        for k in reads:
            if k in self.lastw:
                deps.append(self.lastw[k])
        for k in writes:
            if k in self.lastw:
                deps.append(self.lastw[k])
            deps.extend(self.readers.get(k, {}).values())
        waits = []
        for tok in deps:
            self._tok_wait(e, tok, waits)
        if dma:
            i = self.dnext
            self.dnext = (self.dnext + 1) % self.ND
            prev = self.dval[i]
            if prev > 0:
                self._tok_wait(e, ("d", i, prev), waits)
            self.dval[i] += 16
            tok = ("d", i, self.dval[i])
            inc = (self.dsems[i], 16)
        elif cc:
            self.ccval += 1
            tok = ("c", 0, self.ccval)
            inc = (self.ccsem, 1)
        elif fn is None:
            tok = None
            inc = None
        else:
            self.cnt[e] += 1
            n = self.cnt[e]
            tok = ("e", e, n)
            inc = (self.esems[e][(n - 1) // self.CH], 1)
        self.recs[e].append((waits, fn, inc))
        if tok is not None:
            for k in reads:
                self.readers.setdefault(k, {})[(tok[0], tok[1])] = tok
            for k in writes:
                self.lastw[k] = tok
                self.readers[k] = {}
        return tok

    def emit(self, block):
        nc = self.nc

        def run(e, eng):
            for waits, fn, inc in self.recs[e]:
                for s, v in waits:
                    eng.wait_ge(s, v)
                if fn is not None:
                    ins = fn(eng)
                    ins.then_inc(inc[0], inc[1])

        @block.tensor
        def _(eng):
            run("pe", eng)

        @block.scalar
        def _(eng):
            run("act", eng)

        @block.vector
        def _(eng):
            run("dve", eng)

        @block.gpsimd
        def _(eng):
            run("pool", eng)

        @block.sync
        def _(eng):
            run("sp", eng)


def build_program(stage="all", debug=False):
    nc = bass.Bass("TRN2", target_bir_lowering=False)
    with ExitStack() as es:
        S = Sched(nc, es)

        def dram(name, shape, dt, kind):
            return nc.dram_tensor(name, shape, dt, kind=kind).ap()

        fused = stage == "all"
        layers = {"all": [0, 1], "l0": [0], "mid": [1], "fin": []}[stage]
        do_fin = stage in ("all", "fin")

        xT = dram("xT", [D_MODEL, SEQ], F32, "ExternalInput") if stage in ("all", "l0", "mid") else None
        cosT = dram("cosT", [128, SEQ], F32, "ExternalInput") if layers else None
        sinT = dram("sinT", [128, SEQ], F32, "ExternalInput") if layers else None
        consts = dram("consts", [128, 192], F32, "ExternalInput")
        wfm, wtm, wout, vecs, lamv = {}, {}, {}, {}, {}
        for l in layers:
            wfm[l] = dram(f"wfm{l}", [D_MODEL, 1026], F32, "ExternalInput")
            wtm[l] = dram(f"wtm{l}", [D_MODEL, 256], F32, "ExternalInput")
            lamv[l] = dram(f"lamv{l}", [128, 256], F32, "ExternalInput")
        for l in range(DEPTH):
            vecs[l] = dram(f"vecs{l}", [128, NV], F32, "ExternalInput")
        need_wout = {"all": [0, 1], "l0": [], "mid": [0], "fin": [1]}[stage]
        for l in need_wout:
            wout[l] = dram(f"wout{l}", [D_MODEL, D_MODEL], F32, "ExternalInput")

        ycl, ycf = {}, {}
        for l in layers:
            kind = "Internal" if fused else "ExternalOutput"
            ycl[l] = [dram(f"ycl{l}_{q}", [256, 2048], BF16, kind) for q in range(4)]
        for l in need_wout:
            kind = "Internal" if fused else "ExternalInput"
            ycf[l] = [dram(f"ycf{l}_{q}", [1024, 2048], BF16, kind) for q in range(4)]
        x1s = None
        if stage == "all":
            x1s = dram("x1s", [D_MODEL, SEQ], F32, "Internal")
        elif stage == "mid":
            x1s = dram("x1s", [D_MODEL, SEQ], F32, "ExternalOutput")
        elif stage == "fin":
            x1s = dram("x1s", [D_MODEL, SEQ], F32, "ExternalInput")
        outT = dram("outT", [D_MODEL, SEQ], F32, "ExternalOutput") if do_fin else None
        dbg = {}
        if debug:
            for nm, shp, dt in [("d_qm", [128, SEQ], BF16), ("d_km", [128, SEQ], BF16),
                                ("d_qa", [128, SEQ], BF16), ("d_ka", [128, SEQ], BF16),
                                ("d_rows", [1, 9 * SEQ], F32)]:
                dbg[nm] = dram(nm, shp, dt, "ExternalOutput")

        def sb(name, shape, dt):
            return es.enter_context(nc.sbuf_tensor(name, shape, dt))

        cst = sb("cst", [128, 192], F32)
        ident_f = cst[:, 0:128]
        identb = sb("identb", [128, 128], BF16)
        onesb = sb("onesb", [128, 128], BF16)
        onesf = sb("onesf", [1, 512], F32)
        vec = [sb(f"vec{l}", [128, NV], F32) for l in range(DEPTH)]
        lamt = sb("lamt", [128, 256], F32)
        lamw = sb("lamw", [128, 4], F32)
        neglam = sb("neglam", [128, 1], F32)
        negfb = sb("negfb", [1, 1], F32)

        wfm_b = sb("wfm_b", [128, KT, 1026], BF16)
        wtm_b = sb("wtm_b", [128, KT, 256], BF16)
        wout_b = sb("wout_b", [128, KT, D_MODEL], BF16)
        wstg = [sb(f"wstg{i}", [128, KT, 128], F32) for i in range(2)]

        xblk = sb("xblk", [128, KT, TB], F32)
        sqb = sb("sqb", [128, KT, TB], BF16)
        xnb = sb("xnb", [128, KT, TB], BF16)
        ycb = sqb
        rstd = sb("rstd", [128, TB], F32)
        cosb = sb("cosb", [128, TB], F32)
        sinb = sb("sinb", [128, TB], F32)
        preq = sb("preq", [128, TB + 4], F32)
        prek = sb("prek", [128, TB + 4], F32)
        cva = sb("tA", [128, TB], F32)
        cvb = sb("tB", [128, TB], F32)
        tC = sb("tC", [128, TB], F32)
        tD = sb("tD", [128, TB], F32)
        rpa, rpb = cva, cvb
        qa2 = [sb(f"qa{i}", [128, TB], BF16) for i in range(2)]
        qm2 = [sb(f"qm{i}", [128, TB], BF16) for i in range(2)]
        km2 = [sb(f"km{i}", [128, TB], BF16) for i in range(2)]
        qs = sb("qs", [128, TB], BF16)
        gm2 = [sb(f"gm{i}", [128, TB], F32) for i in range(2)]
        gtA = sb("gtA", [128, TB], F32)
        gtB = sb("gtB", [128, TB], F32)
        ga2 = [sb(f"ga{i}", [128, TB], F32) for i in range(2)]
        kaT = sb("kaT", [128, SEQ], BF16)
        va = sb("va", [128, SEQ // 128, 132], BF16)
        vmc2 = [sb(f"vmc{i}", [64, 8, 132], BF16) for i in range(2)]
        vmw = sb("vmw", [64, 8, 132], BF16)
        ktok = sb("ktok", [64, 8, 128], BF16)
        NR = 9
        rows = sb("rows", [1, NR, TB], F32)
        gif2 = [sb(f"gif{i}", [1, 2, TB], F32) for i in range(2)]
        carry = sb("carry", [1, 4], F32)
        ape = sb("ape", [1, 8], F32)
        dec = sb("dec", [1, 8], F32)
        cols = sb("cols", [64, 24], F32)
        decb = sb("decb", [128, 8], F32)
        wT = sb("wT", [64, 8, 64], F32)
        stl = [sb(f"stl{i}", [64, 64], BF16) for i in range(2)]
        cf = sb("cf", [128, 132], F32)
        cb = sb("cb", [128, 132], BF16)
        nums = sb("nums", [64, 8, 132], F32)
        sq8 = sb("sq8", [64, 8, 128], F32)
        sm = sb("sm", [64, 8, 8], F32)
        hmb = sb("hmb", [64, 8, 128], BF16)
        gt1, gt2 = gtA, gtB
        yo = sb("yo", [128, TB], BF16)
        pT = [sb(f"pT{i}", [128, 512], BF16) for i in range(2)]
        at1 = sb("at1", [128, 128], F32)
        at2 = sb("at2", [128, 128], F32)
        atj = sb("atj", [128, 128], F32)
        asm = sb("asm", [128, 8], F32)
        hab = sb("hab", [128, 128], BF16)
        yab = sb("yab", [128, TB], BF16)
        ob = xblk

        ps_all = es.enter_context(nc.psum_tensor("ps_all", [128, 8, 512], F32))
        ps = [ps_all[:, i, :] for i in range(8)]

        def I(e, fn, r=(), w=(), **kw):
            return S.issue(e, fn, r, w, **kw)

        def dma(q, out, in_, r, w):
            return I(q, lambda e: e.dma_start(out=out, in_=in_), r, w, dma=True)

        def act(out, in_, func, r, w, scale=1.0, bias=0.0, accum=None):
            if accum is None:
                return I("act", lambda e: e.activation(out, in_, func, bias=bias, scale=scale), r, w)
            return I("act", lambda e: e.activation(out, in_, func, bias=bias, scale=scale, accum_out=accum), r, w)

        def tt(eng, out, a, b, op, r, w):
            return I(eng, lambda e: e.tensor_tensor(out, a, b, op), r, w)

        def ts(eng, out, a, s1, op0, r, w, s2=None, op1=ALU.bypass):
            return I(eng, lambda e: e.tensor_scalar(out, a, s1, s2, op0, op1), r, w)

        def stt(out, a, sc, b, op0, op1, r, w):
            return I("dve", lambda e: e.scalar_tensor_tensor(out, a, sc, b, op0, op1), r, w)

        def cp(eng, out, in_, r, w):
            if eng == "act":
                return I(eng, lambda e: e.copy(out, in_), r, w)
            return I(eng, lambda e: e.tensor_copy(out, in_), r, w)

        def mm(out, lhsT, rhs, start, stop, r, w):
            return I("pe", lambda e: e.matmul(out, lhsT, rhs, start=start, stop=stop), r, w)

        def tr(out, in_, idn, r, w):
            return I("pe", lambda e: e.transpose(out, in_, idn), r, w)

        def psb(i):
            return ps[i].bitcast(BF16)

        XB = tuple(("xblk", k) for k in range(KT))
        SQ = tuple(("sqb", k) for k in range(KT))
        XN = tuple(("xnb", k) for k in range(KT))

        dma("sp", cst[:, :], consts[:, :], (), ("cst",))
        for l in range(DEPTH):
            dma("sp", vec[l][:, :], vecs[l][:, :], (), (("vec", l),))
        cp("dve", identb[:, :], cst[:, 0:128], ("cst",), ("identb",))
        I("pool", lambda e: e.memset(onesb[:, :], 1.0), (), ("onesb",))
        I("pool", lambda e: e.memset(onesf[:, :], 1.0), (), ("onesf",))
        I("pool", lambda e: e.memset(va[:, :, 128:132], 1.0), (), ("va_ones",))
        for i in range(2):
            I("pool", lambda e, i=i: e.memset(vmc2[i][:, :, 128:132], 1.0), (), ("vmc_ones",))
        mask64 = cst[0:64, 128:192]

        def load_weights(l):
            nchunk = 11
            for ci in range(nchunk):
                st = wstg[ci % 2]
                sk = ("wstg", ci % 2)
                if ci < 8:
                    src = wfm[l][:, ci * 128:(ci + 1) * 128]
                    dst = wfm_b[:, :, ci * 128:(ci + 1) * 128]
                    ncol = 128
                elif ci == 8:
                    src = wfm[l][:, 1024:1026]
                    dst = wfm_b[:, :, 1024:1026]
                    ncol = 2
                else:
                    src = wtm[l][:, (ci - 9) * 128:(ci - 8) * 128]
                    dst = wtm_b[:, :, (ci - 9) * 128:(ci - 8) * 128]
                    ncol = 128
                dma("sp", st[:, :, 0:ncol], src.rearrange("(kt p) c -> p kt c", p=128), (), (sk,))
                eng = "pool" if ci % 2 == 0 else "dve"
                cp(eng, dst, st[:, :, 0:ncol], (sk,), ("win",))
                if ci in (4, 6):
                    for m in range(2):
                        sl = wfm_b[:, :, ci * 128 + m * 64: ci * 128 + m * 64 + 32]
                        ts("pool", sl, sl, -1.0, ALU.mult, ("win",), ("win",))
        def load_lam(l):
            dma("sp", lamt[:, :], lamv[l][:, :], (), ("lamt",))
            for i in range(2):
                tt("dve", lamt[:, i * 128:i * 128 + 64], lamt[:, i * 128:i * 128 + 64],
                   lamt[:, i * 128 + 64:i * 128 + 128], ALU.mult, ("lamt",), ("lamt",))
                I("dve", lambda e, i=i: e.reduce_sum(lamw[:, i:i + 1], lamt[:, i * 128:i * 128 + 64], AX.X),
                  ("lamt",), ("lamw",))
            act(lamw[:, 2:4], lamw[:, 0:2], AF.Exp, ("lamw",), ("lamw",))
            lam_init = 0.8 - 0.6 * math.exp(-0.3 * l)
            stt(neglam[:, :], lamw[:, 3:4], -lam_init, lamw[:, 2:3], ALU.add, ALU.subtract, ("lamw",), ("neglam",))
            ts("pool", negfb[:, :], vec[l][0:1, 22:23], -1.0, ALU.mult, (("vec", l),), ("negfb",))

        def load_wout(l):
            for ci in range(8):
                st = wstg[ci % 2]
                sk = ("wstg", ci % 2)
                dma("sp", st[:, :, :], wout[l][:, ci * 128:(ci + 1) * 128].rearrange("(kt p) c -> p kt c", p=128),
                    (), (sk,))
                eng = "pool" if ci % 2 == 0 else "dve"
                cp(eng, wout_b[:, :, ci * 128:(ci + 1) * 128], st[:, :, :], (sk,), ("wout",))

        def outproj_loads(l_prev, tb, xsrc, xkey):
            t0 = tb * TB
            q4, tq = tb // 4, (tb % 4) * TB
            dma("sp", ycb[:, :, :], ycf[l_prev][q4][:, tq:tq + TB].rearrange("(kt p) t -> p kt t", p=128),
                (("ycf", l_prev, q4),), SQ)
            dma("sp", xblk[:, :, :], xsrc[:, t0:t0 + TB].rearrange("(kt p) t -> p kt t", p=128),
                (xkey,), XB)

        def outproj_block(l_prev, tb, xsrc, xkey, banks=(0,), load=True):
            if load:
                outproj_loads(l_prev, tb, xsrc, xkey)
            for m in range(KT):
                bank = banks[m % len(banks)]
                for et in range(KT):
                    mm(ps[bank][:, :], wout_b[:, et, m * 128:(m + 1) * 128], ycb[:, et, :], et == 0, et == KT - 1,
                       ("wout", ("sqb", et)), (("ps", bank),))
                tt("dve", xblk[:, m, :], xblk[:, m, :], ps[bank][:, :], ALU.add, (("ps", bank), ("xblk", m)),
                   (("xblk", m),))
                yield

        def norm_block(nw_ap, to_out):
            for kt in range(KT):
                tt("pool" if kt % 2 == 0 else "dve", sqb[:, kt, :], xblk[:, kt, :], xblk[:, kt, :], ALU.mult,
                   (("xblk", kt),), (("sqb", kt),))
            for kt in range(KT):
                mm(ps[0][:, :], onesb[:, :], sqb[:, kt, :], kt == 0, kt == KT - 1, ("onesb", ("sqb", kt)), (("ps", 0),))
            yield
            ts("dve", rstd[:, :], ps[0][:, :], 1.0 / D_MODEL, ALU.mult, (("ps", 0),), ("rstd",), s2=EPS, op1=ALU.add)
            act(rstd[:, :], rstd[:, :], AF.Ln, ("rstd",), ("rstd",))
            act(rstd[:, :], rstd[:, :], AF.Exp, ("rstd",), ("rstd",), scale=-0.5)
            for kt in range(KT):
                if to_out:
                    stt(ob[:, kt, :], xblk[:, kt, :], nw_ap[:, kt:kt + 1], rstd[:, :], ALU.mult, ALU.mult,
                        (("xblk", kt), "rstd"), (("xblk", kt),))
                else:
                    stt(xnb[:, kt, :], xblk[:, kt, :], nw_ap[:, kt:kt + 1], rstd[:, :], ALU.mult, ALU.mult,
                        (("xblk", kt), "rstd"), (("xnb", kt),))
            yield

        def inproj_block(l, tb):
            t0 = tb * TB
            pp = tb % 2
            qa, ga = qa2[pp], ga2[pp]
            qak, gak = ("qa", pp), ("ga", pp)
            qm, km, gm, vmc, gif = qm2[pp], km2[pp], gm2[pp], vmc2[pp], gif2[pp]
            V = vec[l]
            vk = ("vec", l)
            dma("sp", cosb[:, :], cosT[:, t0:t0 + TB], (), ("cosb",))
            dma("sp", sinb[:, :], sinT[:, t0:t0 + TB], (), ("sinb",))

            def fm(c0, ncol):
                def f(b):
                    for kt in range(KT):
                        mm(ps[b][0:ncol, :], wfm_b[:, kt, c0:c0 + ncol], xnb[:, kt, :], kt == 0, kt == KT - 1,
                           ("win", ("xnb", kt)), (("ps", b),))
                return f

            def conv_ev(which):
                pre, dst, cw0, cbc, fin, fk = [(preq, qm, 8, 16, tC, "tC"), (prek, km, 12, 17, tD, "tD")][which]
                pk = ("pre", which)

                def e1(b):
                    if tb == 0:
                        I("pool", lambda e: e.memset(pre[:, 0:4], 0.0), (), (pk,))
                    else:
                        cp("pool", pre[:, 1:4], pre[:, TB + 1:TB + 4], (pk,), (pk,))
                    cp("dve", pre[:, 4:4 + TB], ps[b][:, :], (("ps", b), pk), (pk,))
                    ts("dve", cva[:, :], pre[:, 4:4 + TB], V[:, cw0 + 3:cw0 + 4], ALU.mult, (pk, vk), ("tA",),
                       s2=V[:, cbc:cbc + 1], op1=ALU.add)
                    stt(cvb[:, :], pre[:, 3:3 + TB], V[:, cw0 + 2:cw0 + 3], cva[:, :], ALU.mult, ALU.add,
                        (pk, vk, "tA"), ("tB",))
                    stt(cva[:, :], pre[:, 2:2 + TB], V[:, cw0 + 1:cw0 + 2], cvb[:, :], ALU.mult, ALU.add,
                        (pk, vk, "tB"), ("tA",))
                    stt(fin[:, :], pre[:, 1:1 + TB], V[:, cw0:cw0 + 1], cva[:, :], ALU.mult, ALU.add,
                        (pk, vk, "tA"), (fk,))

                def e2(b):
                    act(dst[:, :], fin[:, :], AF.Silu, (fk,), (("qm", pp) if which == 0 else ("km", pp),))
                return [e1, (SILU_LATE, e2)]

            SILU_LATE = 99

            def silu_ev(dst, dk):
                return [lambda b: cp("dve", dst[:, :], ps[b][:, :], (("ps", b),), (dk,)),
                        (SILU_LATE, lambda b: act(dst[:, :], dst[:, :], AF.Silu, (dk,), (dk,)))]

            def rope_a(b):
                tt("dve", rpa[:, :], ps[b][:, :], cosb[:, :], ALU.mult, (("ps", b), "cosb"), ("tA",))

            def rope_b(which):
                def f(b):
                    tt("dve", rpb[:, :], ps[b][:, :], sinb[:, :], ALU.mult, (("ps", b), "sinb"), ("tB",))
                    if which == 0:
                        tt("pool", qa[:, :], rpa[:, :], rpb[:, :], ALU.add, ("tA", "tB"), (qak,))
                    else:
                        tt("pool", kaT[:, t0:t0 + TB], rpa[:, :], rpb[:, :], ALU.add, ("tA", "tB"), (("ka", tb),))
                return f

            def row_ev(g):
                return [lambda b: cp("dve", gif[:, g, :], ps[b][0:1, :], (("ps", b),), (("gif", pp),))]

            def vm_mm(h):
                def f(b):
                    for c4 in range(4):
                        c8 = h * 4 + c4
                        for kt in range(KT):
                            mm(ps[b][0:64, c4 * 128:(c4 + 1) * 128], xnb[:, kt, c8 * 64:(c8 + 1) * 64],
                               wtm_b[:, kt, 0:128], kt == 0, kt == KT - 1, ("win", ("xnb", kt)), (("ps", b),))
                return f

            def vm_ev(h):
                return [lambda b: cp("dve", vmc[:, h * 4:(h + 1) * 4, 0:128],
                                     ps[b][0:64, :].rearrange("p (c d) -> p c d", d=128), (("ps", b),), (("vmc", pp),))]

            def va_mm(b):
                for t4 in range(4):
                    for kt in range(KT):
                        mm(ps[b][:, t4 * 128:(t4 + 1) * 128], xnb[:, kt, t4 * 128:(t4 + 1) * 128],
                           wtm_b[:, kt, 128:256], kt == 0, kt == KT - 1, ("win", ("xnb", kt)), (("ps", b),))

            def va_ev(b):
                cp("dve", va[:, tb * 4:(tb + 1) * 4, 0:128], ps[b][:, :].rearrange("p (c d) -> p c d", d=128),
                   (("ps", b), "va_ones"), tuple(("va", tb * 4 + i) for i in range(4)))

            stages = [
                (fm(0, 128), conv_ev(0)),
                (fm(128, 128), conv_ev(1)),
                (fm(256, 128), silu_ev(gm, ("gm", pp))),
                (fm(3 * 128, 128), [rope_a]),
                (fm(4 * 128, 128), [rope_b(0)]),
                (fm(5 * 128, 128), [rope_a]),
                (fm(6 * 128, 128), [rope_b(1)]),
                (fm(7 * 128, 128), silu_ev(ga, gak)),
                (fm(1024, 1), row_ev(0)),
                (fm(1025, 1), row_ev(1)),
                (vm_mm(0), vm_ev(0)),
                (vm_mm(1), vm_ev(1)),
                (va_mm, [va_ev]),
            ]
            pending = []
            for k, (mmf, evs) in enumerate(stages):
                b = 0
                mmf(b)
                for i, fn in enumerate(evs):
                    if isinstance(fn, tuple):
                        pending.append((fn[0], fn[1], b))
                    else:
                        pending.append((k + i, fn, b))
                for due, fn, bb in [p for p in pending if p[0] <= k]:
                    fn(bb)
                pending = [p for p in pending if p[0] > k]
                yield
            for due, fn, bb in sorted(pending, key=lambda p: p[0]):
                fn(bb)
            yield

        R_I, R_F, R_L1, R_BN, R_A_, R_AA, R_WI, R_WK, R_EM = range(9)
        R_T2, R_T1, R_ALS = R_I, R_F, R_L1

        def rw(i):
            return rows[:, i, :]

        def gates_block(l, tb):
            V = vec[l]
            vk = ("vec", l)
            rk = lambda i: ("row", i)
            pp = tb % 2
            qm, vmc, gif = qm2[pp], vmc2[pp], gif2[pp]
            gk = ("gif", pp)
            if tb == 0:
                I("pool", lambda e: e.memset(carry[:, :], 0.0), (), ("carry",))
            act(rw(R_T1), gif[:, 1, :], AF.Exp, (gk, "negfb"), (rk(R_T1),), scale=-1.0, bias=negfb[:, :])
            act(rw(R_L1), rw(R_T1), AF.Ln, (rk(R_T1),), (rk(R_L1),), bias=1.0)
            I("dve", lambda e: e.tensor_tensor_scan(rw(R_BN), onesf[:, :], rw(R_L1), carry[:, 0:1], ALU.mult, ALU.add),
              ("onesf", rk(R_L1), "carry"), (rk(R_BN),))
            stt(rw(R_A_), gif[:, 0, :], V[0:1, 21:22], rw(R_BN), ALU.add, ALU.add, (gk, vk, rk(R_BN)), (rk(R_A_),))
            I("dve", lambda e: e.tensor_tensor_scan(rw(R_AA), onesf[:, :], rw(R_A_), carry[:, 1:2], ALU.mult, ALU.max),
              ("onesf", rk(R_A_), "carry"), (rk(R_AA),))
            yield
            A3 = rw(R_AA).rearrange("p (c i) -> p c i", i=64)
            aend = A3[:, :, 63]
            cp("pool", ape[:, 0:1], carry[:, 1:2], ("carry",), ("ape",))
            cp("pool", ape[:, 1:8], A3[:, 0:7, 63], (rk(R_AA),), ("ape",))
            tt("dve", rw(R_T1).rearrange("p (c i) -> p c i", i=64), ape[:, :].unsqueeze(2).broadcast_to([1, 8, 64]),
               A3, ALU.subtract, ("ape", rk(R_AA)), (rk(R_T1),))
            act(rw(R_WI), rw(R_T1), AF.Exp, (rk(R_T1),), (rk(R_WI),))
            tt("dve", dec[:, :], ape[:, :], aend, ALU.subtract, ("ape", rk(R_AA)), ("dec",))
            act(dec[:, :], dec[:, :], AF.Exp, ("dec",), ("dec",))
            tt("dve", rw(R_T2).rearrange("p (c i) -> p c i", i=64), rw(R_A_).rearrange("p (c i) -> p c i", i=64),
               aend.unsqueeze(2).broadcast_to([1, 8, 64]), ALU.subtract, (rk(R_A_), rk(R_AA)), (rk(R_T2),))
            act(rw(R_WK), rw(R_T2), AF.Exp, (rk(R_T2),), (rk(R_WK),), bias=LN_KSCALE)
            tt("dve", rw(R_T1), rw(R_BN), rw(R_AA), ALU.subtract, (rk(R_BN), rk(R_AA)), (rk(R_T1),))
            act(rw(R_EM), rw(R_T1), AF.Exp, (rk(R_T1),), (rk(R_EM),))
            ts("dve", rw(R_ALS), rw(R_A_), LN_KSCALE, ALU.add, (rk(R_A_),), (rk(R_ALS),))
            cp("pool", carry[:, 0:1], rw(R_BN)[:, TB - 1:TB], (rk(R_BN),), ("carry",))
            cp("pool", carry[:, 1:2], rw(R_AA)[:, TB - 1:TB], (rk(R_AA), "ape"), ("carry",))
            yield
            for qi, ri in enumerate([R_ALS, R_WK, R_EM]):
                for c8 in range(8):
                    mm(ps[1][0:64, 480 + qi * 8 + c8:480 + qi * 8 + c8 + 1], rw(ri)[:, c8 * 64:(c8 + 1) * 64],
                       onesf[:, 0:1], True, True, (rk(ri), "onesf"), (("ps", 1),))
            cp("dve", cols[:, :], ps[1][0:64, 480:504], (("ps", 1),), ("cols",))
            mm(ps[1][:, 504:512], onesf[:, 0:128], dec[:, :], True, True, ("onesf", "dec"), (("ps", 1),))
            cp("dve", decb[:, :], ps[1][:, 504:512], (("ps", 1),), ("decb",))
            yield
            mm(ps[1][0:64, :], onesf[:, 0:64], rw(R_AA), True, True, ("onesf", rk(R_AA)), (("ps", 1),))
            for c8 in range(8):
                act(wT[:, c8, :], ps[1][0:64, c8 * 64:(c8 + 1) * 64], AF.Exp, (("ps", 1), "cols"), (("wT", c8),),
                    scale=-1.0, bias=cols[:, c8:c8 + 1])
            tt("pool", wT[:, :, :], wT[:, :, :], mask64.unsqueeze(1).broadcast_to([64, 8, 64]), ALU.mult,
               tuple(("wT", c) for c in range(8)) + ("cst",), tuple(("wT", c) for c in range(8)))
            yield
            mm(ps[1][:, :], onesf[:, 0:128], rw(R_WI), True, True, ("onesf", rk(R_WI)), (("ps", 1),))
            tt("dve", qs[:, :], qm[:, :], ps[1][:, :], ALU.mult, (("qm", pp), ("ps", 1)), ("qs",))
            tt("pool", vmw[:, :, 0:129], vmc[:, :, 0:129], cols[:, 8:16].unsqueeze(2).broadcast_to([64, 8, 129]),
               ALU.mult, (("vmc", pp), "vmc_ones", "cols"), ("vmw",))
            yield

        def mlstm_block(l, tb, ycl_ap, okey, res):
            V = vec[l]
            vk = ("vec", l)
            pp = tb % 2
            qm, km, gm, vmc = qm2[pp], km2[pp], gm2[pp], vmc2[pp]
            qmk, kmk, gmk, vmck = ("qm", pp), ("km", pp), ("gm", pp), ("vmc", pp)
            if tb == 0:
                I("pool", lambda e: e.memset(cf[:, :], 0.0), (), ("cf",))
                I("pool", lambda e: e.memset(cb[:, :], 0.0), (), ("cb",))
            for c8 in range(8):
                tr(psb(1)[0:64, c8 * 128:(c8 + 1) * 128], km[:, c8 * 64:(c8 + 1) * 64], identb[:, :],
                   (kmk, "identb"), (("ps", 1),))
            cp("dve", ktok[:, :, :], psb(1)[0:64, :].rearrange("p (c d) -> p c d", d=128), (("ps", 1),), ("ktok",))
            yield

            def st(c8):
                cs = c8 * 64
                so = (c8 % 2) * 64
                mm(ps[1][0:64, so:so + 64], km[:, cs:cs + 64], qm[:, cs:cs + 64], True, True, (kmk, qmk), (("ps", 1),))

            st(0)
            for c8 in range(8):
                cs = c8 * 64
                so = (c8 % 2) * 64
                if c8 + 1 < 8:
                    st(c8 + 1)
                tt("dve", stl[c8 % 2][:, :], ps[1][0:64, so:so + 64], wT[:, c8, :], ALU.mult, (("ps", 1), ("wT", c8)),
                   (("stl", c8 % 2),))
                mm(ps[1][:, 257:386], ktok[:, c8, :], vmw[:, c8, 0:129], True, True, ("ktok", "vmw"), (("ps", 1),))
                mm(ps[1][0:64, 128:257], qs[:, cs:cs + 64], cb[:, 0:129], True, False, ("qs", "cb"), (("ps", 1),))
                mm(ps[1][0:64, 128:257], stl[c8 % 2][:, :], vmc[:, c8, 0:129], False, True,
                   (("stl", c8 % 2), vmck, "vmc_ones"), (("ps", 1),))
                stt(cf[:, 0:129], cf[:, 0:129], decb[:, c8:c8 + 1], ps[1][:, 257:386], ALU.mult, ALU.add,
                    ("cf", "decb", ("ps", 1)), ("cf",))
                cp("dve", cb[:, 0:129], cf[:, 0:129], ("cf",), ("cb",))
                cp("dve", nums[:, c8, 0:129], ps[1][0:64, 128:257], (("ps", 1),), (("nums", c8),))
                yield
            NUMS = tuple(("nums", c) for c in range(8))
            den = nums[:, :, 128]
            tt("dve", sq8[:, :, :], nums[:, :, 0:128], nums[:, :, 0:128], ALU.mult, NUMS, ("sq8",))
            I("dve", lambda e: e.reduce_sum(sm[:, :, 0], sq8[:, :, :], AX.X), ("sq8",), ("sm",))
            ts("dve", sm[:, :, 1], den, -1.0, ALU.mult, NUMS, ("sm",))
            tt("dve", sm[:, :, 1], sm[:, :, 1], den, ALU.max, ("sm",) + NUMS, ("sm",))
            tt("dve", sm[:, :, 1], sm[:, :, 1], cols[:, 16:24], ALU.max, ("sm", "cols"), ("sm",))
            tt("dve", sm[:, :, 2], sm[:, :, 1], sm[:, :, 1], ALU.mult, ("sm",), ("sm",))
            ts("dve", sm[:, :, 0], sm[:, :, 0], 1.0 / 128.0, ALU.mult, ("sm",), ("sm",))
            stt(sm[:, :, 3], sm[:, :, 2], EPS, sm[:, :, 0], ALU.mult, ALU.add, ("sm",), ("sm",))
            act(sm[:, :, 4], sm[:, :, 3], AF.Ln, ("sm",), ("sm",))
            act(sm[:, :, 5], sm[:, :, 4], AF.Exp, ("sm",), ("sm",), scale=-0.5)
            tt("dve", hmb[:, :, :], nums[:, :, 0:128], sm[:, :, 5:6].broadcast_to([64, 8, 128]), ALU.mult,
               NUMS + ("sm",), ("hmb",))
            for c8 in range(8):
                tr(psb(1)[:, c8 * 64:(c8 + 1) * 64], hmb[:, c8, :], identb[0:64, 0:64], ("hmb", "identb"),
                   (("ps", 1),))
            yield
            ts("dve", gt1[:, :], qm[:, :], V[:, 19:20], ALU.mult, (qmk, vk), ("gtA",))
            stt(gt2[:, :], psb(1)[:, 0:TB], V[:, 18:19], gt1[:, :], ALU.mult, ALU.add, (("ps", 1), vk, "gtA"),
                ("gtB",))
            tt("dve", yo[:, :], gt2[:, :], gm[:, :], ALU.mult, ("gtB", gmk), ("yo",))
            res.append(dma("sp", ycl_ap, yo[:, :], ("yo",), (okey,)))
            yield

        def attn_block(l, tb, ycl_ap, okey, res):
            V = vec[l]
            vk = ("vec", l)
            qa, ga = qa2[tb % 2], ga2[tb % 2]
            qak, gak = ("qa", tb % 2), ("ga", tb % 2)
            lam_init = 0.8 - 0.6 * math.exp(-0.3 * l)
            tiles = []
            for g2 in range(2):
                qt0 = tb * 4 + g2 * 2
                for j in range(qt0 + 2):
                    tiles.append((g2, qt0, j))

            def emit_S(ti):
                g2, qt0, j = tiles[ti]
                qoff = g2 * 256
                lo = 0 if j <= qt0 else 128
                sb0 = 2 + 2 * (ti % 2)
                kb = j // 4
                for m in range(2):
                    mm(ps[sb0 + m][:, lo:256], kaT[m * 64:(m + 1) * 64, j * 128:(j + 1) * 128],
                       qa[m * 64:(m + 1) * 64, qoff + lo:qoff + 256], True, True, (("ka", kb), qak),
                       (("ps", sb0), ("ps", sb0 + 1)))

            touched = set()
            emit_S(0)
            if len(tiles) > 1:
                emit_S(1)
            for ti, (g2, qt0, j) in enumerate(tiles):
                if j == 0:
                    touched = set()
                qoff = g2 * 256
                lo = 0 if j <= qt0 else 128
                sb0 = 2 + 2 * (ti % 2)
                pt = pT[ti % 2]
                ptk = ("pT", ti % 2)
                src_ = ps_all[:, sb0:sb0 + 2, lo:256]
                dst = pt[:, :].rearrange("p (m q) -> p m q", m=2)[:, :, lo:256]
                act(dst, src_, AF.Exp, (("ps", sb0), ("ps", sb0 + 1)), (ptk,), scale=0.125)
                if j >= qt0:
                    d0 = (j - qt0) * 128
                    msl = pt[64:128, :].rearrange("p (m q) -> p m q", m=2)[:, :, d0:d0 + 64]
                    I("pool", lambda e, msl=msl: e.memset(msl, 0.0), (ptk,), (ptk,))
                if ti + 2 < len(tiles):
                    emit_S(ti + 2)
                for qt in range(2):
                    if j > qt0 + qt:
                        continue
                    pb = 6 + qt
                    for m in range(2):
                        first = pb not in touched
                        touched.add(pb)
                        last = (j == qt0 + qt) and m == 1
                        mm(ps[pb][:, m * 129:(m + 1) * 129], pt[:, m * 256 + qt * 128:m * 256 + (qt + 1) * 128],
                           va[:, j, 0:129], first, last, (ptk, ("va", j), "va_ones"), (("ps", pb),))
                    if j == qt0 + qt:
                        P = ps[pb]
                        pk = ("ps", pb)
                        I("dve", lambda e, P=P: e.reciprocal(asm[:, 0:1], P[:, 128:129]), (pk,), ("asm0",))
                        I("dve", lambda e, P=P: e.reciprocal(asm[:, 1:2], P[:, 257:258]), (pk,), ("asm1",))
                        tt("dve", asm[:, 1:2], asm[:, 1:2], neglam[:, :], ALU.mult, ("asm1", "neglam"), ("asm1",))
                        ts("dve", at1[:, :], P[:, 0:128], asm[:, 0:1], ALU.mult, (pk, "asm0"), ("at1",))
                        stt(at2[:, :], P[:, 129:257], asm[:, 1:2], at1[:, :], ALU.mult, ALU.add,
                            (pk, "asm1", "at1"), ("at2",))
                        tt("pool", atj[:, :], at2[:, :], at2[:, :], ALU.mult, ("at2",), ("atj",))
                        I("dve", lambda e: e.reduce_sum(asm[:, 2:3], atj[:, :], AX.X), ("atj",), ("asm2",))
                        ts("dve", asm[:, 3:4], asm[:, 2:3], 1.0 / 128.0, ALU.mult, ("asm2",), ("asm3",),
                           s2=EPS, op1=ALU.add)
                        act(asm[:, 4:5], asm[:, 3:4], AF.Ln, ("asm3",), ("asm4",))
                        act(asm[:, 5:6], asm[:, 4:5], AF.Exp, ("asm4",), ("asm5",), scale=-0.5)
                        ts("dve", hab[:, :], at2[:, :], asm[:, 5:6], ALU.mult, ("at2", "asm5"), ("hab",),
                           s2=1.0 - lam_init, op1=ALU.mult)
                        tr(psb(pb)[:, 768:896], hab[:, :], identb[:, :], ("hab", "identb"), (pk,))
                        c0 = qoff + qt * 128
                        stt(yab[:, c0:c0 + 128], psb(pb)[:, 768:896], V[:, 20:21], ga[:, c0:c0 + 128], ALU.mult,
                            ALU.mult, (pk, vk, gak), ("yab",))
                yield
            res.append(dma("sp", ycl_ap, yab[:, :], ("yab",), (okey,)))

        out_toks = []

        def frontA(l, tb):
            t0 = tb * TB
            if l == 0:
                if tb == 0:
                    dma("sp", xblk[:, :, :], xT[:, t0:t0 + TB].rearrange("(kt p) t -> p kt t", p=128), (), XB)
            else:
                yield from outproj_block(l - 1, tb, xT, ("xsrc0", tb), load=(tb == 0))
                tok = dma("sp", x1s[:, t0:t0 + TB].rearrange("(kt p) t -> p kt t", p=128), xblk[:, :, :],
                          XB, (("xsrc1", tb),))
                if not do_fin:
                    out_toks.append(tok)
            yield from norm_block(vec[l][:, 0:8], False)
            if tb + 1 < _NB:
                t1 = (tb + 1) * TB
                if l == 0:
                    dma("sp", xblk[:, :, :], xT[:, t1:t1 + TB].rearrange("(kt p) t -> p kt t", p=128), (), XB)
                else:
                    outproj_loads(l - 1, tb + 1, xT, ("xsrc0", tb + 1))
            if _STOP >= 2:
                yield from inproj_block(l, tb)

        def frontB(l, tb):
            q4, tq = tb // 4, (tb % 4) * TB
            if _STOP >= 3:
                yield from gates_block(l, tb)
            res = []
            if _STOP >= 4:
                yield from mlstm_block(l, tb, ycl[l][q4][0:128, tq:tq + TB], ("ycl", l, q4, tb % 4, 0), res)
            if not fused:
                out_toks.extend(res)

        def drain(g):
            for _ in g:
                pass

        def adv(g, n):
            for _ in range(n):
                try:
                    next(g)
                except StopIteration:
                    return False
            return True

        NBU, NAU = float(os.environ.get("K_NBU", "16")), float(os.environ.get("K_NAU", "20"))

        load_weights(layers[0]) if layers else None
        if layers and layers[0] > 0:
            load_wout(layers[0] - 1)
        elif len(layers) > 1:
            load_wout(layers[0])
        for li, l in enumerate(layers):
            load_lam(l)
            drain(frontA(l, 0))
            for tb in range(_NB):
                q4, tq = tb // 4, (tb % 4) * TB
                res = []
                ag = (attn_block(l, tb, ycl[l][q4][128:256, tq:tq + TB], ("ycl", l, q4, tb % 4, 1), res)
                      if _STOP >= 5 else iter(()))
                bg = frontB(l, tb)
                fg = frontA(l, tb + 1) if tb + 1 < _NB else iter(())
                if tb == _NB - 1:
                    if li + 1 < len(layers):
                        load_weights(layers[li + 1])
                        if li + 1 >= 2:
                            load_wout(layers[li + 1] - 1)
                    elif do_fin and l > layers[0]:
                        load_wout(DEPTH - 1)
                ntile = 8 * tb + 6
                accb = acca = 0.0
                bl = fl = True
                for _ in ag:
                    accb += NBU / ntile
                    acca += NAU / ntile
                    nb_, na_ = int(accb), int(acca)
                    accb -= nb_
                    acca -= na_
                    while nb_ > 0 or na_ > 0:
                        if nb_ > 0:
                            bl = bl and adv(bg, 1)
                            nb_ -= 1
                        if na_ > 0:
                            fl = fl and adv(fg, 1)
                            na_ -= 1
                while bl or fl:
                    if bl:
                        bl = adv(bg, 1)
                    if fl:
                        fl = adv(fg, 1)
                if not fused:
                    out_toks.extend(res)
                if fused and tb % 4 == 3:
                    rk = tuple(("ycl", l, q4, i, w) for i in range(4) for w in range(2))
                    I("pool", lambda e, l=l, q4=q4: e.collective_compute(
                        "AllGather", ALU.bypass, replica_groups=GROUPS,
                        ins=[ycl[l][q4][:, :]], outs=[ycf[l][q4][:, :]]), rk, (("ycf", l, q4),), cc=True)

        if do_fin:
            if not (layers and layers[-1] > layers[0]):
                load_wout(DEPTH - 1)
            lf = DEPTH - 1
            nw_ap = vec[lf][:, 23:31]
            xb = [xblk[:, :, :], kaT[:, :].bitcast(F32).rearrange("p (kt t) -> p kt t", kt=KT)]
            yb = [sqb[:, :, :], va[:, :, :].rearrange("p a b -> p (a b)")[:, 0:KT * TB].rearrange("p (kt t) -> p kt t", kt=KT)]
            xk = [XB, tuple(("ka", i) for i in range(NBLK))]
            yk = [SQ, tuple(("va", i) for i in range(SEQ // 128)) + ("va_ones",)]

            def fin_load(tb):
                p = tb % 2
                t0 = tb * TB
                q4, tq = tb // 4, (tb % 4) * TB
                dma("sp", yb[p], ycf[lf][q4][:, tq:tq + TB].rearrange("(kt p) t -> p kt t", p=128),
                    (("ycf", lf, q4),), yk[p])
                dma("sp", xb[p], x1s[:, t0:t0 + TB].rearrange("(kt p) t -> p kt t", p=128),
                    (("xsrc1", tb),), xk[p])

            fin_load(0)
            for tb in range(_NB):
                p = tb % 2
                t0 = tb * TB
                if tb + 1 < _NB:
                    fin_load(tb + 1)
                X, Y = xb[p], yb[p]
                for m in range(KT):
                    bank = m % 2
                    for et in range(KT):
                        mm(ps[bank][:, :], wout_b[:, et, m * 128:(m + 1) * 128], Y[:, et, :], et == 0, et == KT - 1,
                           ("wout",) + yk[p], (("ps", bank),))
                    tt("dve", X[:, m, :], X[:, m, :], ps[bank][:, :], ALU.add, (("ps", bank),) + xk[p], xk[p])
                    if m % 2 == 0:
                        tt("pool", xnb[:, m, :], X[:, m, :], X[:, m, :], ALU.mult, xk[p], (("xnb", m),))
                    else:
                        tt("dve", xnb[:, m, :], X[:, m, :], X[:, m, :], ALU.mult, xk[p], (("xnb", m),))
                for kt in range(KT):
                    mm(ps[2][:, :], onesb[:, :], xnb[:, kt, :], kt == 0, kt == KT - 1, ("onesb", ("xnb", kt)), (("ps", 2),))
                ts("dve", rstd[:, :], ps[2][:, :], 1.0 / D_MODEL, ALU.mult, (("ps", 2),), ("rstd",), s2=EPS, op1=ALU.add)
                act(rstd[:, :], rstd[:, :], AF.Ln, ("rstd",), ("rstd",))
                act(rstd[:, :], rstd[:, :], AF.Exp, ("rstd",), ("rstd",), scale=-0.5)
                for kt in range(KT):
                    stt(X[:, kt, :], X[:, kt, :], nw_ap[:, kt:kt + 1], rstd[:, :], ALU.mult, ALU.mult,
                        xk[p] + ("rstd", ("vec", lf)), xk[p])
                out_toks.append(dma("sp", outT[:, t0:t0 + TB].rearrange("(kt p) t -> p kt t", p=128), X, xk[p], ()))

        waits = []
        for tok in out_toks:
            S._tok_wait("sp", tok, waits)
        S.recs["sp"].append((waits, None, None))

        with nc.Block() as block:
            S.emit(block)
    return nc


_OFF = {"mq": 0, "mk": 512, "mv": 1024, "mi": 1536, "mf": 1540, "mz": 1544,
        "aq": 2056, "ak": 2568, "av": 3080, "az": 3592}


def _rope_tables():
    inv = (1.0 / (np.float32(10000.0) ** (np.arange(0, 64, 2, dtype=np.float32) / np.float32(64.0)))).astype(np.float32)
    ang = np.arange(SEQ, dtype=np.float32)[:, None] * inv[None, :]
    cos = np.cos(ang).astype(np.float32)
    sin = np.sin(ang).astype(np.float32)
    cosT = np.ascontiguousarray(np.concatenate([cos, cos, cos, cos], 1).T)
    sinT = np.ascontiguousarray(np.concatenate([sin, sin, sin, sin], 1).T)
    return cosT, sinT


def _consts():
    c = np.zeros((128, 192), np.float32)
    c[:, 0:128] = np.eye(128, dtype=np.float32)
    c[0:64, 128:192] = np.triu(np.ones((64, 64), np.float32))
    return c


def _core_inputs(inp, c, stage_layers, need_wout):
    b, hd = c // 4, c % 4
    f32 = np.float32
    d = {}
    for l in stage_layers:
        w = np.asarray(inp["w_in"][l], f32)
        hs = slice(hd * 128, (hd + 1) * 128)

        def blk(name):
            return w[:, _OFF[name] + hd * 128:_OFF[name] + (hd + 1) * 128]

        def perm(m):
            return np.concatenate([m[:, 32:64], m[:, 0:32], m[:, 96:128], m[:, 64:96]], 1)

        aq, ak = blk("aq"), blk("ak")
        gi = w[:, _OFF["mi"] + hd:_OFF["mi"] + hd + 1]
        gf = w[:, _OFF["mf"] + hd:_OFF["mf"] + hd + 1]
        d[f"wfm{l}"] = np.ascontiguousarray(np.concatenate(
            [blk("mq"), blk("mk"), blk("mz"), aq, perm(aq), ak, perm(ak), blk("az"), gi, gf], 1))
        d[f"wtm{l}"] = np.ascontiguousarray(np.concatenate([blk("mv"), blk("av")], 1))
        lam = np.concatenate([np.asarray(inp[k][l], f32) for k in ("lam_q1", "lam_k1", "lam_q2", "lam_k2")])
        d[f"lamv{l}"] = np.ascontiguousarray(np.tile(lam[None, :], (128, 1)))
    for l in range(DEPTH):
        v = np.zeros((128, NV), f32)
        v[:, 0:8] = np.asarray(inp["norm_w"][l], f32).reshape(8, 128).T
        cw = np.asarray(inp["conv_w"][l], f32)
        cbv = np.asarray(inp["conv_b"][l], f32)
        v[:, 8:12] = cw[:, hd * 128:(hd + 1) * 128].T
        v[:, 12:16] = cw[:, 512 + hd * 128:512 + (hd + 1) * 128].T
        v[:, 16] = cbv[hd * 128:(hd + 1) * 128]
        v[:, 17] = cbv[512 + hd * 128:512 + (hd + 1) * 128]
        v[:, 18] = np.asarray(inp["m_norm_w"][l], f32)[hd * 128:(hd + 1) * 128]
        v[:, 19] = np.asarray(inp["m_skip"][l], f32)[hd * 128:(hd + 1) * 128]
        v[:, 20] = np.asarray(inp["a_norm_w"][l], f32)
        v[:, 21] = np.asarray(inp["i_bias"][l], f32)[hd]
        v[:, 22] = np.asarray(inp["f_bias"][l], f32)[hd]
        v[:, 23:31] = np.asarray(inp["final_norm_w"], f32).reshape(8, 128).T
        d[f"vecs{l}"] = v
    for l in need_wout:
        wo = np.asarray(inp["w_out"][l], f32)
        rows = []
        for r in range(4):
            rows.append(wo[r * 128:(r + 1) * 128])
            rows.append(wo[512 + r * 128:512 + (r + 1) * 128])
        d[f"wout{l}"] = np.ascontiguousarray(np.concatenate(rows, 0))
    return d


_PROG = {}


def _prog(stage, debug=False):
    key = (stage, debug)
    if key not in _PROG:
        _PROG[key] = build_program(stage, debug)
    return _PROG[key]


def kernel(**inp):
    x = np.asarray(inp["x"], np.float32)
    xTs = [np.ascontiguousarray(x[b].T) for b in range(BATCH)]
    cosT, sinT = _rope_tables()
    cst = _consts()
    nc = _prog("all")
    in_maps = []
    for c in range(NCORES):
        d = _core_inputs(inp, c, [0, 1], [0, 1])
        d.update({"xT": xTs[c // 4], "cosT": cosT, "sinT": sinT, "consts": cst})
        in_maps.append(d)
    res = run_bass_kernel_spmd(nc, in_maps, core_ids=list(range(NCORES)))
    out = np.empty((BATCH, SEQ, D_MODEL), np.float32)
    for b in range(BATCH):
        out[b] = res.results[4 * b]["outT"].T
    return out
```
